# Optimizing a Trainium2 kernel written in Bass

```python
import math
import jax
import jax.numpy as jnp
from jax import lax
import numpy as np

D_MODEL = 1024
BATCH = 8
SEQ = 8192
DEPTH = 2

CHUNK = 64
Q_BLOCK = 128

N_MIXERS = 4
W_GROUP = D_MODEL // N_MIXERS

RWKV_HEAD = 64
RWKV_HEADS = W_GROUP // RWKV_HEAD
RWKV_W_RANK = 32
RWKV_A_RANK = 32
RWKV_G_RANK = 64
RWKV_LN_EPS = 64e-5

S5_GROUP = 16
S5_GROUPS = W_GROUP // S5_GROUP
S5_STATE = 64
S5_DT_MIN = 1e-3
S5_DT_MAX = 1e-1

MLA_HEADS = 4
MLA_NOPE = 64
MLA_ROPE = 32
MLA_QK = MLA_NOPE + MLA_ROPE
MLA_V = W_GROUP // MLA_HEADS
MLA_Q_RANK = 256
MLA_KV_RANK = 128
ROPE_THETA = 10000.0

LRU_BLOCKS = 4
LRU_BLOCK = W_GROUP // LRU_BLOCKS
LRU_C = 8.0
CONV_WIDTH = 4

N_GROUPS = 4
EXPERTS_PER_GROUP = 8
N_EXPERTS = N_GROUPS * EXPERTS_PER_GROUP
TOP_K = 2
D_FF_EXPERT = 512
MOE_BLOCK = 256

NORM_EPS = 1e-6

RWKV_SPLITS = (W_GROUP, W_GROUP, W_GROUP, RWKV_W_RANK, RWKV_A_RANK, RWKV_G_RANK)
RWKV_IN = sum(RWKV_SPLITS)
IN_SPLITS = (RWKV_IN, W_GROUP, MLA_Q_RANK, MLA_KV_RANK, MLA_ROPE, W_GROUP, W_GROUP)
D_IN = sum(IN_SPLITS)

kernel_name = 'hybrid_chunk_causal_streaming_trunk'


def _split(t, sizes):
    idx = [int(i) for i in np.cumsum(sizes)[:-1]]
    return jnp.split(t, idx, axis=-1)


def rms_norm(x, g, eps=NORM_EPS):
    xf = x.astype(jnp.float32)
    y = xf * lax.rsqrt(jnp.mean(xf * xf, axis=-1, keepdims=True) + eps)
    return (y * g.astype(jnp.float32)).astype(x.dtype)


def rope(t, pos):
    half = MLA_ROPE // 2
    inv_freq = jnp.power(ROPE_THETA, -jnp.arange(half, dtype=jnp.float32) * 2.0 / MLA_ROPE)
    ang = pos.astype(jnp.float32)[..., None] * inv_freq
    cos = jnp.cos(ang)[:, :, None, :]
    sin = jnp.sin(ang)[:, :, None, :]
    tf = t.astype(jnp.float32)
    t1, t2 = tf[..., :half], tf[..., half:]
    return jnp.concatenate([t1 * cos - t2 * sin, t1 * sin + t2 * cos], axis=-1).astype(t.dtype)


def rwkv7_mixer(z, mu, w0, w2, a0, a2, g2, k_k, k_a, r_k, ln_w, ln_b):
    Bsz, S, _ = z.shape
    z = z.astype(jnp.float32)
    z_prev = jnp.pad(z, ((0, 0), (1, 0), (0, 0)))[:, :-1]
    z = z + (z_prev - z) * mu
    r, k, v, w_lo, a_lo, g_lo = _split(z, RWKV_SPLITS)
    w = -jax.nn.softplus(-(w0 + jnp.tanh(w_lo) @ w2)) - 0.5
    decay = jnp.exp(-jnp.exp(w))
    a = jax.nn.sigmoid(a0 + a_lo @ a2)
    g = jax.nn.sigmoid(g_lo) @ g2

    def heads(t):
        return t.reshape(Bsz, S, RWKV_HEADS, RWKV_HEAD)

    kk = heads(k * k_k)
    kk = kk / jnp.maximum(jnp.linalg.norm(kk, axis=-1, keepdims=True), 1e-12)
    k = k * (1.0 + (a - 1.0) * k_a)
    r_h, k_h, v_h, w_h, a_h = heads(r), heads(k), heads(v), heads(decay), heads(a)

    def step(state, inp):
        r_t, w_t, k_t, v_t, kk_t, a_t = inp
        sa = jnp.einsum('bhvk,bhk->bhv', state, -kk_t)
        state = (state * w_t[:, :, None, :]
                 + sa[..., None] * (kk_t * a_t)[:, :, None, :]
                 + v_t[..., None] * k_t[:, :, None, :])
        return state, jnp.einsum('bhvk,bhk->bhv', state, r_t)

    def tm(t):
        return jnp.swapaxes(t, 0, 1)

    s0 = jnp.zeros((Bsz, RWKV_HEADS, RWKV_HEAD, RWKV_HEAD), jnp.float32)
    _, y = lax.scan(step, s0, (tm(r_h), tm(w_h), tm(k_h), tm(v_h), tm(kk), tm(a_h)))
    y = tm(y)
    mean = jnp.mean(y, axis=-1, keepdims=True)
    var = jnp.mean(jnp.square(y - mean), axis=-1, keepdims=True)
    y = ((y - mean) * lax.rsqrt(var + RWKV_LN_EPS) * ln_w.reshape(RWKV_HEADS, RWKV_HEAD)
         + ln_b.reshape(RWKV_HEADS, RWKV_HEAD))
    bonus = jnp.sum(r_h * k_h * r_k, axis=-1, keepdims=True) * v_h
    return (y + bonus).reshape(Bsz, S, W_GROUP) * g


def s5_mixer(u, lam_re, lam_im, b_re, b_im, c_re, c_im, d, log_dt, glu_w, glu_b):
    Bsz, S, _ = u.shape
    f32 = jnp.float32
    lam_re, lam_im, b_re, b_im, c_re, c_im = (t.astype(f32) for t in (lam_re, lam_im, b_re, b_im, c_re, c_im))
    uf = u.astype(f32)
    dt = jnp.exp(log_dt.astype(f32))[:, None]
    mag = jnp.exp(lam_re * dt)
    a_re = mag * jnp.cos(lam_im * dt)
    a_im = mag * jnp.sin(lam_im * dt)
    den = lam_re * lam_re + lam_im * lam_im
    q_re = ((a_re - 1.0) * lam_re + a_im * lam_im) / den
    q_im = (a_im * lam_re - (a_re - 1.0) * lam_im) / den
    bb_re = q_re[..., None] * b_re - q_im[..., None] * b_im
    bb_im = q_re[..., None] * b_im + q_im[..., None] * b_re
    ug = uf.reshape(Bsz, S, S5_GROUPS, S5_GROUP)
    bu_re = jnp.einsum('bsgi,gpi->bsgp', ug, bb_re)
    bu_im = jnp.einsum('bsgi,gpi->bsgp', ug, bb_im)
    shape = (1, S, S5_GROUPS, S5_STATE)
    ar = jnp.broadcast_to(a_re, shape)
    ai = jnp.broadcast_to(a_im, shape)

    def combine(e1, e2):
        a1r, a1i, b1r, b1i = e1
        a2r, a2i, b2r, b2i = e2
        return (a2r * a1r - a2i * a1i, a2r * a1i + a2i * a1r,
                a2r * b1r - a2i * b1i + b2r, a2r * b1i + a2i * b1r + b2i)

    _, _, h_re, h_im = lax.associative_scan(combine, (ar, ai, bu_re, bu_im), axis=1)
    y = jnp.einsum('bsgp,gip->bsgi', h_re, c_re) - jnp.einsum('bsgp,gip->bsgi', h_im, c_im)
    y = y.reshape(Bsz, S, W_GROUP) + d * uf
    y = jax.nn.gelu(y)
    return y * jax.nn.sigmoid(y @ glu_w + glu_b)


def mla_mixer(q_a, kv_a, k_pe, pos, q_norm_g, w_uq, kv_norm_g, w_ukv, q_head_g, k_head_g):
    Bsz, S, _ = q_a.shape
    q = (rms_norm(q_a, q_norm_g) @ w_uq).reshape(Bsz, S, MLA_HEADS, MLA_QK)
    kv = (rms_norm(kv_a, kv_norm_g) @ w_ukv).reshape(Bsz, S, MLA_HEADS, MLA_NOPE + MLA_V)
    k_nope, v = kv[..., :MLA_NOPE], kv[..., MLA_NOPE:]
    k = jnp.concatenate([k_nope, jnp.broadcast_to(k_pe[:, :, None, :], (Bsz, S, MLA_HEADS, MLA_ROPE))], axis=-1)
    q = rms_norm(q, q_head_g)
    k = rms_norm(k, k_head_g)
    q = jnp.concatenate([q[..., :MLA_NOPE], rope(q[..., MLA_NOPE:], pos)], axis=-1)
    k = jnp.concatenate([k[..., :MLA_NOPE], rope(k[..., MLA_NOPE:], pos)], axis=-1)
    n_blk = S // Q_BLOCK
    q_blocks = q.reshape(Bsz, n_blk, Q_BLOCK, MLA_HEADS, MLA_QK).transpose(1, 0, 2, 3, 4)
    key_chunk = jnp.arange(S) // CHUNK
    scale = MLA_QK ** -0.5

    def attend(args):
        qb, j = args
        s = jnp.einsum('bqhd,bkhd->bhqk', qb, k).astype(jnp.float32) * scale
        q_chunk = (j * Q_BLOCK + jnp.arange(Q_BLOCK)) // CHUNK
        mask = key_chunk[None, :] <= q_chunk[:, None]
        s = jnp.where(mask, s, -jnp.inf)
        p = jax.nn.softmax(s, axis=-1).astype(v.dtype)
        return jnp.einsum('bhqk,bkhd->bqhd', p, v)

    o = lax.map(attend, (q_blocks, jnp.arange(n_blk)))
    return o.transpose(1, 0, 2, 3, 4).reshape(Bsz, S, MLA_HEADS * MLA_V)


def rglru_mixer(x_in, gate, conv_w, conv_b, w_a, b_a, w_x, b_x, lam):
    Bsz, S, W = x_in.shape
    xc = lax.conv_general_dilated(x_in, conv_w[:, None, :], window_strides=(1,),
                                  padding=[(CONV_WIDTH - 1, 0)],
                                  dimension_numbers=('NWC', 'WIO', 'NWC'),
                                  feature_group_count=W) + conv_b
    xb = xc.reshape(Bsz, S, LRU_BLOCKS, LRU_BLOCK)
    r = jax.nn.sigmoid(jnp.einsum('bsnk,nkj->bsnj', xb, w_a).reshape(Bsz, S, W) + b_a)
    i = jax.nn.sigmoid(jnp.einsum('bsnk,nkj->bsnj', xb, w_x).reshape(Bsz, S, W) + b_x)
    log_a = (-LRU_C * r * jax.nn.softplus(-lam)).astype(jnp.float32)
    a = jnp.exp(log_a)
    b = jnp.sqrt(-jnp.expm1(2.0 * log_a)) * (i * xc).astype(jnp.float32)

    def combine(e1, e2):
        a1, b1 = e1
        a2, b2 = e2
        return (a1 * a2, a2 * b1 + b2)

    _, h = lax.associative_scan(combine, (a, b), axis=1)
    return h.astype(x_in.dtype) * jax.nn.gelu(gate)


def hier_moe(h, w_group, b_group, w_expert, b_expert, w1, w3, w2):
    Bsz, S, D = h.shape
    T = Bsz * S
    xt = h.reshape(T, D)
    p_group = jax.nn.softmax((xt @ w_group + b_group).astype(jnp.float32), axis=-1)
    g_sel = jnp.argmax(p_group, axis=-1).astype(jnp.int32)
    g_prob = jnp.max(p_group, axis=-1)
    logit_e = (xt @ w_expert + b_expert).astype(jnp.float32).reshape(T, N_GROUPS, EXPERTS_PER_GROUP)
    logit_e = jnp.take_along_axis(logit_e, g_sel[:, None, None], axis=1)[:, 0]
    p_e = jax.nn.softmax(logit_e, axis=-1)
    top_p, top_i = lax.top_k(p_e, TOP_K)
    gate = (g_prob[:, None] * top_p / jnp.sum(top_p, axis=-1, keepdims=True)).reshape(-1)
    expert = (g_sel[:, None] * EXPERTS_PER_GROUP + top_i.astype(jnp.int32)).reshape(-1)
    tok = jnp.repeat(jnp.arange(T, dtype=jnp.int32), TOP_K)

    n_assign = T * TOP_K
    n_blocks = -(-n_assign // MOE_BLOCK) + N_EXPERTS
    n_rows = n_blocks * MOE_BLOCK
    order = jnp.argsort(expert)
    e_s, tok_s, w_s = expert[order], tok[order], gate[order]
    counts = jax.ops.segment_sum(jnp.ones_like(e_s), e_s, num_segments=N_EXPERTS)
    start = jnp.cumsum(counts) - counts
    pcounts = (counts + MOE_BLOCK - 1) // MOE_BLOCK * MOE_BLOCK
    pend = jnp.cumsum(pcounts)
    pstart = pend - pcounts
    dest = pstart[e_s] + (jnp.arange(n_assign, dtype=jnp.int32) - start[e_s])
    row_tok = jnp.zeros((n_rows,), jnp.int32).at[dest].set(tok_s)
    row_w = jnp.zeros((n_rows,), w_s.dtype).at[dest].set(w_s)
    blk_start = jnp.arange(n_blocks, dtype=jnp.int32) * MOE_BLOCK
    blk_expert = jnp.minimum(jnp.searchsorted(pend, blk_start, side='right'), N_EXPERTS - 1)

    def expert_block(args):
        toks, e = args
        xb = xt[toks]
        hid = jax.nn.silu(xb @ w1[e]) * (xb @ w3[e])
        return hid @ w2[e]

    yb = lax.map(expert_block, (row_tok.reshape(n_blocks, MOE_BLOCK), blk_expert))
    contrib = yb.reshape(n_rows, D) * row_w[:, None].astype(yb.dtype)
    y = jnp.zeros((T, D), yb.dtype).at[row_tok].add(contrib)
    return y.reshape(Bsz, S, D)


def setup_inputs(seed: int = 0) -> dict:
    key = jax.random.key(seed)
    ks = iter(jax.random.split(key, 64))
    L, D, W = DEPTH, D_MODEL, W_GROUP

    def nrm(shape, scale):
        return jax.random.normal(next(ks), shape, jnp.float32) * scale

    def unif(shape, lo, hi):
        return jax.random.uniform(next(ks), shape, jnp.float32, lo, hi)

    x = nrm((BATCH, SEQ, D), 1.0)
    c = nrm((BATCH, D), 1.0)
    pos_offset = jax.random.randint(next(ks), (BATCH,), 0, 65536, dtype=jnp.int32)
    ada_w = nrm((L, D, 6 * D), 0.3 * D ** -0.5)
    ada_b = nrm((L, 6 * D), 0.02)
    norm1_g = 1.0 + nrm((L, D), 0.02)
    w_in = nrm((L, D, D_IN), D ** -0.5)
    rwkv_mu = unif((L, RWKV_IN), 0.0, 1.0)
    rwkv_w0 = jnp.linspace(-6.0, -1.0, W, dtype=jnp.float32)[None, :] + nrm((L, W), 0.1)
    rwkv_w2 = nrm((L, RWKV_W_RANK, W), 0.1 * RWKV_W_RANK ** -0.5)
    rwkv_a0 = nrm((L, W), 0.1)
    rwkv_a2 = nrm((L, RWKV_A_RANK, W), 0.1 * RWKV_A_RANK ** -0.5)
    rwkv_g2 = nrm((L, RWKV_G_RANK, W), RWKV_G_RANK ** -0.5)
    rwkv_k_k = 0.85 + nrm((L, W), 0.02)
    rwkv_k_a = 1.0 + nrm((L, W), 0.02)
    rwkv_r_k = nrm((L, RWKV_HEADS, RWKV_HEAD), 0.1)
    rwkv_ln_w = 1.0 + nrm((L, W), 0.02)
    rwkv_ln_b = nrm((L, W), 0.02)
    s5_lambda_re = -0.5 + nrm((L, S5_GROUPS, S5_STATE), 0.01)
    s5_lambda_im = (math.pi * jnp.arange(S5_STATE, dtype=jnp.float32))[None, None, :] + nrm((L, S5_GROUPS, S5_STATE), 0.01)
    s5_b_re = nrm((L, S5_GROUPS, S5_STATE, S5_GROUP), (2 * S5_GROUP) ** -0.5)
    s5_b_im = nrm((L, S5_GROUPS, S5_STATE, S5_GROUP), (2 * S5_GROUP) ** -0.5)
    s5_c_re = nrm((L, S5_GROUPS, S5_GROUP, S5_STATE), 0.7)
    s5_c_im = nrm((L, S5_GROUPS, S5_GROUP, S5_STATE), 0.7)
    s5_d = nrm((L, W), 0.5)
    s5_log_dt = unif((L, S5_GROUPS), math.log(S5_DT_MIN), math.log(S5_DT_MAX))
    s5_glu_w = nrm((L, W, W), W ** -0.5)
    s5_glu_b = nrm((L, W), 0.02)
    mla_q_norm_g = 1.0 + nrm((L, MLA_Q_RANK), 0.02)
    mla_w_uq = nrm((L, MLA_Q_RANK, MLA_HEADS * MLA_QK), MLA_Q_RANK ** -0.5)
    mla_kv_norm_g = 1.0 + nrm((L, MLA_KV_RANK), 0.02)
    mla_w_ukv = nrm((L, MLA_KV_RANK, MLA_HEADS * (MLA_NOPE + MLA_V)), MLA_KV_RANK ** -0.5)
    mla_q_head_g = 1.0 + nrm((L, MLA_QK), 0.02)
    mla_k_head_g = 1.0 + nrm((L, MLA_QK), 0.02)
    lru_conv_w = nrm((L, CONV_WIDTH, W), CONV_WIDTH ** -0.5)
    lru_conv_b = nrm((L, W), 0.02)
    lru_w_a = nrm((L, LRU_BLOCKS, LRU_BLOCK, LRU_BLOCK), LRU_BLOCK ** -0.5)
    lru_b_a = nrm((L, W), 0.02)
    lru_w_x = nrm((L, LRU_BLOCKS, LRU_BLOCK, LRU_BLOCK), LRU_BLOCK ** -0.5)
    lru_b_x = nrm((L, W), 0.02)
    a_c = unif((L, W), 0.9, 0.999) ** (1.0 / LRU_C)
    lru_lambda = jnp.log(a_c) - jnp.log1p(-a_c)
    branch_norm_g = 1.0 + nrm((L, 3, W), 0.02)
    w_out = nrm((L, D, D), D ** -0.5)
    norm2_g = 1.0 + nrm((L, D), 0.02)
    moe_w_group = nrm((L, D, N_GROUPS), D ** -0.5)
    moe_b_group = nrm((L, N_GROUPS), 0.01)
    moe_w_expert = nrm((L, D, N_EXPERTS), D ** -0.5)
    moe_b_expert = nrm((L, N_EXPERTS), 0.01)
    moe_w1 = nrm((L, N_EXPERTS, D, D_FF_EXPERT), D ** -0.5)
    moe_w3 = nrm((L, N_EXPERTS, D, D_FF_EXPERT), D ** -0.5)
    moe_w2 = nrm((L, N_EXPERTS, D_FF_EXPERT, D), D_FF_EXPERT ** -0.5)
    return {'x': x, 'c': c, 'pos_offset': pos_offset, 'ada_w': ada_w, 'ada_b': ada_b,
            'norm1_g': norm1_g, 'w_in': w_in, 'rwkv_mu': rwkv_mu, 'rwkv_w0': rwkv_w0,
            'rwkv_w2': rwkv_w2, 'rwkv_a0': rwkv_a0, 'rwkv_a2': rwkv_a2, 'rwkv_g2': rwkv_g2,
            'rwkv_k_k': rwkv_k_k, 'rwkv_k_a': rwkv_k_a, 'rwkv_r_k': rwkv_r_k,
            'rwkv_ln_w': rwkv_ln_w, 'rwkv_ln_b': rwkv_ln_b, 's5_lambda_re': s5_lambda_re,
            's5_lambda_im': s5_lambda_im, 's5_b_re': s5_b_re, 's5_b_im': s5_b_im,
            's5_c_re': s5_c_re, 's5_c_im': s5_c_im, 's5_d': s5_d, 's5_log_dt': s5_log_dt,
            's5_glu_w': s5_glu_w, 's5_glu_b': s5_glu_b, 'mla_q_norm_g': mla_q_norm_g,
            'mla_w_uq': mla_w_uq, 'mla_kv_norm_g': mla_kv_norm_g, 'mla_w_ukv': mla_w_ukv,
            'mla_q_head_g': mla_q_head_g, 'mla_k_head_g': mla_k_head_g,
            'lru_conv_w': lru_conv_w, 'lru_conv_b': lru_conv_b, 'lru_w_a': lru_w_a,
            'lru_b_a': lru_b_a, 'lru_w_x': lru_w_x, 'lru_b_x': lru_b_x, 'lru_lambda': lru_lambda,
            'branch_norm_g': branch_norm_g, 'w_out': w_out, 'norm2_g': norm2_g,
            'moe_w_group': moe_w_group, 'moe_b_group': moe_b_group,
            'moe_w_expert': moe_w_expert, 'moe_b_expert': moe_b_expert,
            'moe_w1': moe_w1, 'moe_w3': moe_w3, 'moe_w2': moe_w2}


def reference(x, c, pos_offset, ada_w, ada_b, norm1_g, w_in, rwkv_mu, rwkv_w0, rwkv_w2,
              rwkv_a0, rwkv_a2, rwkv_g2, rwkv_k_k, rwkv_k_a, rwkv_r_k, rwkv_ln_w, rwkv_ln_b,
              s5_lambda_re, s5_lambda_im, s5_b_re, s5_b_im, s5_c_re, s5_c_im, s5_d, s5_log_dt,
              s5_glu_w, s5_glu_b, mla_q_norm_g, mla_w_uq, mla_kv_norm_g, mla_w_ukv,
              mla_q_head_g, mla_k_head_g, lru_conv_w, lru_conv_b, lru_w_a, lru_b_a, lru_w_x,
              lru_b_x, lru_lambda, branch_norm_g, w_out, norm2_g, moe_w_group, moe_b_group,
              moe_w_expert, moe_b_expert, moe_w1, moe_w3, moe_w2):
    Bsz, S, _ = x.shape
    pos = pos_offset[:, None] + jnp.arange(S, dtype=jnp.int32)[None, :]
    cond = jax.nn.silu(c)
    for l in range(DEPTH):
        mod = cond @ ada_w[l] + ada_b[l]
        sh1, sc1, gt1, sh2, sc2, gt2 = [m[:, None, :] for m in jnp.split(mod, 6, axis=-1)]
        h = rms_norm(x, norm1_g[l]) * (1.0 + sc1) + sh1
        z = h @ w_in[l]
        z_rwkv, z_s5, q_a, kv_a, k_pe, z_lru, z_gate = _split(z, IN_SPLITS)
        y_a = rwkv7_mixer(z_rwkv, rwkv_mu[l], rwkv_w0[l], rwkv_w2[l], rwkv_a0[l], rwkv_a2[l],
                          rwkv_g2[l], rwkv_k_k[l], rwkv_k_a[l], rwkv_r_k[l], rwkv_ln_w[l], rwkv_ln_b[l])
        y_b = s5_mixer(z_s5, s5_lambda_re[l], s5_lambda_im[l], s5_b_re[l], s5_b_im[l], s5_c_re[l],
                       s5_c_im[l], s5_d[l], s5_log_dt[l], s5_glu_w[l], s5_glu_b[l])
        y_c = mla_mixer(q_a, kv_a, k_pe, pos, mla_q_norm_g[l], mla_w_uq[l], mla_kv_norm_g[l],
                        mla_w_ukv[l], mla_q_head_g[l], mla_k_head_g[l])
        y_d = rglru_mixer(z_lru, z_gate, lru_conv_w[l], lru_conv_b[l], lru_w_a[l], lru_b_a[l],
                          lru_w_x[l], lru_b_x[l], lru_lambda[l])
        mix = jnp.concatenate([y_a.astype(h.dtype),
                               rms_norm(y_b.astype(h.dtype), branch_norm_g[l, 0]),
                               rms_norm(y_c.astype(h.dtype), branch_norm_g[l, 1]),
                               rms_norm(y_d.astype(h.dtype), branch_norm_g[l, 2])], axis=-1)
        x = x + gt1 * (mix @ w_out[l])
        h2 = rms_norm(x, norm2_g[l]) * (1.0 + sc2) + sh2
        x = x + gt2 * hier_moe(h2, moe_w_group[l], moe_b_group[l], moe_w_expert[l],
                               moe_b_expert[l], moe_w1[l], moe_w3[l], moe_w2[l])
    return x
```

```python
import math
import numpy as np
from contextlib import ExitStack
import concourse.bass as bass
import concourse.mybir as mybir
from concourse.bass_utils import run_bass_kernel_spmd

F32 = mybir.dt.float32
BF16 = mybir.dt.bfloat16
I32 = mybir.dt.int32
AF = mybir.ActivationFunctionType
ALU = mybir.AluOpType
AX = mybir.AxisListType

D = 1024
DIN = 2080
NCORES = 8
ENGS = ["pe", "act", "dve", "pool", "sp"]


class Sched:
    def __init__(self, nc):
        self.nc = nc
        self.ops = {e: [] for e in ENGS}
        self.count = {e: 0 for e in ENGS}
        self.seen = {e: {} for e in ENGS}
        self.last_write = {}
        self.readers = {}
        self.dma_count = {}
        self.sem_names = list(ENGS)
        self.sems = {}
        self.rr = 0

    def _deps(self, eng, reads, writes):
        deps = {}

        def add(tok):
            if tok is not None and deps.get(tok[0], 0) < tok[1]:
                deps[tok[0]] = tok[1]

        for r in reads:
            add(self.last_write.get(r))
        for w in writes:
            add(self.last_write.get(w))
            for t in self.readers.get(w, ()):
                add(t)
        out = []
        for k, v in deps.items():
            if self.seen[eng].get(k, 0) < v:
                self.seen[eng][k] = v
                out.append((k, v))
        return out

    def _commit(self, tok, reads, writes):
        for r in reads:
            lst = self.readers.setdefault(r, [])
            lst[:] = [t for t in lst if t[0] != tok[0]]
            lst.append(tok)
        for w in writes:
            self.last_write[w] = tok
            self.readers[w] = []

    def op(self, eng, fn, reads=(), writes=()):
        waits = self._deps(eng, reads, writes)
        self.count[eng] += 1
        tok = (eng, self.count[eng])
        self.ops[eng].append((waits, fn, (eng, 1)))
        self._commit(tok, reads, writes)
        return tok

    def dma(self, eng, fn, key, reads=(), writes=()):
        waits = self._deps(eng, reads, writes)
        k = ("dma", key)
        if k not in self.dma_count:
            self.dma_count[k] = 0
            self.sem_names.append(k)
        self.dma_count[k] += 16
        tok = (k, self.dma_count[k])
        self.ops[eng].append((waits, fn, (k, 16)))
        self._commit(tok, reads, writes)
        return tok

    def barrier(self):
        toks = [(e, self.count[e]) for e in ENGS if self.count[e] > 0]
        toks += [(k, v) for k, v in self.dma_count.items()]
        for e in ENGS:
            waits = []
            for k, v in toks:
                if k != e and self.seen[e].get(k, 0) < v:
                    self.seen[e][k] = v
                    waits.append((k, v))
            if waits:
                self.ops[e].append((waits, None, None))

    def flush(self, stack):
        nc = self.nc
        for k in self.sem_names:
            if k not in self.sems:
                nm = "s_" + "_".join(str(x) for x in (k if isinstance(k, tuple) else (k,)))
                self.sems[k] = stack.enter_context(nc.semaphore(nm))
        sems = self.sems
        ops = self.ops
        self.ops = {e: [] for e in ENGS}
        with nc.Block() as block:
            def replay(engname):
                def body(e):
                    for waits, fn, inc in ops[engname]:
                        for k, v in waits:
                            e.wait_ge(sems[k], v)
                        if fn is not None:
                            fn(e).then_inc(sems[inc[0]], inc[1])
                return body

            block.tensor(replay("pe"))
            block.scalar(replay("act"))
            block.vector(replay("dve"))
            block.gpsimd(replay("pool"))
            block.sync(replay("sp"))


def _pk(v, p=128):
    v = np.asarray(v)
    n = v.shape[-1] // p
    return np.ascontiguousarray(np.swapaxes(v.reshape(v.shape[:-1] + (n, p)), -1, -2))


def prep_inputs(inp, S):
    L = inp["ada_w"].shape[0]
    f32 = np.float32
    sh = {}
    sh["ada_w"] = np.ascontiguousarray(inp["ada_w"], f32)
    sh["ada_b"] = _pk(inp["ada_b"])
    sh["n1g"] = _pk(inp["norm1_g"])
    sh["n2g"] = _pk(inp["norm2_g"])
    sh["w_in"] = np.ascontiguousarray(inp["w_in"], f32)
    sh["w_out"] = np.ascontiguousarray(inp["w_out"], f32)
    sh["mu"] = _pk(inp["rwkv_mu"])
    rv = np.stack([inp["rwkv_w0"], inp["rwkv_a0"], inp["rwkv_k_k"], inp["rwkv_k_a"],
                   inp["rwkv_r_k"].reshape(L, 256), inp["rwkv_ln_w"], inp["rwkv_ln_b"]], axis=1)
    sh["rvec"] = np.ascontiguousarray(np.transpose(_pk(rv), (0, 2, 1, 3)))
    sh["lora"] = np.ascontiguousarray(np.concatenate([inp["rwkv_w2"], inp["rwkv_a2"], inp["rwkv_g2"]], axis=1))

    def s5vec(a):
        return np.ascontiguousarray(a.reshape(L, 8, 2, 64).transpose(0, 2, 3, 1).reshape(L, 128, 8))
    ldt = np.broadcast_to(inp["s5_log_dt"][:, :, None], (L, 16, 64))
    sh["s5v"] = np.ascontiguousarray(np.stack([s5vec(inp["s5_lambda_re"]), s5vec(inp["s5_lambda_im"]), s5vec(ldt)], axis=2))
    bpad = np.zeros((L, 2, 8, 128, 128), f32)
    cpad = np.zeros((L, 2, 8, 128, 128), f32)
    for ri, (bb, cc) in enumerate([(inp["s5_b_re"], inp["s5_c_re"]), (inp["s5_b_im"], inp["s5_c_im"])]):
        for g in range(16):
            jt, q = g // 2, g % 2
            r0 = (g % 8) * 16
            bpad[:, ri, jt, r0:r0 + 16, q * 64:(q + 1) * 64] = np.transpose(bb[:, g], (0, 2, 1))
            cpad[:, ri, jt, q * 64:(q + 1) * 64, r0:r0 + 16] = np.transpose(cc[:, g], (0, 2, 1))
    sh["s5b"] = bpad
    sh["s5c"] = cpad
    sv = np.stack([inp["s5_d"], inp["s5_glu_b"], inp["branch_norm_g"][:, 0]], axis=1)
    sh["s5vec2"] = np.ascontiguousarray(np.transpose(_pk(sv), (0, 2, 1, 3)))
    sh["glu_w"] = np.ascontiguousarray(inp["s5_glu_w"], f32)
    sh["qng"] = _pk(inp["mla_q_norm_g"])
    sh["w_uq"] = np.ascontiguousarray(inp["mla_w_uq"], f32)
    sh["kvng"] = _pk(inp["mla_kv_norm_g"])
    wkv = inp["mla_w_ukv"].reshape(L, 128, 4, 128)
    kn = np.zeros((L, 128, 4, 96), f32)
    kn[..., :64] = wkv[..., :64]
    sh["w_ukn"] = kn
    sh["w_ukv"] = np.ascontiguousarray(wkv[..., 64:].reshape(L, 128, 256))
    sh["qkhg"] = np.ascontiguousarray(np.stack([inp["mla_q_head_g"], inp["mla_k_head_g"]], axis=2))
    cw = inp["lru_conv_w"]
    lv = np.concatenate([cw, inp["lru_conv_b"][:, None], inp["lru_b_a"][:, None], inp["lru_b_x"][:, None],
                         inp["lru_lambda"][:, None], inp["branch_norm_g"][:, 2][:, None]], axis=1)
    sh["lruv"] = np.ascontiguousarray(np.transpose(_pk(lv), (0, 2, 1, 3)))
    bd = np.zeros((L, 2, 2, 128, 128), f32)
    for wi, wmat in enumerate([inp["lru_w_a"], inp["lru_w_x"]]):
        for n in range(4):
            t, q = n // 2, n % 2
            bd[:, wi, t, q * 64:(q + 1) * 64, q * 64:(q + 1) * 64] = wmat[:, n]
    sh["lrubd"] = bd
    sh["bng_c"] = np.ascontiguousarray(inp["branch_norm_g"][:, 1].reshape(L, 4, 64).transpose(0, 2, 1))
    sh["wr"] = np.ascontiguousarray(np.concatenate([inp["moe_w_group"], inp["moe_w_expert"]], axis=2))
    br = np.concatenate([inp["moe_b_group"], inp["moe_b_expert"]], axis=1)
    sh["br"] = np.ascontiguousarray(np.broadcast_to(br[:, None, :], (L, 128, 36)))
    sh["w1"] = np.ascontiguousarray(inp["moe_w1"], f32)
    sh["w3"] = np.ascontiguousarray(inp["moe_w3"], f32)
    sh["w2"] = np.ascontiguousarray(inp["moe_w2"], f32)
    cst = {}
    cst["ident"] = np.eye(128, dtype=f32)
    bo = np.zeros((128, 128), f32)
    bo[:64, :64] = 1.0
    bo[64:, 64:] = 1.0
    cst["blk"] = bo
    half = 16
    invf = np.power(np.float32(10000.0), -np.arange(half, dtype=f32) * np.float32(2.0) / np.float32(32)).astype(f32)
    iv = np.zeros((96, 1), f32)
    iv[64:80, 0] = invf
    iv[80:96, 0] = invf
    cst["invf"] = iv
    PT = np.zeros((128, 128), f32)
    for i in range(16):
        PT[80 + i, 64 + i] = -1.0
        PT[64 + i, 80 + i] = 1.0
    cst["rotT"] = PT
    E = np.zeros((32, 96), f32)
    for i in range(32):
        E[i, 64 + i] = 1.0
    cst["epe"] = E
    jj = np.arange(64)[:, None]
    ii = np.arange(64)[None, :]
    mk = np.concatenate([(jj < ii), (jj <= ii)], axis=1).astype(f32)
    cst["mk"] = np.ascontiguousarray(np.broadcast_to(mk[:, None, :], (64, 4, 128)))
    cst["mkl"] = np.ascontiguousarray(np.broadcast_to((jj > ii).astype(f32)[:, None, :], (64, 4, 64)))
    cst["id4"] = np.ascontiguousarray(np.broadcast_to(np.eye(64, dtype=f32)[:, None, :], (64, 4, 64)))
    sel = np.zeros((128, 64), f32)
    sel[64, :] = 1.0
    cst["sel65"] = sel
    s32 = np.zeros((128, 32, 128), f32)
    for e in range(32):
        s32[e, e, :] = 1.0
    cst["sel32"] = s32
    cst["tvals"] = np.ascontiguousarray(np.broadcast_to(np.arange(NB, dtype=f32)[None, :], (128, NB)))
    per_core = []
    for b in range(NCORES):
        d = {}
        d["xT"] = np.ascontiguousarray(inp["x"][b, :S].T, f32)
        d["c"] = _pk(inp["c"][b])
        d["pos"] = np.ascontiguousarray(np.broadcast_to(inp["pos_offset"][b].astype(np.int32).reshape(1, 1), (96, 1)))
        per_core.append(d)
    return sh, cst, per_core


class K:
    pass


def build(S, L, shapes, stop=None, dbg=None):
    nc = bass.Bass("TRN2", target_bir_lowering=False)
    k = K()
    k.nc = nc
    k.S = S
    k.L = L
    dr = {}
    for name, (shp, dt) in shapes.items():
        dr[name] = nc.dram_tensor(name, list(shp), dt, kind="ExternalInput").ap()
    dr["out"] = nc.dram_tensor("outT", [D, S], F32, kind="ExternalOutput").ap()
    k.mixT = nc.dram_tensor("mixT", [D, S], BF16, kind="Internal").ap()
    k.x1T = nc.dram_tensor("x1T", [D, S], F32, kind="Internal").ap()
    k.x2T = nc.dram_tensor("x2T", [D, S], F32, kind="Internal").ap()
    k.h2T = nc.dram_tensor("h2T", [D, S], BF16, kind="Internal").ap()
    k.gT = nc.dram_tensor("gT", [32, S], F32, kind="Internal").ap()
    NE = shapes["w1"][0][1]
    k.w1b = nc.dram_tensor("w1b", [NE, 128, 4096], BF16, kind="Internal").ap()
    k.w3b = nc.dram_tensor("w3b", [NE, 128, 4096], BF16, kind="Internal").ap()
    k.w2b = nc.dram_tensor("w2b", [NE, 128, 4096], BF16, kind="Internal").ap()
    k.qs = nc.dram_tensor("qs", [96, 4, S], BF16, kind="Internal").ap()
    k.ks = nc.dram_tensor("ks", [96, 4, S], BF16, kind="Internal").ap()
    k.vs = nc.dram_tensor("vs", [S // 128, 128, 260], BF16, kind="Internal").ap()
    if dbg is not None:
        dr["dbg"] = nc.dram_tensor("dbg", list(dbg), F32, kind="ExternalOutput").ap()
    k.dr = dr
    with ExitStack() as st:
        k.st = st
        S_ = Sched(nc)
        k.S_ = S_
        emit_all(k, stop)
        S_.barrier()
        S_.flush(st)
    return nc


def sb(k, st, name, shape, dt=F32):
    k.uid = getattr(k, "uid", 0) + 1
    return st.enter_context(k.nc.sbuf_tensor("sb%d_%s" % (k.uid, name), list(shape), dt))


class Ops:
    def __init__(self, S_):
        self.S = S_

    def dma(self, eng, out, in_, key, r=(), w=()):
        return self.S.dma(eng, lambda e: e.dma_start(out=out, in_=in_), key, r, w)

    def act(self, out, in_, func, r, w, bias=None, scale=None, eng="act"):
        kw = {}
        if bias is not None:
            kw["bias"] = bias
        if scale is not None:
            kw["scale"] = scale
        return self.S.op(eng, lambda e: e.activation(out=out, in_=in_, func=func, **kw), r, w)

    def tt(self, out, in0, in1, op, r, w, eng="dve"):
        return self.S.op(eng, lambda e: e.tensor_tensor(out=out, in0=in0, in1=in1, op=op), r, w)

    def ts(self, out, in0, s1, s2, op0, op1, r, w, eng="dve"):
        if s2 is None:
            return self.S.op(eng, lambda e: e.tensor_scalar(out=out, in0=in0, scalar1=s1, scalar2=None, op0=op0), r, w)
        return self.S.op(eng, lambda e: e.tensor_scalar(out=out, in0=in0, scalar1=s1, scalar2=s2, op0=op0, op1=op1), r, w)

    def stt(self, out, in0, scalar, in1, op0, op1, r, w, eng="dve"):
        return self.S.op(eng, lambda e: e.scalar_tensor_tensor(out=out, in0=in0, scalar=scalar, in1=in1, op0=op0, op1=op1), r, w)

    def copy(self, out, in_, r, w, eng="dve"):
        if eng == "act":
            return self.S.op(eng, lambda e: e.activation(out=out, in_=in_, func=AF.Copy), r, w)
        return self.S.op(eng, lambda e: e.tensor_copy(out=out, in_=in_), r, w)

    def memset(self, out, val, w, eng="pool"):
        return self.S.op(eng, lambda e: e.memset(out, val), (), w)

    def scan(self, out, d0, d1, init, r, w, eng="dve"):
        return self.S.op(eng, lambda e: e.tensor_tensor_scan(out=out, data0=d0, data1=d1, initial=init, op0=ALU.mult, op1=ALU.add), r, w)

    def mm(self, out, lhsT, rhs, start, stop, r, w):
        return self.S.op("pe", lambda e: e.matmul(out, lhsT, rhs, start=start, stop=stop), r, w)

    def tr(self, out, in_, ident, r, w):
        return self.S.op("pe", lambda e: e.transpose(out, in_, ident), r, w)


NB = 256


def emit_all(k, stop):
    nc, S_, dr, S, L = k.nc, k.S_, k.dr, k.S, k.L
    st = k.st
    o = Ops(S_)
    nblk = S // NB
    ps = [st.enter_context(nc.psum_tensor(f"ps{i}", [128, 512], F32)) for i in range(8)]
    P = [f"ps{i}" for i in range(8)]
    ident = sb(k, st, "ident", [128, 128])
    blk = sb(k, st, "blk", [128, 128])
    blkb = sb(k, st, "blkb", [128, 128], BF16)
    onesb = sb(k, st, "onesb", [128, 128], BF16)
    mods = sb(k, st, "mods", [128, L, 48])
    g1s = sb(k, st, "g1s", [128, L, 8])
    g2s = sb(k, st, "g2s", [128, L, 8])
    k.ident, k.blk, k.blkb, k.onesb, k.mods, k.g1s, k.g2s, k.ps, k.P, k.o = ident, blk, blkb, onesb, mods, g1s, g2s, ps, P, o
    o.dma("sp", ident[:], dr["ident"], "ident", w=["ident"])
    o.dma("sp", blk[:], dr["blk"], "blk", w=["blk"])
    o.copy(blkb[:], blk[:], ["blk"], ["blkb"])
    o.memset(onesb[:], 1.0, ["onesb"])
    ones32 = sb(k, st, "ones32", [128, 128])
    k.ones32 = ones32
    o.memset(ones32[:], 1.0, ["ones32"])
    cc = sb(k, st, "cc", [128, 8])
    k.cc = cc
    for val, c in CC.items():
        o.memset(cc[:, c:c + 1], float(val), ["cc"])

    with ExitStack() as p0:
        stg = [sb(k, p0, f"adst{i}", [128, 8, 768]) for i in range(2)]
        cond = sb(k, p0, "cond", [128, 8])
        adab = sb(k, p0, "adab", [128, 48])
        ng = sb(k, p0, "ng", [128, 8])
        tmp8 = sb(k, p0, "tmp8", [128, 8])
        o.dma("sp", cond[:], dr["c"], "cond", w=["cond"])
        o.act(cond[:], cond[:], AF.Silu, ["cond"], ["cond"])
        for l in range(L):
            o.dma("sp", adab[:], dr["ada_b"][l], "adab", w=["adab"])
            wv = dr["ada_w"][l].rearrange("(kk p) n -> p kk n", p=128)
            for c in range(8):
                bk = f"adst{c % 2}"
                o.dma("sp" if c % 2 == 0 else "pool", stg[c % 2][:], wv[:, :, c * 768:(c + 1) * 768], bk, w=[bk])
                for j in range(6):
                    col = c * 6 + j
                    for kk in range(8):
                        o.mm(ps[0][:, col:col + 1], stg[c % 2][:, kk, j * 128:(j + 1) * 128], cond[:, kk:kk + 1],
                             kk == 0, kk == 7, [bk, "cond"], [P[0]])
            o.tt(mods[:, l, :], ps[0][:, 0:48], adab[:], ALU.add, [P[0], "adab"], ["mods"])
            for (gs, nm, c0) in ((g1s, "n1g", 8), (g2s, "n2g", 32)):
                o.dma("sp", ng[:], dr[nm][l], "ng", w=["ng"])
                o.ts(tmp8[:], mods[:, l, c0:c0 + 8], 1.0, None, ALU.add, None, ["mods"], ["tmp8"])
                o.tt(gs[:, l, :], tmp8[:], ng[:], ALU.mult, ["tmp8", "ng"], ["g1s" if c0 == 8 else "g2s"])
        S_.barrier()
        S_.flush(st)
    if stop == "p0":
        o.dma("sp", dr["dbg"][:, 0:L * 48], mods[:].rearrange("p l c -> p (l c)"), "dbg", r=["mods"], w=["dbg"])
        return

    xin = dr["xT"]
    for l in range(L):
        phaseA(k, l, xin, stop)
        if stop is not None and stop.startswith("A"):
            return
        phaseB(k, l, xin, k.x1T, stop)
        if stop is not None and stop.startswith("B"):
            return
        phaseW(k, l)
        xo = dr["out"] if l == L - 1 else k.x2T
        phaseC(k, l, k.x1T, xo, stop)
        if stop is not None and stop.startswith("C"):
            return
        xin = xo


CC = {1e-6: 0, 64e-5: 1, 1.0: 2, -math.pi: 3, 0.0: 4, 1e-24: 5}


def rsqrt(k, out, in_, scale, eps, rkeys, wkey, rows=128):
    c = CC[eps]
    k.o.act(out, in_, AF.Sqrt, list(rkeys) + ["cc"], [wkey], bias=k.cc[0:rows, c:c + 1], scale=scale)
    k.o.S.op("dve", lambda e: e.reciprocal(out=out, in_=out), [wkey], [wkey])


def rms_stats(k, srcs, n_feat, eps, ps_ap, rstd_ap, sq_bufs, rkeys, pkey, wkey, lhsT=None, rows=128):
    o = k.o
    n = len(srcs)
    lhs = k.onesb[:rows, :rows] if lhsT is None else lhsT
    for i, (ap, key) in enumerate(srcs):
        sq, sqk = sq_bufs[i]
        o.act(sq, ap, AF.Square, [key], [sqk])
        o.mm(ps_ap, lhs, sq, i == 0, i == n - 1, [sqk, "onesb"], [pkey])
    rsqrt(k, rstd_ap, ps_ap, 1.0 / n_feat, eps, [pkey], wkey, rows)


def phaseA(k, l, xin, stop):
    nc, S_, dr, S, L, o = k.nc, k.S_, k.dr, k.S, k.L, k.o
    ps, P = k.ps, k.P
    nblk = S // NB
    with ExitStack() as pa:
        w_in = sb(k, pa, "w_in", [128, 8, DIN], BF16)
        xblk = sb(k, pa, "xblk", [128, 8, NB])
        wv = dr["w_in"][l].rearrange("(kk p) n -> p kk n", p=128)
        xflat = xblk[:].rearrange("p a b -> p (a b)")
        for c in range(20):
            stv = xflat[:, (c % 2) * 832:(c % 2 + 1) * 832].rearrange("p (kk n) -> p kk n", n=104)
            o.dma("sp", stv, wv[:, :, c * 104:(c + 1) * 104], f"xblk{c % 2}", w=["xblk"])
            o.copy(w_in[:, :, c * 104:(c + 1) * 104], stv, ["xblk"], ["w_in"], eng=("dve" if c % 2 == 0 else "pool"))
        rstd = sb(k, pa, "rstd", [128, NB])
        xt = sb(k, pa, "xt", [128, NB])
        hT = sb(k, pa, "hT", [128, 8, NB], BF16)
        sqb = hT
        zr = sb(k, pa, "zr", [128, 7, NB + 1])
        s5u = sb(k, pa, "s5u", [128, 2, NB])
        qab = sb(k, pa, "qab", [128, 2, NB], BF16)
        kvab = sb(k, pa, "kvab", [128, NB], BF16)
        kpeb = sb(k, pa, "kpeb", [32, NB], BF16)
        lrx = sb(k, pa, "lrx", [128, 2, NB + 3])
        lrg = sb(k, pa, "lrg", [128, 2, NB])
        R = rwkv_setup(k, pa, l)
        Q = s5_setup(k, pa, l, xblk) if stop not in ("A1",) and not (stop or "").startswith("A2") else None
        U = lru_setup(k, pa, l, xblk) if stop not in ("A1",) and not (stop or "").startswith("A2") else None
        mixT = k.mixT
        A = mla_setup(k, pa, l, xblk, Q, U) if Q is not None else None
        o.memset(zr[:, :, 0:1], 0.0, ["zr"])
        o.memset(lrx[:, :, 0:3], 0.0, ["lrx"])
        xv = xin.rearrange("(kk p) s -> p kk s", p=128)
        tiles = [(t * 128, 128) for t in range(12)] + [(1536, 32)] + [(1568 + t * 128, 128) for t in range(4)]
        for i in range(nblk):
            t0 = i * NB
            o.dma("sp", xblk[:], xv[:, :, t0:t0 + NB], "xblk", w=["xblk"])
            rms_stats(k, [(xblk[:, kk, :], "xblk") for kk in range(8)], 1024.0, 1e-6, ps[0][:, 0:NB], rstd[:],
                      [(sqb[:, kk, :], "hT") for kk in range(8)], None, P[0], "rstd")
            for kk in range(8):
                o.tt(xt[:], xblk[:, kk, :], rstd[:], ALU.mult, ["xblk", "rstd"], ["xt"])
                o.act(hT[:, kk, :], xt[:], AF.Identity, ["xt", "g1s", "mods"], ["hT"],
                      bias=k.mods[:, l, kk:kk + 1], scale=k.g1s[:, l, kk:kk + 1])
            for ti, (c0, m) in enumerate(tiles):
                pb = 1 + (ti % 3)
                pt = ps[pb][0:m, 0:NB]
                for kk in range(8):
                    o.mm(pt, w_in[:, kk, c0:c0 + m], hT[:, kk, :], kk == 0, kk == 7, ["w_in", "hT"], [P[pb]])
                if ti < 7:
                    o.copy(zr[:, ti, 1:NB + 1], pt, [P[pb]], ["zr"], eng=("act" if ti % 2 == 0 else "dve"))
                elif ti < 9:
                    o.copy(s5u[:, ti - 7, :], pt, [P[pb]], ["s5u"], eng="dve")
                elif ti < 11:
                    o.copy(qab[:, ti - 9, :], pt, [P[pb]], ["qab"], eng="act")
                elif ti == 11:
                    o.copy(kvab[:], pt, [P[pb]], ["kvab"], eng="act")
                elif ti == 12:
                    o.copy(kpeb[:], pt, [P[pb]], ["kpeb"], eng="act")
                elif ti < 15:
                    o.copy(lrx[:, ti - 13, 3:NB + 3], pt, [P[pb]], ["lrx"], eng="dve")
                else:
                    o.copy(lrg[:, ti - 15, :], pt, [P[pb]], ["lrg"], eng="act")
            if stop != "A1":
                rwkv_block(k, l, i, R, zr, stop)
            if stop is None or not stop.startswith("A2"):
                o.dma("pool", mixT[0:256, t0:t0 + NB].rearrange("(t p) s -> p t s", p=128), R["youtb"][:], "mix_a", r=["youtb"], w=["mixT_a"])
                s5_block(k, l, i, Q, s5u, mixT, t0)
                lru_block(k, l, i, U, lrx, lrg, mixT, t0)
                mla_block(k, l, i, A, qab, kvab, kpeb, t0)
            if stop == "A4":
                dbg = dr["dbg"]
                o.copy(A["t1"][:], A["qrb"][:, 1, :], ["a_out"], ["l_aa"], eng="dve")
                o.dma("sp", dbg[0:96, t0:t0 + NB], A["t1"][:], "dbg", r=["l_aa"], w=["dbg"])
                o.copy(A["t2"][:], A["krb"][:, 2, :], ["a_out"], ["l_t1"], eng="dve")
                o.dma("sp", dbg[96:192, t0:t0 + NB], A["t2"][:], "dbg", r=["l_t1"], w=["dbg"])
                for tt in range(NB // 128):
                    o.copy(A["qf"][0:64, 0:128], A["Vt"][0:64, tt, 3, 0:128].rearrange("p c -> p c") if False else A["Vt"][0:64, tt, :, :].rearrange("p h c -> p (h c)")[:, 0:128], ["a_Vt"], ["l_xc"], eng="dve")
                    o.dma("sp", dbg[192:256, t0 + tt * 128:t0 + (tt + 1) * 128], A["qf"][0:64, 0:128], "dbg", r=["l_xc"], w=["dbg"])
                continue
            if stop == "A3":
                dbg = dr["dbg"]
                o.dma("sp", dbg[256:512, t0:t0 + NB].rearrange("(t p) s -> p t s", p=128), Q["yo"][:], "dbg", r=["s_yo"], w=["dbg"])
                o.dma("sp", dbg[768:1024, t0:t0 + NB].rearrange("(t p) s -> p t s", p=128), U["yo"][:], "dbg", r=["l_yo"], w=["dbg"])
                o.dma("sp", dbg[0:256, t0:t0 + NB].rearrange("(t p) s -> p t s", p=128), R["yout"][:], "dbg", r=["yout"], w=["dbg"])
                continue
            if stop is not None and stop.startswith("A2"):
                dbg = dr["dbg"]
                o.dma("sp", dbg[0:256, t0:t0 + NB].rearrange("(t p) s -> p t s", p=128), R["yout"][:], "dbg", r=["yout"], w=["dbg"])
                continue
            if stop == "A1":
                dbg = dr["dbg"]
                o.dma("sp", dbg[0:896, t0:t0 + NB].rearrange("(t p) s -> p t s", p=128), zr[:, :, 1:NB + 1], "dbg", r=["zr"], w=["dbg"])
                o.dma("sp", dbg[896:1152, t0:t0 + NB].rearrange("(t p) s -> p t s", p=128), s5u[:], "dbg", r=["s5u"], w=["dbg"])
                o.dma("sp", dbg[1568:1824, t0:t0 + NB].rearrange("(t p) s -> p t s", p=128), lrx[:, :, 3:NB + 3], "dbg", r=["lrx"], w=["dbg"])
                o.dma("sp", dbg[1824:2080, t0:t0 + NB].rearrange("(t p) s -> p t s", p=128), lrg[:], "dbg", r=["lrg"], w=["dbg"])
                continue
        S_.barrier()
        S_.flush(k.st)


def rwkv_setup(k, pa, l):
    o, dr = k.o, k.dr
    R = {}
    nch = NB // 64
    f2 = [128, 2, NB]
    for nm in ("gg", "bonus", "epos", "bt", "kt", "ya", "yout"):
        R[nm] = sb(k, pa, "r_" + nm, f2)
    for nm in ("sg", "aa", "kk", "tA", "tB", "kmod", "cls", "eneg", "eex"):
        R[nm] = sb(k, pa, "r_" + nm, [128, 1, NB])
    R["zs"] = sb(k, pa, "r_zs", [128, 7, NB])
    R["youtb"] = sb(k, pa, "r_youtb", [128, 2, NB], BF16)
    R["lob"] = sb(k, pa, "r_lob", [128, NB], BF16)
    R["sqb"] = sb(k, pa, "r_sqb", [128, NB], BF16)
    R["ar"] = sb(k, pa, "r_ar", [128, 2, nch, 2, 64])
    R["tok"] = sb(k, pa, "r_tok", [128, 3, 2, 128])
    R["NL"] = [sb(k, pa, f"r_NL{i}", [64, 2, 4, 64]) for i in range(2)]
    R["Pm"] = [sb(k, pa, f"r_Pm{i}", [64, 4, 64]) for i in range(2)]
    R["LakT"] = sb(k, pa, "r_LakT", [64, 4, 64])
    R["QrbT"] = sb(k, pa, "r_QrbT", [128, 4, 64])
    R["QrkT"] = sb(k, pa, "r_QrkT", [128, 4, 64])
    R["Xs"] = sb(k, pa, "r_Xs", [64, 256])
    R["Mtmp"] = sb(k, pa, "r_Mtmp", [128, 128])
    R["Us"] = sb(k, pa, "r_Us", [128, 256])
    R["M"] = [sb(k, pa, f"r_M{i}", [128, 2, 128]) for i in range(2)]
    R["mu"] = sb(k, pa, "r_mu", [128, 7])
    R["omu"] = sb(k, pa, "r_omu", [128, 7])
    R["rvec"] = sb(k, pa, "r_rvec", [128, 7, 2])
    R["lst"] = sb(k, pa, "r_lst", [128, 256])
    R["lora"] = sb(k, pa, "r_lora", [128, 256], BF16)
    R["mk"] = sb(k, pa, "r_mk", [64, 4, 128])
    R["mkl"] = sb(k, pa, "r_mkl", [64, 4, 64])
    R["id4"] = sb(k, pa, "r_id4", [64, 4, 64])
    R["cmask"] = sb(k, pa, "r_cmask", [128, NB])
    o.dma("sp", R["mu"][:], dr["mu"][l], "r_mu", w=["r_mu"])
    o.ts(R["omu"][:], R["mu"][:], -1.0, 1.0, ALU.mult, ALU.add, ["r_mu"], ["r_omu"])
    o.dma("sp", R["rvec"][:], dr["rvec"][l], "r_rvec", w=["r_rvec"])
    o.dma("sp", R["lst"][:], dr["lora"][l], "r_lst", w=["r_lst"])
    o.copy(R["lora"][:], R["lst"][:], ["r_lst"], ["r_lora"])
    o.dma("sp", R["mk"][:], dr["mk"], "r_mk", w=["r_mk"])
    o.dma("sp", R["mkl"][:], dr["mkl"], "r_mkl", w=["r_mkl"])
    o.dma("sp", R["id4"][:], dr["id4"], "r_id4", w=["r_id4"])
    o.memset(R["cmask"][:], 1.0, ["r_cmask"])
    o.memset(R["cmask"][:].rearrange("p (c j) -> p c j", j=64)[:, :, 0:1], 0.0, ["r_cmask"])
    o.memset(R["M"][0][:], 0.0, ["r_M0"])
    o.memset(R["tok"][:], 0.0, ["r_tok"])
    o.memset(R["Us"][:], 0.0, ["r_Us"])
    o.memset(R["QrbT"][:], 0.0, ["r_QrbT"])
    o.memset(R["QrkT"][:], 0.0, ["r_QrkT"])
    return R


C0 = -math.exp(-0.5)


def rwkv_block(k, l, i, R, zr, stop):
    o, ps, P = k.o, k.ps, k.P
    nch = NB // 64
    zs, rv = R["zs"], R["rvec"]
    W0, A0, KK, KA, RK, LNW, LNB = range(7)
    for t in range(7):
        eng = "dve" if t % 2 == 0 else "pool"
        o.ts(zs[:, t, :], zr[:, t, 0:NB], R["mu"][:, t:t + 1], None, ALU.mult, None, ["zr", "r_mu"], [("zs", t)], eng=eng)
        o.stt(zs[:, t, :], zr[:, t, 1:NB + 1], R["omu"][:, t:t + 1], zs[:, t, :], ALU.mult, ALU.add, ["zr", "r_omu", ("zs", t)], [("zs", t)])
    o.copy(zr[:, :, 0:1], zr[:, :, NB:NB + 1], ["zr"], ["zr"], eng="dve")
    lob = R["lob"]
    o.act(lob[0:32, :], zs[0:32, 6, :], AF.Tanh, [("zs", 6)], ["r_lob"])
    o.act(lob[64:128, :], zs[64:128, 6, :], AF.Sigmoid, [("zs", 6)], ["r_lob"])
    o.copy(lob[32:64, :], zs[32:64, 6, :], [("zs", 6)], ["r_lob"], eng="dve")
    lora = R["lora"]
    for p in range(2):
        cs = slice(p * 128, (p + 1) * 128)
        rT, kT, vT = zs[:, p, :], zs[:, 2 + p, :], zs[:, 4 + p, :]
        rk_, kk_, vk_ = ("zs", p), ("zs", 2 + p), ("zs", 4 + p)
        o.mm(ps[1][:, 0:NB], lora[0:32, cs], lob[0:32, :], True, True, ["r_lora", "r_lob"], [P[1]])
        o.mm(ps[2][:, 0:NB], lora[32:64, cs], lob[32:64, :], True, True, ["r_lora", "r_lob"], [P[2]])
        o.mm(ps[3][:, 0:NB], lora[64:128, cs], lob[64:128, :], True, True, ["r_lora", "r_lob"], [P[3]])
        o.act(R["sg"][:, 0, :], ps[1][:, 0:NB], AF.Sigmoid, [P[1], "r_rvec"], ["r_sg"], bias=rv[:, W0, p:p + 1])
        o.act(R["aa"][:, 0, :], ps[2][:, 0:NB], AF.Sigmoid, [P[2], "r_rvec"], ["r_aa"], bias=rv[:, A0, p:p + 1])
        o.copy(R["gg"][:, p, :], ps[3][:, 0:NB], [P[3]], ["r_gg"], eng="dve")
        kk = R["kk"][:, 0, :]
        o.ts(kk, kT, rv[:, KK, p:p + 1], None, ALU.mult, None, [kk_, "r_rvec"], ["r_kk"])
        o.act(R["sqb"][:], kk, AF.Square, ["r_kk"], ["r_sqb"])
        o.mm(ps[1][:, 0:NB], k.blkb[:], R["sqb"][:], True, True, ["blkb", "r_sqb"], [P[1]])
        rsqrt(k, R["tA"][:, 0, :], ps[1][:, 0:NB], 1.0, 1e-24, [P[1]], "r_tA")
        o.tt(kk, kk, R["tA"][:, 0, :], ALU.mult, ["r_kk", "r_tA"], ["r_kk"])
        o.ts(R["tA"][:, 0, :], R["aa"][:, 0, :], -1.0, rv[:, KA, p:p + 1], ALU.add, ALU.mult, ["r_aa", "r_rvec"], ["r_tA"])
        o.stt(R["kmod"][:, 0, :], R["tA"][:, 0, :], 1.0, kT, ALU.add, ALU.mult, ["r_tA", kk_], ["r_kmod"])
        o.tt(R["tA"][:, 0, :], rT, R["kmod"][:, 0, :], ALU.mult, [rk_, "r_kmod"], ["r_tA"])
        o.ts(R["sqb"][:], R["tA"][:, 0, :], rv[:, RK, p:p + 1], None, ALU.mult, None, ["r_tA", "r_rvec"], ["r_sqb"])
        o.mm(ps[2][:, 0:NB], k.blkb[:], R["sqb"][:], True, True, ["blkb", "r_sqb"], [P[2]])
        o.tt(R["bonus"][:, p, :], ps[2][:, 0:NB], vT, ALU.mult, [P[2], vk_], ["r_bonus"])
        o.scan(R["cls"][:, 0, :], R["cmask"][:], R["sg"][:, 0, :], 0.0, ["r_cmask", "r_sg"], ["r_cls"])
        o.act(R["epos"][:, p, :], R["cls"][:, 0, :], AF.Exp, ["r_cls"], ["r_epos"], scale=C0)
        o.act(R["eneg"][:, 0, :], R["cls"][:, 0, :], AF.Exp, ["r_cls"], ["r_eneg"], scale=-C0)
        o.tt(R["tB"][:, 0, :], R["cls"][:, 0, :], R["sg"][:, 0, :], ALU.subtract, ["r_cls", "r_sg"], ["r_tB"])
        o.act(R["eex"][:, 0, :], R["tB"][:, 0, :], AF.Exp, ["r_tB"], ["r_eex"], scale=C0)
        arv = R["ar"][:, p, :, :, :]
        o.tt(arv[:, :, 1, :], rT.rearrange("p (c j) -> p c j", j=64), R["epos"][:, p, :].rearrange("p (c j) -> p c j", j=64),
             ALU.mult, [rk_, "r_epos"], ["r_ar"])
        o.stt(arv[:, :, 0, :], kk.rearrange("p (c j) -> p c j", j=64), -1.0, R["eex"][:, 0, :].rearrange("p (c j) -> p c j", j=64),
              ALU.mult, ALU.mult, ["r_kk", "r_eex"], ["r_ar"])
        o.tt(R["kt"][:, p, :], R["kmod"][:, 0, :], R["eneg"][:, 0, :], ALU.mult, ["r_kmod", "r_eneg"], ["r_kt"])
        o.tt(R["tA"][:, 0, :], kk, R["aa"][:, 0, :], ALU.mult, ["r_kk", "r_aa"], ["r_tA"])
        o.tt(R["bt"][:, p, :], R["tA"][:, 0, :], R["eneg"][:, 0, :], ALU.mult, ["r_tA", "r_eneg"], ["r_bt"])
    ar, bt, kt, tok = R["ar"], R["bt"], R["kt"], R["tok"]
    NL, Pm = R["NL"], R["Pm"]
    if stop == "A2a":
        return
    for c in range(nch):
        gc = i * nch + c
        cs = slice(c * 64, (c + 1) * 64)
        for p in range(2):
            o.tr(ps[4][0:64, p * 128:(p + 1) * 128], bt[:, p, cs], k.ident[:], ["r_bt", "ident"], [P[4]])
            o.tr(ps[4][0:64, 256 + p * 128:256 + (p + 1) * 128], kt[:, p, cs], k.ident[:], ["r_kt", "ident"], [P[4]])
            o.tr(ps[5][0:64, p * 128:(p + 1) * 128], zs[:, 4 + p, cs], k.ident[:], [("zs", 4 + p), "ident"], [P[5]])
        o.copy(tok[0:64, 0:2, :, :].rearrange("j a p c -> j (a p c)"), ps[4][0:64, :], [P[4]], ["r_tok"], eng="act")
        o.copy(tok[0:64, 2, :, :].rearrange("j p c -> j (p c)"), ps[5][0:64, 0:256], [P[5]], ["r_tok"], eng="dve")
        if stop == "A2b":
            continue
        for h in range(4):
            p, q = h // 2, h % 2
            rs = slice(q * 64, (q + 1) * 64)
            arh = ar[rs, p, c, :, :].rearrange("k a j -> k (a j)")
            o.mm(ps[6][0:64, h * 128:(h + 1) * 128], bt[rs, p, cs], arh, True, True, ["r_bt", "r_ar"], [P[6]])
            o.mm(ps[7][0:64, h * 128:(h + 1) * 128], kt[rs, p, cs], arh, True, True, ["r_kt", "r_ar"], [P[7]])
            o.mm(ps[5][0:64, 256 + h * 64:256 + (h + 1) * 64], ar[rs, p, c, 0, :], bt[rs, p, cs], True, True, ["r_bt", "r_ar"], [P[5]])
        v6 = ps[6][0:64, :].rearrange("j (h x) -> j h x", x=128)
        v7 = ps[7][0:64, :].rearrange("j (h x) -> j h x", x=128)
        mk = R["mk"]
        o.tt(NL[0][:, 0, :, :], v6[:, :, 0:64], mk[:, :, 0:64], ALU.mult, [P[6], "r_mk"], ["r_NL0"])
        o.tt(R["QrbT"][0:64], v6[:, :, 64:128], mk[:, :, 64:128], ALU.mult, [P[6], "r_mk"], ["r_QrbT"])
        o.tt(R["LakT"][:], v7[:, :, 0:64], mk[:, :, 0:64], ALU.mult, [P[7], "r_mk"], ["r_LakT"])
        o.tt(R["QrkT"][0:64], v7[:, :, 64:128], mk[:, :, 64:128], ALU.mult, [P[7], "r_mk"], ["r_QrkT"])
        o.tt(NL[0][:, 1, :, :], ps[5][0:64, 256:512].rearrange("j (h x) -> j h x", x=64), R["mkl"][:], ALU.mult, [P[5], "r_mkl"], ["r_NL0"])
        o.tt(Pm[1][:], NL[0][:, 0, :, :], R["id4"][:], ALU.add, ["r_NL0", "r_id4"], ["r_Pm1"], eng="pool")
        if stop == "A2c":
            continue
        for m in range(1, 7):
            src, dst = NL[(m - 1) % 2], NL[m % 2]
            sk, dk = f"r_NL{(m - 1) % 2}", f"r_NL{m % 2}"
            pin, pout = Pm[(m - 1) % 2], Pm[m % 2]
            pik, pok = f"r_Pm{(m - 1) % 2}", f"r_Pm{m % 2}"
            for h in range(4):
                if m <= 5:
                    o.mm(ps[6][0:64, h * 64:(h + 1) * 64], src[:, 1, h, :], src[:, 0, h, :], True, True, [sk], [P[6]])
                    o.mm(ps[6][0:64, 256 + h * 64:256 + (h + 1) * 64], src[:, 0, h, :], src[:, 1, h, :], True, True, [sk], [P[6]])
                if m >= 2:
                    o.mm(ps[7][0:64, h * 64:(h + 1) * 64], src[:, 1, h, :], pin[:, h, :], True, True, [sk, pik], [P[7]])
            if m <= 5:
                o.copy(dst[:].rearrange("j a h x -> j (a h x)"), ps[6][0:64, :], [P[6]], [dk], eng="act")
            if m >= 2:
                o.tt(pout[:].rearrange("j h x -> j (h x)"), ps[7][0:64, 0:256], pin[:].rearrange("j h x -> j (h x)"), ALU.add, [P[7], pik], [pok])
            else:
                pass
        TT_ = Pm[0]
        if stop == "A2d":
            continue
        Mc, Mn = R["M"][gc % 2], R["M"][(gc + 1) % 2]
        mck, mnk = f"r_M{gc % 2}", f"r_M{(gc + 1) % 2}"
        for p in range(2):
            o.mm(ps[4][0:64, p * 128:(p + 1) * 128], ar[:, p, c, 0, :], Mc[:, p, :], True, False, ["r_ar", mck], [P[4]])
            for q in range(2):
                h = 2 * p + q
                o.mm(ps[4][0:64, p * 128 + q * 64:p * 128 + (q + 1) * 64], R["LakT"][:, h, :], tok[0:64, 2, p, q * 64:(q + 1) * 64],
                     False, q == 1, ["r_LakT", "r_tok"], [P[4]])
        o.copy(R["Xs"][:], ps[4][0:64, 0:256], [P[4]], ["r_Xs"], eng="dve")
        if stop == "A2e":
            continue
        for h in range(4):
            o.mm(ps[4][0:64, 256 + h * 64:256 + (h + 1) * 64], TT_[:, h, :], R["Xs"][:, h * 64:(h + 1) * 64], True, True, ["r_Pm0", "r_Xs"], [P[4]])
        o.copy(R["Us"][0:64, :], ps[4][0:64, 256:512], [P[4]], ["r_Us"], eng="dve")
        if stop == "A2f":
            continue
        for p in range(2):
            pc = slice(p * 128, (p + 1) * 128)
            o.mm(ps[5][:, pc], k.ident[:], Mc[:, p, :], True, False, ["ident", mck], [P[5]])
            o.mm(ps[5][:, pc], tok[:, 0, p, :], R["Us"][:, pc], False, False, ["r_tok", "r_Us"], [P[5]])
            o.mm(ps[5][:, pc], tok[:, 1, p, :], tok[:, 2, p, :], False, True, ["r_tok"], [P[5]])
            for q in range(2):
                if stop == "A2g":
                    continue
                h = 2 * p + q
                yc = slice(256 + h * 64, 256 + (h + 1) * 64)
                o.mm(ps[5][:, yc], Mc[:, p, :], ar[:, p, c, 1, :], True, False, [mck, "r_ar"], [P[5]])
                o.mm(ps[5][:, yc], R["Us"][:, pc], R["QrbT"][:, h, :], False, False, ["r_Us", "r_QrbT"], [P[5]])
                o.mm(ps[5][:, yc], tok[:, 2, p, :], R["QrkT"][:, h, :], False, True, ["r_tok", "r_QrkT"], [P[5]])
        for p in range(2):
            pc = slice(p * 128, (p + 1) * 128)
            o.act(R["Mtmp"][:], ps[5][:, pc], AF.Identity, [P[5], "r_epos"], ["r_Mtmp"], scale=R["epos"][:, p, c * 64 + 63:c * 64 + 64])
            o.tt(Mn[:, p, :], R["Mtmp"][:], k.blk[:], ALU.mult, ["r_Mtmp", "blk"], [mnk])
            for q in range(2):
                if stop == "A2g":
                    continue
                h = 2 * p + q
                rs = slice(q * 64, (q + 1) * 64)
                o.copy(R["ya"][rs, p, cs], ps[5][rs, 256 + h * 64:256 + (h + 1) * 64], [P[5]], ["r_ya"], eng="act")
    for p in range(2):
        ya = R["ya"][:, p, :]
        o.mm(ps[1][:, 0:NB], k.blk[:], ya, True, True, ["blk", "r_ya"], [P[1]])
        o.stt(R["tA"][:, 0, :], ps[1][:, 0:NB], -1.0 / 64, ya, ALU.mult, ALU.add, [P[1], "r_ya"], ["r_tA"])
        o.act(R["tB"][:, 0, :], R["tA"][:, 0, :], AF.Square, ["r_tA"], ["r_tB"])
        o.mm(ps[2][:, 0:NB], k.blk[:], R["tB"][:, 0, :], True, True, ["blk", "r_tB"], [P[2]])
        rsqrt(k, R["tB"][:, 0, :], ps[2][:, 0:NB], 1.0 / 64, 64e-5, [P[2]], "r_tB")
        o.tt(R["tA"][:, 0, :], R["tA"][:, 0, :], R["tB"][:, 0, :], ALU.mult, ["r_tA", "r_tB"], ["r_tA"])
        o.ts(R["tA"][:, 0, :], R["tA"][:, 0, :], rv[:, LNW, p:p + 1], rv[:, LNB, p:p + 1], ALU.mult, ALU.add, ["r_tA", "r_rvec"], ["r_tA"])
        o.tt(R["tA"][:, 0, :], R["tA"][:, 0, :], R["bonus"][:, p, :], ALU.add, ["r_tA", "r_bonus"], ["r_tA"])
        o.tt(R["yout"][:, p, :], R["tA"][:, 0, :], R["gg"][:, p, :], ALU.mult, ["r_tA", "r_gg"], ["yout"])
    o.copy(R["youtb"][:], R["yout"][:], ["yout"], ["youtb"], eng="pool")


TWO_PI = 2.0 * math.pi
CW1 = 6.28125
CW2 = TWO_PI - CW1


def sin_of(k, out, ang, shift, tmp, tmpi, shape_rows, r, w, tk):
    o = k.o
    o.ts(tmp, ang, 1.0 / TWO_PI, 0.5 + shift / TWO_PI, ALU.mult, ALU.add, r, [tk])
    o.copy(tmpi, tmp, [tk], [tk + "i"])
    o.copy(tmp, tmpi, [tk + "i"], [tk])
    o.stt(out, tmp, -CW1, ang, ALU.mult, ALU.add, [tk] + list(r), w)
    o.stt(out, tmp, -CW2, out, ALU.mult, ALU.add, [tk] + list(w), w)
    if shift != 0.0:
        o.ts(out, out, float(shift), None, ALU.add, None, w, w)
    o.ts(tmp, out, -math.pi, TWO_PI, ALU.is_lt, ALU.mult, w, [tk])
    o.tt(out, out, tmp, ALU.add, list(w) + [tk], w)
    o.ts(tmp, out, math.pi, -TWO_PI, ALU.is_gt, ALU.mult, w, [tk])
    o.tt(out, out, tmp, ALU.add, list(w) + [tk], w)
    o.ts(out, out, math.pi, -math.pi, ALU.min, ALU.max, w, w)
    o.act(out, out, AF.Sin, w, w)


def gelu_tanh(k, out, x, t1, r, w, tk):
    o = k.o
    o.tt(t1, x, x, ALU.mult, r, [tk])
    o.ts(t1, t1, 0.044715, 1.0, ALU.mult, ALU.add, [tk], [tk])
    o.tt(t1, t1, x, ALU.mult, [tk] + list(r), [tk])
    o.act(t1, t1, AF.Sigmoid, [tk], [tk], scale=1.5957691216057308)
    o.tt(out, x, t1, ALU.mult, [tk] + list(r), w)


def s5_setup(k, pa, l, xblk):
    o, dr, ps, P = k.o, k.dr, k.ps, k.P
    Q = {}
    Q["v"] = sb(k, pa, "s_v", [128, 3, 8])
    for nm in ("dt", "mag", "th", "c8", "s8", "qre", "qim", "den", "t8a", "t8b", "Ere", "Eim", "cre", "cim", "glr", "gli"):
        Q[nm] = sb(k, pa, "s_" + nm, [128, 8])
    Q["t8i"] = sb(k, pa, "s_t8i", [128, 8], I32)
    for nm in ("cosT", "sinT"):
        Q[nm] = sb(k, pa, "s_" + nm, [128, 8, NB])
    Q["hre"] = sb(k, pa, "s_hre", [128, 8, NB], BF16)
    Q["him"] = sb(k, pa, "s_him", [128, 8, NB], BF16)
    Q["tv"] = sb(k, pa, "s_tv", [128, NB])
    Q["B"] = sb(k, pa, "s_B", [128, 2, 8, 128], BF16)
    Q["C"] = sb(k, pa, "s_C", [128, 2, 8, 128], BF16)
    Q["vec2"] = sb(k, pa, "s_vec2", [128, 3, 2])
    Q["glu"] = sb(k, pa, "s_glu", [128, 2, 256], BF16)
    Q["ub"] = sb(k, pa, "s_ub", [128, 2, NB], BF16)
    for nm in ("w1", "w2", "w3", "w4", "w5", "w6", "w7"):
        Q[nm] = sb(k, pa, "s_" + nm, [128, NB])
    Q["wi"] = sb(k, pa, "s_wi", [128, NB], I32)
    Q["yv"] = sb(k, pa, "s_yv", [128, 2, NB])
    Q["ge"] = sb(k, pa, "s_ge", [128, 2, NB])
    Q["geb"] = sb(k, pa, "s_geb", [128, 2, NB], BF16)
    Q["sqb"] = sb(k, pa, "s_sqb", [128, 2, NB], BF16)
    Q["yo"] = sb(k, pa, "s_yo", [128, 2, NB])
    Q["yob"] = sb(k, pa, "s_yob", [128, 2, NB], BF16)
    Q["dq"] = sb(k, pa, "s_dq", [128, 2, 128])
    v = Q["v"]
    wst = xblk[:].rearrange("p a b -> p (a b)").rearrange("p (a j c) -> p a j c", a=2, j=8)
    o.dma("sp", v[:], dr["s5v"][l], "s_v", w=["s_v"])
    o.dma("sp", Q["tv"][:], dr["tvals"], "s_tv", w=["s_tv"])
    o.dma("sp", Q["vec2"][:], dr["s5vec2"][l], "s_vec2", w=["s_vec2"])
    S = ["s_small"]
    o.act(Q["dt"][:], v[:, 2, :], AF.Exp, ["s_v"], S)
    o.tt(Q["t8a"][:], v[:, 0, :], Q["dt"][:], ALU.mult, ["s_v"] + S, S)
    o.act(Q["mag"][:], Q["t8a"][:], AF.Exp, S, S)
    o.tt(Q["th"][:], v[:, 1, :], Q["dt"][:], ALU.mult, ["s_v"] + S, S)
    sin_of(k, Q["s8"][:], Q["th"][:], 0.0, Q["t8b"][:], Q["t8i"][:], 128, S, S, "s_t8")
    sin_of(k, Q["c8"][:], Q["th"][:], math.pi / 2, Q["t8b"][:], Q["t8i"][:], 128, S, S, "s_t8")
    o.tt(Q["t8a"][:], Q["mag"][:], Q["c8"][:], ALU.mult, S, S)
    o.ts(Q["t8a"][:], Q["t8a"][:], -1.0, None, ALU.add, None, S, S)
    o.tt(Q["t8b"][:], Q["mag"][:], Q["s8"][:], ALU.mult, S + ["s_t8"], ["s_t8"])
    o.tt(Q["den"][:], v[:, 0, :], v[:, 0, :], ALU.mult, ["s_v"], S)
    o.tt(Q["qre"][:], v[:, 1, :], v[:, 1, :], ALU.mult, ["s_v"], S)
    o.tt(Q["den"][:], Q["den"][:], Q["qre"][:], ALU.add, S, S)
    o.S.op("dve", lambda e: e.reciprocal(out=Q["den"][:], in_=Q["den"][:]), S, S)
    o.tt(Q["qre"][:], Q["t8a"][:], v[:, 0, :], ALU.mult, S + ["s_v"], S)
    o.tt(Q["qim"][:], Q["t8b"][:], v[:, 1, :], ALU.mult, S + ["s_v", "s_t8"], S)
    o.tt(Q["qre"][:], Q["qre"][:], Q["qim"][:], ALU.add, S, S)
    o.tt(Q["qre"][:], Q["qre"][:], Q["den"][:], ALU.mult, S, S)
    o.tt(Q["qim"][:], Q["t8b"][:], v[:, 0, :], ALU.mult, S + ["s_v", "s_t8"], S)
    o.tt(Q["cre"][:], Q["t8a"][:], v[:, 1, :], ALU.mult, S + ["s_v"], S)
    o.tt(Q["qim"][:], Q["qim"][:], Q["cre"][:], ALU.subtract, S, S)
    o.tt(Q["qim"][:], Q["qim"][:], Q["den"][:], ALU.mult, S, S)
    o.ts(Q["t8a"][:], Q["th"][:], float(NB), None, ALU.mult, None, S, S)
    sin_of(k, Q["Eim"][:], Q["t8a"][:], 0.0, Q["t8b"][:], Q["t8i"][:], 128, S, S, "s_t8")
    sin_of(k, Q["Ere"][:], Q["t8a"][:], math.pi / 2, Q["t8b"][:], Q["t8i"][:], 128, S, S, "s_t8")
    o.dma("sp", wst, dr["s5b"][l].rearrange("a j p c -> p a j c"), "xblk0", w=["xblk"])
    for jt in range(8):
        o.ts(Q["dq"][:, 0, :], k.ident[:], Q["qre"][:, jt:jt + 1], None, ALU.mult, None, ["ident"] + S, ["s_dq"])
        o.ts(Q["dq"][:, 1, :], k.ident[:], Q["qim"][:, jt:jt + 1], None, ALU.mult, None, ["ident"] + S, ["s_dq"])
        o.mm(ps[1][:, 0:128], k.ones32[:], Q["dq"][:, 0, :], True, True, ["ones32", "s_dq"], [P[1]])
        o.mm(ps[1][:, 128:256], k.ones32[:], Q["dq"][:, 1, :], True, True, ["ones32", "s_dq"], [P[1]])
        qrb, qib = ps[1][:, 0:128], ps[1][:, 128:256]
        bre, bim = wst[:, 0, jt, :], wst[:, 1, jt, :]
        w1, w2 = Q["w1"][:, 0:128], Q["w2"][:, 0:128]
        o.tt(w1, qrb, bre, ALU.mult, [P[1], "xblk"], ["s_w1"])
        o.tt(w2, qib, bim, ALU.mult, [P[1], "xblk"], ["s_w2"])
        o.tt(Q["B"][:, 0, jt, :], w1, w2, ALU.subtract, ["s_w1", "s_w2"], ["s_B"])
        o.tt(w1, qrb, bim, ALU.mult, [P[1], "xblk"], ["s_w1"])
        o.tt(w2, qib, bre, ALU.mult, [P[1], "xblk"], ["s_w2"])
        o.tt(Q["B"][:, 1, jt, :], w1, w2, ALU.add, ["s_w1", "s_w2"], ["s_B"])
    o.dma("sp", wst, dr["s5c"][l].rearrange("a j p c -> p a j c"), "xblk0", r=["s_B"], w=["xblk"])
    o.copy(Q["C"][:, 0], wst[:, 0], ["xblk"], ["s_C"])
    o.ts(Q["C"][:, 1], wst[:, 1], -1.0, None, ALU.mult, None, ["xblk"], ["s_C"])
    gst = xblk[:, 0:2, :].rearrange("p a b -> p (a b)")[:, 0:512].rearrange("p (kt n) -> p kt n", n=256)
    o.dma("sp", gst, dr["glu_w"][l].rearrange("(kt p) n -> p kt n", p=128), "xblk0", r=["s_C"], w=["xblk"])
    o.copy(Q["glu"][:], gst, ["xblk"], ["s_glu"])
    T = ["s_tab"]
    for jt in range(8):
        o.ts(Q["w5"][:], Q["tv"][:], Q["th"][:, jt:jt + 1], None, ALU.mult, None, ["s_tv"] + S, ["s_w5"])
        sin_of(k, Q["sinT"][:, jt, :], Q["w5"][:], 0.0, Q["w6"][:], Q["wi"][:], 128, ["s_w5"], T, "s_w6")
        sin_of(k, Q["cosT"][:, jt, :], Q["w5"][:], math.pi / 2, Q["w6"][:], Q["wi"][:], 128, ["s_w5"], T, "s_w6")
    o.memset(Q["cre"][:], 0.0, ["s_carry"], eng="dve")
    o.memset(Q["cim"][:], 0.0, ["s_carry"], eng="dve")
    return Q


def s5_block(k, l, i, Q, s5u, mixT, t0):
    o, ps, P = k.o, k.ps, k.P
    o.copy(Q["ub"][:], s5u[:], ["s5u"], ["s_ub"], eng="pool")
    T = ["s_tab"]
    w1, w2, w3, w4, w5, w6 = (Q[n][:] for n in ("w1", "w2", "w3", "w4", "w5", "w6"))
    for jt in range(8):
        ct = jt // 4
        pb = 1 + (jt % 2)
        o.mm(ps[pb][:, 0:NB], Q["B"][:, 0, jt, :], Q["ub"][:, ct, :], True, True, ["s_B", "s_ub"], [P[pb]])
        o.mm(ps[pb][:, NB:2 * NB], Q["B"][:, 1, jt, :], Q["ub"][:, ct, :], True, True, ["s_B", "s_ub"], [P[pb]])
        bre, bim = ps[pb][:, 0:NB], ps[pb][:, NB:2 * NB]
        cs_, sn_ = Q["cosT"][:, jt, :], Q["sinT"][:, jt, :]
        o.tt(w1, bre, cs_, ALU.mult, [P[pb]] + T, ["s_w1"])
        o.tt(w2, bim, sn_, ALU.mult, [P[pb]] + T, ["s_w2"])
        o.tt(w3, bim, cs_, ALU.mult, [P[pb]] + T, ["s_w3"])
        o.tt(w4, bre, sn_, ALU.mult, [P[pb]] + T, ["s_w4"])
        o.tt(w1, w1, w2, ALU.add, ["s_w1", "s_w2"], ["s_w1"], eng="pool")
        o.tt(w3, w3, w4, ALU.subtract, ["s_w3", "s_w4"], ["s_w3"], eng="pool")
        magb = Q["w7"][:]
        o.ts(magb, Q["tv"][:], 0.0, Q["mag"][:, jt:jt + 1], ALU.mult, ALU.add, ["s_tv", "s_small"], ["s_w7"], eng="pool")
        o.scan(w5, magb, w1, Q["cre"][:, jt:jt + 1], ["s_w7", "s_w1", "s_carry"], ["s_w5"])
        o.scan(w6, magb, w3, Q["cim"][:, jt:jt + 1], ["s_w7", "s_w3", "s_carry"], ["s_w6"])
        o.copy(Q["glr"][:, jt:jt + 1], w5[:, NB - 1:NB], ["s_w5"], ["s_gl"], eng="dve")
        o.copy(Q["gli"][:, jt:jt + 1], w6[:, NB - 1:NB], ["s_w6"], ["s_gl"], eng="dve")
        o.tt(w2, w5, cs_, ALU.mult, ["s_w5"] + T, ["s_w2"], eng="pool")
        o.tt(w4, w6, sn_, ALU.mult, ["s_w6"] + T, ["s_w4"], eng="pool")
        o.tt(Q["hre"][:, jt, :], w2, w4, ALU.subtract, ["s_w2", "s_w4"], [("s_h", jt)], eng="pool")
        o.tt(w2, w5, sn_, ALU.mult, ["s_w5"] + T, ["s_w2"], eng="pool")
        o.tt(w4, w6, cs_, ALU.mult, ["s_w6"] + T, ["s_w4"], eng="pool")
        o.tt(Q["him"][:, jt, :], w2, w4, ALU.add, ["s_w2", "s_w4"], [("s_h", jt)], eng="pool")
    o.tt(Q["t8a"][:], Q["glr"][:], Q["Ere"][:], ALU.mult, ["s_gl", "s_small"], ["s_c1"])
    o.tt(Q["t8b"][:], Q["gli"][:], Q["Eim"][:], ALU.mult, ["s_gl", "s_small"], ["s_c2"])
    o.tt(Q["cre"][:], Q["t8a"][:], Q["t8b"][:], ALU.subtract, ["s_c1", "s_c2"], ["s_carry"])
    o.tt(Q["t8a"][:], Q["glr"][:], Q["Eim"][:], ALU.mult, ["s_gl", "s_small"], ["s_c1"])
    o.tt(Q["t8b"][:], Q["gli"][:], Q["Ere"][:], ALU.mult, ["s_gl", "s_small"], ["s_c2"])
    o.tt(Q["cim"][:], Q["t8a"][:], Q["t8b"][:], ALU.add, ["s_c1", "s_c2"], ["s_carry"])
    vec2 = Q["vec2"]
    for ct in range(2):
        pb = 3
        for j in range(4):
            jt = ct * 4 + j
            o.mm(ps[pb][:, 0:NB], Q["C"][:, 0, jt, :], Q["hre"][:, jt, :], j == 0, False, ["s_C", ("s_h", jt)], [P[pb]])
            o.mm(ps[pb][:, 0:NB], Q["C"][:, 1, jt, :], Q["him"][:, jt, :], False, j == 3, ["s_C", ("s_h", jt)], [P[pb]])
        o.copy(Q["yv"][:, ct, :], ps[pb][:, 0:NB], [P[pb]], ["s_yv"], eng="act")
        o.stt(Q["yv"][:, ct, :], s5u[:, ct, :], vec2[:, 0, ct:ct + 1], Q["yv"][:, ct, :], ALU.mult, ALU.add, ["s5u", "s_vec2", "s_yv"], ["s_yv"])
        gelu_tanh(k, Q["ge"][:, ct, :], Q["yv"][:, ct, :], Q["w1"][:], ["s_yv"], ["s_ge"], "s_w1")
        o.copy(Q["geb"][:, ct, :], Q["ge"][:, ct, :], ["s_ge"], ["s_geb"], eng="pool")
    for ct in range(2):
        pb = 3
        for kt in range(2):
            o.mm(ps[pb][:, 0:NB], Q["glu"][:, kt, ct * 128:(ct + 1) * 128], Q["geb"][:, kt, :], kt == 0, kt == 1, ["s_glu", "s_geb"], [P[pb]])
        o.act(Q["w1"][:], ps[pb][:, 0:NB], AF.Sigmoid, [P[pb], "s_vec2"], ["s_w1"], bias=vec2[:, 1, ct:ct + 1])
        o.tt(Q["yv"][:, ct, :], Q["ge"][:, ct, :], Q["w1"][:], ALU.mult, ["s_ge", "s_w1"], ["s_yv"])
    branch_norm(k, Q["yv"], "s_yv", Q["sqb"], "s_sqb", Q["w2"], "s_w2", vec2[:, 2, :], "s_vec2", Q["yo"], "s_yo", Q["yob"], "s_yob", 2)
    o.dma("pool", mixT[256:512, t0:t0 + NB].rearrange("(t p) s -> p t s", p=128), Q["yob"][:], "mix_b", r=["s_yob"], w=["mixT_b"])


def branch_norm(k, y, yk, sqb, sqk, rstd, rk, g, gk, yo, yok, yob, yobk, nt, rows=128):
    o, ps, P = k.o, k.ps, k.P
    pb = 2
    for t in range(nt):
        o.act(sqb[0:rows, t, :], y[0:rows, t, :], AF.Square, [yk], [sqk])
    for t in range(nt):
        o.mm(ps[pb][0:rows, 0:y.shape[2]], k.onesb[0:rows, 0:rows], sqb[0:rows, t, :], t == 0, t == nt - 1, [sqk, "onesb"], [P[pb]])
    rsqrt(k, rstd[0:rows, :], ps[pb][0:rows, 0:y.shape[2]], 1.0 / (nt * rows), 1e-6, [P[pb]], rk, rows)
    for t in range(nt):
        o.stt(yo[0:rows, t, :], y[0:rows, t, :], g[0:rows, t:t + 1], rstd[0:rows, :], ALU.mult, ALU.mult, [yk, gk, rk], [yok])
    o.copy(yob[0:rows], yo[0:rows], [yok], [yobk], eng="pool")


def lru_setup(k, pa, l, xblk):
    o, dr = k.o, k.dr
    U = {}
    U["v"] = sb(k, pa, "l_v", [128, 9, 2])
    U["c1"] = sb(k, pa, "l_c1", [128, 2])
    U["bd"] = sb(k, pa, "l_bd", [128, 2, 2, 128], BF16)
    for nm in ("xc", "rr", "ii", "aa", "t1", "t2", "hh", "gg"):
        U[nm] = sb(k, pa, "l_" + nm, [128, NB])
    U["xcb"] = sb(k, pa, "l_xcb", [128, NB], BF16)
    U["hc"] = sb(k, pa, "l_hc", [128, 2])
    U["yd"] = sb(k, pa, "l_yd", [128, 2, NB])
    U["sqb"] = sb(k, pa, "l_sqb", [128, 2, NB], BF16)
    U["yo"] = sb(k, pa, "l_yo", [128, 2, NB])
    U["yob"] = sb(k, pa, "l_yob", [128, 2, NB], BF16)
    o.dma("sp", U["v"][:], dr["lruv"][l], "l_v", w=["l_v"])
    bst = xblk[:, 0:2, :].rearrange("p a b -> p (a b)")[:, 0:512].rearrange("p (a t c) -> p a t c", a=2, t=2)
    o.dma("sp", bst, dr["lrubd"][l].rearrange("a t p c -> p a t c"), "xblk0", w=["xblk"])
    o.copy(U["bd"][:], bst, ["xblk"], ["l_bd"])
    o.act(U["c1"][:], U["v"][:, 7, :], AF.Exp, ["l_v"], ["l_c1"], scale=-1.0)
    o.act(U["c1"][:], U["c1"][:], AF.Ln, ["l_c1", "cc"], ["l_c1"], bias=k.cc[:, CC[1.0]:CC[1.0] + 1])
    o.ts(U["c1"][:], U["c1"][:], -8.0, None, ALU.mult, None, ["l_c1"], ["l_c1"])
    o.memset(U["hc"][:], 0.0, ["l_hc"], eng="dve")
    return U


def lru_block(k, l, i, U, lrx, lrg, mixT, t0):
    o, ps, P = k.o, k.ps, k.P
    v = U["v"]
    for t in range(2):
        xc = U["xc"][:]
        o.ts(xc, lrx[:, t, 3:NB + 3], v[:, 3, t:t + 1], v[:, 4, t:t + 1], ALU.mult, ALU.add, ["lrx", "l_v"], ["l_xc"])
        for j in range(3):
            o.stt(xc, lrx[:, t, j:NB + j], v[:, j, t:t + 1], xc, ALU.mult, ALU.add, ["lrx", "l_v", "l_xc"], ["l_xc"])
        o.copy(U["xcb"][:], xc, ["l_xc"], ["l_xcb"], eng="pool")
        o.mm(ps[1][:, 0:NB], U["bd"][:, 0, t, :], U["xcb"][:], True, True, ["l_bd", "l_xcb"], [P[1]])
        o.mm(ps[1][:, NB:2 * NB], U["bd"][:, 1, t, :], U["xcb"][:], True, True, ["l_bd", "l_xcb"], [P[1]])
        o.act(U["rr"][:], ps[1][:, 0:NB], AF.Sigmoid, [P[1], "l_v"], ["l_rr"], bias=v[:, 5, t:t + 1])
        o.act(U["ii"][:], ps[1][:, NB:2 * NB], AF.Sigmoid, [P[1], "l_v"], ["l_ii"], bias=v[:, 6, t:t + 1])
        o.act(U["aa"][:], U["rr"][:], AF.Exp, ["l_rr", "l_c1"], ["l_aa"], scale=U["c1"][:, t:t + 1])
        o.tt(U["t1"][:], U["aa"][:], U["aa"][:], ALU.mult, ["l_aa"], ["l_t1"])
        o.ts(U["t1"][:], U["t1"][:], -1.0, 1.0, ALU.mult, ALU.add, ["l_t1"], ["l_t1"])
        o.ts(U["t1"][:], U["t1"][:], 0.0, None, ALU.max, None, ["l_t1"], ["l_t1"])
        o.act(U["t1"][:], U["t1"][:], AF.Sqrt, ["l_t1"], ["l_t1"])
        o.tt(U["t2"][:], U["ii"][:], xc, ALU.mult, ["l_ii", "l_xc"], ["l_t2"])
        o.tt(U["t2"][:], U["t2"][:], U["t1"][:], ALU.mult, ["l_t2", "l_t1"], ["l_t2"])
        o.scan(U["hh"][:], U["aa"][:], U["t2"][:], U["hc"][:, t:t + 1], ["l_aa", "l_t2", "l_hc"], ["l_hh"])
        o.copy(U["hc"][:, t:t + 1], U["hh"][:, NB - 1:NB], ["l_hh"], ["l_hc"], eng="dve")
        gelu_tanh(k, U["gg"][:], lrg[:, t, :], U["t1"][:], ["lrg"], ["l_gg"], "l_t1")
        o.tt(U["yd"][:, t, :], U["hh"][:], U["gg"][:], ALU.mult, ["l_hh", "l_gg"], ["l_yd"])
    o.copy(lrx[:, :, 0:3], lrx[:, :, NB:NB + 3], ["lrx"], ["lrx"], eng="dve")
    branch_norm(k, U["yd"], "l_yd", U["sqb"], "l_sqb", U["t2"], "l_t2", v[:, 8, :], "l_v", U["yo"], "l_yo", U["yob"], "l_yob", 2)
    o.dma("pool", mixT[768:1024, t0:t0 + NB].rearrange("(t p) s -> p t s", p=128), U["yob"][:], "mix_d", r=["l_yob"], w=["mixT_d"])


QSCALE = 96 ** -0.5


def mla_setup(k, pa, l, xblk, Q, U):
    o, dr = k.o, k.dr
    A = {}
    xflat = xblk[:].rearrange("p a b -> p (a b)")
    A["wuq"] = sb(k, pa, "a_wuq", [128, 2, 384], BF16)
    A["wukn"] = sb(k, pa, "a_wukn", [128, 4, 96], BF16)
    A["wukv"] = sb(k, pa, "a_wukv", [128, 256], BF16)
    A["qng"] = sb(k, pa, "a_qng", [128, 2])
    A["kvng"] = sb(k, pa, "a_kvng", [128, 1])
    A["qkhg"] = sb(k, pa, "a_qkhg", [96, 2])
    A["invf"] = sb(k, pa, "a_invf", [96, 1])
    A["rotT"] = sb(k, pa, "a_rotT", [128, 128])
    A["epe"] = sb(k, pa, "a_epe", [32, 96], BF16)
    A["posi"] = sb(k, pa, "a_posi", [96, 1], I32)
    A["posf"] = sb(k, pa, "a_posf", [96, 1])
    A["tv"] = Q["tv"]
    for nm in ("rk96", "COS", "SIN"):
        A[nm] = sb(k, pa, "a_" + nm, [96, NB])
    A["rq"] = U["t2"]
    A["ang"], A["tmpS"], A["angi"] = Q["w5"][0:96, :], Q["w6"][0:96, :], Q["wi"][0:96, :]
    A["qf"], A["rsh"], A["qn"], A["t1"], A["t2"] = (U[n][0:96, :] for n in ("xc", "rr", "ii", "aa", "t1"))
    A["qn128"] = U["ii"]
    A["sq"] = sb(k, pa, "a_sq", [128, 2, NB], BF16)
    A["sqk"] = sb(k, pa, "a_sqk", [128, NB], BF16)
    A["sqh"] = sb(k, pa, "a_sqh", [96, NB], BF16)
    A["rkt"] = sb(k, pa, "a_rkt", [128, NB // 128])
    A["qrb"] = sb(k, pa, "a_qrb", [96, 4, NB], BF16)
    A["krb"] = sb(k, pa, "a_krb", [96, 4, NB], BF16)
    A["Vt"] = sb(k, pa, "a_Vt", [128, NB // 128, 4, 65], BF16)
    for nm, src in (("qng", "qng"), ("kvng", "kvng"), ("qkhg", "qkhg")):
        o.dma("sp", A[nm][:], dr[src][l], "a_" + nm, w=["a_small"])
    o.dma("sp", A["invf"][:], dr["invf"], "a_invf", w=["a_small"])
    o.dma("sp", A["rotT"][:], dr["rotT"], "a_rotT", w=["a_small"])
    o.dma("sp", A["posi"][:], dr["pos"], "a_posi", w=["a_posi"])
    o.copy(A["posf"][:], A["posi"][:], ["a_posi"], ["a_small"])
    ste = xflat[0:32, 0:96]
    o.dma("sp", ste, dr["epe"], "xblk0", w=["xblk"])
    o.copy(A["epe"][:], ste, ["xblk"], ["a_w"])
    stq = xflat[:, 0:768].rearrange("p (kt n) -> p kt n", n=384)
    o.dma("sp", stq, dr["w_uq"][l].rearrange("(kt p) n -> p kt n", p=128), "xblk0", r=["a_w"], w=["xblk"])
    for kt in range(2):
        o.ts(A["wuq"][:, kt, :], stq[:, kt, :], A["qng"][:, kt:kt + 1], None, ALU.mult, None, ["xblk", "a_small"], ["a_w"])
    stk = xflat[:, 0:384]
    o.dma("sp", stk, dr["w_ukn"][l].rearrange("p h c -> p (h c)"), "xblk0", r=["a_w"], w=["xblk"])
    o.ts(A["wukn"][:].rearrange("p h c -> p (h c)"), stk, A["kvng"][:, 0:1], None, ALU.mult, None, ["xblk", "a_small"], ["a_w"])
    stv = xflat[:, 0:256]
    o.dma("sp", stv, dr["w_ukv"][l], "xblk0", r=["a_w"], w=["xblk"])
    o.ts(A["wukv"][:], stv, A["kvng"][:, 0:1], None, ALU.mult, None, ["xblk", "a_small"], ["a_w"])
    o.memset(A["rk96"][:], 1.0, ["a_rk96"], eng="dve")
    o.memset(A["Vt"][:], 1.0, ["a_Vt"], eng="dve")
    return A


def head_norm_rope(k, A, src, gcol, out):
    o, ps, P = k.o, k.ps, k.P
    o.act(A["sqh"][:], src, AF.Square, ["l_xc"], ["a_sqh"])
    o.mm(ps[4][0:96, 0:NB], k.onesb[0:96, 0:96], A["sqh"][:], True, True, ["onesb", "a_sqh"], [P[4]])
    rsqrt(k, A["rsh"][:], ps[4][0:96, 0:NB], 1.0 / 96, 1e-6, [P[4]], "l_rr", 96)
    o.stt(A["qn"][:], src, A["qkhg"][:, gcol:gcol + 1], A["rsh"][:], ALU.mult, ALU.mult, ["l_xc", "a_small", "l_rr"], ["l_ii"])
    o.mm(ps[4][:, NB:2 * NB], A["rotT"][:], A["qn128"][:], True, True, ["a_small", "l_ii"], [P[4]])
    o.tt(A["t1"][:], A["qn"][:], A["COS"][:], ALU.mult, ["l_ii", "a_cs"], ["l_aa"])
    o.tt(A["t2"][:], ps[4][0:96, NB:2 * NB], A["SIN"][:], ALU.mult, [P[4], "a_cs"], ["l_t1"])
    o.tt(out, A["t1"][:], A["t2"][:], ALU.add, ["l_aa", "l_t1"], ["a_out"])


def mla_block(k, l, i, A, qab, kvab, kpeb, t0):
    o, ps, P = k.o, k.ps, k.P
    ntt = NB // 128
    rms_stats(k, [(qab[:, kt, :], "qab") for kt in range(2)], 256.0, 1e-6, ps[1][:, 0:NB], A["rq"][:],
              [(A["sq"][:, kt, :], "a_sq") for kt in range(2)], None, P[1], "l_t2")
    o.act(A["sqk"][:], kvab[:], AF.Square, ["kvab"], ["a_sqk"])
    o.mm(ps[2][:, 0:NB], k.onesb[:], A["sqk"][:], True, True, ["onesb", "a_sqk"], [P[2]])
    for tt in range(ntt):
        o.mm(ps[2][:, NB + tt:NB + tt + 1], A["sqk"][:, tt * 128:(tt + 1) * 128], k.onesb[:, 0:1], True, True, ["onesb", "a_sqk"], [P[2]])
    rsqrt(k, A["rk96"][0:64, :], ps[2][0:64, 0:NB], 1.0 / 128, 1e-6, [P[2]], "a_rk96", 64)
    rsqrt(k, A["rkt"][:], ps[2][:, NB:NB + ntt], 1.0 / 128, 1e-6, [P[2]], "a_rkt", 128)
    o.ts(A["ang"][:], A["tv"][0:96, :], A["posf"][:, 0:1], None, ALU.add, None, ["s_tv", "a_small"], ["s_w5"])
    o.ts(A["ang"][:], A["ang"][:], float(t0), A["invf"][:, 0:1], ALU.add, ALU.mult, ["s_w5", "a_small"], ["s_w5"])
    sin_of(k, A["SIN"][:], A["ang"][:], 0.0, A["tmpS"][:], A["angi"][:], 96, ["s_w5"], ["a_cs"], "s_w6")
    sin_of(k, A["COS"][:], A["ang"][:], math.pi / 2, A["tmpS"][:], A["angi"][:], 96, ["s_w5"], ["a_cs"], "s_w6")
    for h in range(4):
        for kt in range(2):
            o.mm(ps[3][0:96, 0:NB], A["wuq"][:, kt, h * 96:(h + 1) * 96], qab[:, kt, :], kt == 0, kt == 1, ["a_w", "qab"], [P[3]])
        o.tt(A["qf"][:], ps[3][0:96, 0:NB], A["rq"][0:96, :], ALU.mult, [P[3], "l_t2"], ["l_xc"])
        head_norm_rope(k, A, A["qf"][:], 0, A["qrb"][:, h, :])
        o.mm(ps[3][0:96, NB:2 * NB], A["wukn"][:, h, :], kvab[:], True, False, ["a_w", "kvab"], [P[3]])
        o.mm(ps[3][0:96, NB:2 * NB], A["epe"][:], kpeb[:], False, True, ["a_w", "kpeb"], [P[3]])
        o.tt(A["qf"][:], ps[3][0:96, NB:2 * NB], A["rk96"][:], ALU.mult, [P[3], "a_rk96"], ["l_xc"])
        head_norm_rope(k, A, A["qf"][:], 1, A["krb"][:, h, :])
    for tt in range(ntt):
        o.mm(ps[1][:, 0:256], kvab[:, tt * 128:(tt + 1) * 128], A["wukv"][:], True, True, ["kvab", "a_w"], [P[1]])
        o.act(A["Vt"][:, tt, :, 0:64], ps[1][:, 0:256].rearrange("p (h c) -> p h c", c=64), AF.Identity, [P[1], "a_rkt"], ["a_Vt"],
              scale=A["rkt"][:, tt:tt + 1])
    o.dma("pool", k.qs[:, :, t0:t0 + NB], A["qrb"][:], "st_q", r=["a_out"], w=["qs"])
    o.dma("pool", k.ks[:, :, t0:t0 + NB], A["krb"][:], "st_k", r=["a_out"], w=["ks"])
    o.dma("pool", k.vs[t0 // 128:t0 // 128 + ntt].rearrange("t p c -> p t c"), A["Vt"][:].rearrange("p t h c -> p t (h c)"), "st_v", r=["a_Vt"], w=["vs"])


QB = 256


def phaseB(k, l, xin, x1T, stop):
    nc, S_, dr, S, L, o = k.nc, k.S_, k.dr, k.S, k.L, k.o
    ps, P = k.ps, k.P
    nqb = S // QB
    with ExitStack() as pb_:
        Kall = sb(k, pb_, "Kall", [96, 4, S], BF16)
        Vall = sb(k, pb_, "Vall", [128, S // 128, 260], BF16)
        qblk = sb(k, pb_, "qblk", [96, 4, QB], BF16)
        PT = [sb(k, pb_, f"PT{i}", [128, QB], BF16) for i in range(2)]
        Oext = sb(k, pb_, "Oext", [128, QB])
        rden = sb(k, pb_, "rden", [64, QB])
        yc = sb(k, pb_, "yc", [64, 4, QB])
        ycsq = sb(k, pb_, "ycsq", [64, 4, QB], BF16)
        ycn = sb(k, pb_, "ycn", [64, 4, QB])
        ycnb = sb(k, pb_, "ycnb", [64, 4, QB], BF16)
        rstc = sb(k, pb_, "rstc", [64, QB])
        mixb = sb(k, pb_, "mixb", [128, 6, QB], BF16)
        wo = sb(k, pb_, "wo", [128, 6, D], BF16)
        woc = sb(k, pb_, "woc", [64, 4, D], BF16)
        xblk = sb(k, pb_, "xblkB", [128, 8, QB])
        sq2 = sb(k, pb_, "sq2", [128, 8, QB], BF16)
        rst2 = sb(k, pb_, "rst2", [128, QB])
        xt2 = sb(k, pb_, "xt2", [128, QB])
        h2 = sb(k, pb_, "h2", [128, 8, QB])
        h2b = sb(k, pb_, "h2b", [128, 8, QB], BF16)
        wr = sb(k, pb_, "wr", [128, 8, 36])
        brt = sb(k, pb_, "brt", [128, 36])
        sel65 = sb(k, pb_, "sel65", [128, 64])
        bngc = sb(k, pb_, "bngc", [64, 4])
        lg = sb(k, pb_, "lg", [128, 36])
        rt = {nm: sb(k, pb_, "rt_" + nm, [128, w_]) for nm, w_ in (("m4", 1), ("nm4", 1), ("e4", 4), ("s4", 1), ("gp", 1), ("ohg", 4), ("sel", 8),
                                                                  ("l1", 1), ("oh1", 8), ("sel2", 8), ("l2", 1), ("oh2", 8), ("nl1", 1), ("d", 1),
                                                                  ("t", 1), ("w1", 1), ("w2", 1), ("ge", 8))}
        gates = sb(k, pb_, "gates", [128, 32])
        gT = sb(k, pb_, "gTb", [32, QB])
        wst = h2[:].rearrange("p a b -> p (a b)")
        wov = dr["w_out"][l]
        rows = [0, 128, 256, 384, 768, 896]
        for t, r0 in enumerate(rows):
            for hf in range(1):
                o.dma("sp", wst[:, 0:1024], wov[r0:r0 + 128, :], "h2st", w=["h2"])
                o.copy(wo[:, t, :], wst[:, 0:1024], ["h2"], ["wo"], eng=("dve" if t % 2 == 0 else "pool"))
        for h in range(4):
            o.dma("sp", wst[0:64, 0:1024], wov[512 + h * 64:512 + (h + 1) * 64, :], "h2st", w=["h2"])
            o.copy(woc[:, h, :], wst[0:64, 0:1024], ["h2"], ["wo"], eng="dve")
        o.dma("sp", wr[:], dr["wr"][l].rearrange("(kk p) n -> p kk n", p=128), "wr", w=["wr"])
        o.dma("sp", brt[:], dr["br"][l], "brt", w=["wr"])
        o.dma("sp", sel65[:], dr["sel65"], "sel65", w=["sel65"])
        o.memset(Oext[:], 0.0, ["Oext"], eng="dve")
        o.dma("sp", bngc[:], dr["bng_c"][l], "bngc", w=["bngc"])
        xv = xin.rearrange("(kk p) s -> p kk s", p=128)
        x1v = x1T.rearrange("(kk p) s -> p kk s", p=128)
        h2v = k.h2T.rearrange("(kk p) s -> p kk s", p=128)
        for qb in range(nqb):
            t0 = qb * QB
            nkt = QB // 128
            o.dma("sp", Kall[:, :, t0:t0 + QB], k.ks[:, :, t0:t0 + QB], "ldK", r=["ks"], w=["Kall"])
            o.dma("sp", Vall[:, t0 // 128:t0 // 128 + nkt, :], k.vs[t0 // 128:t0 // 128 + nkt].rearrange("t p c -> p t c"), "ldV", r=["vs"], w=["Vall"])
            o.dma("sp", qblk[:], k.qs[:, :, t0:t0 + QB], "ldQ", r=["qs"], w=["qblk"])
            o.dma("pool", mixb[:, 0:2, :], k.mixT[0:256, t0:t0 + QB].rearrange("(t p) s -> p t s", p=128), "ldm", r=["mixT_a"], w=["mixb"])
            o.dma("pool", mixb[:, 2:4, :], k.mixT[256:512, t0:t0 + QB].rearrange("(t p) s -> p t s", p=128), "ldm", r=["mixT_b"], w=["mixb"])
            o.dma("pool", mixb[:, 4:6, :], k.mixT[768:1024, t0:t0 + QB].rearrange("(t p) s -> p t s", p=128), "ldm", r=["mixT_d"], w=["mixb"])
            o.dma("sp", xblk[:], xv[:, :, t0:t0 + QB], "xblkB", w=["xblkB"])
            nk_tot = (t0 + QB) // 128
            it = 0
            for h in range(4):
                for kt in range(nk_tot):
                    a = kt - (t0 // 128)
                    c0 = max(a, 0) * 128
                    sb_ = 1 + (it % 2)
                    pt = PT[it % 2]
                    ptk = f"PT{it % 2}"
                    it += 1
                    o.mm(ps[sb_][:, c0:QB], Kall[:, h, kt * 128:(kt + 1) * 128], qblk[:, h, c0:QB], True, True, ["Kall", "qblk"], [P[sb_]])
                    o.act(pt[:, c0:QB], ps[sb_][:, c0:QB], AF.Exp, [P[sb_]], [ptk], scale=QSCALE)
                    if a >= 0:
                        o.memset(pt[64:128, c0:c0 + 64], 0.0, [ptk], eng="pool")
                    o.mm(ps[3][0:65, c0:QB], Vall[:, kt, h * 65:(h + 1) * 65], pt[:, c0:QB], kt == 0, kt == nk_tot - 1, ["Vall", ptk], [P[3]])
                o.copy(Oext[0:65, :], ps[3][0:65, 0:QB], [P[3]], ["Oext"], eng="dve")
                o.mm(ps[4][0:64, 0:QB], sel65[:], Oext[:], True, True, ["sel65", "Oext"], [P[4]])
                o.S.op("dve", lambda e: e.reciprocal(out=rden[:], in_=ps[4][0:64, 0:QB]), [P[4]], ["rden"])
                o.tt(yc[:, h, :], Oext[0:64, :], rden[:], ALU.mult, ["Oext", "rden"], ["yc"])
            branch_norm(k, yc, "yc", ycsq, "ycsq", rstc, "rstc", bngc, "bngc", ycn, "ycn", ycnb, "ycnb", 4, rows=64)
            if stop == "B1":
                dbg = dr["dbg"]
                o.dma("sp", dbg[512:768, t0:t0 + QB].rearrange("(h p) s -> p h s", p=64), ycn[:], "dbg", r=["ycn"], w=["dbg"])
            for f in range(8):
                pb = 5 + (f % 2)
                fc = slice(f * 128, (f + 1) * 128)
                for t in range(6):
                    o.mm(ps[pb][:, 0:QB], wo[:, t, fc], mixb[:, t, :], t == 0, False, ["wo", "mixb"], [P[pb]])
                for h in range(4):
                    o.mm(ps[pb][:, 0:QB], woc[:, h, fc], ycnb[:, h, :], False, h == 3, ["wo", "ycnb"], [P[pb]])
                o.act(xt2[:], ps[pb][:, 0:QB], AF.Identity, [P[pb], "mods"], ["xt2"], scale=k.mods[:, l, 16 + f:17 + f])
                o.tt(xblk[:, f, :], xt2[:], xblk[:, f, :], ALU.add, ["xt2", "xblkB"], ["xblkB"])
            o.dma("pool", x1v[:, :, t0:t0 + QB], xblk[:], "stx1", r=["xblkB"], w=["x1T"])
            if stop == "B1":
                dbg = dr["dbg"]
                o.dma("sp", dbg[1024:2048, t0:t0 + QB].rearrange("(t p) s -> p t s", p=128), xblk[:], "dbg", r=["xblkB"], w=["dbg"])
            rms_stats(k, [(xblk[:, kk, :], "xblkB") for kk in range(8)], 1024.0, 1e-6, ps[7][:, 0:QB], rst2[:],
                      [(sq2[:, kk, :], "sq2") for kk in range(8)], None, P[7], "rst2")
            for kk in range(8):
                o.tt(xt2[:], xblk[:, kk, :], rst2[:], ALU.mult, ["xblkB", "rst2"], ["xt2"])
                o.act(h2[:, kk, :], xt2[:], AF.Identity, ["xt2", "g2s", "mods"], ["h2"], bias=k.mods[:, l, 24 + kk:25 + kk], scale=k.g2s[:, l, kk:kk + 1])
            o.copy(h2b[:], h2[:], ["h2"], ["h2b"], eng="pool")
            o.dma("pool", h2v[:, :, t0:t0 + QB], h2b[:], "sth2", r=["h2b"], w=["h2T"])
            for tq in range(QB // 128):
                tsl = slice(tq * 128, (tq + 1) * 128)
                for kk in range(8):
                    o.mm(ps[7][:, 256:292], h2[:, kk, tsl], wr[:, kk, :], kk == 0, kk == 7, ["h2", "wr"], [P[7]])
                o.tt(lg[:], ps[7][:, 256:292], brt[:], ALU.add, [P[7], "wr"], ["lg"])
                route(k, lg, rt, gates)
                o.tr(ps[7][0:32, 384:512], gates[:], k.ident[:], ["gates", "ident"], [P[7]])
                o.copy(gT[:, tsl], ps[7][0:32, 384:512], [P[7]], ["gTb"], eng="dve")
            o.dma("pool", k.gT[:, t0:t0 + QB], gT[:], "stg", r=["gTb"], w=["gT"])
            if stop == "B1":
                o.dma("sp", dr["dbg"][0:32, t0:t0 + QB], gT[:], "dbg", r=["gTb"], w=["dbg"])
        S_.barrier()
        S_.flush(k.st)


def route(k, lg, rt, gates):
    o = k.o
    R_ = ["rt"]
    red = lambda out, in_, op: o.S.op("dve", lambda e: e.tensor_reduce(out=out, in_=in_, axis=AX.X, op=op), ["lg"] + R_, R_)
    red(rt["m4"][:], lg[:, 0:4], ALU.max)
    o.ts(rt["nm4"][:], rt["m4"][:], -1.0, None, ALU.mult, None, R_, R_)
    o.act(rt["e4"][:], lg[:, 0:4], AF.Exp, ["lg"] + R_, R_, bias=rt["nm4"][:, 0:1])
    red(rt["s4"][:], rt["e4"][:], ALU.add)
    o.S.op("dve", lambda e: e.reciprocal(out=rt["gp"][:], in_=rt["s4"][:]), R_, R_)
    o.ts(rt["ohg"][:], lg[:, 0:4], rt["m4"][:, 0:1], None, ALU.is_equal, None, ["lg"] + R_, R_)
    for g in range(4):
        le = lg[:, 4 + 8 * g:12 + 8 * g]
        if g == 0:
            o.ts(rt["sel"][:], le, rt["ohg"][:, 0:1], None, ALU.mult, None, ["lg"] + R_, R_)
        else:
            o.stt(rt["sel"][:], le, rt["ohg"][:, g:g + 1], rt["sel"][:], ALU.mult, ALU.add, ["lg"] + R_, R_)
    red(rt["l1"][:], rt["sel"][:], ALU.max)
    o.ts(rt["oh1"][:], rt["sel"][:], rt["l1"][:, 0:1], None, ALU.is_equal, None, R_, R_)
    o.stt(rt["sel2"][:], rt["oh1"][:], -1e30, rt["sel"][:], ALU.mult, ALU.add, R_, R_)
    red(rt["l2"][:], rt["sel2"][:], ALU.max)
    o.ts(rt["oh2"][:], rt["sel2"][:], rt["l2"][:, 0:1], None, ALU.is_equal, None, R_, R_)
    o.ts(rt["nl1"][:], rt["l1"][:], -1.0, None, ALU.mult, None, R_, R_)
    o.act(rt["d"][:], rt["l2"][:], AF.Exp, R_, R_, bias=rt["nl1"][:, 0:1])
    o.ts(rt["t"][:], rt["d"][:], 1.0, None, ALU.add, None, R_, R_)
    o.S.op("dve", lambda e: e.reciprocal(out=rt["t"][:], in_=rt["t"][:]), R_, R_)
    o.tt(rt["w1"][:], rt["gp"][:], rt["t"][:], ALU.mult, R_, R_)
    o.tt(rt["w2"][:], rt["w1"][:], rt["d"][:], ALU.mult, R_, R_)
    o.ts(rt["ge"][:], rt["oh1"][:], rt["w1"][:, 0:1], None, ALU.mult, None, R_, R_)
    o.stt(rt["ge"][:], rt["oh2"][:], rt["w2"][:, 0:1], rt["ge"][:], ALU.mult, ALU.add, R_, R_)
    for g in range(4):
        o.ts(gates[:, 8 * g:8 * g + 8], rt["ge"][:], rt["ohg"][:, g:g + 1], None, ALU.mult, None, R_, ["gates"])


def phaseW(k, l):
    nc, S_, dr, o = k.nc, k.S_, k.dr, k.o
    NE = dr["w1"].shape[1]
    with ExitStack() as pw:
        stg = [sb(k, pw, f"wstg{i}", [128, 4096]) for i in range(2)]
        wb = [sb(k, pw, f"wbf{i}", [128, 4096], BF16) for i in range(2)]
        it = 0
        for e in range(NE):
            for nm, dst, kk in (("w1", k.w1b, 8), ("w3", k.w3b, 8), ("w2", k.w2b, 4)):
                i2 = it % 2
                n = 4096 // kk
                src = dr[nm][l, e].rearrange("(kk p) n -> p kk n", p=128)
                o.dma("sp", stg[i2][:].rearrange("p (kk n) -> p kk n", kk=kk), src, f"wstg{i2}", w=[f"wstg{i2}"])
                o.copy(wb[i2][:], stg[i2][:], [f"wstg{i2}"], [f"wbf{i2}"], eng=("dve", "pool", "act")[it % 3])
                o.dma("pool", dst[e], wb[i2][:], f"wbst{i2}", r=[f"wbf{i2}"], w=["wscr"])
                it += 1
        S_.barrier()
        S_.flush(k.st)


def phaseC(k, l, x1T, xoutT, stop):
    nc, S_, dr, S, o = k.nc, k.S_, k.dr, k.S, k.o
    ps, P = k.ps, k.P
    NE = dr["w1"].shape[1]
    TB = min(1024, S)
    nh = TB // 512
    with ExitStack() as pc:
        h2b = sb(k, pc, "c_h2b", [128, 8, TB], BF16)
        acc = sb(k, pc, "c_acc", [128, 8, TB])
        gbc = [sb(k, pc, f"c_gbc{i}", [128, TB]) for i in range(2)]
        W1 = [sb(k, pc, f"c_w1_{i}", [128, 8, 512], BF16) for i in range(2)]
        W3 = [sb(k, pc, f"c_w3_{i}", [128, 8, 512], BF16) for i in range(2)]
        W2 = [sb(k, pc, f"c_w2_{i}", [128, 4, 1024], BF16) for i in range(2)]
        hid = [sb(k, pc, f"c_hid{i}", [128, 4, 512], BF16) for i in range(2)]
        sil = [sb(k, pc, f"c_sil{i}", [128, 512]) for i in range(2)]
        t3 = [sb(k, pc, f"c_t3{i}", [128, 512]) for i in range(2)]
        xr = [sb(k, pc, f"c_xr{i}", [128, TB]) for i in range(2)]
        sel32 = sb(k, pc, "c_sel32", [128, 32, 128])
        gt128 = sb(k, pc, "c_gt128", [128, TB])
        o.dma("sp", sel32[:], dr["sel32"], "c_sel32", w=["sel32"])
        o.memset(gt128[:], 0.0, ["gt128"], eng="dve")
        h2v = k.h2T.rearrange("(kk p) s -> p kk s", p=128)
        x1v = x1T.rearrange("(kk p) s -> p kk s", p=128)
        xov = xoutT.rearrange("(kk p) s -> p kk s", p=128)
        cnt = 0
        for tb in range(S // TB):
            t0 = tb * TB
            o.dma("sp", h2b[:], h2v[:, :, t0:t0 + TB], "c_h2b", r=["h2T"], w=["c_h2b"])
            o.dma("sp", gt128[0:32, :], k.gT[:, t0:t0 + TB], "c_gt128", r=["gT"], w=["gt128"])
            for e in range(NE):
                i2 = e % 2
                wk = f"c_w{i2}"
                o.dma("sp", W1[i2][:].rearrange("p a b -> p (a b)"), k.w1b[e], f"c_w1_{i2}", r=["wscr"], w=[wk + "a"])
                o.dma("sp", W3[i2][:].rearrange("p a b -> p (a b)"), k.w3b[e], f"c_w3_{i2}", r=["wscr"], w=[wk + "b"])
                o.dma("sp", W2[i2][:].rearrange("p a b -> p (a b)"), k.w2b[e], f"c_w2_{i2}", r=["wscr"], w=[wk + "c"])
                for hf in range(nh):
                    o.mm(ps[4 + hf][:, :], sel32[:, e, :], gt128[:, hf * 512:(hf + 1) * 512], True, True, ["sel32", "gt128"], [P[4 + hf]])
                    o.copy(gbc[i2][:, hf * 512:(hf + 1) * 512], ps[4 + hf][:, :], [P[4 + hf]], [f"c_gbc{i2}"], eng="act")
                for hf in range(nh):
                    hs = slice(hf * 512, (hf + 1) * 512)
                    j2 = cnt % 2
                    cnt += 1
                    for ht in range(4):
                        pa_, pb_ = ht % 2, 2 + ht % 2
                        hc = slice(ht * 128, (ht + 1) * 128)
                        for kk in range(8):
                            o.mm(ps[pa_][:, :], W1[i2][:, kk, hc], h2b[:, kk, hs], kk == 0, kk == 7, [wk + "a", "c_h2b"], [P[pa_]])
                        for kk in range(8):
                            o.mm(ps[pb_][:, :], W3[i2][:, kk, hc], h2b[:, kk, hs], kk == 0, kk == 7, [wk + "b", "c_h2b"], [P[pb_]])
                        s2 = ht % 2
                        o.act(sil[s2][:], ps[pa_][:, :], AF.Silu, [P[pa_]], [f"c_sil{s2}"])
                        o.tt(t3[s2][:], ps[pb_][:, :], gbc[i2][:, hs], ALU.mult, [P[pb_], f"c_gbc{i2}"], [f"c_t3{s2}"])
                        o.tt(hid[j2][:, ht, :], sil[s2][:], t3[s2][:], ALU.mult, [f"c_sil{s2}", f"c_t3{s2}"], [f"c_hid{j2}"],
                             eng=("pool" if ht % 2 == 0 else "dve"))
                    for f in range(8):
                        po = 4 + f % 4
                        fc = slice(f * 128, (f + 1) * 128)
                        for ht in range(4):
                            o.mm(ps[po][:, :], W2[i2][:, ht, fc], hid[j2][:, ht, :], ht == 0, ht == 3, [wk + "c", f"c_hid{j2}"], [P[po]])
                        if e == 0:
                            o.copy(acc[:, f, hs], ps[po][:, :], [P[po]], ["c_acc"], eng="act")
                        else:
                            o.tt(acc[:, f, hs], ps[po][:, :], acc[:, f, hs], ALU.add, [P[po], "c_acc"], ["c_acc"])
            for f in range(8):
                i2 = f % 2
                o.dma("sp", xr[i2][:], x1v[:, f, t0:t0 + TB], f"c_xr{i2}", r=["x1T"], w=[f"c_xr{i2}"])
                o.stt(xr[i2][:], acc[:, f, :], k.mods[:, l, 40 + f:41 + f], xr[i2][:], ALU.mult, ALU.add, ["c_acc", "mods", f"c_xr{i2}"], [f"c_xr{i2}"])
                o.dma("pool", xov[:, f, t0:t0 + TB], xr[i2][:], f"c_xo{i2}", r=[f"c_xr{i2}"], w=["xout%d" % l])
                if stop == "C1":
                    o.dma("sp", dr["dbg"][f * 128:(f + 1) * 128, t0:t0 + TB], xr[i2][:], "dbg", r=[f"c_xr{i2}"], w=["dbg"])
        S_.barrier()
        S_.flush(k.st)


def make_shapes(sh, cst, pc):
    shapes = {}
    for d_ in (sh, cst, pc):
        for kname, v in d_.items():
            shapes[kname] = (v.shape, I32 if v.dtype == np.int32 else F32)
    return shapes


def run(inputs, S, L, stop=None, dbg=None):
    sh, cst, per_core = prep_inputs(inputs, S)
    sh = {kk: (v[:L] if v.shape[0] == inputs["ada_w"].shape[0] and kk not in () else v) for kk, v in sh.items()}
    shapes = make_shapes(sh, cst, per_core[0])
    nc = build(S, L, shapes, stop=stop, dbg=dbg)
    in_maps = []
    for b in range(NCORES):
        m = dict(sh)
        m.update(cst)
        m.update(per_core[b])
        in_maps.append(m)
    res = run_bass_kernel_spmd(nc, in_maps, core_ids=list(range(NCORES)))
    return res


def kernel(**inputs):
    S = inputs["x"].shape[1]
    L = inputs["ada_w"].shape[0]
    res = run(inputs, S, L)
    out = np.stack([np.ascontiguousarray(res.results[b]["outT"].T) for b in range(NCORES)], axis=0)
    return out.astype(np.float32)
```

```python
import math
import numpy as np
from contextlib import ExitStack
import concourse.bass as bass
import concourse.mybir as mybir
from concourse.bass_utils import run_bass_kernel_spmd

F32 = mybir.dt.float32
BF16 = mybir.dt.bfloat16
I32 = mybir.dt.int32
AF = mybir.ActivationFunctionType
ALU = mybir.AluOpType
AX = mybir.AxisListType

D = 1024
DIN = 2080
NCORES = 8
ENGS = ["pe", "act", "dve", "pool", "sp"]


class Sched:
    def __init__(self, nc):
        self.nc = nc
        self.ops = {e: [] for e in ENGS}
        self.count = {e: 0 for e in ENGS}
        self.seen = {e: {} for e in ENGS}
        self.last_write = {}
        self.readers = {}
        self.dma_count = {}
        self.sem_names = list(ENGS)
        self.sems = {}
        self.rr = 0

    def _deps(self, eng, reads, writes):
        deps = {}

        def add(tok):
            if tok is not None and deps.get(tok[0], 0) < tok[1]:
                deps[tok[0]] = tok[1]

        for r in reads:
            add(self.last_write.get(r))
        for w in writes:
            add(self.last_write.get(w))
            for t in self.readers.get(w, ()):
                add(t)
        out = []
        for k, v in deps.items():
            if eng == "pe" and k == "pe":
                continue
            if self.seen[eng].get(k, 0) < v:
                self.seen[eng][k] = v
                out.append((k, v))
        return out

    def _commit(self, tok, reads, writes):
        for r in reads:
            lst = self.readers.setdefault(r, [])
            lst[:] = [t for t in lst if t[0] != tok[0]]
            lst.append(tok)
        for w in writes:
            self.last_write[w] = tok
            self.readers[w] = []

    def op(self, eng, fn, reads=(), writes=()):
        waits = self._deps(eng, reads, writes)
        self.count[eng] += 1
        tok = (eng, self.count[eng])
        self.ops[eng].append((waits, fn, (eng, 1)))
        self._commit(tok, reads, writes)
        return tok

    def dma(self, eng, fn, key, reads=(), writes=()):
        waits = self._deps(eng, reads, writes)
        k = ("dma", key)
        if k not in self.dma_count:
            self.dma_count[k] = 0
            self.sem_names.append(k)
        self.dma_count[k] += 16
        tok = (k, self.dma_count[k])
        self.ops[eng].append((waits, fn, (k, 16)))
        self._commit(tok, reads, writes)
        return tok

    def barrier(self):
        toks = [(e, self.count[e]) for e in ENGS if self.count[e] > 0]
        toks += [(k, v) for k, v in self.dma_count.items()]
        for e in ENGS:
            waits = []
            for k, v in toks:
                if k != e and self.seen[e].get(k, 0) < v:
                    self.seen[e][k] = v
                    waits.append((k, v))
            if waits:
                self.ops[e].append((waits, None, None))

    def flush(self, stack):
        nc = self.nc
        for k in self.sem_names:
            if k not in self.sems:
                nm = "s_" + "_".join(str(x) for x in (k if isinstance(k, tuple) else (k,)))
                self.sems[k] = stack.enter_context(nc.semaphore(nm))
        sems = self.sems
        ops = self.ops
        self.ops = {e: [] for e in ENGS}
        with nc.Block() as block:
            def replay(engname):
                def body(e):
                    for waits, fn, inc in ops[engname]:
                        for k, v in waits:
                            e.wait_ge(sems[k], v)
                        if fn is not None:
                            fn(e).then_inc(sems[inc[0]], inc[1])
                return body

            block.tensor(replay("pe"))
            block.scalar(replay("act"))
            block.vector(replay("dve"))
            block.gpsimd(replay("pool"))
            block.sync(replay("sp"))


def _pk(v, p=128):
    v = np.asarray(v)
    n = v.shape[-1] // p
    return np.ascontiguousarray(np.swapaxes(v.reshape(v.shape[:-1] + (n, p)), -1, -2))


def prep_inputs(inp, S):
    L = inp["ada_w"].shape[0]
    f32 = np.float32
    sh = {}
    sh["ada_w"] = np.ascontiguousarray(inp["ada_w"], f32)
    sh["ada_b"] = _pk(inp["ada_b"])
    sh["n1g"] = _pk(inp["norm1_g"])
    sh["n2g"] = _pk(inp["norm2_g"])
    sh["w_in"] = np.ascontiguousarray(inp["w_in"], f32)
    sh["w_out"] = np.ascontiguousarray(inp["w_out"], f32)
    sh["mu"] = _pk(inp["rwkv_mu"])
    rv = np.stack([inp["rwkv_w0"], inp["rwkv_a0"], inp["rwkv_k_k"], inp["rwkv_k_a"],
                   inp["rwkv_r_k"].reshape(L, 256), inp["rwkv_ln_w"], inp["rwkv_ln_b"]], axis=1)
    sh["rvec"] = np.ascontiguousarray(np.transpose(_pk(rv), (0, 2, 1, 3)))
    sh["lora"] = np.ascontiguousarray(np.concatenate([inp["rwkv_w2"], inp["rwkv_a2"], inp["rwkv_g2"]], axis=1))

    def s5vec(a):
        return np.ascontiguousarray(a.reshape(L, 8, 2, 64).transpose(0, 2, 3, 1).reshape(L, 128, 8))
    ldt = np.broadcast_to(inp["s5_log_dt"][:, :, None], (L, 16, 64))
    sh["s5v"] = np.ascontiguousarray(np.stack([s5vec(inp["s5_lambda_re"]), s5vec(inp["s5_lambda_im"]), s5vec(ldt)], axis=2))
    bpad = np.zeros((L, 2, 8, 128, 128), f32)
    cpad = np.zeros((L, 2, 8, 128, 128), f32)
    for ri, (bb, cc) in enumerate([(inp["s5_b_re"], inp["s5_c_re"]), (inp["s5_b_im"], inp["s5_c_im"])]):
        for g in range(16):
            jt, q = g // 2, g % 2
            r0 = (g % 8) * 16
            bpad[:, ri, jt, r0:r0 + 16, q * 64:(q + 1) * 64] = np.transpose(bb[:, g], (0, 2, 1))
            cpad[:, ri, jt, q * 64:(q + 1) * 64, r0:r0 + 16] = np.transpose(cc[:, g], (0, 2, 1))
    sh["s5b"] = bpad
    sh["s5c"] = cpad
    sv = np.stack([inp["s5_d"], inp["s5_glu_b"], inp["branch_norm_g"][:, 0]], axis=1)
    sh["s5vec2"] = np.ascontiguousarray(np.transpose(_pk(sv), (0, 2, 1, 3)))
    sh["glu_w"] = np.ascontiguousarray(inp["s5_glu_w"], f32)
    sh["qng"] = _pk(inp["mla_q_norm_g"])
    sh["w_uq"] = np.ascontiguousarray(inp["mla_w_uq"], f32)
    sh["kvng"] = _pk(inp["mla_kv_norm_g"])
    wkv = inp["mla_w_ukv"].reshape(L, 128, 4, 128)
    kn = np.zeros((L, 128, 4, 96), f32)
    kn[..., :64] = wkv[..., :64]
    sh["w_ukn"] = kn
    sh["w_ukv"] = np.ascontiguousarray(wkv[..., 64:].reshape(L, 128, 256))
    sh["qkhg"] = np.ascontiguousarray(np.stack([inp["mla_q_head_g"], inp["mla_k_head_g"]], axis=2))
    cw = inp["lru_conv_w"]
    lv = np.concatenate([cw, inp["lru_conv_b"][:, None], inp["lru_b_a"][:, None], inp["lru_b_x"][:, None],
                         inp["lru_lambda"][:, None], inp["branch_norm_g"][:, 2][:, None]], axis=1)
    sh["lruv"] = np.ascontiguousarray(np.transpose(_pk(lv), (0, 2, 1, 3)))
    bd = np.zeros((L, 2, 2, 128, 128), f32)
    for wi, wmat in enumerate([inp["lru_w_a"], inp["lru_w_x"]]):
        for n in range(4):
            t, q = n // 2, n % 2
            bd[:, wi, t, q * 64:(q + 1) * 64, q * 64:(q + 1) * 64] = wmat[:, n]
    sh["lrubd"] = bd
    sh["bng_c"] = np.ascontiguousarray(inp["branch_norm_g"][:, 1].reshape(L, 4, 64).transpose(0, 2, 1))
    sh["wr"] = np.ascontiguousarray(np.concatenate([inp["moe_w_group"], inp["moe_w_expert"]], axis=2))
    br = np.concatenate([inp["moe_b_group"], inp["moe_b_expert"]], axis=1)
    sh["br"] = np.ascontiguousarray(np.broadcast_to(br[:, None, :], (L, 128, 36)))
    sh["w1"] = np.ascontiguousarray(inp["moe_w1"], f32)
    sh["w3"] = np.ascontiguousarray(inp["moe_w3"], f32)
    sh["w2"] = np.ascontiguousarray(inp["moe_w2"], f32)
    cst = {}
    cst["ident"] = np.eye(128, dtype=f32)
    bo = np.zeros((128, 128), f32)
    bo[:64, :64] = 1.0
    bo[64:, 64:] = 1.0
    cst["blk"] = bo
    half = 16
    invf = np.power(np.float32(10000.0), -np.arange(half, dtype=f32) * np.float32(2.0) / np.float32(32)).astype(f32)
    iv = np.zeros((96, 1), f32)
    iv[64:80, 0] = invf
    iv[80:96, 0] = invf
    cst["invf"] = iv
    PT = np.zeros((128, 128), f32)
    for i in range(16):
        PT[80 + i, 64 + i] = -1.0
        PT[64 + i, 80 + i] = 1.0
    cst["rotT"] = PT
    E = np.zeros((32, 96), f32)
    for i in range(32):
        E[i, 64 + i] = 1.0
    cst["epe"] = E
    jj = np.arange(64)[:, None]
    ii = np.arange(64)[None, :]
    mk = np.concatenate([(jj < ii), (jj <= ii)], axis=1).astype(f32)
    cst["mk"] = np.ascontiguousarray(np.broadcast_to(mk[:, None, :], (64, 4, 128)))
    cst["mkl"] = np.ascontiguousarray(np.broadcast_to((jj > ii).astype(f32)[:, None, :], (64, 4, 64)))
    cst["id4"] = np.ascontiguousarray(np.broadcast_to(np.eye(64, dtype=f32)[:, None, :], (64, 4, 64)))
    sel = np.zeros((128, 64), f32)
    sel[64, :] = 1.0
    cst["sel65"] = sel
    s32 = np.zeros((128, 32, 128), f32)
    for e in range(32):
        s32[e, e, :] = 1.0
    cst["sel32"] = s32
    cst["tvals"] = np.ascontiguousarray(np.broadcast_to(np.arange(NB, dtype=f32)[None, :], (128, NB)))
    per_core = []
    for b in range(NCORES):
        d = {}
        d["xT"] = np.ascontiguousarray(inp["x"][b, :S].T, f32)
        d["c"] = _pk(inp["c"][b])
        d["pos"] = np.ascontiguousarray(np.broadcast_to(inp["pos_offset"][b].astype(np.int32).reshape(1, 1), (96, 1)))
        per_core.append(d)
    return sh, cst, per_core


class K:
    pass


def build(S, L, shapes, stop=None, dbg=None):
    nc = bass.Bass("TRN2", target_bir_lowering=False)
    k = K()
    k.nc = nc
    k.S = S
    k.L = L
    dr = {}
    for name, (shp, dt) in shapes.items():
        dr[name] = nc.dram_tensor(name, list(shp), dt, kind="ExternalInput").ap()
    dr["out"] = nc.dram_tensor("outT", [D, S], F32, kind="ExternalOutput").ap()
    k.mixT = nc.dram_tensor("mixT", [D, S], BF16, kind="Internal").ap()
    k.x1T = nc.dram_tensor("x1T", [D, S], F32, kind="Internal").ap()
    k.x2T = nc.dram_tensor("x2T", [D, S], F32, kind="Internal").ap()
    k.h2T = nc.dram_tensor("h2T", [D, S], BF16, kind="Internal").ap()
    k.gT = nc.dram_tensor("gT", [32, S], F32, kind="Internal").ap()
    NE = shapes["w1"][0][1]
    k.w1b = nc.dram_tensor("w1b", [NE, 128, 4096], BF16, kind="Internal").ap()
    k.w3b = nc.dram_tensor("w3b", [NE, 128, 4096], BF16, kind="Internal").ap()
    k.w2b = nc.dram_tensor("w2b", [NE, 128, 4096], BF16, kind="Internal").ap()
    k.qs = nc.dram_tensor("qs", [96, 4, S], BF16, kind="Internal").ap()
    k.ks = nc.dram_tensor("ks", [96, 4, S], BF16, kind="Internal").ap()
    k.vs = nc.dram_tensor("vs", [S // 128, 128, 260], BF16, kind="Internal").ap()
    if dbg is not None:
        dr["dbg"] = nc.dram_tensor("dbg", list(dbg), F32, kind="ExternalOutput").ap()
    k.dr = dr
    with ExitStack() as st:
        k.st = st
        S_ = Sched(nc)
        k.S_ = S_
        emit_all(k, stop)
        S_.barrier()
        S_.flush(st)
    return nc


def sb(k, st, name, shape, dt=F32):
    k.uid = getattr(k, "uid", 0) + 1
    return st.enter_context(k.nc.sbuf_tensor("sb%d_%s" % (k.uid, name), list(shape), dt))


class Ops:
    def __init__(self, S_):
        self.S = S_

    def dma(self, eng, out, in_, key, r=(), w=()):
        return self.S.dma(eng, lambda e: e.dma_start(out=out, in_=in_), key, r, w)

    def act(self, out, in_, func, r, w, bias=None, scale=None, eng="act"):
        kw = {}
        if bias is not None:
            kw["bias"] = bias
        if scale is not None:
            kw["scale"] = scale
        return self.S.op(eng, lambda e: e.activation(out=out, in_=in_, func=func, **kw), r, w)

    def tt(self, out, in0, in1, op, r, w, eng="dve"):
        return self.S.op(eng, lambda e: e.tensor_tensor(out=out, in0=in0, in1=in1, op=op), r, w)

    def ts(self, out, in0, s1, s2, op0, op1, r, w, eng="dve"):
        if s2 is None:
            return self.S.op(eng, lambda e: e.tensor_scalar(out=out, in0=in0, scalar1=s1, scalar2=None, op0=op0), r, w)
        return self.S.op(eng, lambda e: e.tensor_scalar(out=out, in0=in0, scalar1=s1, scalar2=s2, op0=op0, op1=op1), r, w)

    def stt(self, out, in0, scalar, in1, op0, op1, r, w, eng="dve"):
        return self.S.op(eng, lambda e: e.scalar_tensor_tensor(out=out, in0=in0, scalar=scalar, in1=in1, op0=op0, op1=op1), r, w)

    def copy(self, out, in_, r, w, eng="dve"):
        if eng == "act":
            return self.S.op(eng, lambda e: e.activation(out=out, in_=in_, func=AF.Copy), r, w)
        return self.S.op(eng, lambda e: e.tensor_copy(out=out, in_=in_), r, w)

    def memset(self, out, val, w, eng="pool"):
        return self.S.op(eng, lambda e: e.memset(out, val), (), w)

    def scan(self, out, d0, d1, init, r, w, eng="dve"):
        return self.S.op(eng, lambda e: e.tensor_tensor_scan(out=out, data0=d0, data1=d1, initial=init, op0=ALU.mult, op1=ALU.add), r, w)

    def mm(self, out, lhsT, rhs, start, stop, r, w):
        return self.S.op("pe", lambda e: e.matmul(out, lhsT, rhs, start=start, stop=stop), r, w)

    def tr(self, out, in_, ident, r, w):
        return self.S.op("pe", lambda e: e.transpose(out, in_, ident), r, w)


NB = 256


def emit_all(k, stop):
    nc, S_, dr, S, L = k.nc, k.S_, k.dr, k.S, k.L
    st = k.st
    o = Ops(S_)
    nblk = S // NB
    ps = [st.enter_context(nc.psum_tensor(f"ps{i}", [128, 512], F32)) for i in range(8)]
    P = [f"ps{i}" for i in range(8)]
    ident = sb(k, st, "ident", [128, 128])
    blk = sb(k, st, "blk", [128, 128])
    blkb = sb(k, st, "blkb", [128, 128], BF16)
    onesb = sb(k, st, "onesb", [128, 128], BF16)
    mods = sb(k, st, "mods", [128, L, 48])
    g1s = sb(k, st, "g1s", [128, L, 8])
    g2s = sb(k, st, "g2s", [128, L, 8])
    k.ident, k.blk, k.blkb, k.onesb, k.mods, k.g1s, k.g2s, k.ps, k.P, k.o = ident, blk, blkb, onesb, mods, g1s, g2s, ps, P, o
    o.dma("sp", ident[:], dr["ident"], "ident", w=["ident"])
    o.dma("sp", blk[:], dr["blk"], "blk", w=["blk"])
    o.copy(blkb[:], blk[:], ["blk"], ["blkb"])
    o.memset(onesb[:], 1.0, ["onesb"])
    ones32 = sb(k, st, "ones32", [128, 128])
    k.ones32 = ones32
    o.memset(ones32[:], 1.0, ["ones32"])
    cc = sb(k, st, "cc", [128, 8])
    k.cc = cc
    for val, c in CC.items():
        o.memset(cc[:, c:c + 1], float(val), ["cc"])

    with ExitStack() as p0:
        stg = [sb(k, p0, f"adst{i}", [128, 8, 768]) for i in range(2)]
        cond = sb(k, p0, "cond", [128, 8])
        adab = sb(k, p0, "adab", [128, 48])
        ng = sb(k, p0, "ng", [128, 8])
        tmp8 = sb(k, p0, "tmp8", [128, 8])
        o.dma("sp", cond[:], dr["c"], "cond", w=["cond"])
        o.act(cond[:], cond[:], AF.Silu, ["cond"], ["cond"])
        for l in range(L):
            o.dma("sp", adab[:], dr["ada_b"][l], "adab", w=["adab"])
            wv = dr["ada_w"][l].rearrange("(kk p) n -> p kk n", p=128)
            for c in range(8):
                bk = f"adst{c % 2}"
                o.dma("sp" if c % 2 == 0 else "pool", stg[c % 2][:], wv[:, :, c * 768:(c + 1) * 768], bk, w=[bk])
                for j in range(6):
                    col = c * 6 + j
                    for kk in range(8):
                        o.mm(ps[0][:, col:col + 1], stg[c % 2][:, kk, j * 128:(j + 1) * 128], cond[:, kk:kk + 1],
                             kk == 0, kk == 7, [bk, "cond"], [P[0]])
            o.tt(mods[:, l, :], ps[0][:, 0:48], adab[:], ALU.add, [P[0], "adab"], ["mods"])
            for (gs, nm, c0) in ((g1s, "n1g", 8), (g2s, "n2g", 32)):
                o.dma("sp", ng[:], dr[nm][l], "ng", w=["ng"])
                o.ts(tmp8[:], mods[:, l, c0:c0 + 8], 1.0, None, ALU.add, None, ["mods"], ["tmp8"])
                o.tt(gs[:, l, :], tmp8[:], ng[:], ALU.mult, ["tmp8", "ng"], ["g1s" if c0 == 8 else "g2s"])
        S_.barrier()
        S_.flush(st)
    if stop == "p0":
        o.dma("sp", dr["dbg"][:, 0:L * 48], mods[:].rearrange("p l c -> p (l c)"), "dbg", r=["mods"], w=["dbg"])
        return

    xin = dr["xT"]
    for l in range(L):
        phaseA(k, l, xin, stop)
        if stop is not None and stop.startswith("A"):
            return
        phaseB(k, l, xin, k.x1T, stop)
        if stop is not None and stop.startswith("B"):
            return
        phaseW(k, l)
        xo = dr["out"] if l == L - 1 else k.x2T
        phaseC(k, l, k.x1T, xo, stop)
        if stop is not None and stop.startswith("C"):
            return
        xin = xo


CC = {1e-6: 0, 64e-5: 1, 1.0: 2, -math.pi: 3, 0.0: 4, 1e-24: 5}


def rsqrt(k, out, in_, scale, eps, rkeys, wkey, rows=128):
    c = CC[eps]
    k.o.act(out, in_, AF.Sqrt, list(rkeys) + ["cc"], [wkey], bias=k.cc[0:rows, c:c + 1], scale=scale)
    k.o.S.op("dve", lambda e: e.reciprocal(out=out, in_=out), [wkey], [wkey])


def rms_stats(k, srcs, n_feat, eps, ps_ap, rstd_ap, sq_bufs, rkeys, pkey, wkey, lhsT=None, rows=128):
    o = k.o
    n = len(srcs)
    lhs = k.onesb[:rows, :rows] if lhsT is None else lhsT
    for i, (ap, key) in enumerate(srcs):
        sq, sqk = sq_bufs[i]
        o.act(sq, ap, AF.Square, [key], [sqk])
        o.mm(ps_ap, lhs, sq, i == 0, i == n - 1, [sqk, "onesb"], [pkey])
    rsqrt(k, rstd_ap, ps_ap, 1.0 / n_feat, eps, [pkey], wkey, rows)


def phaseA(k, l, xin, stop):
    nc, S_, dr, S, L, o = k.nc, k.S_, k.dr, k.S, k.L, k.o
    ps, P = k.ps, k.P
    nblk = S // NB
    with ExitStack() as pa:
        w_in = sb(k, pa, "w_in", [128, 8, DIN], BF16)
        xblk = sb(k, pa, "xblk", [128, 8, NB])
        wv = dr["w_in"][l].rearrange("(kk p) n -> p kk n", p=128)
        xflat = xblk[:].rearrange("p a b -> p (a b)")
        for c in range(20):
            stv = xflat[:, (c % 2) * 832:(c % 2 + 1) * 832].rearrange("p (kk n) -> p kk n", n=104)
            o.dma("sp", stv, wv[:, :, c * 104:(c + 1) * 104], f"xblk{c % 2}", w=["xblk"])
            o.copy(w_in[:, :, c * 104:(c + 1) * 104], stv, ["xblk"], ["w_in"], eng=("dve" if c % 2 == 0 else "pool"))
        rstd = sb(k, pa, "rstd", [128, NB])
        xt = sb(k, pa, "xt", [128, NB])
        hT = sb(k, pa, "hT", [128, 8, NB], BF16)
        sqb = hT
        zr = sb(k, pa, "zr", [128, 7, NB + 1])
        s5u = sb(k, pa, "s5u", [128, 2, NB])
        qab = sb(k, pa, "qab", [128, 2, NB], BF16)
        kvab = sb(k, pa, "kvab", [128, NB], BF16)
        kpeb = sb(k, pa, "kpeb", [32, NB], BF16)
        lrx = sb(k, pa, "lrx", [128, 2, NB + 3])
        lrg = sb(k, pa, "lrg", [128, 2, NB])
        R = rwkv_setup(k, pa, l)
        Q = s5_setup(k, pa, l, xblk) if stop not in ("A1",) and not (stop or "").startswith("A2") else None
        U = lru_setup(k, pa, l, xblk) if stop not in ("A1",) and not (stop or "").startswith("A2") else None
        mixT = k.mixT
        A = mla_setup(k, pa, l, xblk, Q, U) if Q is not None else None
        o.memset(zr[:, :, 0:1], 0.0, ["zr"])
        o.memset(lrx[:, :, 0:3], 0.0, ["lrx"])
        xv = xin.rearrange("(kk p) s -> p kk s", p=128)
        tiles = [(t * 128, 128) for t in range(12)] + [(1536, 32)] + [(1568 + t * 128, 128) for t in range(4)]
        for i in range(nblk):
            t0 = i * NB
            o.dma("sp", xblk[:], xv[:, :, t0:t0 + NB], "xblk", w=["xblk"])
            rms_stats(k, [(xblk[:, kk, :], "xblk") for kk in range(8)], 1024.0, 1e-6, ps[0][:, 0:NB], rstd[:],
                      [(sqb[:, kk, :], "hT") for kk in range(8)], None, P[0], "rstd")
            for kk in range(8):
                o.tt(xt[:], xblk[:, kk, :], rstd[:], ALU.mult, ["xblk", "rstd"], ["xt"])
                o.act(hT[:, kk, :], xt[:], AF.Identity, ["xt", "g1s", "mods"], ["hT"],
                      bias=k.mods[:, l, kk:kk + 1], scale=k.g1s[:, l, kk:kk + 1])
            for ti, (c0, m) in enumerate(tiles):
                pb = 1 + (ti % 3)
                pt = ps[pb][0:m, 0:NB]
                for kk in range(8):
                    o.mm(pt, w_in[:, kk, c0:c0 + m], hT[:, kk, :], kk == 0, kk == 7, ["w_in", "hT"], [P[pb]])
                if ti < 7:
                    o.copy(zr[:, ti, 1:NB + 1], pt, [P[pb]], ["zr"], eng=("act" if ti % 2 == 0 else "dve"))
                elif ti < 9:
                    o.copy(s5u[:, ti - 7, :], pt, [P[pb]], ["s5u"], eng="dve")
                elif ti < 11:
                    o.copy(qab[:, ti - 9, :], pt, [P[pb]], ["qab"], eng="act")
                elif ti == 11:
                    o.copy(kvab[:], pt, [P[pb]], ["kvab"], eng="act")
                elif ti == 12:
                    o.copy(kpeb[:], pt, [P[pb]], ["kpeb"], eng="act")
                elif ti < 15:
                    o.copy(lrx[:, ti - 13, 3:NB + 3], pt, [P[pb]], ["lrx"], eng="dve")
                else:
                    o.copy(lrg[:, ti - 15, :], pt, [P[pb]], ["lrg"], eng="act")
            if stop != "A1":
                rwkv_block(k, l, i, R, zr, stop)
            if stop is None or not stop.startswith("A2"):
                o.dma("pool", mixT[0:256, t0:t0 + NB].rearrange("(t p) s -> p t s", p=128), R["youtb"][:], "mix_a", r=["youtb"], w=["mixT_a"])
                s5_block(k, l, i, Q, s5u, mixT, t0)
                lru_block(k, l, i, U, lrx, lrg, mixT, t0)
                mla_block(k, l, i, A, qab, kvab, kpeb, t0)
            if stop == "A4":
                dbg = dr["dbg"]
                o.copy(A["t1"][:], A["qrb"][:, 1, :], ["a_out"], ["l_aa"], eng="dve")
                o.dma("sp", dbg[0:96, t0:t0 + NB], A["t1"][:], "dbg", r=["l_aa"], w=["dbg"])
                o.copy(A["t2"][:], A["krb"][:, 2, :], ["a_out"], ["l_t1"], eng="dve")
                o.dma("sp", dbg[96:192, t0:t0 + NB], A["t2"][:], "dbg", r=["l_t1"], w=["dbg"])
                for tt in range(NB // 128):
                    o.copy(A["qf"][0:64, 0:128], A["Vt"][0:64, tt, 3, 0:128].rearrange("p c -> p c") if False else A["Vt"][0:64, tt, :, :].rearrange("p h c -> p (h c)")[:, 0:128], ["a_Vt"], ["l_xc"], eng="dve")
                    o.dma("sp", dbg[192:256, t0 + tt * 128:t0 + (tt + 1) * 128], A["qf"][0:64, 0:128], "dbg", r=["l_xc"], w=["dbg"])
                continue
            if stop == "A3":
                dbg = dr["dbg"]
                o.dma("sp", dbg[256:512, t0:t0 + NB].rearrange("(t p) s -> p t s", p=128), Q["yo"][:], "dbg", r=["s_yo"], w=["dbg"])
                o.dma("sp", dbg[768:1024, t0:t0 + NB].rearrange("(t p) s -> p t s", p=128), U["yo"][:], "dbg", r=["l_yo"], w=["dbg"])
                o.dma("sp", dbg[0:256, t0:t0 + NB].rearrange("(t p) s -> p t s", p=128), R["yout"][:], "dbg", r=["yout"], w=["dbg"])
                continue
            if stop is not None and stop.startswith("A2"):
                dbg = dr["dbg"]
                o.dma("sp", dbg[0:256, t0:t0 + NB].rearrange("(t p) s -> p t s", p=128), R["yout"][:], "dbg", r=["yout"], w=["dbg"])
                continue
            if stop == "A1":
                dbg = dr["dbg"]
                o.dma("sp", dbg[0:896, t0:t0 + NB].rearrange("(t p) s -> p t s", p=128), zr[:, :, 1:NB + 1], "dbg", r=["zr"], w=["dbg"])
                o.dma("sp", dbg[896:1152, t0:t0 + NB].rearrange("(t p) s -> p t s", p=128), s5u[:], "dbg", r=["s5u"], w=["dbg"])
                o.dma("sp", dbg[1568:1824, t0:t0 + NB].rearrange("(t p) s -> p t s", p=128), lrx[:, :, 3:NB + 3], "dbg", r=["lrx"], w=["dbg"])
                o.dma("sp", dbg[1824:2080, t0:t0 + NB].rearrange("(t p) s -> p t s", p=128), lrg[:], "dbg", r=["lrg"], w=["dbg"])
                continue
        S_.barrier()
        S_.flush(k.st)


def rwkv_setup(k, pa, l):
    o, dr = k.o, k.dr
    R = {}
    nch = NB // 64
    f2 = [128, 2, NB]
    for nm in ("gg", "bonus", "epos", "bt", "kt", "ya", "yout"):
        R[nm] = sb(k, pa, "r_" + nm, f2)
    for nm in ("sg", "aa", "kk", "tA", "tB", "kmod", "cls", "eneg", "eex"):
        R[nm] = sb(k, pa, "r_" + nm, [128, 1, NB])
    R["zs"] = sb(k, pa, "r_zs", [128, 7, NB])
    R["youtb"] = sb(k, pa, "r_youtb", [128, 2, NB], BF16)
    R["lob"] = sb(k, pa, "r_lob", [128, NB], BF16)
    R["sqb"] = sb(k, pa, "r_sqb", [128, NB], BF16)
    R["ar"] = sb(k, pa, "r_ar", [128, 2, nch, 2, 64])
    R["tok"] = sb(k, pa, "r_tok", [128, 3, 2, 128])
    R["NL"] = [sb(k, pa, f"r_NL{i}", [64, 2, 4, 64]) for i in range(2)]
    R["Pm"] = [sb(k, pa, f"r_Pm{i}", [64, 4, 64]) for i in range(2)]
    R["LakT"] = sb(k, pa, "r_LakT", [64, 4, 64])
    R["QrbT"] = sb(k, pa, "r_QrbT", [128, 4, 64])
    R["QrkT"] = sb(k, pa, "r_QrkT", [128, 4, 64])
    R["Xs"] = sb(k, pa, "r_Xs", [64, 256])
    R["Mtmp"] = sb(k, pa, "r_Mtmp", [128, 128])
    R["Us"] = sb(k, pa, "r_Us", [128, 256])
    R["M"] = [sb(k, pa, f"r_M{i}", [128, 2, 128]) for i in range(2)]
    R["mu"] = sb(k, pa, "r_mu", [128, 7])
    R["omu"] = sb(k, pa, "r_omu", [128, 7])
    R["rvec"] = sb(k, pa, "r_rvec", [128, 7, 2])
    R["lst"] = sb(k, pa, "r_lst", [128, 256])
    R["lora"] = sb(k, pa, "r_lora", [128, 256], BF16)
    R["mk"] = sb(k, pa, "r_mk", [64, 4, 128])
    R["mkl"] = sb(k, pa, "r_mkl", [64, 4, 64])
    R["id4"] = sb(k, pa, "r_id4", [64, 4, 64])
    R["cmask"] = sb(k, pa, "r_cmask", [128, NB])
    o.dma("sp", R["mu"][:], dr["mu"][l], "r_mu", w=["r_mu"])
    o.ts(R["omu"][:], R["mu"][:], -1.0, 1.0, ALU.mult, ALU.add, ["r_mu"], ["r_omu"])
    o.dma("sp", R["rvec"][:], dr["rvec"][l], "r_rvec", w=["r_rvec"])
    o.dma("sp", R["lst"][:], dr["lora"][l], "r_lst", w=["r_lst"])
    o.copy(R["lora"][:], R["lst"][:], ["r_lst"], ["r_lora"])
    o.dma("sp", R["mk"][:], dr["mk"], "r_mk", w=["r_mk"])
    o.dma("sp", R["mkl"][:], dr["mkl"], "r_mkl", w=["r_mkl"])
    o.dma("sp", R["id4"][:], dr["id4"], "r_id4", w=["r_id4"])
    o.memset(R["cmask"][:], 1.0, ["r_cmask"])
    o.memset(R["cmask"][:].rearrange("p (c j) -> p c j", j=64)[:, :, 0:1], 0.0, ["r_cmask"])
    o.memset(R["M"][0][:], 0.0, ["r_M0"])
    o.memset(R["tok"][:], 0.0, ["r_tok"])
    o.memset(R["Us"][:], 0.0, ["r_Us"])
    o.memset(R["QrbT"][:], 0.0, ["r_QrbT"])
    o.memset(R["QrkT"][:], 0.0, ["r_QrkT"])
    return R


C0 = -math.exp(-0.5)


def rwkv_block(k, l, i, R, zr, stop):
    o, ps, P = k.o, k.ps, k.P
    nch = NB // 64
    zs, rv = R["zs"], R["rvec"]
    W0, A0, KK, KA, RK, LNW, LNB = range(7)
    for t in range(7):
        eng = "dve" if t % 2 == 0 else "pool"
        o.ts(zs[:, t, :], zr[:, t, 0:NB], R["mu"][:, t:t + 1], None, ALU.mult, None, ["zr", "r_mu"], [("zs", t)], eng=eng)
        o.stt(zs[:, t, :], zr[:, t, 1:NB + 1], R["omu"][:, t:t + 1], zs[:, t, :], ALU.mult, ALU.add, ["zr", "r_omu", ("zs", t)], [("zs", t)])
    o.copy(zr[:, :, 0:1], zr[:, :, NB:NB + 1], ["zr"], ["zr"], eng="dve")
    lob = R["lob"]
    o.act(lob[0:32, :], zs[0:32, 6, :], AF.Tanh, [("zs", 6)], ["r_lob"])
    o.act(lob[64:128, :], zs[64:128, 6, :], AF.Sigmoid, [("zs", 6)], ["r_lob"])
    o.copy(lob[32:64, :], zs[32:64, 6, :], [("zs", 6)], ["r_lob"], eng="dve")
    lora = R["lora"]
    for p in range(2):
        cs = slice(p * 128, (p + 1) * 128)
        rT, kT, vT = zs[:, p, :], zs[:, 2 + p, :], zs[:, 4 + p, :]
        rk_, kk_, vk_ = ("zs", p), ("zs", 2 + p), ("zs", 4 + p)
        o.mm(ps[1][:, 0:NB], lora[0:32, cs], lob[0:32, :], True, True, ["r_lora", "r_lob"], [P[1]])
        o.mm(ps[2][:, 0:NB], lora[32:64, cs], lob[32:64, :], True, True, ["r_lora", "r_lob"], [P[2]])
        o.mm(ps[3][:, 0:NB], lora[64:128, cs], lob[64:128, :], True, True, ["r_lora", "r_lob"], [P[3]])
        o.act(R["sg"][:, 0, :], ps[1][:, 0:NB], AF.Sigmoid, [P[1], "r_rvec"], ["r_sg"], bias=rv[:, W0, p:p + 1])
        o.act(R["aa"][:, 0, :], ps[2][:, 0:NB], AF.Sigmoid, [P[2], "r_rvec"], ["r_aa"], bias=rv[:, A0, p:p + 1])
        o.copy(R["gg"][:, p, :], ps[3][:, 0:NB], [P[3]], ["r_gg"], eng="dve")
        kk = R["kk"][:, 0, :]
        o.ts(kk, kT, rv[:, KK, p:p + 1], None, ALU.mult, None, [kk_, "r_rvec"], ["r_kk"])
        o.act(R["sqb"][:], kk, AF.Square, ["r_kk"], ["r_sqb"])
        o.mm(ps[1][:, 0:NB], k.blkb[:], R["sqb"][:], True, True, ["blkb", "r_sqb"], [P[1]])
        rsqrt(k, R["tA"][:, 0, :], ps[1][:, 0:NB], 1.0, 1e-24, [P[1]], "r_tA")
        o.tt(kk, kk, R["tA"][:, 0, :], ALU.mult, ["r_kk", "r_tA"], ["r_kk"])
        o.ts(R["tA"][:, 0, :], R["aa"][:, 0, :], -1.0, rv[:, KA, p:p + 1], ALU.add, ALU.mult, ["r_aa", "r_rvec"], ["r_tA"])
        o.stt(R["kmod"][:, 0, :], R["tA"][:, 0, :], 1.0, kT, ALU.add, ALU.mult, ["r_tA", kk_], ["r_kmod"])
        o.tt(R["tA"][:, 0, :], rT, R["kmod"][:, 0, :], ALU.mult, [rk_, "r_kmod"], ["r_tA"])
        o.ts(R["sqb"][:], R["tA"][:, 0, :], rv[:, RK, p:p + 1], None, ALU.mult, None, ["r_tA", "r_rvec"], ["r_sqb"])
        o.mm(ps[2][:, 0:NB], k.blkb[:], R["sqb"][:], True, True, ["blkb", "r_sqb"], [P[2]])
        o.tt(R["bonus"][:, p, :], ps[2][:, 0:NB], vT, ALU.mult, [P[2], vk_], ["r_bonus"])
        o.scan(R["cls"][:, 0, :], R["cmask"][:], R["sg"][:, 0, :], 0.0, ["r_cmask", "r_sg"], ["r_cls"])
        o.act(R["epos"][:, p, :], R["cls"][:, 0, :], AF.Exp, ["r_cls"], ["r_epos"], scale=C0)
        o.act(R["eneg"][:, 0, :], R["cls"][:, 0, :], AF.Exp, ["r_cls"], ["r_eneg"], scale=-C0)
        o.tt(R["tB"][:, 0, :], R["cls"][:, 0, :], R["sg"][:, 0, :], ALU.subtract, ["r_cls", "r_sg"], ["r_tB"])
        o.act(R["eex"][:, 0, :], R["tB"][:, 0, :], AF.Exp, ["r_tB"], ["r_eex"], scale=C0)
        arv = R["ar"][:, p, :, :, :]
        o.tt(arv[:, :, 1, :], rT.rearrange("p (c j) -> p c j", j=64), R["epos"][:, p, :].rearrange("p (c j) -> p c j", j=64),
             ALU.mult, [rk_, "r_epos"], ["r_ar"])
        o.stt(arv[:, :, 0, :], kk.rearrange("p (c j) -> p c j", j=64), -1.0, R["eex"][:, 0, :].rearrange("p (c j) -> p c j", j=64),
              ALU.mult, ALU.mult, ["r_kk", "r_eex"], ["r_ar"])
        o.tt(R["kt"][:, p, :], R["kmod"][:, 0, :], R["eneg"][:, 0, :], ALU.mult, ["r_kmod", "r_eneg"], ["r_kt"])
        o.tt(R["tA"][:, 0, :], kk, R["aa"][:, 0, :], ALU.mult, ["r_kk", "r_aa"], ["r_tA"])
        o.tt(R["bt"][:, p, :], R["tA"][:, 0, :], R["eneg"][:, 0, :], ALU.mult, ["r_tA", "r_eneg"], ["r_bt"])
    ar, bt, kt, tok = R["ar"], R["bt"], R["kt"], R["tok"]
    NL, Pm = R["NL"], R["Pm"]
    if stop == "A2a":
        return
    for c in range(nch):
        gc = i * nch + c
        cs = slice(c * 64, (c + 1) * 64)
        for p in range(2):
            o.tr(ps[4][0:64, p * 128:(p + 1) * 128], bt[:, p, cs], k.ident[:], ["r_bt", "ident"], [P[4]])
            o.tr(ps[4][0:64, 256 + p * 128:256 + (p + 1) * 128], kt[:, p, cs], k.ident[:], ["r_kt", "ident"], [P[4]])
            o.tr(ps[5][0:64, p * 128:(p + 1) * 128], zs[:, 4 + p, cs], k.ident[:], [("zs", 4 + p), "ident"], [P[5]])
        o.copy(tok[0:64, 0:2, :, :].rearrange("j a p c -> j (a p c)"), ps[4][0:64, :], [P[4]], ["r_tok"], eng="act")
        o.copy(tok[0:64, 2, :, :].rearrange("j p c -> j (p c)"), ps[5][0:64, 0:256], [P[5]], ["r_tok"], eng="dve")
        if stop == "A2b":
            continue
        for h in range(4):
            p, q = h // 2, h % 2
            rs = slice(q * 64, (q + 1) * 64)
            arh = ar[rs, p, c, :, :].rearrange("k a j -> k (a j)")
            o.mm(ps[6][0:64, h * 128:(h + 1) * 128], bt[rs, p, cs], arh, True, True, ["r_bt", "r_ar"], [P[6]])
            o.mm(ps[7][0:64, h * 128:(h + 1) * 128], kt[rs, p, cs], arh, True, True, ["r_kt", "r_ar"], [P[7]])
            o.mm(ps[5][0:64, 256 + h * 64:256 + (h + 1) * 64], ar[rs, p, c, 0, :], bt[rs, p, cs], True, True, ["r_bt", "r_ar"], [P[5]])
        v6 = ps[6][0:64, :].rearrange("j (h x) -> j h x", x=128)
        v7 = ps[7][0:64, :].rearrange("j (h x) -> j h x", x=128)
        mk = R["mk"]
        o.tt(NL[0][:, 0, :, :], v6[:, :, 0:64], mk[:, :, 0:64], ALU.mult, [P[6], "r_mk"], ["r_NL0"])
        o.tt(R["QrbT"][0:64], v6[:, :, 64:128], mk[:, :, 64:128], ALU.mult, [P[6], "r_mk"], ["r_QrbT"])
        o.tt(R["LakT"][:], v7[:, :, 0:64], mk[:, :, 0:64], ALU.mult, [P[7], "r_mk"], ["r_LakT"])
        o.tt(R["QrkT"][0:64], v7[:, :, 64:128], mk[:, :, 64:128], ALU.mult, [P[7], "r_mk"], ["r_QrkT"])
        o.tt(NL[0][:, 1, :, :], ps[5][0:64, 256:512].rearrange("j (h x) -> j h x", x=64), R["mkl"][:], ALU.mult, [P[5], "r_mkl"], ["r_NL0"])
        o.tt(Pm[1][:], NL[0][:, 0, :, :], R["id4"][:], ALU.add, ["r_NL0", "r_id4"], ["r_Pm1"], eng="pool")
        if stop == "A2c":
            continue
        for m in range(1, 7):
            src, dst = NL[(m - 1) % 2], NL[m % 2]
            sk, dk = f"r_NL{(m - 1) % 2}", f"r_NL{m % 2}"
            pin, pout = Pm[(m - 1) % 2], Pm[m % 2]
            pik, pok = f"r_Pm{(m - 1) % 2}", f"r_Pm{m % 2}"
            for h in range(4):
                if m <= 5:
                    o.mm(ps[6][0:64, h * 64:(h + 1) * 64], src[:, 1, h, :], src[:, 0, h, :], True, True, [sk], [P[6]])
                    o.mm(ps[6][0:64, 256 + h * 64:256 + (h + 1) * 64], src[:, 0, h, :], src[:, 1, h, :], True, True, [sk], [P[6]])
                if m >= 2:
                    o.mm(ps[7][0:64, h * 64:(h + 1) * 64], src[:, 1, h, :], pin[:, h, :], True, True, [sk, pik], [P[7]])
            if m <= 5:
                o.copy(dst[:].rearrange("j a h x -> j (a h x)"), ps[6][0:64, :], [P[6]], [dk], eng="act")
            if m >= 2:
                o.tt(pout[:].rearrange("j h x -> j (h x)"), ps[7][0:64, 0:256], pin[:].rearrange("j h x -> j (h x)"), ALU.add, [P[7], pik], [pok])
            else:
                pass
        TT_ = Pm[0]
        if stop == "A2d":
            continue
        Mc, Mn = R["M"][gc % 2], R["M"][(gc + 1) % 2]
        mck, mnk = f"r_M{gc % 2}", f"r_M{(gc + 1) % 2}"
        for p in range(2):
            o.mm(ps[4][0:64, p * 128:(p + 1) * 128], ar[:, p, c, 0, :], Mc[:, p, :], True, False, ["r_ar", mck], [P[4]])
            for q in range(2):
                h = 2 * p + q
                o.mm(ps[4][0:64, p * 128 + q * 64:p * 128 + (q + 1) * 64], R["LakT"][:, h, :], tok[0:64, 2, p, q * 64:(q + 1) * 64],
                     False, q == 1, ["r_LakT", "r_tok"], [P[4]])
        o.copy(R["Xs"][:], ps[4][0:64, 0:256], [P[4]], ["r_Xs"], eng="dve")
        if stop == "A2e":
            continue
        for h in range(4):
            o.mm(ps[4][0:64, 256 + h * 64:256 + (h + 1) * 64], TT_[:, h, :], R["Xs"][:, h * 64:(h + 1) * 64], True, True, ["r_Pm0", "r_Xs"], [P[4]])
        o.copy(R["Us"][0:64, :], ps[4][0:64, 256:512], [P[4]], ["r_Us"], eng="dve")
        if stop == "A2f":
            continue
        for p in range(2):
            pc = slice(p * 128, (p + 1) * 128)
            o.mm(ps[5][:, pc], k.ident[:], Mc[:, p, :], True, False, ["ident", mck], [P[5]])
            o.mm(ps[5][:, pc], tok[:, 0, p, :], R["Us"][:, pc], False, False, ["r_tok", "r_Us"], [P[5]])
            o.mm(ps[5][:, pc], tok[:, 1, p, :], tok[:, 2, p, :], False, True, ["r_tok"], [P[5]])
            for q in range(2):
                if stop == "A2g":
                    continue
                h = 2 * p + q
                yc = slice(256 + h * 64, 256 + (h + 1) * 64)
                o.mm(ps[5][:, yc], Mc[:, p, :], ar[:, p, c, 1, :], True, False, [mck, "r_ar"], [P[5]])
                o.mm(ps[5][:, yc], R["Us"][:, pc], R["QrbT"][:, h, :], False, False, ["r_Us", "r_QrbT"], [P[5]])
                o.mm(ps[5][:, yc], tok[:, 2, p, :], R["QrkT"][:, h, :], False, True, ["r_tok", "r_QrkT"], [P[5]])
        for p in range(2):
            pc = slice(p * 128, (p + 1) * 128)
            o.act(R["Mtmp"][:], ps[5][:, pc], AF.Identity, [P[5], "r_epos"], ["r_Mtmp"], scale=R["epos"][:, p, c * 64 + 63:c * 64 + 64])
            o.tt(Mn[:, p, :], R["Mtmp"][:], k.blk[:], ALU.mult, ["r_Mtmp", "blk"], [mnk])
            for q in range(2):
                if stop == "A2g":
                    continue
                h = 2 * p + q
                rs = slice(q * 64, (q + 1) * 64)
                o.copy(R["ya"][rs, p, cs], ps[5][rs, 256 + h * 64:256 + (h + 1) * 64], [P[5]], ["r_ya"], eng="act")
    for p in range(2):
        ya = R["ya"][:, p, :]
        o.mm(ps[1][:, 0:NB], k.blk[:], ya, True, True, ["blk", "r_ya"], [P[1]])
        o.stt(R["tA"][:, 0, :], ps[1][:, 0:NB], -1.0 / 64, ya, ALU.mult, ALU.add, [P[1], "r_ya"], ["r_tA"])
        o.act(R["tB"][:, 0, :], R["tA"][:, 0, :], AF.Square, ["r_tA"], ["r_tB"])
        o.mm(ps[2][:, 0:NB], k.blk[:], R["tB"][:, 0, :], True, True, ["blk", "r_tB"], [P[2]])
        rsqrt(k, R["tB"][:, 0, :], ps[2][:, 0:NB], 1.0 / 64, 64e-5, [P[2]], "r_tB")
        o.tt(R["tA"][:, 0, :], R["tA"][:, 0, :], R["tB"][:, 0, :], ALU.mult, ["r_tA", "r_tB"], ["r_tA"])
        o.ts(R["tA"][:, 0, :], R["tA"][:, 0, :], rv[:, LNW, p:p + 1], rv[:, LNB, p:p + 1], ALU.mult, ALU.add, ["r_tA", "r_rvec"], ["r_tA"])
        o.tt(R["tA"][:, 0, :], R["tA"][:, 0, :], R["bonus"][:, p, :], ALU.add, ["r_tA", "r_bonus"], ["r_tA"])
        o.tt(R["yout"][:, p, :], R["tA"][:, 0, :], R["gg"][:, p, :], ALU.mult, ["r_tA", "r_gg"], ["yout"])
    o.copy(R["youtb"][:], R["yout"][:], ["yout"], ["youtb"], eng="pool")


TWO_PI = 2.0 * math.pi
CW1 = 6.28125
CW2 = TWO_PI - CW1


def sin_of(k, out, ang, shift, tmp, tmpi, shape_rows, r, w, tk):
    o = k.o
    o.ts(tmp, ang, 1.0 / TWO_PI, 0.5 + shift / TWO_PI, ALU.mult, ALU.add, r, [tk])
    o.copy(tmpi, tmp, [tk], [tk + "i"])
    o.copy(tmp, tmpi, [tk + "i"], [tk])
    o.stt(out, tmp, -CW1, ang, ALU.mult, ALU.add, [tk] + list(r), w)
    o.stt(out, tmp, -CW2, out, ALU.mult, ALU.add, [tk] + list(w), w)
    if shift != 0.0:
        o.ts(out, out, float(shift), None, ALU.add, None, w, w)
    o.ts(tmp, out, -math.pi, TWO_PI, ALU.is_lt, ALU.mult, w, [tk])
    o.tt(out, out, tmp, ALU.add, list(w) + [tk], w)
    o.ts(tmp, out, math.pi, -TWO_PI, ALU.is_gt, ALU.mult, w, [tk])
    o.tt(out, out, tmp, ALU.add, list(w) + [tk], w)
    o.ts(out, out, math.pi, -math.pi, ALU.min, ALU.max, w, w)
    o.act(out, out, AF.Sin, w, w)


def gelu_tanh(k, out, x, t1, r, w, tk):
    o = k.o
    o.tt(t1, x, x, ALU.mult, r, [tk])
    o.ts(t1, t1, 0.044715, 1.0, ALU.mult, ALU.add, [tk], [tk])
    o.tt(t1, t1, x, ALU.mult, [tk] + list(r), [tk])
    o.act(t1, t1, AF.Sigmoid, [tk], [tk], scale=1.5957691216057308)
    o.tt(out, x, t1, ALU.mult, [tk] + list(r), w)


def s5_setup(k, pa, l, xblk):
    o, dr, ps, P = k.o, k.dr, k.ps, k.P
    Q = {}
    Q["v"] = sb(k, pa, "s_v", [128, 3, 8])
    for nm in ("dt", "mag", "th", "c8", "s8", "qre", "qim", "den", "t8a", "t8b", "Ere", "Eim", "cre", "cim", "glr", "gli"):
        Q[nm] = sb(k, pa, "s_" + nm, [128, 8])
    Q["t8i"] = sb(k, pa, "s_t8i", [128, 8], I32)
    for nm in ("cosT", "sinT"):
        Q[nm] = sb(k, pa, "s_" + nm, [128, 8, NB])
    Q["hre"] = sb(k, pa, "s_hre", [128, 8, NB], BF16)
    Q["him"] = sb(k, pa, "s_him", [128, 8, NB], BF16)
    Q["tv"] = sb(k, pa, "s_tv", [128, NB])
    Q["B"] = sb(k, pa, "s_B", [128, 2, 8, 128], BF16)
    Q["C"] = sb(k, pa, "s_C", [128, 2, 8, 128], BF16)
    Q["vec2"] = sb(k, pa, "s_vec2", [128, 3, 2])
    Q["glu"] = sb(k, pa, "s_glu", [128, 2, 256], BF16)
    Q["ub"] = sb(k, pa, "s_ub", [128, 2, NB], BF16)
    for nm in ("w1", "w2", "w3", "w4", "w5", "w6", "w7"):
        Q[nm] = sb(k, pa, "s_" + nm, [128, NB])
    Q["wi"] = sb(k, pa, "s_wi", [128, NB], I32)
    Q["yv"] = sb(k, pa, "s_yv", [128, 2, NB])
    Q["ge"] = sb(k, pa, "s_ge", [128, 2, NB])
    Q["geb"] = sb(k, pa, "s_geb", [128, 2, NB], BF16)
    Q["sqb"] = sb(k, pa, "s_sqb", [128, 2, NB], BF16)
    Q["yo"] = sb(k, pa, "s_yo", [128, 2, NB])
    Q["yob"] = sb(k, pa, "s_yob", [128, 2, NB], BF16)
    Q["dq"] = sb(k, pa, "s_dq", [128, 2, 128])
    v = Q["v"]
    wst = xblk[:].rearrange("p a b -> p (a b)").rearrange("p (a j c) -> p a j c", a=2, j=8)
    o.dma("sp", v[:], dr["s5v"][l], "s_v", w=["s_v"])
    o.dma("sp", Q["tv"][:], dr["tvals"], "s_tv", w=["s_tv"])
    o.dma("sp", Q["vec2"][:], dr["s5vec2"][l], "s_vec2", w=["s_vec2"])
    S = ["s_small"]
    o.act(Q["dt"][:], v[:, 2, :], AF.Exp, ["s_v"], S)
    o.tt(Q["t8a"][:], v[:, 0, :], Q["dt"][:], ALU.mult, ["s_v"] + S, S)
    o.act(Q["mag"][:], Q["t8a"][:], AF.Exp, S, S)
    o.tt(Q["th"][:], v[:, 1, :], Q["dt"][:], ALU.mult, ["s_v"] + S, S)
    sin_of(k, Q["s8"][:], Q["th"][:], 0.0, Q["t8b"][:], Q["t8i"][:], 128, S, S, "s_t8")
    sin_of(k, Q["c8"][:], Q["th"][:], math.pi / 2, Q["t8b"][:], Q["t8i"][:], 128, S, S, "s_t8")
    o.tt(Q["t8a"][:], Q["mag"][:], Q["c8"][:], ALU.mult, S, S)
    o.ts(Q["t8a"][:], Q["t8a"][:], -1.0, None, ALU.add, None, S, S)
    o.tt(Q["t8b"][:], Q["mag"][:], Q["s8"][:], ALU.mult, S + ["s_t8"], ["s_t8"])
    o.tt(Q["den"][:], v[:, 0, :], v[:, 0, :], ALU.mult, ["s_v"], S)
    o.tt(Q["qre"][:], v[:, 1, :], v[:, 1, :], ALU.mult, ["s_v"], S)
    o.tt(Q["den"][:], Q["den"][:], Q["qre"][:], ALU.add, S, S)
    o.S.op("dve", lambda e: e.reciprocal(out=Q["den"][:], in_=Q["den"][:]), S, S)
    o.tt(Q["qre"][:], Q["t8a"][:], v[:, 0, :], ALU.mult, S + ["s_v"], S)
    o.tt(Q["qim"][:], Q["t8b"][:], v[:, 1, :], ALU.mult, S + ["s_v", "s_t8"], S)
    o.tt(Q["qre"][:], Q["qre"][:], Q["qim"][:], ALU.add, S, S)
    o.tt(Q["qre"][:], Q["qre"][:], Q["den"][:], ALU.mult, S, S)
    o.tt(Q["qim"][:], Q["t8b"][:], v[:, 0, :], ALU.mult, S + ["s_v", "s_t8"], S)
    o.tt(Q["cre"][:], Q["t8a"][:], v[:, 1, :], ALU.mult, S + ["s_v"], S)
    o.tt(Q["qim"][:], Q["qim"][:], Q["cre"][:], ALU.subtract, S, S)
    o.tt(Q["qim"][:], Q["qim"][:], Q["den"][:], ALU.mult, S, S)
    o.ts(Q["t8a"][:], Q["th"][:], float(NB), None, ALU.mult, None, S, S)
    sin_of(k, Q["Eim"][:], Q["t8a"][:], 0.0, Q["t8b"][:], Q["t8i"][:], 128, S, S, "s_t8")
    sin_of(k, Q["Ere"][:], Q["t8a"][:], math.pi / 2, Q["t8b"][:], Q["t8i"][:], 128, S, S, "s_t8")
    o.dma("sp", wst, dr["s5b"][l].rearrange("a j p c -> p a j c"), "xblk0", w=["xblk"])
    for jt in range(8):
        o.ts(Q["dq"][:, 0, :], k.ident[:], Q["qre"][:, jt:jt + 1], None, ALU.mult, None, ["ident"] + S, ["s_dq"])
        o.ts(Q["dq"][:, 1, :], k.ident[:], Q["qim"][:, jt:jt + 1], None, ALU.mult, None, ["ident"] + S, ["s_dq"])
        o.mm(ps[1][:, 0:128], k.ones32[:], Q["dq"][:, 0, :], True, True, ["ones32", "s_dq"], [P[1]])
        o.mm(ps[1][:, 128:256], k.ones32[:], Q["dq"][:, 1, :], True, True, ["ones32", "s_dq"], [P[1]])
        qrb, qib = ps[1][:, 0:128], ps[1][:, 128:256]
        bre, bim = wst[:, 0, jt, :], wst[:, 1, jt, :]
        w1, w2 = Q["w1"][:, 0:128], Q["w2"][:, 0:128]
        o.tt(w1, qrb, bre, ALU.mult, [P[1], "xblk"], ["s_w1"])
        o.tt(w2, qib, bim, ALU.mult, [P[1], "xblk"], ["s_w2"])
        o.tt(Q["B"][:, 0, jt, :], w1, w2, ALU.subtract, ["s_w1", "s_w2"], ["s_B"])
        o.tt(w1, qrb, bim, ALU.mult, [P[1], "xblk"], ["s_w1"])
        o.tt(w2, qib, bre, ALU.mult, [P[1], "xblk"], ["s_w2"])
        o.tt(Q["B"][:, 1, jt, :], w1, w2, ALU.add, ["s_w1", "s_w2"], ["s_B"])
    o.dma("sp", wst, dr["s5c"][l].rearrange("a j p c -> p a j c"), "xblk0", r=["s_B"], w=["xblk"])
    o.copy(Q["C"][:, 0], wst[:, 0], ["xblk"], ["s_C"])
    o.ts(Q["C"][:, 1], wst[:, 1], -1.0, None, ALU.mult, None, ["xblk"], ["s_C"])
    gst = xblk[:, 0:2, :].rearrange("p a b -> p (a b)")[:, 0:512].rearrange("p (kt n) -> p kt n", n=256)
    o.dma("sp", gst, dr["glu_w"][l].rearrange("(kt p) n -> p kt n", p=128), "xblk0", r=["s_C"], w=["xblk"])
    o.copy(Q["glu"][:], gst, ["xblk"], ["s_glu"])
    T = ["s_tab"]
    for jt in range(8):
        o.ts(Q["w5"][:], Q["tv"][:], Q["th"][:, jt:jt + 1], None, ALU.mult, None, ["s_tv"] + S, ["s_w5"])
        sin_of(k, Q["sinT"][:, jt, :], Q["w5"][:], 0.0, Q["w6"][:], Q["wi"][:], 128, ["s_w5"], T, "s_w6")
        sin_of(k, Q["cosT"][:, jt, :], Q["w5"][:], math.pi / 2, Q["w6"][:], Q["wi"][:], 128, ["s_w5"], T, "s_w6")
    o.memset(Q["cre"][:], 0.0, ["s_carry"], eng="dve")
    o.memset(Q["cim"][:], 0.0, ["s_carry"], eng="dve")
    return Q


def s5_block(k, l, i, Q, s5u, mixT, t0):
    o, ps, P = k.o, k.ps, k.P
    o.copy(Q["ub"][:], s5u[:], ["s5u"], ["s_ub"], eng="pool")
    T = ["s_tab"]
    w1, w2, w3, w4, w5, w6 = (Q[n][:] for n in ("w1", "w2", "w3", "w4", "w5", "w6"))
    for jt in range(8):
        ct = jt // 4
        pb = 1 + (jt % 2)
        o.mm(ps[pb][:, 0:NB], Q["B"][:, 0, jt, :], Q["ub"][:, ct, :], True, True, ["s_B", "s_ub"], [P[pb]])
        o.mm(ps[pb][:, NB:2 * NB], Q["B"][:, 1, jt, :], Q["ub"][:, ct, :], True, True, ["s_B", "s_ub"], [P[pb]])
        bre, bim = ps[pb][:, 0:NB], ps[pb][:, NB:2 * NB]
        cs_, sn_ = Q["cosT"][:, jt, :], Q["sinT"][:, jt, :]
        o.tt(w1, bre, cs_, ALU.mult, [P[pb]] + T, ["s_w1"])
        o.tt(w2, bim, sn_, ALU.mult, [P[pb]] + T, ["s_w2"])
        o.tt(w3, bim, cs_, ALU.mult, [P[pb]] + T, ["s_w3"])
        o.tt(w4, bre, sn_, ALU.mult, [P[pb]] + T, ["s_w4"])
        o.tt(w1, w1, w2, ALU.add, ["s_w1", "s_w2"], ["s_w1"], eng="pool")
        o.tt(w3, w3, w4, ALU.subtract, ["s_w3", "s_w4"], ["s_w3"], eng="pool")
        magb = Q["w7"][:]
        o.ts(magb, Q["tv"][:], 0.0, Q["mag"][:, jt:jt + 1], ALU.mult, ALU.add, ["s_tv", "s_small"], ["s_w7"], eng="pool")
        o.scan(w5, magb, w1, Q["cre"][:, jt:jt + 1], ["s_w7", "s_w1", "s_carry"], ["s_w5"])
        o.scan(w6, magb, w3, Q["cim"][:, jt:jt + 1], ["s_w7", "s_w3", "s_carry"], ["s_w6"])
        o.copy(Q["glr"][:, jt:jt + 1], w5[:, NB - 1:NB], ["s_w5"], ["s_gl"], eng="dve")
        o.copy(Q["gli"][:, jt:jt + 1], w6[:, NB - 1:NB], ["s_w6"], ["s_gl"], eng="dve")
        o.tt(w2, w5, cs_, ALU.mult, ["s_w5"] + T, ["s_w2"], eng="pool")
        o.tt(w4, w6, sn_, ALU.mult, ["s_w6"] + T, ["s_w4"], eng="pool")
        o.tt(Q["hre"][:, jt, :], w2, w4, ALU.subtract, ["s_w2", "s_w4"], [("s_h", jt)], eng="pool")
        o.tt(w2, w5, sn_, ALU.mult, ["s_w5"] + T, ["s_w2"], eng="pool")
        o.tt(w4, w6, cs_, ALU.mult, ["s_w6"] + T, ["s_w4"], eng="pool")
        o.tt(Q["him"][:, jt, :], w2, w4, ALU.add, ["s_w2", "s_w4"], [("s_h", jt)], eng="pool")
    o.tt(Q["t8a"][:], Q["glr"][:], Q["Ere"][:], ALU.mult, ["s_gl", "s_small"], ["s_c1"])
    o.tt(Q["t8b"][:], Q["gli"][:], Q["Eim"][:], ALU.mult, ["s_gl", "s_small"], ["s_c2"])
    o.tt(Q["cre"][:], Q["t8a"][:], Q["t8b"][:], ALU.subtract, ["s_c1", "s_c2"], ["s_carry"])
    o.tt(Q["t8a"][:], Q["glr"][:], Q["Eim"][:], ALU.mult, ["s_gl", "s_small"], ["s_c1"])
    o.tt(Q["t8b"][:], Q["gli"][:], Q["Ere"][:], ALU.mult, ["s_gl", "s_small"], ["s_c2"])
    o.tt(Q["cim"][:], Q["t8a"][:], Q["t8b"][:], ALU.add, ["s_c1", "s_c2"], ["s_carry"])
    vec2 = Q["vec2"]
    for ct in range(2):
        pb = 3
        for j in range(4):
            jt = ct * 4 + j
            o.mm(ps[pb][:, 0:NB], Q["C"][:, 0, jt, :], Q["hre"][:, jt, :], j == 0, False, ["s_C", ("s_h", jt)], [P[pb]])
            o.mm(ps[pb][:, 0:NB], Q["C"][:, 1, jt, :], Q["him"][:, jt, :], False, j == 3, ["s_C", ("s_h", jt)], [P[pb]])
        o.copy(Q["yv"][:, ct, :], ps[pb][:, 0:NB], [P[pb]], ["s_yv"], eng="act")
        o.stt(Q["yv"][:, ct, :], s5u[:, ct, :], vec2[:, 0, ct:ct + 1], Q["yv"][:, ct, :], ALU.mult, ALU.add, ["s5u", "s_vec2", "s_yv"], ["s_yv"])
        gelu_tanh(k, Q["ge"][:, ct, :], Q["yv"][:, ct, :], Q["w1"][:], ["s_yv"], ["s_ge"], "s_w1")
        o.copy(Q["geb"][:, ct, :], Q["ge"][:, ct, :], ["s_ge"], ["s_geb"], eng="pool")
    for ct in range(2):
        pb = 3
        for kt in range(2):
            o.mm(ps[pb][:, 0:NB], Q["glu"][:, kt, ct * 128:(ct + 1) * 128], Q["geb"][:, kt, :], kt == 0, kt == 1, ["s_glu", "s_geb"], [P[pb]])
        o.act(Q["w1"][:], ps[pb][:, 0:NB], AF.Sigmoid, [P[pb], "s_vec2"], ["s_w1"], bias=vec2[:, 1, ct:ct + 1])
        o.tt(Q["yv"][:, ct, :], Q["ge"][:, ct, :], Q["w1"][:], ALU.mult, ["s_ge", "s_w1"], ["s_yv"])
    branch_norm(k, Q["yv"], "s_yv", Q["sqb"], "s_sqb", Q["w2"], "s_w2", vec2[:, 2, :], "s_vec2", Q["yo"], "s_yo", Q["yob"], "s_yob", 2)
    o.dma("pool", mixT[256:512, t0:t0 + NB].rearrange("(t p) s -> p t s", p=128), Q["yob"][:], "mix_b", r=["s_yob"], w=["mixT_b"])


def branch_norm(k, y, yk, sqb, sqk, rstd, rk, g, gk, yo, yok, yob, yobk, nt, rows=128):
    o, ps, P = k.o, k.ps, k.P
    pb = 2
    for t in range(nt):
        o.act(sqb[0:rows, t, :], y[0:rows, t, :], AF.Square, [yk], [sqk])
    for t in range(nt):
        o.mm(ps[pb][0:rows, 0:y.shape[2]], k.onesb[0:rows, 0:rows], sqb[0:rows, t, :], t == 0, t == nt - 1, [sqk, "onesb"], [P[pb]])
    rsqrt(k, rstd[0:rows, :], ps[pb][0:rows, 0:y.shape[2]], 1.0 / (nt * rows), 1e-6, [P[pb]], rk, rows)
    for t in range(nt):
        o.stt(yo[0:rows, t, :], y[0:rows, t, :], g[0:rows, t:t + 1], rstd[0:rows, :], ALU.mult, ALU.mult, [yk, gk, rk], [yok])
    o.copy(yob[0:rows], yo[0:rows], [yok], [yobk], eng="pool")


def lru_setup(k, pa, l, xblk):
    o, dr = k.o, k.dr
    U = {}
    U["v"] = sb(k, pa, "l_v", [128, 9, 2])
    U["c1"] = sb(k, pa, "l_c1", [128, 2])
    U["bd"] = sb(k, pa, "l_bd", [128, 2, 2, 128], BF16)
    for nm in ("xc", "rr", "ii", "aa", "t1", "t2", "hh", "gg"):
        U[nm] = sb(k, pa, "l_" + nm, [128, NB])
    U["xcb"] = sb(k, pa, "l_xcb", [128, NB], BF16)
    U["hc"] = sb(k, pa, "l_hc", [128, 2])
    U["yd"] = sb(k, pa, "l_yd", [128, 2, NB])
    U["sqb"] = sb(k, pa, "l_sqb", [128, 2, NB], BF16)
    U["yo"] = sb(k, pa, "l_yo", [128, 2, NB])
    U["yob"] = sb(k, pa, "l_yob", [128, 2, NB], BF16)
    o.dma("sp", U["v"][:], dr["lruv"][l], "l_v", w=["l_v"])
    bst = xblk[:, 0:2, :].rearrange("p a b -> p (a b)")[:, 0:512].rearrange("p (a t c) -> p a t c", a=2, t=2)
    o.dma("sp", bst, dr["lrubd"][l].rearrange("a t p c -> p a t c"), "xblk0", w=["xblk"])
    o.copy(U["bd"][:], bst, ["xblk"], ["l_bd"])
    o.act(U["c1"][:], U["v"][:, 7, :], AF.Exp, ["l_v"], ["l_c1"], scale=-1.0)
    o.act(U["c1"][:], U["c1"][:], AF.Ln, ["l_c1", "cc"], ["l_c1"], bias=k.cc[:, CC[1.0]:CC[1.0] + 1])
    o.ts(U["c1"][:], U["c1"][:], -8.0, None, ALU.mult, None, ["l_c1"], ["l_c1"])
    o.memset(U["hc"][:], 0.0, ["l_hc"], eng="dve")
    return U


def lru_block(k, l, i, U, lrx, lrg, mixT, t0):
    o, ps, P = k.o, k.ps, k.P
    v = U["v"]
    for t in range(2):
        xc = U["xc"][:]
        o.ts(xc, lrx[:, t, 3:NB + 3], v[:, 3, t:t + 1], v[:, 4, t:t + 1], ALU.mult, ALU.add, ["lrx", "l_v"], ["l_xc"])
        for j in range(3):
            o.stt(xc, lrx[:, t, j:NB + j], v[:, j, t:t + 1], xc, ALU.mult, ALU.add, ["lrx", "l_v", "l_xc"], ["l_xc"])
        o.copy(U["xcb"][:], xc, ["l_xc"], ["l_xcb"], eng="pool")
        o.mm(ps[1][:, 0:NB], U["bd"][:, 0, t, :], U["xcb"][:], True, True, ["l_bd", "l_xcb"], [P[1]])
        o.mm(ps[1][:, NB:2 * NB], U["bd"][:, 1, t, :], U["xcb"][:], True, True, ["l_bd", "l_xcb"], [P[1]])
        o.act(U["rr"][:], ps[1][:, 0:NB], AF.Sigmoid, [P[1], "l_v"], ["l_rr"], bias=v[:, 5, t:t + 1])
        o.act(U["ii"][:], ps[1][:, NB:2 * NB], AF.Sigmoid, [P[1], "l_v"], ["l_ii"], bias=v[:, 6, t:t + 1])
        o.act(U["aa"][:], U["rr"][:], AF.Exp, ["l_rr", "l_c1"], ["l_aa"], scale=U["c1"][:, t:t + 1])
        o.tt(U["t1"][:], U["aa"][:], U["aa"][:], ALU.mult, ["l_aa"], ["l_t1"])
        o.ts(U["t1"][:], U["t1"][:], -1.0, 1.0, ALU.mult, ALU.add, ["l_t1"], ["l_t1"])
        o.ts(U["t1"][:], U["t1"][:], 0.0, None, ALU.max, None, ["l_t1"], ["l_t1"])
        o.act(U["t1"][:], U["t1"][:], AF.Sqrt, ["l_t1"], ["l_t1"])
        o.tt(U["t2"][:], U["ii"][:], xc, ALU.mult, ["l_ii", "l_xc"], ["l_t2"])
        o.tt(U["t2"][:], U["t2"][:], U["t1"][:], ALU.mult, ["l_t2", "l_t1"], ["l_t2"])
        o.scan(U["hh"][:], U["aa"][:], U["t2"][:], U["hc"][:, t:t + 1], ["l_aa", "l_t2", "l_hc"], ["l_hh"])
        o.copy(U["hc"][:, t:t + 1], U["hh"][:, NB - 1:NB], ["l_hh"], ["l_hc"], eng="dve")
        gelu_tanh(k, U["gg"][:], lrg[:, t, :], U["t1"][:], ["lrg"], ["l_gg"], "l_t1")
        o.tt(U["yd"][:, t, :], U["hh"][:], U["gg"][:], ALU.mult, ["l_hh", "l_gg"], ["l_yd"])
    o.copy(lrx[:, :, 0:3], lrx[:, :, NB:NB + 3], ["lrx"], ["lrx"], eng="dve")
    branch_norm(k, U["yd"], "l_yd", U["sqb"], "l_sqb", U["t2"], "l_t2", v[:, 8, :], "l_v", U["yo"], "l_yo", U["yob"], "l_yob", 2)
    o.dma("pool", mixT[768:1024, t0:t0 + NB].rearrange("(t p) s -> p t s", p=128), U["yob"][:], "mix_d", r=["l_yob"], w=["mixT_d"])


QSCALE = 96 ** -0.5


def mla_setup(k, pa, l, xblk, Q, U):
    o, dr = k.o, k.dr
    A = {}
    xflat = xblk[:].rearrange("p a b -> p (a b)")
    A["wuq"] = sb(k, pa, "a_wuq", [128, 2, 384], BF16)
    A["wukn"] = sb(k, pa, "a_wukn", [128, 4, 96], BF16)
    A["wukv"] = sb(k, pa, "a_wukv", [128, 256], BF16)
    A["qng"] = sb(k, pa, "a_qng", [128, 2])
    A["kvng"] = sb(k, pa, "a_kvng", [128, 1])
    A["qkhg"] = sb(k, pa, "a_qkhg", [96, 2])
    A["invf"] = sb(k, pa, "a_invf", [96, 1])
    A["rotT"] = sb(k, pa, "a_rotT", [128, 128])
    A["epe"] = sb(k, pa, "a_epe", [32, 96], BF16)
    A["posi"] = sb(k, pa, "a_posi", [96, 1], I32)
    A["posf"] = sb(k, pa, "a_posf", [96, 1])
    A["tv"] = Q["tv"]
    for nm in ("rk96", "COS", "SIN"):
        A[nm] = sb(k, pa, "a_" + nm, [96, NB])
    A["rq"] = U["t2"]
    A["ang"], A["tmpS"], A["angi"] = Q["w5"][0:96, :], Q["w6"][0:96, :], Q["wi"][0:96, :]
    A["qf"], A["rsh"], A["qn"], A["t1"], A["t2"] = (U[n][0:96, :] for n in ("xc", "rr", "ii", "aa", "t1"))
    A["qn128"] = U["ii"]
    A["sq"] = sb(k, pa, "a_sq", [128, 2, NB], BF16)
    A["sqk"] = sb(k, pa, "a_sqk", [128, NB], BF16)
    A["sqh"] = sb(k, pa, "a_sqh", [96, NB], BF16)
    A["rkt"] = sb(k, pa, "a_rkt", [128, NB // 128])
    A["qrb"] = sb(k, pa, "a_qrb", [96, 4, NB], BF16)
    A["krb"] = sb(k, pa, "a_krb", [96, 4, NB], BF16)
    A["Vt"] = sb(k, pa, "a_Vt", [128, NB // 128, 4, 65], BF16)
    for nm, src in (("qng", "qng"), ("kvng", "kvng"), ("qkhg", "qkhg")):
        o.dma("sp", A[nm][:], dr[src][l], "a_" + nm, w=["a_small"])
    o.dma("sp", A["invf"][:], dr["invf"], "a_invf", w=["a_small"])
    o.dma("sp", A["rotT"][:], dr["rotT"], "a_rotT", w=["a_small"])
    o.dma("sp", A["posi"][:], dr["pos"], "a_posi", w=["a_posi"])
    o.copy(A["posf"][:], A["posi"][:], ["a_posi"], ["a_small"])
    ste = xflat[0:32, 0:96]
    o.dma("sp", ste, dr["epe"], "xblk0", w=["xblk"])
    o.copy(A["epe"][:], ste, ["xblk"], ["a_w"])
    stq = xflat[:, 0:768].rearrange("p (kt n) -> p kt n", n=384)
    o.dma("sp", stq, dr["w_uq"][l].rearrange("(kt p) n -> p kt n", p=128), "xblk0", r=["a_w"], w=["xblk"])
    for kt in range(2):
        o.ts(A["wuq"][:, kt, :], stq[:, kt, :], A["qng"][:, kt:kt + 1], None, ALU.mult, None, ["xblk", "a_small"], ["a_w"])
    stk = xflat[:, 0:384]
    o.dma("sp", stk, dr["w_ukn"][l].rearrange("p h c -> p (h c)"), "xblk0", r=["a_w"], w=["xblk"])
    o.ts(A["wukn"][:].rearrange("p h c -> p (h c)"), stk, A["kvng"][:, 0:1], None, ALU.mult, None, ["xblk", "a_small"], ["a_w"])
    stv = xflat[:, 0:256]
    o.dma("sp", stv, dr["w_ukv"][l], "xblk0", r=["a_w"], w=["xblk"])
    o.ts(A["wukv"][:], stv, A["kvng"][:, 0:1], None, ALU.mult, None, ["xblk", "a_small"], ["a_w"])
    o.memset(A["rk96"][:], 1.0, ["a_rk96"], eng="dve")
    o.memset(A["Vt"][:], 1.0, ["a_Vt"], eng="dve")
    return A


def head_norm_rope(k, A, src, gcol, out):
    o, ps, P = k.o, k.ps, k.P
    o.act(A["sqh"][:], src, AF.Square, ["l_xc"], ["a_sqh"])
    o.mm(ps[4][0:96, 0:NB], k.onesb[0:96, 0:96], A["sqh"][:], True, True, ["onesb", "a_sqh"], [P[4]])
    rsqrt(k, A["rsh"][:], ps[4][0:96, 0:NB], 1.0 / 96, 1e-6, [P[4]], "l_rr", 96)
    o.stt(A["qn"][:], src, A["qkhg"][:, gcol:gcol + 1], A["rsh"][:], ALU.mult, ALU.mult, ["l_xc", "a_small", "l_rr"], ["l_ii"])
    o.mm(ps[4][:, NB:2 * NB], A["rotT"][:], A["qn128"][:], True, True, ["a_small", "l_ii"], [P[4]])
    o.tt(A["t1"][:], A["qn"][:], A["COS"][:], ALU.mult, ["l_ii", "a_cs"], ["l_aa"])
    o.tt(A["t2"][:], ps[4][0:96, NB:2 * NB], A["SIN"][:], ALU.mult, [P[4], "a_cs"], ["l_t1"])
    o.tt(out, A["t1"][:], A["t2"][:], ALU.add, ["l_aa", "l_t1"], ["a_out"])


def mla_block(k, l, i, A, qab, kvab, kpeb, t0):
    o, ps, P = k.o, k.ps, k.P
    ntt = NB // 128
    rms_stats(k, [(qab[:, kt, :], "qab") for kt in range(2)], 256.0, 1e-6, ps[1][:, 0:NB], A["rq"][:],
              [(A["sq"][:, kt, :], "a_sq") for kt in range(2)], None, P[1], "l_t2")
    o.act(A["sqk"][:], kvab[:], AF.Square, ["kvab"], ["a_sqk"])
    o.mm(ps[2][:, 0:NB], k.onesb[:], A["sqk"][:], True, True, ["onesb", "a_sqk"], [P[2]])
    for tt in range(ntt):
        o.mm(ps[2][:, NB + tt:NB + tt + 1], A["sqk"][:, tt * 128:(tt + 1) * 128], k.onesb[:, 0:1], True, True, ["onesb", "a_sqk"], [P[2]])
    rsqrt(k, A["rk96"][0:64, :], ps[2][0:64, 0:NB], 1.0 / 128, 1e-6, [P[2]], "a_rk96", 64)
    rsqrt(k, A["rkt"][:], ps[2][:, NB:NB + ntt], 1.0 / 128, 1e-6, [P[2]], "a_rkt", 128)
    o.ts(A["ang"][:], A["tv"][0:96, :], A["posf"][:, 0:1], None, ALU.add, None, ["s_tv", "a_small"], ["s_w5"])
    o.ts(A["ang"][:], A["ang"][:], float(t0), A["invf"][:, 0:1], ALU.add, ALU.mult, ["s_w5", "a_small"], ["s_w5"])
    sin_of(k, A["SIN"][:], A["ang"][:], 0.0, A["tmpS"][:], A["angi"][:], 96, ["s_w5"], ["a_cs"], "s_w6")
    sin_of(k, A["COS"][:], A["ang"][:], math.pi / 2, A["tmpS"][:], A["angi"][:], 96, ["s_w5"], ["a_cs"], "s_w6")
    for h in range(4):
        for kt in range(2):
            o.mm(ps[3][0:96, 0:NB], A["wuq"][:, kt, h * 96:(h + 1) * 96], qab[:, kt, :], kt == 0, kt == 1, ["a_w", "qab"], [P[3]])
        o.tt(A["qf"][:], ps[3][0:96, 0:NB], A["rq"][0:96, :], ALU.mult, [P[3], "l_t2"], ["l_xc"])
        head_norm_rope(k, A, A["qf"][:], 0, A["qrb"][:, h, :])
        o.mm(ps[3][0:96, NB:2 * NB], A["wukn"][:, h, :], kvab[:], True, False, ["a_w", "kvab"], [P[3]])
        o.mm(ps[3][0:96, NB:2 * NB], A["epe"][:], kpeb[:], False, True, ["a_w", "kpeb"], [P[3]])
        o.tt(A["qf"][:], ps[3][0:96, NB:2 * NB], A["rk96"][:], ALU.mult, [P[3], "a_rk96"], ["l_xc"])
        head_norm_rope(k, A, A["qf"][:], 1, A["krb"][:, h, :])
    for tt in range(ntt):
        o.mm(ps[1][:, 0:256], kvab[:, tt * 128:(tt + 1) * 128], A["wukv"][:], True, True, ["kvab", "a_w"], [P[1]])
        o.act(A["Vt"][:, tt, :, 0:64], ps[1][:, 0:256].rearrange("p (h c) -> p h c", c=64), AF.Identity, [P[1], "a_rkt"], ["a_Vt"],
              scale=A["rkt"][:, tt:tt + 1])
    o.dma("pool", k.qs[:, :, t0:t0 + NB], A["qrb"][:], "st_q", r=["a_out"], w=["qs"])
    o.dma("pool", k.ks[:, :, t0:t0 + NB], A["krb"][:], "st_k", r=["a_out"], w=["ks"])
    o.dma("pool", k.vs[t0 // 128:t0 // 128 + ntt].rearrange("t p c -> p t c"), A["Vt"][:].rearrange("p t h c -> p t (h c)"), "st_v", r=["a_Vt"], w=["vs"])


QB = 256


def phaseB(k, l, xin, x1T, stop):
    nc, S_, dr, S, L, o = k.nc, k.S_, k.dr, k.S, k.L, k.o
    ps, P = k.ps, k.P
    nqb = S // QB
    with ExitStack() as pb_:
        Kall = sb(k, pb_, "Kall", [96, 4, S], BF16)
        Vall = sb(k, pb_, "Vall", [128, S // 128, 260], BF16)
        qblk = sb(k, pb_, "qblk", [96, 4, QB], BF16)
        PT = [sb(k, pb_, f"PT{i}", [128, QB], BF16) for i in range(2)]
        Oext = sb(k, pb_, "Oext", [128, QB])
        rden = sb(k, pb_, "rden", [64, QB])
        yc = sb(k, pb_, "yc", [64, 4, QB])
        ycsq = sb(k, pb_, "ycsq", [64, 4, QB], BF16)
        ycn = sb(k, pb_, "ycn", [64, 4, QB])
        ycnb = sb(k, pb_, "ycnb", [64, 4, QB], BF16)
        rstc = sb(k, pb_, "rstc", [64, QB])
        mixb = sb(k, pb_, "mixb", [128, 6, QB], BF16)
        wo = sb(k, pb_, "wo", [128, 6, D], BF16)
        woc = sb(k, pb_, "woc", [64, 4, D], BF16)
        xblk = sb(k, pb_, "xblkB", [128, 8, QB])
        sq2 = sb(k, pb_, "sq2", [128, 8, QB], BF16)
        rst2 = sb(k, pb_, "rst2", [128, QB])
        xt2 = sb(k, pb_, "xt2", [128, QB])
        h2 = sb(k, pb_, "h2", [128, 8, QB])
        h2b = sb(k, pb_, "h2b", [128, 8, QB], BF16)
        wr = sb(k, pb_, "wr", [128, 8, 36])
        brt = sb(k, pb_, "brt", [128, 36])
        sel65 = sb(k, pb_, "sel65", [128, 64])
        bngc = sb(k, pb_, "bngc", [64, 4])
        lg = sb(k, pb_, "lg", [128, 36])
        rt = {nm: sb(k, pb_, "rt_" + nm, [128, w_]) for nm, w_ in (("m4", 1), ("nm4", 1), ("e4", 4), ("s4", 1), ("gp", 1), ("ohg", 4), ("sel", 8),
                                                                  ("l1", 1), ("oh1", 8), ("sel2", 8), ("l2", 1), ("oh2", 8), ("nl1", 1), ("d", 1),
                                                                  ("t", 1), ("w1", 1), ("w2", 1), ("ge", 8))}
        gates = sb(k, pb_, "gates", [128, 32])
        gT = sb(k, pb_, "gTb", [32, QB])
        wst = h2[:].rearrange("p a b -> p (a b)")
        wov = dr["w_out"][l]
        rows = [0, 128, 256, 384, 768, 896]
        for t, r0 in enumerate(rows):
            for hf in range(1):
                o.dma("sp", wst[:, 0:1024], wov[r0:r0 + 128, :], "h2st", w=["h2"])
                o.copy(wo[:, t, :], wst[:, 0:1024], ["h2"], ["wo"], eng=("dve" if t % 2 == 0 else "pool"))
        for h in range(4):
            o.dma("sp", wst[0:64, 0:1024], wov[512 + h * 64:512 + (h + 1) * 64, :], "h2st", w=["h2"])
            o.copy(woc[:, h, :], wst[0:64, 0:1024], ["h2"], ["wo"], eng="dve")
        o.dma("sp", wr[:], dr["wr"][l].rearrange("(kk p) n -> p kk n", p=128), "wr", w=["wr"])
        o.dma("sp", brt[:], dr["br"][l], "brt", w=["wr"])
        o.dma("sp", sel65[:], dr["sel65"], "sel65", w=["sel65"])
        o.memset(Oext[:], 0.0, ["Oext"], eng="dve")
        o.dma("sp", bngc[:], dr["bng_c"][l], "bngc", w=["bngc"])
        xv = xin.rearrange("(kk p) s -> p kk s", p=128)
        x1v = x1T.rearrange("(kk p) s -> p kk s", p=128)
        h2v = k.h2T.rearrange("(kk p) s -> p kk s", p=128)
        for qb in range(nqb):
            t0 = qb * QB
            nkt = QB // 128
            o.dma("sp", Kall[:, :, t0:t0 + QB], k.ks[:, :, t0:t0 + QB], "ldK", r=["ks"], w=["Kall"])
            o.dma("sp", Vall[:, t0 // 128:t0 // 128 + nkt, :], k.vs[t0 // 128:t0 // 128 + nkt].rearrange("t p c -> p t c"), "ldV", r=["vs"], w=["Vall"])
            o.dma("sp", qblk[:], k.qs[:, :, t0:t0 + QB], "ldQ", r=["qs"], w=["qblk"])
            o.dma("pool", mixb[:, 0:2, :], k.mixT[0:256, t0:t0 + QB].rearrange("(t p) s -> p t s", p=128), "ldm", r=["mixT_a"], w=["mixb"])
            o.dma("pool", mixb[:, 2:4, :], k.mixT[256:512, t0:t0 + QB].rearrange("(t p) s -> p t s", p=128), "ldm", r=["mixT_b"], w=["mixb"])
            o.dma("pool", mixb[:, 4:6, :], k.mixT[768:1024, t0:t0 + QB].rearrange("(t p) s -> p t s", p=128), "ldm", r=["mixT_d"], w=["mixb"])
            o.dma("sp", xblk[:], xv[:, :, t0:t0 + QB], "xblkB", w=["xblkB"])
            nk_tot = (t0 + QB) // 128
            it = 0
            for h in range(4):
                for kt in range(nk_tot):
                    a = kt - (t0 // 128)
                    c0 = max(a, 0) * 128
                    sb_ = 1 + (it % 2)
                    pt = PT[it % 2]
                    ptk = f"PT{it % 2}"
                    it += 1
                    o.mm(ps[sb_][:, c0:QB], Kall[:, h, kt * 128:(kt + 1) * 128], qblk[:, h, c0:QB], True, True, ["Kall", "qblk"], [P[sb_]])
                    o.act(pt[:, c0:QB], ps[sb_][:, c0:QB], AF.Exp, [P[sb_]], [ptk], scale=QSCALE)
                    if a >= 0:
                        o.memset(pt[64:128, c0:c0 + 64], 0.0, [ptk], eng="pool")
                    o.mm(ps[3][0:65, c0:QB], Vall[:, kt, h * 65:(h + 1) * 65], pt[:, c0:QB], kt == 0, kt == nk_tot - 1, ["Vall", ptk], [P[3]])
                o.copy(Oext[0:65, :], ps[3][0:65, 0:QB], [P[3]], ["Oext"], eng="dve")
                o.mm(ps[4][0:64, 0:QB], sel65[:], Oext[:], True, True, ["sel65", "Oext"], [P[4]])
                o.S.op("dve", lambda e: e.reciprocal(out=rden[:], in_=ps[4][0:64, 0:QB]), [P[4]], ["rden"])
                o.tt(yc[:, h, :], Oext[0:64, :], rden[:], ALU.mult, ["Oext", "rden"], ["yc"])
            branch_norm(k, yc, "yc", ycsq, "ycsq", rstc, "rstc", bngc, "bngc", ycn, "ycn", ycnb, "ycnb", 4, rows=64)
            if stop == "B1":
                dbg = dr["dbg"]
                o.dma("sp", dbg[512:768, t0:t0 + QB].rearrange("(h p) s -> p h s", p=64), ycn[:], "dbg", r=["ycn"], w=["dbg"])
            for f in range(8):
                pb = 5 + (f % 2)
                fc = slice(f * 128, (f + 1) * 128)
                for t in range(6):
                    o.mm(ps[pb][:, 0:QB], wo[:, t, fc], mixb[:, t, :], t == 0, False, ["wo", "mixb"], [P[pb]])
                for h in range(4):
                    o.mm(ps[pb][:, 0:QB], woc[:, h, fc], ycnb[:, h, :], False, h == 3, ["wo", "ycnb"], [P[pb]])
                o.act(xt2[:], ps[pb][:, 0:QB], AF.Identity, [P[pb], "mods"], ["xt2"], scale=k.mods[:, l, 16 + f:17 + f])
                o.tt(xblk[:, f, :], xt2[:], xblk[:, f, :], ALU.add, ["xt2", "xblkB"], ["xblkB"])
            o.dma("pool", x1v[:, :, t0:t0 + QB], xblk[:], "stx1", r=["xblkB"], w=["x1T"])
            if stop == "B1":
                dbg = dr["dbg"]
                o.dma("sp", dbg[1024:2048, t0:t0 + QB].rearrange("(t p) s -> p t s", p=128), xblk[:], "dbg", r=["xblkB"], w=["dbg"])
            rms_stats(k, [(xblk[:, kk, :], "xblkB") for kk in range(8)], 1024.0, 1e-6, ps[7][:, 0:QB], rst2[:],
                      [(sq2[:, kk, :], "sq2") for kk in range(8)], None, P[7], "rst2")
            for kk in range(8):
                o.tt(xt2[:], xblk[:, kk, :], rst2[:], ALU.mult, ["xblkB", "rst2"], ["xt2"])
                o.act(h2[:, kk, :], xt2[:], AF.Identity, ["xt2", "g2s", "mods"], ["h2"], bias=k.mods[:, l, 24 + kk:25 + kk], scale=k.g2s[:, l, kk:kk + 1])
            o.copy(h2b[:], h2[:], ["h2"], ["h2b"], eng="pool")
            o.dma("pool", h2v[:, :, t0:t0 + QB], h2b[:], "sth2", r=["h2b"], w=["h2T"])
            for tq in range(QB // 128):
                tsl = slice(tq * 128, (tq + 1) * 128)
                for kk in range(8):
                    o.mm(ps[7][:, 256:292], h2[:, kk, tsl], wr[:, kk, :], kk == 0, kk == 7, ["h2", "wr"], [P[7]])
                o.tt(lg[:], ps[7][:, 256:292], brt[:], ALU.add, [P[7], "wr"], ["lg"])
                route(k, lg, rt, gates)
                o.tr(ps[7][0:32, 384:512], gates[:], k.ident[:], ["gates", "ident"], [P[7]])
                o.copy(gT[:, tsl], ps[7][0:32, 384:512], [P[7]], ["gTb"], eng="dve")
            o.dma("pool", k.gT[:, t0:t0 + QB], gT[:], "stg", r=["gTb"], w=["gT"])
            if stop == "B1":
                o.dma("sp", dr["dbg"][0:32, t0:t0 + QB], gT[:], "dbg", r=["gTb"], w=["dbg"])
        S_.barrier()
        S_.flush(k.st)


def route(k, lg, rt, gates):
    o = k.o
    R_ = ["rt"]
    red = lambda out, in_, op: o.S.op("dve", lambda e: e.tensor_reduce(out=out, in_=in_, axis=AX.X, op=op), ["lg"] + R_, R_)
    red(rt["m4"][:], lg[:, 0:4], ALU.max)
    o.ts(rt["nm4"][:], rt["m4"][:], -1.0, None, ALU.mult, None, R_, R_)
    o.act(rt["e4"][:], lg[:, 0:4], AF.Exp, ["lg"] + R_, R_, bias=rt["nm4"][:, 0:1])
    red(rt["s4"][:], rt["e4"][:], ALU.add)
    o.S.op("dve", lambda e: e.reciprocal(out=rt["gp"][:], in_=rt["s4"][:]), R_, R_)
    o.ts(rt["ohg"][:], lg[:, 0:4], rt["m4"][:, 0:1], None, ALU.is_equal, None, ["lg"] + R_, R_)
    for g in range(4):
        le = lg[:, 4 + 8 * g:12 + 8 * g]
        if g == 0:
            o.ts(rt["sel"][:], le, rt["ohg"][:, 0:1], None, ALU.mult, None, ["lg"] + R_, R_)
        else:
            o.stt(rt["sel"][:], le, rt["ohg"][:, g:g + 1], rt["sel"][:], ALU.mult, ALU.add, ["lg"] + R_, R_)
    red(rt["l1"][:], rt["sel"][:], ALU.max)
    o.ts(rt["oh1"][:], rt["sel"][:], rt["l1"][:, 0:1], None, ALU.is_equal, None, R_, R_)
    o.stt(rt["sel2"][:], rt["oh1"][:], -1e30, rt["sel"][:], ALU.mult, ALU.add, R_, R_)
    red(rt["l2"][:], rt["sel2"][:], ALU.max)
    o.ts(rt["oh2"][:], rt["sel2"][:], rt["l2"][:, 0:1], None, ALU.is_equal, None, R_, R_)
    o.ts(rt["nl1"][:], rt["l1"][:], -1.0, None, ALU.mult, None, R_, R_)
    o.act(rt["d"][:], rt["l2"][:], AF.Exp, R_, R_, bias=rt["nl1"][:, 0:1])
    o.ts(rt["t"][:], rt["d"][:], 1.0, None, ALU.add, None, R_, R_)
    o.S.op("dve", lambda e: e.reciprocal(out=rt["t"][:], in_=rt["t"][:]), R_, R_)
    o.tt(rt["w1"][:], rt["gp"][:], rt["t"][:], ALU.mult, R_, R_)
    o.tt(rt["w2"][:], rt["w1"][:], rt["d"][:], ALU.mult, R_, R_)
    o.ts(rt["ge"][:], rt["oh1"][:], rt["w1"][:, 0:1], None, ALU.mult, None, R_, R_)
    o.stt(rt["ge"][:], rt["oh2"][:], rt["w2"][:, 0:1], rt["ge"][:], ALU.mult, ALU.add, R_, R_)
    for g in range(4):
        o.ts(gates[:, 8 * g:8 * g + 8], rt["ge"][:], rt["ohg"][:, g:g + 1], None, ALU.mult, None, R_, ["gates"])


def phaseW(k, l):
    nc, S_, dr, o = k.nc, k.S_, k.dr, k.o
    NE = dr["w1"].shape[1]
    with ExitStack() as pw:
        stg = [sb(k, pw, f"wstg{i}", [128, 4096]) for i in range(2)]
        wb = [sb(k, pw, f"wbf{i}", [128, 4096], BF16) for i in range(2)]
        it = 0
        for e in range(NE):
            for nm, dst, kk in (("w1", k.w1b, 8), ("w3", k.w3b, 8), ("w2", k.w2b, 4)):
                i2 = it % 2
                n = 4096 // kk
                src = dr[nm][l, e].rearrange("(kk p) n -> p kk n", p=128)
                o.dma("sp", stg[i2][:].rearrange("p (kk n) -> p kk n", kk=kk), src, f"wstg{i2}", w=[f"wstg{i2}"])
                o.copy(wb[i2][:], stg[i2][:], [f"wstg{i2}"], [f"wbf{i2}"], eng=("dve", "pool", "act")[it % 3])
                o.dma("pool", dst[e], wb[i2][:], f"wbst{i2}", r=[f"wbf{i2}"], w=["wscr"])
                it += 1
        S_.barrier()
        S_.flush(k.st)


def phaseC(k, l, x1T, xoutT, stop):
    nc, S_, dr, S, o = k.nc, k.S_, k.dr, k.S, k.o
    ps, P = k.ps, k.P
    NE = dr["w1"].shape[1]
    TB = min(1024, S)
    nh = TB // 512
    with ExitStack() as pc:
        h2b = sb(k, pc, "c_h2b", [128, 8, TB], BF16)
        acc = sb(k, pc, "c_acc", [128, 8, TB])
        gbc = [sb(k, pc, f"c_gbc{i}", [128, TB]) for i in range(2)]
        W1 = [sb(k, pc, f"c_w1_{i}", [128, 8, 512], BF16) for i in range(2)]
        W3 = [sb(k, pc, f"c_w3_{i}", [128, 8, 512], BF16) for i in range(2)]
        W2 = [sb(k, pc, f"c_w2_{i}", [128, 4, 1024], BF16) for i in range(2)]
        hid = [sb(k, pc, f"c_hid{i}", [128, 4, 512], BF16) for i in range(2)]
        sil = [sb(k, pc, f"c_sil{i}", [128, 512]) for i in range(2)]
        t3 = [sb(k, pc, f"c_t3{i}", [128, 512]) for i in range(2)]
        xr = [sb(k, pc, f"c_xr{i}", [128, TB]) for i in range(2)]
        sel32 = sb(k, pc, "c_sel32", [128, 32, 128])
        gt128 = sb(k, pc, "c_gt128", [128, TB])
        o.dma("sp", sel32[:], dr["sel32"], "c_sel32", w=["sel32"])
        o.memset(gt128[:], 0.0, ["gt128"], eng="dve")
        h2v = k.h2T.rearrange("(kk p) s -> p kk s", p=128)
        x1v = x1T.rearrange("(kk p) s -> p kk s", p=128)
        xov = xoutT.rearrange("(kk p) s -> p kk s", p=128)
        cnt = 0
        for tb in range(S // TB):
            t0 = tb * TB
            o.dma("sp", h2b[:], h2v[:, :, t0:t0 + TB], "c_h2b", r=["h2T"], w=["c_h2b"])
            o.dma("sp", gt128[0:32, :], k.gT[:, t0:t0 + TB], "c_gt128", r=["gT"], w=["gt128"])
            for e in range(NE):
                i2 = e % 2
                wk = f"c_w{i2}"
                o.dma("sp", W1[i2][:].rearrange("p a b -> p (a b)"), k.w1b[e], f"c_w1_{i2}", r=["wscr"], w=[wk + "a"])
                o.dma("sp", W3[i2][:].rearrange("p a b -> p (a b)"), k.w3b[e], f"c_w3_{i2}", r=["wscr"], w=[wk + "b"])
                o.dma("sp", W2[i2][:].rearrange("p a b -> p (a b)"), k.w2b[e], f"c_w2_{i2}", r=["wscr"], w=[wk + "c"])
                for hf in range(nh):
                    o.mm(ps[4 + hf][:, :], sel32[:, e, :], gt128[:, hf * 512:(hf + 1) * 512], True, True, ["sel32", "gt128"], [P[4 + hf]])
                    o.copy(gbc[i2][:, hf * 512:(hf + 1) * 512], ps[4 + hf][:, :], [P[4 + hf]], [f"c_gbc{i2}"], eng="act")
                for hf in range(nh):
                    hs = slice(hf * 512, (hf + 1) * 512)
                    j2 = cnt % 2
                    cnt += 1
                    for ht in range(4):
                        pa_, pb_ = ht % 2, 2 + ht % 2
                        hc = slice(ht * 128, (ht + 1) * 128)
                        for kk in range(8):
                            o.mm(ps[pa_][:, :], W1[i2][:, kk, hc], h2b[:, kk, hs], kk == 0, kk == 7, [wk + "a", "c_h2b"], [P[pa_]])
                        for kk in range(8):
                            o.mm(ps[pb_][:, :], W3[i2][:, kk, hc], h2b[:, kk, hs], kk == 0, kk == 7, [wk + "b", "c_h2b"], [P[pb_]])
                        s2 = ht % 2
                        o.act(sil[s2][:], ps[pa_][:, :], AF.Silu, [P[pa_]], [f"c_sil{s2}"])
                        o.tt(t3[s2][:], ps[pb_][:, :], gbc[i2][:, hs], ALU.mult, [P[pb_], f"c_gbc{i2}"], [f"c_t3{s2}"])
                        o.tt(hid[j2][:, ht, :], sil[s2][:], t3[s2][:], ALU.mult, [f"c_sil{s2}", f"c_t3{s2}"], [f"c_hid{j2}"],
                             eng=("pool" if ht % 2 == 0 else "dve"))
                    for f in range(8):
                        po = 4 + f % 4
                        fc = slice(f * 128, (f + 1) * 128)
                        for ht in range(4):
                            o.mm(ps[po][:, :], W2[i2][:, ht, fc], hid[j2][:, ht, :], ht == 0, ht == 3, [wk + "c", f"c_hid{j2}"], [P[po]])
                        if e == 0:
                            o.copy(acc[:, f, hs], ps[po][:, :], [P[po]], ["c_acc"], eng="act")
                        else:
                            o.tt(acc[:, f, hs], ps[po][:, :], acc[:, f, hs], ALU.add, [P[po], "c_acc"], ["c_acc"])
            for f in range(8):
                i2 = f % 2
                o.dma("sp", xr[i2][:], x1v[:, f, t0:t0 + TB], f"c_xr{i2}", r=["x1T"], w=[f"c_xr{i2}"])
                o.stt(xr[i2][:], acc[:, f, :], k.mods[:, l, 40 + f:41 + f], xr[i2][:], ALU.mult, ALU.add, ["c_acc", "mods", f"c_xr{i2}"], [f"c_xr{i2}"])
                o.dma("pool", xov[:, f, t0:t0 + TB], xr[i2][:], f"c_xo{i2}", r=[f"c_xr{i2}"], w=["xout%d" % l])
                if stop == "C1":
                    o.dma("sp", dr["dbg"][f * 128:(f + 1) * 128, t0:t0 + TB], xr[i2][:], "dbg", r=[f"c_xr{i2}"], w=["dbg"])
        S_.barrier()
        S_.flush(k.st)


def make_shapes(sh, cst, pc):
    shapes = {}
    for d_ in (sh, cst, pc):
        for kname, v in d_.items():
            shapes[kname] = (v.shape, I32 if v.dtype == np.int32 else F32)
    return shapes


def run(inputs, S, L, stop=None, dbg=None):
    sh, cst, per_core = prep_inputs(inputs, S)
    sh = {kk: (v[:L] if v.shape[0] == inputs["ada_w"].shape[0] and kk not in () else v) for kk, v in sh.items()}
    shapes = make_shapes(sh, cst, per_core[0])
    nc = build(S, L, shapes, stop=stop, dbg=dbg)
    in_maps = []
    for b in range(NCORES):
        m = dict(sh)
        m.update(cst)
        m.update(per_core[b])
        in_maps.append(m)
    res = run_bass_kernel_spmd(nc, in_maps, core_ids=list(range(NCORES)))
    return res


def kernel(**inputs):
    S = inputs["x"].shape[1]
    L = inputs["ada_w"].shape[0]
    res = run(inputs, S, L)
    out = np.stack([np.ascontiguousarray(res.results[b]["outT"].T) for b in range(NCORES)], axis=0)
    return out.astype(np.float32)
```

```python
import math
import numpy as np
from contextlib import ExitStack
import concourse.bass as bass
import concourse.mybir as mybir
from concourse.bass_utils import run_bass_kernel_spmd

F32 = mybir.dt.float32
BF16 = mybir.dt.bfloat16
I32 = mybir.dt.int32
AF = mybir.ActivationFunctionType
ALU = mybir.AluOpType
AX = mybir.AxisListType

D = 1024
DIN = 2080
NCORES = 8
ENGS = ["pe", "act", "dve", "pool", "sp"]


class Sched:
    def __init__(self, nc):
        self.nc = nc
        self.ops = {e: [] for e in ENGS}
        self.count = {e: 0 for e in ENGS}
        self.seen = {e: {} for e in ENGS}
        self.last_write = {}
        self.readers = {}
        self.dma_count = {}
        self.sem_names = list(ENGS)
        self.sems = {}
        self.rr = 0

    def _deps(self, eng, reads, writes):
        deps = {}

        def add(tok):
            if tok is not None and deps.get(tok[0], 0) < tok[1]:
                deps[tok[0]] = tok[1]

        for r in reads:
            add(self.last_write.get(r))
        for w in writes:
            add(self.last_write.get(w))
            for t in self.readers.get(w, ()):
                add(t)
        out = []
        for k, v in deps.items():
            if eng == "pe" and k == "pe":
                continue
            if self.seen[eng].get(k, 0) < v:
                self.seen[eng][k] = v
                out.append((k, v))
        return out

    def _commit(self, tok, reads, writes):
        for r in reads:
            lst = self.readers.setdefault(r, [])
            lst[:] = [t for t in lst if t[0] != tok[0]]
            lst.append(tok)
        for w in writes:
            self.last_write[w] = tok
            self.readers[w] = []

    def op(self, eng, fn, reads=(), writes=()):
        waits = self._deps(eng, reads, writes)
        self.count[eng] += 1
        tok = (eng, self.count[eng])
        self.ops[eng].append((waits, fn, (eng, 1)))
        self._commit(tok, reads, writes)
        return tok

    def dma(self, eng, fn, key, reads=(), writes=()):
        waits = self._deps(eng, reads, writes)
        k = ("dma", key)
        if k not in self.dma_count:
            self.dma_count[k] = 0
            self.sem_names.append(k)
        self.dma_count[k] += 16
        tok = (k, self.dma_count[k])
        self.ops[eng].append((waits, fn, (k, 16)))
        self._commit(tok, reads, writes)
        return tok

    def barrier(self):
        toks = [(e, self.count[e]) for e in ENGS if self.count[e] > 0]
        toks += [(k, v) for k, v in self.dma_count.items()]
        for e in ENGS:
            waits = []
            for k, v in toks:
                if k != e and self.seen[e].get(k, 0) < v:
                    self.seen[e][k] = v
                    waits.append((k, v))
            if waits:
                self.ops[e].append((waits, None, None))

    def flush(self, stack):
        nc = self.nc
        for k in self.sem_names:
            if k not in self.sems:
                nm = "s_" + "_".join(str(x) for x in (k if isinstance(k, tuple) else (k,)))
                self.sems[k] = stack.enter_context(nc.semaphore(nm))
        sems = self.sems
        ops = self.ops
        self.ops = {e: [] for e in ENGS}
        with nc.Block() as block:
            def replay(engname):
                def body(e):
                    for waits, fn, inc in ops[engname]:
                        for k, v in waits:
                            e.wait_ge(sems[k], v)
                        if fn is not None:
                            fn(e).then_inc(sems[inc[0]], inc[1])
                return body

            block.tensor(replay("pe"))
            block.scalar(replay("act"))
            block.vector(replay("dve"))
            block.gpsimd(replay("pool"))
            block.sync(replay("sp"))


def _pk(v, p=128):
    v = np.asarray(v)
    n = v.shape[-1] // p
    return np.ascontiguousarray(np.swapaxes(v.reshape(v.shape[:-1] + (n, p)), -1, -2))


def prep_inputs(inp, S):
    L = inp["ada_w"].shape[0]
    f32 = np.float32
    sh = {}
    sh["ada_w"] = np.ascontiguousarray(inp["ada_w"], f32)
    sh["ada_b"] = _pk(inp["ada_b"])
    sh["n1g"] = _pk(inp["norm1_g"])
    sh["n2g"] = _pk(inp["norm2_g"])
    sh["w_in"] = np.ascontiguousarray(inp["w_in"], f32)
    sh["w_out"] = np.ascontiguousarray(inp["w_out"], f32)
    sh["mu"] = _pk(inp["rwkv_mu"])
    rv = np.stack([inp["rwkv_w0"], inp["rwkv_a0"], inp["rwkv_k_k"], inp["rwkv_k_a"],
                   inp["rwkv_r_k"].reshape(L, 256), inp["rwkv_ln_w"], inp["rwkv_ln_b"]], axis=1)
    sh["rvec"] = np.ascontiguousarray(np.transpose(_pk(rv), (0, 2, 1, 3)))
    sh["lora"] = np.ascontiguousarray(np.concatenate([inp["rwkv_w2"], inp["rwkv_a2"], inp["rwkv_g2"]], axis=1))

    def s5vec(a):
        return np.ascontiguousarray(a.reshape(L, 8, 2, 64).transpose(0, 2, 3, 1).reshape(L, 128, 8))
    ldt = np.broadcast_to(inp["s5_log_dt"][:, :, None], (L, 16, 64))
    sh["s5v"] = np.ascontiguousarray(np.stack([s5vec(inp["s5_lambda_re"]), s5vec(inp["s5_lambda_im"]), s5vec(ldt)], axis=2))
    bpad = np.zeros((L, 2, 8, 128, 128), f32)
    cpad = np.zeros((L, 2, 8, 128, 128), f32)
    for ri, (bb, cc) in enumerate([(inp["s5_b_re"], inp["s5_c_re"]), (inp["s5_b_im"], inp["s5_c_im"])]):
        for g in range(16):
            jt, q = g // 2, g % 2
            r0 = (g % 8) * 16
            bpad[:, ri, jt, r0:r0 + 16, q * 64:(q + 1) * 64] = np.transpose(bb[:, g], (0, 2, 1))
            cpad[:, ri, jt, q * 64:(q + 1) * 64, r0:r0 + 16] = np.transpose(cc[:, g], (0, 2, 1))
    sh["s5b"] = bpad
    sh["s5c"] = cpad
    sv = np.stack([inp["s5_d"], inp["s5_glu_b"], inp["branch_norm_g"][:, 0]], axis=1)
    sh["s5vec2"] = np.ascontiguousarray(np.transpose(_pk(sv), (0, 2, 1, 3)))
    sh["glu_w"] = np.ascontiguousarray(inp["s5_glu_w"], f32)
    sh["qng"] = _pk(inp["mla_q_norm_g"])
    sh["w_uq"] = np.ascontiguousarray(inp["mla_w_uq"], f32)
    sh["kvng"] = _pk(inp["mla_kv_norm_g"])
    wkv = inp["mla_w_ukv"].reshape(L, 128, 4, 128)
    kn = np.zeros((L, 128, 4, 96), f32)
    kn[..., :64] = wkv[..., :64]
    sh["w_ukn"] = kn
    sh["w_ukv"] = np.ascontiguousarray(wkv[..., 64:].reshape(L, 128, 256))
    sh["qkhg"] = np.ascontiguousarray(np.stack([inp["mla_q_head_g"], inp["mla_k_head_g"]], axis=2))
    cw = inp["lru_conv_w"]
    lv = np.concatenate([cw, inp["lru_conv_b"][:, None], inp["lru_b_a"][:, None], inp["lru_b_x"][:, None],
                         inp["lru_lambda"][:, None], inp["branch_norm_g"][:, 2][:, None]], axis=1)
    sh["lruv"] = np.ascontiguousarray(np.transpose(_pk(lv), (0, 2, 1, 3)))
    bd = np.zeros((L, 2, 2, 128, 128), f32)
    for wi, wmat in enumerate([inp["lru_w_a"], inp["lru_w_x"]]):
        for n in range(4):
            t, q = n // 2, n % 2
            bd[:, wi, t, q * 64:(q + 1) * 64, q * 64:(q + 1) * 64] = wmat[:, n]
    sh["lrubd"] = bd
    sh["bng_c"] = np.ascontiguousarray(inp["branch_norm_g"][:, 1].reshape(L, 4, 64).transpose(0, 2, 1))
    sh["wr"] = np.ascontiguousarray(np.concatenate([inp["moe_w_group"], inp["moe_w_expert"]], axis=2))
    br = np.concatenate([inp["moe_b_group"], inp["moe_b_expert"]], axis=1)
    sh["br"] = np.ascontiguousarray(np.broadcast_to(br[:, None, :], (L, 128, 36)))
    sh["w1"] = np.ascontiguousarray(inp["moe_w1"], f32)
    sh["w3"] = np.ascontiguousarray(inp["moe_w3"], f32)
    sh["w2"] = np.ascontiguousarray(inp["moe_w2"], f32)
    cst = {}
    cst["ident"] = np.eye(128, dtype=f32)
    bo = np.zeros((128, 128), f32)
    bo[:64, :64] = 1.0
    bo[64:, 64:] = 1.0
    cst["blk"] = bo
    half = 16
    invf = np.power(np.float32(10000.0), -np.arange(half, dtype=f32) * np.float32(2.0) / np.float32(32)).astype(f32)
    iv = np.zeros((96, 1), f32)
    iv[64:80, 0] = invf
    iv[80:96, 0] = invf
    cst["invf"] = iv
    PT = np.zeros((128, 128), f32)
    for i in range(16):
        PT[80 + i, 64 + i] = -1.0
        PT[64 + i, 80 + i] = 1.0
    cst["rotT"] = PT
    E = np.zeros((32, 96), f32)
    for i in range(32):
        E[i, 64 + i] = 1.0
    cst["epe"] = E
    jj = np.arange(64)[:, None]
    ii = np.arange(64)[None, :]
    mk = np.concatenate([(jj < ii), (jj <= ii)], axis=1).astype(f32)
    cst["mk"] = np.ascontiguousarray(np.broadcast_to(mk[:, None, :], (64, 4, 128)))
    cst["mkl"] = np.ascontiguousarray(np.broadcast_to((jj > ii).astype(f32)[:, None, :], (64, 4, 64)))
    cst["id4"] = np.ascontiguousarray(np.broadcast_to(np.eye(64, dtype=f32)[:, None, :], (64, 4, 64)))
    sel = np.zeros((128, 64), f32)
    sel[64, :] = 1.0
    cst["sel65"] = sel
    s32 = np.zeros((128, 32, 128), f32)
    for e in range(32):
        s32[e, e, :] = 1.0
    cst["sel32"] = s32
    cst["tvals"] = np.ascontiguousarray(np.broadcast_to(np.arange(NB, dtype=f32)[None, :], (128, NB)))
    per_core = []
    for b in range(NCORES):
        d = {}
        d["xT"] = np.ascontiguousarray(inp["x"][b, :S].T, f32)
        d["c"] = _pk(inp["c"][b])
        d["pos"] = np.ascontiguousarray(np.broadcast_to(inp["pos_offset"][b].astype(np.int32).reshape(1, 1), (96, 1)))
        per_core.append(d)
    return sh, cst, per_core


class K:
    pass


def build(S, L, shapes, stop=None, dbg=None):
    nc = bass.Bass("TRN2", target_bir_lowering=False)
    k = K()
    k.nc = nc
    k.S = S
    k.L = L
    dr = {}
    for name, (shp, dt) in shapes.items():
        dr[name] = nc.dram_tensor(name, list(shp), dt, kind="ExternalInput").ap()
    dr["out"] = nc.dram_tensor("outT", [D, S], F32, kind="ExternalOutput").ap()
    k.mixT = nc.dram_tensor("mixT", [D, S], BF16, kind="Internal").ap()
    k.x1T = nc.dram_tensor("x1T", [D, S], F32, kind="Internal").ap()
    k.x2T = nc.dram_tensor("x2T", [D, S], F32, kind="Internal").ap()
    k.h2T = nc.dram_tensor("h2T", [D, S], BF16, kind="Internal").ap()
    k.gT = nc.dram_tensor("gT", [32, S], F32, kind="Internal").ap()
    NE = shapes["w1"][0][1]
    k.w1b = nc.dram_tensor("w1b", [NE, 128, 4096], BF16, kind="Internal").ap()
    k.w3b = nc.dram_tensor("w3b", [NE, 128, 4096], BF16, kind="Internal").ap()
    k.w2b = nc.dram_tensor("w2b", [NE, 128, 4096], BF16, kind="Internal").ap()
    k.qs = nc.dram_tensor("qs", [96, 4, S], BF16, kind="Internal").ap()
    k.ks = nc.dram_tensor("ks", [96, 4, S], BF16, kind="Internal").ap()
    k.vs = nc.dram_tensor("vs", [S // 128, 128, 260], BF16, kind="Internal").ap()
    if dbg is not None:
        dr["dbg"] = nc.dram_tensor("dbg", list(dbg), F32, kind="ExternalOutput").ap()
    k.dr = dr
    with ExitStack() as st:
        k.st = st
        S_ = Sched(nc)
        k.S_ = S_
        emit_all(k, stop)
        S_.barrier()
        S_.flush(st)
    return nc


def sb(k, st, name, shape, dt=F32):
    k.uid = getattr(k, "uid", 0) + 1
    return st.enter_context(k.nc.sbuf_tensor("sb%d_%s" % (k.uid, name), list(shape), dt))


class Ops:
    def __init__(self, S_):
        self.S = S_

    def dma(self, eng, out, in_, key, r=(), w=()):
        return self.S.dma(eng, lambda e: e.dma_start(out=out, in_=in_), key, r, w)

    def act(self, out, in_, func, r, w, bias=None, scale=None, eng="act"):
        kw = {}
        if bias is not None:
            kw["bias"] = bias
        if scale is not None:
            kw["scale"] = scale
        return self.S.op(eng, lambda e: e.activation(out=out, in_=in_, func=func, **kw), r, w)

    def tt(self, out, in0, in1, op, r, w, eng="dve"):
        return self.S.op(eng, lambda e: e.tensor_tensor(out=out, in0=in0, in1=in1, op=op), r, w)

    def ts(self, out, in0, s1, s2, op0, op1, r, w, eng="dve"):
        if s2 is None:
            return self.S.op(eng, lambda e: e.tensor_scalar(out=out, in0=in0, scalar1=s1, scalar2=None, op0=op0), r, w)
        return self.S.op(eng, lambda e: e.tensor_scalar(out=out, in0=in0, scalar1=s1, scalar2=s2, op0=op0, op1=op1), r, w)

    def stt(self, out, in0, scalar, in1, op0, op1, r, w, eng="dve"):
        return self.S.op(eng, lambda e: e.scalar_tensor_tensor(out=out, in0=in0, scalar=scalar, in1=in1, op0=op0, op1=op1), r, w)

    def copy(self, out, in_, r, w, eng="dve"):
        if eng == "act":
            return self.S.op(eng, lambda e: e.activation(out=out, in_=in_, func=AF.Copy), r, w)
        return self.S.op(eng, lambda e: e.tensor_copy(out=out, in_=in_), r, w)

    def memset(self, out, val, w, eng="pool"):
        return self.S.op(eng, lambda e: e.memset(out, val), (), w)

    def scan(self, out, d0, d1, init, r, w, eng="dve"):
        return self.S.op(eng, lambda e: e.tensor_tensor_scan(out=out, data0=d0, data1=d1, initial=init, op0=ALU.mult, op1=ALU.add), r, w)

    def mm(self, out, lhsT, rhs, start, stop, r, w):
        return self.S.op("pe", lambda e: e.matmul(out, lhsT, rhs, start=start, stop=stop), r, w)

    def tr(self, out, in_, ident, r, w):
        return self.S.op("pe", lambda e: e.transpose(out, in_, ident), r, w)


NB = 256


def emit_all(k, stop):
    nc, S_, dr, S, L = k.nc, k.S_, k.dr, k.S, k.L
    st = k.st
    o = Ops(S_)
    nblk = S // NB
    ps = [st.enter_context(nc.psum_tensor(f"ps{i}", [128, 512], F32)) for i in range(8)]
    P = [f"ps{i}" for i in range(8)]
    ident = sb(k, st, "ident", [128, 128])
    blk = sb(k, st, "blk", [128, 128])
    blkb = sb(k, st, "blkb", [128, 128], BF16)
    onesb = sb(k, st, "onesb", [128, 128], BF16)
    mods = sb(k, st, "mods", [128, L, 48])
    g1s = sb(k, st, "g1s", [128, L, 8])
    g2s = sb(k, st, "g2s", [128, L, 8])
    k.ident, k.blk, k.blkb, k.onesb, k.mods, k.g1s, k.g2s, k.ps, k.P, k.o = ident, blk, blkb, onesb, mods, g1s, g2s, ps, P, o
    o.dma("sp", ident[:], dr["ident"], "ident", w=["ident"])
    o.dma("sp", blk[:], dr["blk"], "blk", w=["blk"])
    o.copy(blkb[:], blk[:], ["blk"], ["blkb"])
    o.memset(onesb[:], 1.0, ["onesb"])
    ones32 = sb(k, st, "ones32", [128, 128])
    k.ones32 = ones32
    o.memset(ones32[:], 1.0, ["ones32"])
    cc = sb(k, st, "cc", [128, 8])
    k.cc = cc
    for val, c in CC.items():
        o.memset(cc[:, c:c + 1], float(val), ["cc"])

    with ExitStack() as p0:
        stg = [sb(k, p0, f"adst{i}", [128, 8, 768]) for i in range(2)]
        cond = sb(k, p0, "cond", [128, 8])
        adab = sb(k, p0, "adab", [128, 48])
        ng = sb(k, p0, "ng", [128, 8])
        tmp8 = sb(k, p0, "tmp8", [128, 8])
        o.dma("sp", cond[:], dr["c"], "cond", w=["cond"])
        o.act(cond[:], cond[:], AF.Silu, ["cond"], ["cond"])
        for l in range(L):
            o.dma("sp", adab[:], dr["ada_b"][l], "adab", w=["adab"])
            wv = dr["ada_w"][l].rearrange("(kk p) n -> p kk n", p=128)
            for c in range(8):
                bk = f"adst{c % 2}"
                o.dma("sp" if c % 2 == 0 else "pool", stg[c % 2][:], wv[:, :, c * 768:(c + 1) * 768], bk, w=[bk])
                for j in range(6):
                    col = c * 6 + j
                    for kk in range(8):
                        o.mm(ps[0][:, col:col + 1], stg[c % 2][:, kk, j * 128:(j + 1) * 128], cond[:, kk:kk + 1],
                             kk == 0, kk == 7, [bk, "cond"], [P[0]])
            o.tt(mods[:, l, :], ps[0][:, 0:48], adab[:], ALU.add, [P[0], "adab"], ["mods"])
            for (gs, nm, c0) in ((g1s, "n1g", 8), (g2s, "n2g", 32)):
                o.dma("sp", ng[:], dr[nm][l], "ng", w=["ng"])
                o.ts(tmp8[:], mods[:, l, c0:c0 + 8], 1.0, None, ALU.add, None, ["mods"], ["tmp8"])
                o.tt(gs[:, l, :], tmp8[:], ng[:], ALU.mult, ["tmp8", "ng"], ["g1s" if c0 == 8 else "g2s"])
        S_.barrier()
        S_.flush(st)
    if stop == "p0":
        o.dma("sp", dr["dbg"][:, 0:L * 48], mods[:].rearrange("p l c -> p (l c)"), "dbg", r=["mods"], w=["dbg"])
        return

    xin = dr["xT"]
    for l in range(L):
        phaseA(k, l, xin, stop)
        if stop is not None and stop.startswith("A"):
            return
        phaseB(k, l, xin, k.x1T, stop)
        if stop is not None and stop.startswith("B"):
            return
        phaseW(k, l)
        xo = dr["out"] if l == L - 1 else k.x2T
        phaseC(k, l, k.x1T, xo, stop)
        if stop is not None and stop.startswith("C"):
            return
        xin = xo


CC = {1e-6: 0, 64e-5: 1, 1.0: 2, -math.pi: 3, 0.0: 4, 1e-24: 5}


def rsqrt(k, out, in_, scale, eps, rkeys, wkey, rows=128):
    c = CC[eps]
    k.o.act(out, in_, AF.Sqrt, list(rkeys) + ["cc"], [wkey], bias=k.cc[0:rows, c:c + 1], scale=scale)
    k.o.S.op("dve", lambda e: e.reciprocal(out=out, in_=out), [wkey], [wkey])


def rms_stats(k, srcs, n_feat, eps, ps_ap, rstd_ap, sq_bufs, rkeys, pkey, wkey, lhsT=None, rows=128):
    o = k.o
    n = len(srcs)
    lhs = k.onesb[:rows, :rows] if lhsT is None else lhsT
    for i, (ap, key) in enumerate(srcs):
        sq, sqk = sq_bufs[i]
        o.act(sq, ap, AF.Square, [key], [sqk])
        o.mm(ps_ap, lhs, sq, i == 0, i == n - 1, [sqk, "onesb"], [pkey])
    rsqrt(k, rstd_ap, ps_ap, 1.0 / n_feat, eps, [pkey], wkey, rows)


def phaseA(k, l, xin, stop):
    nc, S_, dr, S, L, o = k.nc, k.S_, k.dr, k.S, k.L, k.o
    ps, P = k.ps, k.P
    nblk = S // NB
    with ExitStack() as pa:
        w_in = sb(k, pa, "w_in", [128, 8, DIN], BF16)
        xblk = sb(k, pa, "xblk", [128, 8, NB])
        wv = dr["w_in"][l].rearrange("(kk p) n -> p kk n", p=128)
        xflat = xblk[:].rearrange("p a b -> p (a b)")
        for c in range(20):
            stv = xflat[:, (c % 2) * 832:(c % 2 + 1) * 832].rearrange("p (kk n) -> p kk n", n=104)
            o.dma("sp", stv, wv[:, :, c * 104:(c + 1) * 104], f"xblk{c % 2}", w=["xblk"])
            o.copy(w_in[:, :, c * 104:(c + 1) * 104], stv, ["xblk"], ["w_in"], eng=("dve" if c % 2 == 0 else "pool"))
        rstd = sb(k, pa, "rstd", [128, NB])
        xt = sb(k, pa, "xt", [128, NB])
        hT = sb(k, pa, "hT", [128, 8, NB], BF16)
        sqb = hT
        zr = sb(k, pa, "zr", [128, 7, NB + 1])
        s5u = sb(k, pa, "s5u", [128, 2, NB])
        qab = sb(k, pa, "qab", [128, 2, NB], BF16)
        kvab = sb(k, pa, "kvab", [128, NB], BF16)
        kpeb = sb(k, pa, "kpeb", [32, NB], BF16)
        lrx = sb(k, pa, "lrx", [128, 2, NB + 3])
        lrg = sb(k, pa, "lrg", [128, 2, NB])
        R = rwkv_setup(k, pa, l)
        Q = s5_setup(k, pa, l, xblk) if stop not in ("A1",) and not (stop or "").startswith("A2") else None
        U = lru_setup(k, pa, l, xblk) if stop not in ("A1",) and not (stop or "").startswith("A2") else None
        mixT = k.mixT
        A = mla_setup(k, pa, l, xblk, Q, U) if Q is not None else None
        o.memset(zr[:, :, 0:1], 0.0, ["zr"])
        o.memset(lrx[:, :, 0:3], 0.0, ["lrx"])
        xv = xin.rearrange("(kk p) s -> p kk s", p=128)
        tiles = [(t * 128, 128) for t in range(12)] + [(1536, 32)] + [(1568 + t * 128, 128) for t in range(4)]
        for i in range(nblk):
            t0 = i * NB
            o.dma("sp", xblk[:], xv[:, :, t0:t0 + NB], "xblk", w=["xblk"])
            rms_stats(k, [(xblk[:, kk, :], "xblk") for kk in range(8)], 1024.0, 1e-6, ps[0][:, 0:NB], rstd[:],
                      [(sqb[:, kk, :], "hT") for kk in range(8)], None, P[0], "rstd")
            for kk in range(8):
                o.tt(xt[:], xblk[:, kk, :], rstd[:], ALU.mult, ["xblk", "rstd"], ["xt"])
                o.act(hT[:, kk, :], xt[:], AF.Identity, ["xt", "g1s", "mods"], ["hT"],
                      bias=k.mods[:, l, kk:kk + 1], scale=k.g1s[:, l, kk:kk + 1])
            for ti, (c0, m) in enumerate(tiles):
                pb = 1 + (ti % 3)
                pt = ps[pb][0:m, 0:NB]
                for kk in range(8):
                    o.mm(pt, w_in[:, kk, c0:c0 + m], hT[:, kk, :], kk == 0, kk == 7, ["w_in", "hT"], [P[pb]])
                if ti < 7:
                    o.copy(zr[:, ti, 1:NB + 1], pt, [P[pb]], ["zr"], eng=("act" if ti % 2 == 0 else "dve"))
                elif ti < 9:
                    o.copy(s5u[:, ti - 7, :], pt, [P[pb]], ["s5u"], eng="dve")
                elif ti < 11:
                    o.copy(qab[:, ti - 9, :], pt, [P[pb]], ["qab"], eng="act")
                elif ti == 11:
                    o.copy(kvab[:], pt, [P[pb]], ["kvab"], eng="act")
                elif ti == 12:
                    o.copy(kpeb[:], pt, [P[pb]], ["kpeb"], eng="act")
                elif ti < 15:
                    o.copy(lrx[:, ti - 13, 3:NB + 3], pt, [P[pb]], ["lrx"], eng="dve")
                else:
                    o.copy(lrg[:, ti - 15, :], pt, [P[pb]], ["lrg"], eng="act")
            gens = []
            if stop != "A1":
                R["mix_dst"] = (mixT, t0) if (stop is None or not stop.startswith("A2")) else None
                gens.append(rwkv_block(k, l, i, R, zr, stop))
            if stop is None or not stop.startswith("A2"):
                gens.append(seq_gen(par_gen([s5_block(k, l, i, Q, s5u, mixT, t0), lru_block(k, l, i, U, lrx, lrg, mixT, t0)]),
                                    mla_block(k, l, i, A, qab, kvab, kpeb, t0)))
            interleave(gens)
            if stop == "A4":
                dbg = dr["dbg"]
                o.copy(A["t1"][:], A["qrb"][:, 1, :], ["a_out"], ["l_aa"], eng="dve")
                o.dma("sp", dbg[0:96, t0:t0 + NB], A["t1"][:], "dbg", r=["l_aa"], w=["dbg"])
                o.copy(A["t2"][:], A["krb"][:, 2, :], ["a_out"], ["l_t1"], eng="dve")
                o.dma("sp", dbg[96:192, t0:t0 + NB], A["t2"][:], "dbg", r=["l_t1"], w=["dbg"])
                for tt in range(NB // 128):
                    o.copy(A["qf"][0:64, 0:128], A["Vt"][0:64, tt, 3, 0:128].rearrange("p c -> p c") if False else A["Vt"][0:64, tt, :, :].rearrange("p h c -> p (h c)")[:, 0:128], ["a_Vt"], ["l_xc"], eng="dve")
                    o.dma("sp", dbg[192:256, t0 + tt * 128:t0 + (tt + 1) * 128], A["qf"][0:64, 0:128], "dbg", r=["l_xc"], w=["dbg"])
                continue
            if stop == "A3":
                dbg = dr["dbg"]
                o.dma("sp", dbg[256:512, t0:t0 + NB].rearrange("(t p) s -> p t s", p=128), Q["yo"][:], "dbg", r=["s_yo"], w=["dbg"])
                o.dma("sp", dbg[768:1024, t0:t0 + NB].rearrange("(t p) s -> p t s", p=128), U["yo"][:], "dbg", r=["l_yo"], w=["dbg"])
                o.dma("sp", dbg[0:256, t0:t0 + NB].rearrange("(t p) s -> p t s", p=128), R["yout"][:], "dbg", r=["yout"], w=["dbg"])
                continue
            if stop is not None and stop.startswith("A2"):
                dbg = dr["dbg"]
                o.dma("sp", dbg[0:256, t0:t0 + NB].rearrange("(t p) s -> p t s", p=128), R["yout"][:], "dbg", r=["yout"], w=["dbg"])
                continue
            if stop == "A1":
                dbg = dr["dbg"]
                o.dma("sp", dbg[0:896, t0:t0 + NB].rearrange("(t p) s -> p t s", p=128), zr[:, :, 1:NB + 1], "dbg", r=["zr"], w=["dbg"])
                o.dma("sp", dbg[896:1152, t0:t0 + NB].rearrange("(t p) s -> p t s", p=128), s5u[:], "dbg", r=["s5u"], w=["dbg"])
                o.dma("sp", dbg[1568:1824, t0:t0 + NB].rearrange("(t p) s -> p t s", p=128), lrx[:, :, 3:NB + 3], "dbg", r=["lrx"], w=["dbg"])
                o.dma("sp", dbg[1824:2080, t0:t0 + NB].rearrange("(t p) s -> p t s", p=128), lrg[:], "dbg", r=["lrg"], w=["dbg"])
                continue
        S_.barrier()
        S_.flush(k.st)


def par_gen(gens):
    gens = list(gens)
    while gens:
        for g in list(gens):
            try:
                next(g)
            except StopIteration:
                gens.remove(g)
        yield


def seq_gen(*gens):
    for g in gens:
        yield from g


def interleave(gens):
    gens = [g for g in gens if g is not None]
    while gens:
        for g in list(gens):
            try:
                next(g)
            except StopIteration:
                gens.remove(g)


def rwkv_setup(k, pa, l):
    o, dr = k.o, k.dr
    R = {}
    nch = NB // 64
    f2 = [128, 2, NB]
    for nm in ("gg", "bonus", "epos", "bt", "kt", "ya", "yout"):
        R[nm] = sb(k, pa, "r_" + nm, f2)
    for nm in ("sg", "aa", "kk", "tA", "tB", "kmod", "cls", "eneg", "eex"):
        R[nm] = sb(k, pa, "r_" + nm, [128, 1, NB])
    R["zs"] = sb(k, pa, "r_zs", [128, 7, NB])
    R["youtb"] = sb(k, pa, "r_youtb", [128, 2, NB], BF16)
    R["lob"] = sb(k, pa, "r_lob", [128, NB], BF16)
    R["sqb"] = sb(k, pa, "r_sqb", [128, NB], BF16)
    R["ar"] = sb(k, pa, "r_ar", [128, 2, nch, 2, 64])
    R["tok"] = sb(k, pa, "r_tok", [128, 3, 2, 128])
    R["NL"] = [sb(k, pa, f"r_NL{i}", [64, 2, 4, 64]) for i in range(2)]
    R["Pm"] = [sb(k, pa, f"r_Pm{i}", [64, 4, 64]) for i in range(2)]
    R["LakT"] = sb(k, pa, "r_LakT", [64, 4, 64])
    R["QrbT"] = sb(k, pa, "r_QrbT", [128, 4, 64])
    R["QrkT"] = sb(k, pa, "r_QrkT", [128, 4, 64])
    R["Xs"] = sb(k, pa, "r_Xs", [64, 256])
    R["Mtmp"] = sb(k, pa, "r_Mtmp", [128, 128])
    R["Us"] = sb(k, pa, "r_Us", [128, 256])
    R["M"] = [sb(k, pa, f"r_M{i}", [128, 2, 128]) for i in range(2)]
    R["mu"] = sb(k, pa, "r_mu", [128, 7])
    R["omu"] = sb(k, pa, "r_omu", [128, 7])
    R["rvec"] = sb(k, pa, "r_rvec", [128, 7, 2])
    R["lst"] = sb(k, pa, "r_lst", [128, 256])
    R["lora"] = sb(k, pa, "r_lora", [128, 256], BF16)
    R["mk"] = sb(k, pa, "r_mk", [64, 4, 128])
    R["mkl"] = sb(k, pa, "r_mkl", [64, 4, 64])
    R["id4"] = sb(k, pa, "r_id4", [64, 4, 64])
    R["cmask"] = sb(k, pa, "r_cmask", [128, NB])
    o.dma("sp", R["mu"][:], dr["mu"][l], "r_mu", w=["r_mu"])
    o.ts(R["omu"][:], R["mu"][:], -1.0, 1.0, ALU.mult, ALU.add, ["r_mu"], ["r_omu"])
    o.dma("sp", R["rvec"][:], dr["rvec"][l], "r_rvec", w=["r_rvec"])
    o.dma("sp", R["lst"][:], dr["lora"][l], "r_lst", w=["r_lst"])
    o.copy(R["lora"][:], R["lst"][:], ["r_lst"], ["r_lora"])
    o.dma("sp", R["mk"][:], dr["mk"], "r_mk", w=["r_mk"])
    o.dma("sp", R["mkl"][:], dr["mkl"], "r_mkl", w=["r_mkl"])
    o.dma("sp", R["id4"][:], dr["id4"], "r_id4", w=["r_id4"])
    o.memset(R["cmask"][:], 1.0, ["r_cmask"])
    o.memset(R["cmask"][:].rearrange("p (c j) -> p c j", j=64)[:, :, 0:1], 0.0, ["r_cmask"])
    o.memset(R["M"][0][:], 0.0, ["r_M0"])
    o.memset(R["tok"][:], 0.0, ["r_tok"])
    o.memset(R["Us"][:], 0.0, ["r_Us"])
    o.memset(R["QrbT"][:], 0.0, ["r_QrbT"])
    o.memset(R["QrkT"][:], 0.0, ["r_QrkT"])
    return R


C0 = -math.exp(-0.5)


def rwkv_block(k, l, i, R, zr, stop):
    o, ps, P = k.o, k.ps, k.P
    nch = NB // 64
    zs, rv = R["zs"], R["rvec"]
    W0, A0, KK, KA, RK, LNW, LNB = range(7)
    for t in range(7):
        eng = "dve" if t % 2 == 0 else "pool"
        o.ts(zs[:, t, :], zr[:, t, 0:NB], R["mu"][:, t:t + 1], None, ALU.mult, None, ["zr", "r_mu"], [("zs", t)], eng=eng)
        o.stt(zs[:, t, :], zr[:, t, 1:NB + 1], R["omu"][:, t:t + 1], zs[:, t, :], ALU.mult, ALU.add, ["zr", "r_omu", ("zs", t)], [("zs", t)])
    o.copy(zr[:, :, 0:1], zr[:, :, NB:NB + 1], ["zr"], ["zr"], eng="dve")
    lob = R["lob"]
    o.act(lob[0:32, :], zs[0:32, 6, :], AF.Tanh, [("zs", 6)], ["r_lob"])
    o.act(lob[64:128, :], zs[64:128, 6, :], AF.Sigmoid, [("zs", 6)], ["r_lob"])
    o.copy(lob[32:64, :], zs[32:64, 6, :], [("zs", 6)], ["r_lob"], eng="dve")
    lora = R["lora"]
    for p in range(2):
        cs = slice(p * 128, (p + 1) * 128)
        rT, kT, vT = zs[:, p, :], zs[:, 2 + p, :], zs[:, 4 + p, :]
        rk_, kk_, vk_ = ("zs", p), ("zs", 2 + p), ("zs", 4 + p)
        o.mm(ps[1][:, 0:NB], lora[0:32, cs], lob[0:32, :], True, True, ["r_lora", "r_lob"], [P[1]])
        o.mm(ps[2][:, 0:NB], lora[32:64, cs], lob[32:64, :], True, True, ["r_lora", "r_lob"], [P[2]])
        o.mm(ps[3][:, 0:NB], lora[64:128, cs], lob[64:128, :], True, True, ["r_lora", "r_lob"], [P[3]])
        o.act(R["sg"][:, 0, :], ps[1][:, 0:NB], AF.Sigmoid, [P[1], "r_rvec"], ["r_sg"], bias=rv[:, W0, p:p + 1])
        o.act(R["aa"][:, 0, :], ps[2][:, 0:NB], AF.Sigmoid, [P[2], "r_rvec"], ["r_aa"], bias=rv[:, A0, p:p + 1])
        o.copy(R["gg"][:, p, :], ps[3][:, 0:NB], [P[3]], ["r_gg"], eng="dve")
        kk = R["kk"][:, 0, :]
        o.ts(kk, kT, rv[:, KK, p:p + 1], None, ALU.mult, None, [kk_, "r_rvec"], ["r_kk"])
        o.act(R["sqb"][:], kk, AF.Square, ["r_kk"], ["r_sqb"])
        o.mm(ps[1][:, 0:NB], k.blkb[:], R["sqb"][:], True, True, ["blkb", "r_sqb"], [P[1]])
        rsqrt(k, R["tA"][:, 0, :], ps[1][:, 0:NB], 1.0, 1e-24, [P[1]], "r_tA")
        o.tt(kk, kk, R["tA"][:, 0, :], ALU.mult, ["r_kk", "r_tA"], ["r_kk"])
        o.ts(R["tA"][:, 0, :], R["aa"][:, 0, :], -1.0, rv[:, KA, p:p + 1], ALU.add, ALU.mult, ["r_aa", "r_rvec"], ["r_tA"])
        o.stt(R["kmod"][:, 0, :], R["tA"][:, 0, :], 1.0, kT, ALU.add, ALU.mult, ["r_tA", kk_], ["r_kmod"])
        o.tt(R["tA"][:, 0, :], rT, R["kmod"][:, 0, :], ALU.mult, [rk_, "r_kmod"], ["r_tA"])
        o.ts(R["sqb"][:], R["tA"][:, 0, :], rv[:, RK, p:p + 1], None, ALU.mult, None, ["r_tA", "r_rvec"], ["r_sqb"])
        o.mm(ps[2][:, 0:NB], k.blkb[:], R["sqb"][:], True, True, ["blkb", "r_sqb"], [P[2]])
        o.tt(R["bonus"][:, p, :], ps[2][:, 0:NB], vT, ALU.mult, [P[2], vk_], ["r_bonus"])
        o.scan(R["cls"][:, 0, :], R["cmask"][:], R["sg"][:, 0, :], 0.0, ["r_cmask", "r_sg"], ["r_cls"])
        o.act(R["epos"][:, p, :], R["cls"][:, 0, :], AF.Exp, ["r_cls"], ["r_epos"], scale=C0)
        o.act(R["eneg"][:, 0, :], R["cls"][:, 0, :], AF.Exp, ["r_cls"], ["r_eneg"], scale=-C0)
        o.tt(R["tB"][:, 0, :], R["cls"][:, 0, :], R["sg"][:, 0, :], ALU.subtract, ["r_cls", "r_sg"], ["r_tB"])
        o.act(R["eex"][:, 0, :], R["tB"][:, 0, :], AF.Exp, ["r_tB"], ["r_eex"], scale=C0)
        arv = R["ar"][:, p, :, :, :]
        o.tt(arv[:, :, 1, :], rT.rearrange("p (c j) -> p c j", j=64), R["epos"][:, p, :].rearrange("p (c j) -> p c j", j=64),
             ALU.mult, [rk_, "r_epos"], ["r_ar"])
        o.stt(arv[:, :, 0, :], kk.rearrange("p (c j) -> p c j", j=64), -1.0, R["eex"][:, 0, :].rearrange("p (c j) -> p c j", j=64),
              ALU.mult, ALU.mult, ["r_kk", "r_eex"], ["r_ar"])
        o.tt(R["kt"][:, p, :], R["kmod"][:, 0, :], R["eneg"][:, 0, :], ALU.mult, ["r_kmod", "r_eneg"], ["r_kt"])
        o.tt(R["tA"][:, 0, :], kk, R["aa"][:, 0, :], ALU.mult, ["r_kk", "r_aa"], ["r_tA"])
        o.tt(R["bt"][:, p, :], R["tA"][:, 0, :], R["eneg"][:, 0, :], ALU.mult, ["r_tA", "r_eneg"], ["r_bt"])
        yield
    ar, bt, kt, tok = R["ar"], R["bt"], R["kt"], R["tok"]
    NL, Pm = R["NL"], R["Pm"]
    if stop == "A2a":
        return
    for c in range(nch):
        gc = i * nch + c
        cs = slice(c * 64, (c + 1) * 64)
        for p in range(2):
            o.tr(ps[4][0:64, p * 128:(p + 1) * 128], bt[:, p, cs], k.ident[:], ["r_bt", "ident"], [P[4]])
            o.tr(ps[4][0:64, 256 + p * 128:256 + (p + 1) * 128], kt[:, p, cs], k.ident[:], ["r_kt", "ident"], [P[4]])
            o.tr(ps[5][0:64, p * 128:(p + 1) * 128], zs[:, 4 + p, cs], k.ident[:], [("zs", 4 + p), "ident"], [P[5]])
        o.copy(tok[0:64, 0:2, :, :].rearrange("j a p c -> j (a p c)"), ps[4][0:64, :], [P[4]], ["r_tok"], eng="act")
        o.copy(tok[0:64, 2, :, :].rearrange("j p c -> j (p c)"), ps[5][0:64, 0:256], [P[5]], ["r_tok"], eng="dve")
        yield
        if stop == "A2b":
            continue
        for h in range(4):
            p, q = h // 2, h % 2
            rs = slice(q * 64, (q + 1) * 64)
            arh = ar[rs, p, c, :, :].rearrange("k a j -> k (a j)")
            o.mm(ps[6][0:64, h * 128:(h + 1) * 128], bt[rs, p, cs], arh, True, True, ["r_bt", "r_ar"], [P[6]])
            o.mm(ps[7][0:64, h * 128:(h + 1) * 128], kt[rs, p, cs], arh, True, True, ["r_kt", "r_ar"], [P[7]])
            o.mm(ps[5][0:64, 256 + h * 64:256 + (h + 1) * 64], ar[rs, p, c, 0, :], bt[rs, p, cs], True, True, ["r_bt", "r_ar"], [P[5]])
        v6 = ps[6][0:64, :].rearrange("j (h x) -> j h x", x=128)
        v7 = ps[7][0:64, :].rearrange("j (h x) -> j h x", x=128)
        mk = R["mk"]
        o.tt(NL[0][:, 0, :, :], v6[:, :, 0:64], mk[:, :, 0:64], ALU.mult, [P[6], "r_mk"], ["r_NL0"])
        o.tt(R["QrbT"][0:64], v6[:, :, 64:128], mk[:, :, 64:128], ALU.mult, [P[6], "r_mk"], ["r_QrbT"])
        o.tt(R["LakT"][:], v7[:, :, 0:64], mk[:, :, 0:64], ALU.mult, [P[7], "r_mk"], ["r_LakT"])
        o.tt(R["QrkT"][0:64], v7[:, :, 64:128], mk[:, :, 64:128], ALU.mult, [P[7], "r_mk"], ["r_QrkT"])
        o.tt(NL[0][:, 1, :, :], ps[5][0:64, 256:512].rearrange("j (h x) -> j h x", x=64), R["mkl"][:], ALU.mult, [P[5], "r_mkl"], ["r_NL0"])
        o.tt(Pm[1][:], NL[0][:, 0, :, :], R["id4"][:], ALU.add, ["r_NL0", "r_id4"], ["r_Pm1"], eng="pool")
        yield
        if stop == "A2c":
            continue
        for m in range(1, 7):
            src, dst = NL[(m - 1) % 2], NL[m % 2]
            sk, dk = f"r_NL{(m - 1) % 2}", f"r_NL{m % 2}"
            pin, pout = Pm[(m - 1) % 2], Pm[m % 2]
            pik, pok = f"r_Pm{(m - 1) % 2}", f"r_Pm{m % 2}"
            for h in range(4):
                if m <= 5:
                    o.mm(ps[6][0:64, h * 64:(h + 1) * 64], src[:, 1, h, :], src[:, 0, h, :], True, True, [sk], [P[6]])
                    o.mm(ps[6][0:64, 256 + h * 64:256 + (h + 1) * 64], src[:, 0, h, :], src[:, 1, h, :], True, True, [sk], [P[6]])
                if m >= 2:
                    o.mm(ps[7][0:64, h * 64:(h + 1) * 64], src[:, 1, h, :], pin[:, h, :], True, True, [sk, pik], [P[7]])
            if m <= 5:
                o.copy(dst[:].rearrange("j a h x -> j (a h x)"), ps[6][0:64, :], [P[6]], [dk], eng="act")
            if m >= 2:
                o.tt(pout[:].rearrange("j h x -> j (h x)"), ps[7][0:64, 0:256], pin[:].rearrange("j h x -> j (h x)"), ALU.add, [P[7], pik], [pok])
                yield
            else:
                pass
        TT_ = Pm[0]
        if stop == "A2d":
            continue
        Mc, Mn = R["M"][gc % 2], R["M"][(gc + 1) % 2]
        mck, mnk = f"r_M{gc % 2}", f"r_M{(gc + 1) % 2}"
        for p in range(2):
            o.mm(ps[4][0:64, p * 128:(p + 1) * 128], ar[:, p, c, 0, :], Mc[:, p, :], True, False, ["r_ar", mck], [P[4]])
            for q in range(2):
                h = 2 * p + q
                o.mm(ps[4][0:64, p * 128 + q * 64:p * 128 + (q + 1) * 64], R["LakT"][:, h, :], tok[0:64, 2, p, q * 64:(q + 1) * 64],
                     False, q == 1, ["r_LakT", "r_tok"], [P[4]])
        o.copy(R["Xs"][:], ps[4][0:64, 0:256], [P[4]], ["r_Xs"], eng="dve")
        yield
        if stop == "A2e":
            continue
        for h in range(4):
            o.mm(ps[4][0:64, 256 + h * 64:256 + (h + 1) * 64], TT_[:, h, :], R["Xs"][:, h * 64:(h + 1) * 64], True, True, ["r_Pm0", "r_Xs"], [P[4]])
        o.copy(R["Us"][0:64, :], ps[4][0:64, 256:512], [P[4]], ["r_Us"], eng="dve")
        yield
        if stop == "A2f":
            continue
        for p in range(2):
            pc = slice(p * 128, (p + 1) * 128)
            o.mm(ps[5][:, pc], k.ident[:], Mc[:, p, :], True, False, ["ident", mck], [P[5]])
            o.mm(ps[5][:, pc], tok[:, 0, p, :], R["Us"][:, pc], False, False, ["r_tok", "r_Us"], [P[5]])
            o.mm(ps[5][:, pc], tok[:, 1, p, :], tok[:, 2, p, :], False, True, ["r_tok"], [P[5]])
            for q in range(2):
                if stop == "A2g":
                    continue
                h = 2 * p + q
                yc = slice(256 + h * 64, 256 + (h + 1) * 64)
                o.mm(ps[5][:, yc], Mc[:, p, :], ar[:, p, c, 1, :], True, False, [mck, "r_ar"], [P[5]])
                o.mm(ps[5][:, yc], R["Us"][:, pc], R["QrbT"][:, h, :], False, False, ["r_Us", "r_QrbT"], [P[5]])
                o.mm(ps[5][:, yc], tok[:, 2, p, :], R["QrkT"][:, h, :], False, True, ["r_tok", "r_QrkT"], [P[5]])
        for p in range(2):
            pc = slice(p * 128, (p + 1) * 128)
            o.act(R["Mtmp"][:], ps[5][:, pc], AF.Identity, [P[5], "r_epos"], ["r_Mtmp"], scale=R["epos"][:, p, c * 64 + 63:c * 64 + 64])
            o.tt(Mn[:, p, :], R["Mtmp"][:], k.blk[:], ALU.mult, ["r_Mtmp", "blk"], [mnk])
            for q in range(2):
                if stop == "A2g":
                    continue
                h = 2 * p + q
                rs = slice(q * 64, (q + 1) * 64)
                o.copy(R["ya"][rs, p, cs], ps[5][rs, 256 + h * 64:256 + (h + 1) * 64], [P[5]], ["r_ya"], eng="act")
    for p in range(2):
        ya = R["ya"][:, p, :]
        o.mm(ps[1][:, 0:NB], k.blk[:], ya, True, True, ["blk", "r_ya"], [P[1]])
        o.stt(R["tA"][:, 0, :], ps[1][:, 0:NB], -1.0 / 64, ya, ALU.mult, ALU.add, [P[1], "r_ya"], ["r_tA"])
        o.act(R["tB"][:, 0, :], R["tA"][:, 0, :], AF.Square, ["r_tA"], ["r_tB"])
        o.mm(ps[2][:, 0:NB], k.blk[:], R["tB"][:, 0, :], True, True, ["blk", "r_tB"], [P[2]])
        rsqrt(k, R["tB"][:, 0, :], ps[2][:, 0:NB], 1.0 / 64, 64e-5, [P[2]], "r_tB")
        o.tt(R["tA"][:, 0, :], R["tA"][:, 0, :], R["tB"][:, 0, :], ALU.mult, ["r_tA", "r_tB"], ["r_tA"])
        o.ts(R["tA"][:, 0, :], R["tA"][:, 0, :], rv[:, LNW, p:p + 1], rv[:, LNB, p:p + 1], ALU.mult, ALU.add, ["r_tA", "r_rvec"], ["r_tA"])
        o.tt(R["tA"][:, 0, :], R["tA"][:, 0, :], R["bonus"][:, p, :], ALU.add, ["r_tA", "r_bonus"], ["r_tA"])
        o.tt(R["yout"][:, p, :], R["tA"][:, 0, :], R["gg"][:, p, :], ALU.mult, ["r_tA", "r_gg"], ["yout"])
        yield
    o.copy(R["youtb"][:], R["yout"][:], ["yout"], ["youtb"], eng="pool")
    if R.get("mix_dst") is not None:
        mixT, t0 = R["mix_dst"]
        o.dma("pool", mixT[0:256, t0:t0 + NB].rearrange("(t p) s -> p t s", p=128), R["youtb"][:], "mix_a", r=["youtb"], w=["mixT_a"])
    yield


TWO_PI = 2.0 * math.pi
CW1 = 6.28125
CW2 = TWO_PI - CW1


def sin_of(k, out, ang, shift, tmp, tmpi, shape_rows, r, w, tk):
    o = k.o
    o.ts(tmp, ang, 1.0 / TWO_PI, 0.5 + shift / TWO_PI, ALU.mult, ALU.add, r, [tk])
    o.copy(tmpi, tmp, [tk], [tk + "i"])
    o.copy(tmp, tmpi, [tk + "i"], [tk])
    o.stt(out, tmp, -CW1, ang, ALU.mult, ALU.add, [tk] + list(r), w)
    o.stt(out, tmp, -CW2, out, ALU.mult, ALU.add, [tk] + list(w), w)
    if shift != 0.0:
        o.ts(out, out, float(shift), None, ALU.add, None, w, w)
    o.ts(tmp, out, -math.pi, TWO_PI, ALU.is_lt, ALU.mult, w, [tk])
    o.tt(out, out, tmp, ALU.add, list(w) + [tk], w)
    o.ts(tmp, out, math.pi, -TWO_PI, ALU.is_gt, ALU.mult, w, [tk])
    o.tt(out, out, tmp, ALU.add, list(w) + [tk], w)
    o.ts(out, out, math.pi, -math.pi, ALU.min, ALU.max, w, w)
    o.act(out, out, AF.Sin, w, w)


def gelu_tanh(k, out, x, t1, r, w, tk):
    o = k.o
    o.tt(t1, x, x, ALU.mult, r, [tk])
    o.ts(t1, t1, 0.044715, 1.0, ALU.mult, ALU.add, [tk], [tk])
    o.tt(t1, t1, x, ALU.mult, [tk] + list(r), [tk])
    o.act(t1, t1, AF.Sigmoid, [tk], [tk], scale=1.5957691216057308)
    o.tt(out, x, t1, ALU.mult, [tk] + list(r), w)


def s5_setup(k, pa, l, xblk):
    o, dr, ps, P = k.o, k.dr, k.ps, k.P
    Q = {}
    Q["v"] = sb(k, pa, "s_v", [128, 3, 8])
    for nm in ("dt", "mag", "th", "c8", "s8", "qre", "qim", "den", "t8a", "t8b", "Ere", "Eim", "cre", "cim", "glr", "gli"):
        Q[nm] = sb(k, pa, "s_" + nm, [128, 8])
    Q["t8i"] = sb(k, pa, "s_t8i", [128, 8], I32)
    for nm in ("cosT", "sinT"):
        Q[nm] = sb(k, pa, "s_" + nm, [128, 8, NB])
    Q["hre"] = sb(k, pa, "s_hre", [128, 8, NB], BF16)
    Q["him"] = sb(k, pa, "s_him", [128, 8, NB], BF16)
    Q["tv"] = sb(k, pa, "s_tv", [128, NB])
    Q["B"] = sb(k, pa, "s_B", [128, 2, 8, 128], BF16)
    Q["C"] = sb(k, pa, "s_C", [128, 2, 8, 128], BF16)
    Q["vec2"] = sb(k, pa, "s_vec2", [128, 3, 2])
    Q["glu"] = sb(k, pa, "s_glu", [128, 2, 256], BF16)
    Q["ub"] = sb(k, pa, "s_ub", [128, 2, NB], BF16)
    for nm in ("w1", "w2", "w3", "w4", "w5", "w6", "w7"):
        Q[nm] = sb(k, pa, "s_" + nm, [128, NB])
    Q["wi"] = sb(k, pa, "s_wi", [128, NB], I32)
    Q["yv"] = sb(k, pa, "s_yv", [128, 2, NB])
    Q["ge"] = sb(k, pa, "s_ge", [128, 2, NB])
    Q["geb"] = sb(k, pa, "s_geb", [128, 2, NB], BF16)
    Q["sqb"] = sb(k, pa, "s_sqb", [128, 2, NB], BF16)
    Q["yo"] = sb(k, pa, "s_yo", [128, 2, NB])
    Q["yob"] = sb(k, pa, "s_yob", [128, 2, NB], BF16)
    Q["dq"] = sb(k, pa, "s_dq", [128, 2, 128])
    v = Q["v"]
    wst = xblk[:].rearrange("p a b -> p (a b)").rearrange("p (a j c) -> p a j c", a=2, j=8)
    o.dma("sp", v[:], dr["s5v"][l], "s_v", w=["s_v"])
    o.dma("sp", Q["tv"][:], dr["tvals"], "s_tv", w=["s_tv"])
    o.dma("sp", Q["vec2"][:], dr["s5vec2"][l], "s_vec2", w=["s_vec2"])
    S = ["s_small"]
    o.act(Q["dt"][:], v[:, 2, :], AF.Exp, ["s_v"], S)
    o.tt(Q["t8a"][:], v[:, 0, :], Q["dt"][:], ALU.mult, ["s_v"] + S, S)
    o.act(Q["mag"][:], Q["t8a"][:], AF.Exp, S, S)
    o.tt(Q["th"][:], v[:, 1, :], Q["dt"][:], ALU.mult, ["s_v"] + S, S)
    sin_of(k, Q["s8"][:], Q["th"][:], 0.0, Q["t8b"][:], Q["t8i"][:], 128, S, S, "s_t8")
    sin_of(k, Q["c8"][:], Q["th"][:], math.pi / 2, Q["t8b"][:], Q["t8i"][:], 128, S, S, "s_t8")
    o.tt(Q["t8a"][:], Q["mag"][:], Q["c8"][:], ALU.mult, S, S)
    o.ts(Q["t8a"][:], Q["t8a"][:], -1.0, None, ALU.add, None, S, S)
    o.tt(Q["t8b"][:], Q["mag"][:], Q["s8"][:], ALU.mult, S + ["s_t8"], ["s_t8"])
    o.tt(Q["den"][:], v[:, 0, :], v[:, 0, :], ALU.mult, ["s_v"], S)
    o.tt(Q["qre"][:], v[:, 1, :], v[:, 1, :], ALU.mult, ["s_v"], S)
    o.tt(Q["den"][:], Q["den"][:], Q["qre"][:], ALU.add, S, S)
    o.S.op("dve", lambda e: e.reciprocal(out=Q["den"][:], in_=Q["den"][:]), S, S)
    o.tt(Q["qre"][:], Q["t8a"][:], v[:, 0, :], ALU.mult, S + ["s_v"], S)
    o.tt(Q["qim"][:], Q["t8b"][:], v[:, 1, :], ALU.mult, S + ["s_v", "s_t8"], S)
    o.tt(Q["qre"][:], Q["qre"][:], Q["qim"][:], ALU.add, S, S)
    o.tt(Q["qre"][:], Q["qre"][:], Q["den"][:], ALU.mult, S, S)
    o.tt(Q["qim"][:], Q["t8b"][:], v[:, 0, :], ALU.mult, S + ["s_v", "s_t8"], S)
    o.tt(Q["cre"][:], Q["t8a"][:], v[:, 1, :], ALU.mult, S + ["s_v"], S)
    o.tt(Q["qim"][:], Q["qim"][:], Q["cre"][:], ALU.subtract, S, S)
    o.tt(Q["qim"][:], Q["qim"][:], Q["den"][:], ALU.mult, S, S)
    o.ts(Q["t8a"][:], Q["th"][:], float(NB), None, ALU.mult, None, S, S)
    sin_of(k, Q["Eim"][:], Q["t8a"][:], 0.0, Q["t8b"][:], Q["t8i"][:], 128, S, S, "s_t8")
    sin_of(k, Q["Ere"][:], Q["t8a"][:], math.pi / 2, Q["t8b"][:], Q["t8i"][:], 128, S, S, "s_t8")
    o.dma("sp", wst, dr["s5b"][l].rearrange("a j p c -> p a j c"), "xblk0", w=["xblk"])
    for jt in range(8):
        o.ts(Q["dq"][:, 0, :], k.ident[:], Q["qre"][:, jt:jt + 1], None, ALU.mult, None, ["ident"] + S, ["s_dq"])
        o.ts(Q["dq"][:, 1, :], k.ident[:], Q["qim"][:, jt:jt + 1], None, ALU.mult, None, ["ident"] + S, ["s_dq"])
        o.mm(ps[1][:, 0:128], k.ones32[:], Q["dq"][:, 0, :], True, True, ["ones32", "s_dq"], [P[1]])
        o.mm(ps[1][:, 128:256], k.ones32[:], Q["dq"][:, 1, :], True, True, ["ones32", "s_dq"], [P[1]])
        qrb, qib = ps[1][:, 0:128], ps[1][:, 128:256]
        bre, bim = wst[:, 0, jt, :], wst[:, 1, jt, :]
        w1, w2 = Q["w1"][:, 0:128], Q["w2"][:, 0:128]
        o.tt(w1, qrb, bre, ALU.mult, [P[1], "xblk"], ["s_w1"])
        o.tt(w2, qib, bim, ALU.mult, [P[1], "xblk"], ["s_w2"])
        o.tt(Q["B"][:, 0, jt, :], w1, w2, ALU.subtract, ["s_w1", "s_w2"], ["s_B"])
        o.tt(w1, qrb, bim, ALU.mult, [P[1], "xblk"], ["s_w1"])
        o.tt(w2, qib, bre, ALU.mult, [P[1], "xblk"], ["s_w2"])
        o.tt(Q["B"][:, 1, jt, :], w1, w2, ALU.add, ["s_w1", "s_w2"], ["s_B"])
    o.dma("sp", wst, dr["s5c"][l].rearrange("a j p c -> p a j c"), "xblk0", r=["s_B"], w=["xblk"])
    o.copy(Q["C"][:, 0], wst[:, 0], ["xblk"], ["s_C"])
    o.ts(Q["C"][:, 1], wst[:, 1], -1.0, None, ALU.mult, None, ["xblk"], ["s_C"])
    gst = xblk[:, 0:2, :].rearrange("p a b -> p (a b)")[:, 0:512].rearrange("p (kt n) -> p kt n", n=256)
    o.dma("sp", gst, dr["glu_w"][l].rearrange("(kt p) n -> p kt n", p=128), "xblk0", r=["s_C"], w=["xblk"])
    o.copy(Q["glu"][:], gst, ["xblk"], ["s_glu"])
    T = ["s_tab"]
    for jt in range(8):
        o.ts(Q["w5"][:], Q["tv"][:], Q["th"][:, jt:jt + 1], None, ALU.mult, None, ["s_tv"] + S, ["s_w5"])
        sin_of(k, Q["sinT"][:, jt, :], Q["w5"][:], 0.0, Q["w6"][:], Q["wi"][:], 128, ["s_w5"], T, "s_w6")
        sin_of(k, Q["cosT"][:, jt, :], Q["w5"][:], math.pi / 2, Q["w6"][:], Q["wi"][:], 128, ["s_w5"], T, "s_w6")
    o.memset(Q["cre"][:], 0.0, ["s_carry"], eng="dve")
    o.memset(Q["cim"][:], 0.0, ["s_carry"], eng="dve")
    return Q


def s5_block(k, l, i, Q, s5u, mixT, t0):
    o, ps, P = k.o, k.ps, k.P
    o.copy(Q["ub"][:], s5u[:], ["s5u"], ["s_ub"], eng="pool")
    T = ["s_tab"]
    w1, w2, w3, w4, w5, w6 = (Q[n][:] for n in ("w1", "w2", "w3", "w4", "w5", "w6"))
    for jt in range(8):
        ct = jt // 4
        pb = 1 + (jt % 2)
        o.mm(ps[pb][:, 0:NB], Q["B"][:, 0, jt, :], Q["ub"][:, ct, :], True, True, ["s_B", "s_ub"], [P[pb]])
        o.mm(ps[pb][:, NB:2 * NB], Q["B"][:, 1, jt, :], Q["ub"][:, ct, :], True, True, ["s_B", "s_ub"], [P[pb]])
        bre, bim = ps[pb][:, 0:NB], ps[pb][:, NB:2 * NB]
        cs_, sn_ = Q["cosT"][:, jt, :], Q["sinT"][:, jt, :]
        o.tt(w1, bre, cs_, ALU.mult, [P[pb]] + T, ["s_w1"])
        o.tt(w2, bim, sn_, ALU.mult, [P[pb]] + T, ["s_w2"])
        o.tt(w3, bim, cs_, ALU.mult, [P[pb]] + T, ["s_w3"])
        o.tt(w4, bre, sn_, ALU.mult, [P[pb]] + T, ["s_w4"])
        o.tt(w1, w1, w2, ALU.add, ["s_w1", "s_w2"], ["s_w1"], eng="pool")
        o.tt(w3, w3, w4, ALU.subtract, ["s_w3", "s_w4"], ["s_w3"], eng="pool")
        magb = Q["w7"][:]
        o.ts(magb, Q["tv"][:], 0.0, Q["mag"][:, jt:jt + 1], ALU.mult, ALU.add, ["s_tv", "s_small"], ["s_w7"], eng="pool")
        o.scan(w5, magb, w1, Q["cre"][:, jt:jt + 1], ["s_w7", "s_w1", "s_carry"], ["s_w5"])
        o.scan(w6, magb, w3, Q["cim"][:, jt:jt + 1], ["s_w7", "s_w3", "s_carry"], ["s_w6"])
        o.copy(Q["glr"][:, jt:jt + 1], w5[:, NB - 1:NB], ["s_w5"], ["s_gl"], eng="dve")
        o.copy(Q["gli"][:, jt:jt + 1], w6[:, NB - 1:NB], ["s_w6"], ["s_gl"], eng="dve")
        o.tt(w2, w5, cs_, ALU.mult, ["s_w5"] + T, ["s_w2"], eng="pool")
        o.tt(w4, w6, sn_, ALU.mult, ["s_w6"] + T, ["s_w4"], eng="pool")
        o.tt(Q["hre"][:, jt, :], w2, w4, ALU.subtract, ["s_w2", "s_w4"], [("s_h", jt)], eng="pool")
        o.tt(w2, w5, sn_, ALU.mult, ["s_w5"] + T, ["s_w2"], eng="pool")
        o.tt(w4, w6, cs_, ALU.mult, ["s_w6"] + T, ["s_w4"], eng="pool")
        o.tt(Q["him"][:, jt, :], w2, w4, ALU.add, ["s_w2", "s_w4"], [("s_h", jt)], eng="pool")
        yield
    o.tt(Q["t8a"][:], Q["glr"][:], Q["Ere"][:], ALU.mult, ["s_gl", "s_small"], ["s_c1"])
    o.tt(Q["t8b"][:], Q["gli"][:], Q["Eim"][:], ALU.mult, ["s_gl", "s_small"], ["s_c2"])
    o.tt(Q["cre"][:], Q["t8a"][:], Q["t8b"][:], ALU.subtract, ["s_c1", "s_c2"], ["s_carry"])
    o.tt(Q["t8a"][:], Q["glr"][:], Q["Eim"][:], ALU.mult, ["s_gl", "s_small"], ["s_c1"])
    o.tt(Q["t8b"][:], Q["gli"][:], Q["Ere"][:], ALU.mult, ["s_gl", "s_small"], ["s_c2"])
    o.tt(Q["cim"][:], Q["t8a"][:], Q["t8b"][:], ALU.add, ["s_c1", "s_c2"], ["s_carry"])
    vec2 = Q["vec2"]
    for ct in range(2):
        pb = 3
        for j in range(4):
            jt = ct * 4 + j
            o.mm(ps[pb][:, 0:NB], Q["C"][:, 0, jt, :], Q["hre"][:, jt, :], j == 0, False, ["s_C", ("s_h", jt)], [P[pb]])
            o.mm(ps[pb][:, 0:NB], Q["C"][:, 1, jt, :], Q["him"][:, jt, :], False, j == 3, ["s_C", ("s_h", jt)], [P[pb]])
        o.copy(Q["yv"][:, ct, :], ps[pb][:, 0:NB], [P[pb]], ["s_yv"], eng="act")
        o.stt(Q["yv"][:, ct, :], s5u[:, ct, :], vec2[:, 0, ct:ct + 1], Q["yv"][:, ct, :], ALU.mult, ALU.add, ["s5u", "s_vec2", "s_yv"], ["s_yv"])
        gelu_tanh(k, Q["ge"][:, ct, :], Q["yv"][:, ct, :], Q["w1"][:], ["s_yv"], ["s_ge"], "s_w1")
        o.copy(Q["geb"][:, ct, :], Q["ge"][:, ct, :], ["s_ge"], ["s_geb"], eng="pool")
        yield
    for ct in range(2):
        pb = 3
        for kt in range(2):
            o.mm(ps[pb][:, 0:NB], Q["glu"][:, kt, ct * 128:(ct + 1) * 128], Q["geb"][:, kt, :], kt == 0, kt == 1, ["s_glu", "s_geb"], [P[pb]])
        o.act(Q["w1"][:], ps[pb][:, 0:NB], AF.Sigmoid, [P[pb], "s_vec2"], ["s_w1"], bias=vec2[:, 1, ct:ct + 1])
        o.tt(Q["yv"][:, ct, :], Q["ge"][:, ct, :], Q["w1"][:], ALU.mult, ["s_ge", "s_w1"], ["s_yv"])
        yield
    branch_norm(k, Q["yv"], "s_yv", Q["sqb"], "s_sqb", Q["w2"], "s_w2", vec2[:, 2, :], "s_vec2", Q["yo"], "s_yo", Q["yob"], "s_yob", 2)
    o.dma("pool", mixT[256:512, t0:t0 + NB].rearrange("(t p) s -> p t s", p=128), Q["yob"][:], "mix_b", r=["s_yob"], w=["mixT_b"])


def branch_norm(k, y, yk, sqb, sqk, rstd, rk, g, gk, yo, yok, yob, yobk, nt, rows=128):
    o, ps, P = k.o, k.ps, k.P
    pb = 2
    for t in range(nt):
        o.act(sqb[0:rows, t, :], y[0:rows, t, :], AF.Square, [yk], [sqk])
    for t in range(nt):
        o.mm(ps[pb][0:rows, 0:y.shape[2]], k.onesb[0:rows, 0:rows], sqb[0:rows, t, :], t == 0, t == nt - 1, [sqk, "onesb"], [P[pb]])
    rsqrt(k, rstd[0:rows, :], ps[pb][0:rows, 0:y.shape[2]], 1.0 / (nt * rows), 1e-6, [P[pb]], rk, rows)
    for t in range(nt):
        o.stt(yo[0:rows, t, :], y[0:rows, t, :], g[0:rows, t:t + 1], rstd[0:rows, :], ALU.mult, ALU.mult, [yk, gk, rk], [yok])
    o.copy(yob[0:rows], yo[0:rows], [yok], [yobk], eng="pool")


def lru_setup(k, pa, l, xblk):
    o, dr = k.o, k.dr
    U = {}
    U["v"] = sb(k, pa, "l_v", [128, 9, 2])
    U["c1"] = sb(k, pa, "l_c1", [128, 2])
    U["bd"] = sb(k, pa, "l_bd", [128, 2, 2, 128], BF16)
    for nm in ("xc", "rr", "ii", "aa", "t1", "t2", "hh", "gg"):
        U[nm] = sb(k, pa, "l_" + nm, [128, NB])
    U["xcb"] = sb(k, pa, "l_xcb", [128, NB], BF16)
    U["hc"] = sb(k, pa, "l_hc", [128, 2])
    U["yd"] = sb(k, pa, "l_yd", [128, 2, NB])
    U["sqb"] = sb(k, pa, "l_sqb", [128, 2, NB], BF16)
    U["yo"] = sb(k, pa, "l_yo", [128, 2, NB])
    U["yob"] = sb(k, pa, "l_yob", [128, 2, NB], BF16)
    o.dma("sp", U["v"][:], dr["lruv"][l], "l_v", w=["l_v"])
    bst = xblk[:, 0:2, :].rearrange("p a b -> p (a b)")[:, 0:512].rearrange("p (a t c) -> p a t c", a=2, t=2)
    o.dma("sp", bst, dr["lrubd"][l].rearrange("a t p c -> p a t c"), "xblk0", w=["xblk"])
    o.copy(U["bd"][:], bst, ["xblk"], ["l_bd"])
    o.act(U["c1"][:], U["v"][:, 7, :], AF.Exp, ["l_v"], ["l_c1"], scale=-1.0)
    o.act(U["c1"][:], U["c1"][:], AF.Ln, ["l_c1", "cc"], ["l_c1"], bias=k.cc[:, CC[1.0]:CC[1.0] + 1])
    o.ts(U["c1"][:], U["c1"][:], -8.0, None, ALU.mult, None, ["l_c1"], ["l_c1"])
    o.memset(U["hc"][:], 0.0, ["l_hc"], eng="dve")
    return U


def lru_block(k, l, i, U, lrx, lrg, mixT, t0):
    o, ps, P = k.o, k.ps, k.P
    v = U["v"]
    for t in range(2):
        xc = U["xc"][:]
        o.ts(xc, lrx[:, t, 3:NB + 3], v[:, 3, t:t + 1], v[:, 4, t:t + 1], ALU.mult, ALU.add, ["lrx", "l_v"], ["l_xc"])
        for j in range(3):
            o.stt(xc, lrx[:, t, j:NB + j], v[:, j, t:t + 1], xc, ALU.mult, ALU.add, ["lrx", "l_v", "l_xc"], ["l_xc"])
        o.copy(U["xcb"][:], xc, ["l_xc"], ["l_xcb"], eng="pool")
        yield
        o.mm(ps[1][:, 0:NB], U["bd"][:, 0, t, :], U["xcb"][:], True, True, ["l_bd", "l_xcb"], [P[1]])
        o.mm(ps[1][:, NB:2 * NB], U["bd"][:, 1, t, :], U["xcb"][:], True, True, ["l_bd", "l_xcb"], [P[1]])
        o.act(U["rr"][:], ps[1][:, 0:NB], AF.Sigmoid, [P[1], "l_v"], ["l_rr"], bias=v[:, 5, t:t + 1])
        o.act(U["ii"][:], ps[1][:, NB:2 * NB], AF.Sigmoid, [P[1], "l_v"], ["l_ii"], bias=v[:, 6, t:t + 1])
        o.act(U["aa"][:], U["rr"][:], AF.Exp, ["l_rr", "l_c1"], ["l_aa"], scale=U["c1"][:, t:t + 1])
        yield
        o.tt(U["t1"][:], U["aa"][:], U["aa"][:], ALU.mult, ["l_aa"], ["l_t1"])
        o.ts(U["t1"][:], U["t1"][:], -1.0, 1.0, ALU.mult, ALU.add, ["l_t1"], ["l_t1"])
        o.ts(U["t1"][:], U["t1"][:], 0.0, None, ALU.max, None, ["l_t1"], ["l_t1"])
        o.act(U["t1"][:], U["t1"][:], AF.Sqrt, ["l_t1"], ["l_t1"])
        o.tt(U["t2"][:], U["ii"][:], xc, ALU.mult, ["l_ii", "l_xc"], ["l_t2"])
        o.tt(U["t2"][:], U["t2"][:], U["t1"][:], ALU.mult, ["l_t2", "l_t1"], ["l_t2"])
        o.scan(U["hh"][:], U["aa"][:], U["t2"][:], U["hc"][:, t:t + 1], ["l_aa", "l_t2", "l_hc"], ["l_hh"])
        o.copy(U["hc"][:, t:t + 1], U["hh"][:, NB - 1:NB], ["l_hh"], ["l_hc"], eng="dve")
        yield
        gelu_tanh(k, U["gg"][:], lrg[:, t, :], U["t1"][:], ["lrg"], ["l_gg"], "l_t1")
        o.tt(U["yd"][:, t, :], U["hh"][:], U["gg"][:], ALU.mult, ["l_hh", "l_gg"], ["l_yd"])
        yield
    o.copy(lrx[:, :, 0:3], lrx[:, :, NB:NB + 3], ["lrx"], ["lrx"], eng="dve")
    branch_norm(k, U["yd"], "l_yd", U["sqb"], "l_sqb", U["t2"], "l_t2", v[:, 8, :], "l_v", U["yo"], "l_yo", U["yob"], "l_yob", 2)
    o.dma("pool", mixT[768:1024, t0:t0 + NB].rearrange("(t p) s -> p t s", p=128), U["yob"][:], "mix_d", r=["l_yob"], w=["mixT_d"])


QSCALE = 96 ** -0.5


def mla_setup(k, pa, l, xblk, Q, U):
    o, dr = k.o, k.dr
    A = {}
    xflat = xblk[:].rearrange("p a b -> p (a b)")
    A["wuq"] = sb(k, pa, "a_wuq", [128, 2, 384], BF16)
    A["wukn"] = sb(k, pa, "a_wukn", [128, 4, 96], BF16)
    A["wukv"] = sb(k, pa, "a_wukv", [128, 256], BF16)
    A["qng"] = sb(k, pa, "a_qng", [128, 2])
    A["kvng"] = sb(k, pa, "a_kvng", [128, 1])
    A["qkhg"] = sb(k, pa, "a_qkhg", [96, 2])
    A["invf"] = sb(k, pa, "a_invf", [96, 1])
    A["rotT"] = sb(k, pa, "a_rotT", [128, 128])
    A["epe"] = sb(k, pa, "a_epe", [32, 96], BF16)
    A["posi"] = sb(k, pa, "a_posi", [96, 1], I32)
    A["posf"] = sb(k, pa, "a_posf", [96, 1])
    A["tv"] = Q["tv"]
    for nm in ("rk96", "COS", "SIN"):
        A[nm] = sb(k, pa, "a_" + nm, [96, NB])
    A["rq"] = U["t2"]
    A["ang"], A["tmpS"], A["angi"] = Q["w5"][0:96, :], Q["w6"][0:96, :], Q["wi"][0:96, :]
    A["qf"], A["rsh"], A["qn"], A["t1"], A["t2"] = (U[n][0:96, :] for n in ("xc", "rr", "ii", "aa", "t1"))
    A["qn128"] = U["ii"]
    A["sq"] = sb(k, pa, "a_sq", [128, 2, NB], BF16)
    A["sqk"] = sb(k, pa, "a_sqk", [128, NB], BF16)
    A["sqh"] = sb(k, pa, "a_sqh", [96, NB], BF16)
    A["rkt"] = sb(k, pa, "a_rkt", [128, NB // 128])
    A["qrb"] = sb(k, pa, "a_qrb", [96, 4, NB], BF16)
    A["krb"] = sb(k, pa, "a_krb", [96, 4, NB], BF16)
    A["Vt"] = sb(k, pa, "a_Vt", [128, NB // 128, 4, 65], BF16)
    for nm, src in (("qng", "qng"), ("kvng", "kvng"), ("qkhg", "qkhg")):
        o.dma("sp", A[nm][:], dr[src][l], "a_" + nm, w=["a_small"])
    o.dma("sp", A["invf"][:], dr["invf"], "a_invf", w=["a_small"])
    o.dma("sp", A["rotT"][:], dr["rotT"], "a_rotT", w=["a_small"])
    o.dma("sp", A["posi"][:], dr["pos"], "a_posi", w=["a_posi"])
    o.copy(A["posf"][:], A["posi"][:], ["a_posi"], ["a_small"])
    ste = xflat[0:32, 0:96]
    o.dma("sp", ste, dr["epe"], "xblk0", w=["xblk"])
    o.copy(A["epe"][:], ste, ["xblk"], ["a_w"])
    stq = xflat[:, 0:768].rearrange("p (kt n) -> p kt n", n=384)
    o.dma("sp", stq, dr["w_uq"][l].rearrange("(kt p) n -> p kt n", p=128), "xblk0", r=["a_w"], w=["xblk"])
    for kt in range(2):
        o.ts(A["wuq"][:, kt, :], stq[:, kt, :], A["qng"][:, kt:kt + 1], None, ALU.mult, None, ["xblk", "a_small"], ["a_w"])
    stk = xflat[:, 0:384]
    o.dma("sp", stk, dr["w_ukn"][l].rearrange("p h c -> p (h c)"), "xblk0", r=["a_w"], w=["xblk"])
    o.ts(A["wukn"][:].rearrange("p h c -> p (h c)"), stk, A["kvng"][:, 0:1], None, ALU.mult, None, ["xblk", "a_small"], ["a_w"])
    stv = xflat[:, 0:256]
    o.dma("sp", stv, dr["w_ukv"][l], "xblk0", r=["a_w"], w=["xblk"])
    o.ts(A["wukv"][:], stv, A["kvng"][:, 0:1], None, ALU.mult, None, ["xblk", "a_small"], ["a_w"])
    o.memset(A["rk96"][:], 1.0, ["a_rk96"], eng="dve")
    o.memset(A["Vt"][:], 1.0, ["a_Vt"], eng="dve")
    return A


def head_norm_rope(k, A, src, gcol, out):
    o, ps, P = k.o, k.ps, k.P
    o.act(A["sqh"][:], src, AF.Square, ["l_xc"], ["a_sqh"])
    o.mm(ps[4][0:96, 0:NB], k.onesb[0:96, 0:96], A["sqh"][:], True, True, ["onesb", "a_sqh"], [P[4]])
    rsqrt(k, A["rsh"][:], ps[4][0:96, 0:NB], 1.0 / 96, 1e-6, [P[4]], "l_rr", 96)
    o.stt(A["qn"][:], src, A["qkhg"][:, gcol:gcol + 1], A["rsh"][:], ALU.mult, ALU.mult, ["l_xc", "a_small", "l_rr"], ["l_ii"])
    o.mm(ps[4][:, NB:2 * NB], A["rotT"][:], A["qn128"][:], True, True, ["a_small", "l_ii"], [P[4]])
    o.tt(A["t1"][:], A["qn"][:], A["COS"][:], ALU.mult, ["l_ii", "a_cs"], ["l_aa"])
    o.tt(A["t2"][:], ps[4][0:96, NB:2 * NB], A["SIN"][:], ALU.mult, [P[4], "a_cs"], ["l_t1"])
    o.tt(out, A["t1"][:], A["t2"][:], ALU.add, ["l_aa", "l_t1"], ["a_out"])


def mla_block(k, l, i, A, qab, kvab, kpeb, t0):
    o, ps, P = k.o, k.ps, k.P
    ntt = NB // 128
    rms_stats(k, [(qab[:, kt, :], "qab") for kt in range(2)], 256.0, 1e-6, ps[1][:, 0:NB], A["rq"][:],
              [(A["sq"][:, kt, :], "a_sq") for kt in range(2)], None, P[1], "l_t2")
    o.act(A["sqk"][:], kvab[:], AF.Square, ["kvab"], ["a_sqk"])
    o.mm(ps[2][:, 0:NB], k.onesb[:], A["sqk"][:], True, True, ["onesb", "a_sqk"], [P[2]])
    for tt in range(ntt):
        o.mm(ps[2][:, NB + tt:NB + tt + 1], A["sqk"][:, tt * 128:(tt + 1) * 128], k.onesb[:, 0:1], True, True, ["onesb", "a_sqk"], [P[2]])
    rsqrt(k, A["rk96"][0:64, :], ps[2][0:64, 0:NB], 1.0 / 128, 1e-6, [P[2]], "a_rk96", 64)
    rsqrt(k, A["rkt"][:], ps[2][:, NB:NB + ntt], 1.0 / 128, 1e-6, [P[2]], "a_rkt", 128)
    yield
    o.ts(A["ang"][:], A["tv"][0:96, :], A["posf"][:, 0:1], None, ALU.add, None, ["s_tv", "a_small"], ["s_w5"])
    o.ts(A["ang"][:], A["ang"][:], float(t0), A["invf"][:, 0:1], ALU.add, ALU.mult, ["s_w5", "a_small"], ["s_w5"])
    sin_of(k, A["SIN"][:], A["ang"][:], 0.0, A["tmpS"][:], A["angi"][:], 96, ["s_w5"], ["a_cs"], "s_w6")
    sin_of(k, A["COS"][:], A["ang"][:], math.pi / 2, A["tmpS"][:], A["angi"][:], 96, ["s_w5"], ["a_cs"], "s_w6")
    yield
    for h in range(4):
        for kt in range(2):
            o.mm(ps[3][0:96, 0:NB], A["wuq"][:, kt, h * 96:(h + 1) * 96], qab[:, kt, :], kt == 0, kt == 1, ["a_w", "qab"], [P[3]])
        o.tt(A["qf"][:], ps[3][0:96, 0:NB], A["rq"][0:96, :], ALU.mult, [P[3], "l_t2"], ["l_xc"])
        head_norm_rope(k, A, A["qf"][:], 0, A["qrb"][:, h, :])
        yield
        o.mm(ps[3][0:96, NB:2 * NB], A["wukn"][:, h, :], kvab[:], True, False, ["a_w", "kvab"], [P[3]])
        o.mm(ps[3][0:96, NB:2 * NB], A["epe"][:], kpeb[:], False, True, ["a_w", "kpeb"], [P[3]])
        o.tt(A["qf"][:], ps[3][0:96, NB:2 * NB], A["rk96"][:], ALU.mult, [P[3], "a_rk96"], ["l_xc"])
        head_norm_rope(k, A, A["qf"][:], 1, A["krb"][:, h, :])
        yield
    for tt in range(ntt):
        o.mm(ps[1][:, 0:256], kvab[:, tt * 128:(tt + 1) * 128], A["wukv"][:], True, True, ["kvab", "a_w"], [P[1]])
        o.act(A["Vt"][:, tt, :, 0:64], ps[1][:, 0:256].rearrange("p (h c) -> p h c", c=64), AF.Identity, [P[1], "a_rkt"], ["a_Vt"],
              scale=A["rkt"][:, tt:tt + 1])
    o.dma("pool", k.qs[:, :, t0:t0 + NB], A["qrb"][:], "st_q", r=["a_out"], w=["qs"])
    o.dma("pool", k.ks[:, :, t0:t0 + NB], A["krb"][:], "st_k", r=["a_out"], w=["ks"])
    o.dma("pool", k.vs[t0 // 128:t0 // 128 + ntt].rearrange("t p c -> p t c"), A["Vt"][:].rearrange("p t h c -> p t (h c)"), "st_v", r=["a_Vt"], w=["vs"])


QB = 256


def phaseB(k, l, xin, x1T, stop):
    nc, S_, dr, S, L, o = k.nc, k.S_, k.dr, k.S, k.L, k.o
    ps, P = k.ps, k.P
    nqb = S // QB
    with ExitStack() as pb_:
        Kall = sb(k, pb_, "Kall", [96, 4, S], BF16)
        Vall = sb(k, pb_, "Vall", [128, S // 128, 260], BF16)
        qblk = sb(k, pb_, "qblk", [96, 4, QB], BF16)
        PT = [sb(k, pb_, f"PT{i}", [128, QB], BF16) for i in range(2)]
        Oext = sb(k, pb_, "Oext", [128, QB])
        rden = sb(k, pb_, "rden", [64, QB])
        yc = sb(k, pb_, "yc", [64, 4, QB])
        ycsq = sb(k, pb_, "ycsq", [64, 4, QB], BF16)
        ycn = sb(k, pb_, "ycn", [64, 4, QB])
        ycnb = sb(k, pb_, "ycnb", [64, 4, QB], BF16)
        rstc = sb(k, pb_, "rstc", [64, QB])
        mixb = sb(k, pb_, "mixb", [128, 6, QB], BF16)
        wo = sb(k, pb_, "wo", [128, 6, D], BF16)
        woc = sb(k, pb_, "woc", [64, 4, D], BF16)
        xblk = sb(k, pb_, "xblkB", [128, 8, QB])
        sq2 = sb(k, pb_, "sq2", [128, 8, QB], BF16)
        rst2 = sb(k, pb_, "rst2", [128, QB])
        xt2 = sb(k, pb_, "xt2", [128, QB])
        h2 = sb(k, pb_, "h2", [128, 8, QB])
        h2b = sb(k, pb_, "h2b", [128, 8, QB], BF16)
        wr = sb(k, pb_, "wr", [128, 8, 36])
        brt = sb(k, pb_, "brt", [128, 36])
        sel65 = sb(k, pb_, "sel65", [128, 64])
        bngc = sb(k, pb_, "bngc", [64, 4])
        lg = sb(k, pb_, "lg", [128, 36])
        rt = {nm: sb(k, pb_, "rt_" + nm, [128, w_]) for nm, w_ in (("m4", 1), ("nm4", 1), ("e4", 4), ("s4", 1), ("gp", 1), ("ohg", 4), ("sel", 8),
                                                                  ("l1", 1), ("oh1", 8), ("sel2", 8), ("l2", 1), ("oh2", 8), ("nl1", 1), ("d", 1),
                                                                  ("t", 1), ("w1", 1), ("w2", 1), ("ge", 8))}
        gates = sb(k, pb_, "gates", [128, 32])
        gT = sb(k, pb_, "gTb", [32, QB])
        wst = h2[:].rearrange("p a b -> p (a b)")
        wov = dr["w_out"][l]
        rows = [0, 128, 256, 384, 768, 896]
        for t, r0 in enumerate(rows):
            for hf in range(1):
                o.dma("sp", wst[:, 0:1024], wov[r0:r0 + 128, :], "h2st", w=["h2"])
                o.copy(wo[:, t, :], wst[:, 0:1024], ["h2"], ["wo"], eng=("dve" if t % 2 == 0 else "pool"))
        for h in range(4):
            o.dma("sp", wst[0:64, 0:1024], wov[512 + h * 64:512 + (h + 1) * 64, :], "h2st", w=["h2"])
            o.copy(woc[:, h, :], wst[0:64, 0:1024], ["h2"], ["wo"], eng="dve")
        o.dma("sp", wr[:], dr["wr"][l].rearrange("(kk p) n -> p kk n", p=128), "wr", w=["wr"])
        o.dma("sp", brt[:], dr["br"][l], "brt", w=["wr"])
        o.dma("sp", sel65[:], dr["sel65"], "sel65", w=["sel65"])
        o.memset(Oext[:], 0.0, ["Oext"], eng="dve")
        o.dma("sp", bngc[:], dr["bng_c"][l], "bngc", w=["bngc"])
        xv = xin.rearrange("(kk p) s -> p kk s", p=128)
        x1v = x1T.rearrange("(kk p) s -> p kk s", p=128)
        h2v = k.h2T.rearrange("(kk p) s -> p kk s", p=128)
        for qb in range(nqb):
            t0 = qb * QB
            nkt = QB // 128
            o.dma("sp", Kall[:, :, t0:t0 + QB], k.ks[:, :, t0:t0 + QB], "ldK", r=["ks"], w=["Kall"])
            o.dma("sp", Vall[:, t0 // 128:t0 // 128 + nkt, :], k.vs[t0 // 128:t0 // 128 + nkt].rearrange("t p c -> p t c"), "ldV", r=["vs"], w=["Vall"])
            o.dma("sp", qblk[:], k.qs[:, :, t0:t0 + QB], "ldQ", r=["qs"], w=["qblk"])
            o.dma("pool", mixb[:, 0:2, :], k.mixT[0:256, t0:t0 + QB].rearrange("(t p) s -> p t s", p=128), "ldm", r=["mixT_a"], w=["mixb"])
            o.dma("pool", mixb[:, 2:4, :], k.mixT[256:512, t0:t0 + QB].rearrange("(t p) s -> p t s", p=128), "ldm", r=["mixT_b"], w=["mixb"])
            o.dma("pool", mixb[:, 4:6, :], k.mixT[768:1024, t0:t0 + QB].rearrange("(t p) s -> p t s", p=128), "ldm", r=["mixT_d"], w=["mixb"])
            o.dma("sp", xblk[:], xv[:, :, t0:t0 + QB], "xblkB", w=["xblkB"])
            nk_tot = (t0 + QB) // 128
            units = [(h, kt) for h in range(4) for kt in range(nk_tot)]

            def s_stage(iu):
                h, kt = units[iu]
                a = kt - (t0 // 128)
                c0 = max(a, 0) * 128
                sb_ = 1 + (iu % 2)
                o.mm(ps[sb_][:, c0:QB], Kall[:, h, kt * 128:(kt + 1) * 128], qblk[:, h, c0:QB], True, True, ["Kall", "qblk"], [P[sb_]])

            s_stage(0)
            for iu, (h, kt) in enumerate(units):
                if iu + 1 < len(units):
                    s_stage(iu + 1)
                a = kt - (t0 // 128)
                c0 = max(a, 0) * 128
                sb_ = 1 + (iu % 2)
                pt = PT[iu % 2]
                ptk = f"PT{iu % 2}"
                o.act(pt[:, c0:QB], ps[sb_][:, c0:QB], AF.Exp, [P[sb_]], [ptk], scale=QSCALE)
                if a >= 0:
                    o.memset(pt[64:128, c0:c0 + 64], 0.0, [ptk], eng="pool")
                o.mm(ps[3][0:65, c0:QB], Vall[:, kt, h * 65:(h + 1) * 65], pt[:, c0:QB], kt == 0, kt == nk_tot - 1, ["Vall", ptk], [P[3]])
                if kt == nk_tot - 1:
                    o.copy(Oext[0:65, :], ps[3][0:65, 0:QB], [P[3]], ["Oext"], eng="dve")
                    o.mm(ps[4][0:64, 0:QB], sel65[:], Oext[:], True, True, ["sel65", "Oext"], [P[4]])
                    o.S.op("dve", lambda e: e.reciprocal(out=rden[:], in_=ps[4][0:64, 0:QB]), [P[4]], ["rden"])
                    o.tt(yc[:, h, :], Oext[0:64, :], rden[:], ALU.mult, ["Oext", "rden"], ["yc"])
            branch_norm(k, yc, "yc", ycsq, "ycsq", rstc, "rstc", bngc, "bngc", ycn, "ycn", ycnb, "ycnb", 4, rows=64)
            if stop == "B1":
                dbg = dr["dbg"]
                o.dma("sp", dbg[512:768, t0:t0 + QB].rearrange("(h p) s -> p h s", p=64), ycn[:], "dbg", r=["ycn"], w=["dbg"])
            for f in range(8):
                pb = 5 + (f % 2)
                fc = slice(f * 128, (f + 1) * 128)
                for t in range(6):
                    o.mm(ps[pb][:, 0:QB], wo[:, t, fc], mixb[:, t, :], t == 0, False, ["wo", "mixb"], [P[pb]])
                for h in range(4):
                    o.mm(ps[pb][:, 0:QB], woc[:, h, fc], ycnb[:, h, :], False, h == 3, ["wo", "ycnb"], [P[pb]])
                o.act(xt2[:], ps[pb][:, 0:QB], AF.Identity, [P[pb], "mods"], ["xt2"], scale=k.mods[:, l, 16 + f:17 + f])
                o.tt(xblk[:, f, :], xt2[:], xblk[:, f, :], ALU.add, ["xt2", "xblkB"], ["xblkB"])
            o.dma("pool", x1v[:, :, t0:t0 + QB], xblk[:], "stx1", r=["xblkB"], w=["x1T"])
            if stop == "B1":
                dbg = dr["dbg"]
                o.dma("sp", dbg[1024:2048, t0:t0 + QB].rearrange("(t p) s -> p t s", p=128), xblk[:], "dbg", r=["xblkB"], w=["dbg"])
            rms_stats(k, [(xblk[:, kk, :], "xblkB") for kk in range(8)], 1024.0, 1e-6, ps[7][:, 0:QB], rst2[:],
                      [(sq2[:, kk, :], "sq2") for kk in range(8)], None, P[7], "rst2")
            for kk in range(8):
                o.tt(xt2[:], xblk[:, kk, :], rst2[:], ALU.mult, ["xblkB", "rst2"], ["xt2"])
                o.act(h2[:, kk, :], xt2[:], AF.Identity, ["xt2", "g2s", "mods"], ["h2"], bias=k.mods[:, l, 24 + kk:25 + kk], scale=k.g2s[:, l, kk:kk + 1])
            o.copy(h2b[:], h2[:], ["h2"], ["h2b"], eng="pool")
            o.dma("pool", h2v[:, :, t0:t0 + QB], h2b[:], "sth2", r=["h2b"], w=["h2T"])
            for tq in range(QB // 128):
                tsl = slice(tq * 128, (tq + 1) * 128)
                for kk in range(8):
                    o.mm(ps[7][:, 256:292], h2[:, kk, tsl], wr[:, kk, :], kk == 0, kk == 7, ["h2", "wr"], [P[7]])
                o.tt(lg[:], ps[7][:, 256:292], brt[:], ALU.add, [P[7], "wr"], ["lg"])
                route(k, lg, rt, gates)
                o.tr(ps[7][0:32, 384:512], gates[:], k.ident[:], ["gates", "ident"], [P[7]])
                o.copy(gT[:, tsl], ps[7][0:32, 384:512], [P[7]], ["gTb"], eng="dve")
            o.dma("pool", k.gT[:, t0:t0 + QB], gT[:], "stg", r=["gTb"], w=["gT"])
            if stop == "B1":
                o.dma("sp", dr["dbg"][0:32, t0:t0 + QB], gT[:], "dbg", r=["gTb"], w=["dbg"])
        S_.barrier()
        S_.flush(k.st)


def route(k, lg, rt, gates):
    o = k.o
    R_ = ["rt"]
    red = lambda out, in_, op: o.S.op("dve", lambda e: e.tensor_reduce(out=out, in_=in_, axis=AX.X, op=op), ["lg"] + R_, R_)
    red(rt["m4"][:], lg[:, 0:4], ALU.max)
    o.ts(rt["nm4"][:], rt["m4"][:], -1.0, None, ALU.mult, None, R_, R_)
    o.act(rt["e4"][:], lg[:, 0:4], AF.Exp, ["lg"] + R_, R_, bias=rt["nm4"][:, 0:1])
    red(rt["s4"][:], rt["e4"][:], ALU.add)
    o.S.op("dve", lambda e: e.reciprocal(out=rt["gp"][:], in_=rt["s4"][:]), R_, R_)
    o.ts(rt["ohg"][:], lg[:, 0:4], rt["m4"][:, 0:1], None, ALU.is_equal, None, ["lg"] + R_, R_)
    for g in range(4):
        le = lg[:, 4 + 8 * g:12 + 8 * g]
        if g == 0:
            o.ts(rt["sel"][:], le, rt["ohg"][:, 0:1], None, ALU.mult, None, ["lg"] + R_, R_)
        else:
            o.stt(rt["sel"][:], le, rt["ohg"][:, g:g + 1], rt["sel"][:], ALU.mult, ALU.add, ["lg"] + R_, R_)
    red(rt["l1"][:], rt["sel"][:], ALU.max)
    o.ts(rt["oh1"][:], rt["sel"][:], rt["l1"][:, 0:1], None, ALU.is_equal, None, R_, R_)
    o.stt(rt["sel2"][:], rt["oh1"][:], -1e30, rt["sel"][:], ALU.mult, ALU.add, R_, R_)
    red(rt["l2"][:], rt["sel2"][:], ALU.max)
    o.ts(rt["oh2"][:], rt["sel2"][:], rt["l2"][:, 0:1], None, ALU.is_equal, None, R_, R_)
    o.ts(rt["nl1"][:], rt["l1"][:], -1.0, None, ALU.mult, None, R_, R_)
    o.act(rt["d"][:], rt["l2"][:], AF.Exp, R_, R_, bias=rt["nl1"][:, 0:1])
    o.ts(rt["t"][:], rt["d"][:], 1.0, None, ALU.add, None, R_, R_)
    o.S.op("dve", lambda e: e.reciprocal(out=rt["t"][:], in_=rt["t"][:]), R_, R_)
    o.tt(rt["w1"][:], rt["gp"][:], rt["t"][:], ALU.mult, R_, R_)
    o.tt(rt["w2"][:], rt["w1"][:], rt["d"][:], ALU.mult, R_, R_)
    o.ts(rt["ge"][:], rt["oh1"][:], rt["w1"][:, 0:1], None, ALU.mult, None, R_, R_)
    o.stt(rt["ge"][:], rt["oh2"][:], rt["w2"][:, 0:1], rt["ge"][:], ALU.mult, ALU.add, R_, R_)
    for g in range(4):
        o.ts(gates[:, 8 * g:8 * g + 8], rt["ge"][:], rt["ohg"][:, g:g + 1], None, ALU.mult, None, R_, ["gates"])


def phaseW(k, l):
    nc, S_, dr, o = k.nc, k.S_, k.dr, k.o
    NE = dr["w1"].shape[1]
    with ExitStack() as pw:
        stg = [sb(k, pw, f"wstg{i}", [128, 4096]) for i in range(2)]
        wb = [sb(k, pw, f"wbf{i}", [128, 4096], BF16) for i in range(2)]
        it = 0
        for e in range(NE):
            for nm, dst, kk in (("w1", k.w1b, 8), ("w3", k.w3b, 8), ("w2", k.w2b, 4)):
                i2 = it % 2
                n = 4096 // kk
                src = dr[nm][l, e].rearrange("(kk p) n -> p kk n", p=128)
                o.dma("sp", stg[i2][:].rearrange("p (kk n) -> p kk n", kk=kk), src, f"wstg{i2}", w=[f"wstg{i2}"])
                o.copy(wb[i2][:], stg[i2][:], [f"wstg{i2}"], [f"wbf{i2}"], eng=("dve", "pool", "act")[it % 3])
                o.dma("pool", dst[e], wb[i2][:], f"wbst{i2}", r=[f"wbf{i2}"], w=["wscr"])
                it += 1
        S_.barrier()
        S_.flush(k.st)


def phaseC(k, l, x1T, xoutT, stop):
    nc, S_, dr, S, o = k.nc, k.S_, k.dr, k.S, k.o
    ps, P = k.ps, k.P
    NE = dr["w1"].shape[1]
    TB = min(1024, S)
    nh = TB // 512
    with ExitStack() as pc:
        h2b = sb(k, pc, "c_h2b", [128, 8, TB], BF16)
        acc = sb(k, pc, "c_acc", [128, 8, TB])
        gbc = [sb(k, pc, f"c_gbc{i}", [128, TB]) for i in range(2)]
        W1 = [sb(k, pc, f"c_w1_{i}", [128, 8, 512], BF16) for i in range(2)]
        W3 = [sb(k, pc, f"c_w3_{i}", [128, 8, 512], BF16) for i in range(2)]
        W2 = [sb(k, pc, f"c_w2_{i}", [128, 4, 1024], BF16) for i in range(2)]
        hid = [sb(k, pc, f"c_hid{i}", [128, 4, 512], BF16) for i in range(2)]
        sil = [sb(k, pc, f"c_sil{i}", [128, 512]) for i in range(2)]
        t3 = [sb(k, pc, f"c_t3{i}", [128, 512]) for i in range(2)]
        xr = [sb(k, pc, f"c_xr{i}", [128, TB]) for i in range(2)]
        sel32 = sb(k, pc, "c_sel32", [128, 32, 128])
        gt128 = sb(k, pc, "c_gt128", [128, TB])
        o.dma("sp", sel32[:], dr["sel32"], "c_sel32", w=["sel32"])
        o.memset(gt128[:], 0.0, ["gt128"], eng="dve")
        h2v = k.h2T.rearrange("(kk p) s -> p kk s", p=128)
        x1v = x1T.rearrange("(kk p) s -> p kk s", p=128)
        xov = xoutT.rearrange("(kk p) s -> p kk s", p=128)
        for tb in range(S // TB):
            t0 = tb * TB
            o.dma("sp", h2b[:], h2v[:, :, t0:t0 + TB], "c_h2b", r=["h2T"], w=["c_h2b"])
            o.dma("sp", gt128[0:32, :], k.gT[:, t0:t0 + TB], "c_gt128", r=["gT"], w=["gt128"])
            units = [(e, hf) for e in range(NE) for hf in range(nh)]

            def stage1(iu):
                e, hf = units[iu]
                i2 = e % 2
                wk = f"c_w{i2}"
                j2 = iu % 2
                if hf == 0:
                    o.dma("sp", W1[i2][:].rearrange("p a b -> p (a b)"), k.w1b[e], f"c_w1_{i2}", r=["wscr"], w=[wk + "a"])
                    o.dma("sp", W3[i2][:].rearrange("p a b -> p (a b)"), k.w3b[e], f"c_w3_{i2}", r=["wscr"], w=[wk + "b"])
                    o.dma("sp", W2[i2][:].rearrange("p a b -> p (a b)"), k.w2b[e], f"c_w2_{i2}", r=["wscr"], w=[wk + "c"])
                    for h_ in range(nh):
                        o.mm(ps[h_][:, :], sel32[:, e, :], gt128[:, h_ * 512:(h_ + 1) * 512], True, True, ["sel32", "gt128"], [P[h_]])
                        o.copy(gbc[i2][:, h_ * 512:(h_ + 1) * 512], ps[h_][:, :], [P[h_]], [f"c_gbc{i2}"], eng="act")
                hs = slice(hf * 512, (hf + 1) * 512)
                for ht in range(4):
                    pa_, pb_ = ht % 2, 2 + ht % 2
                    hc = slice(ht * 128, (ht + 1) * 128)
                    for kk in range(8):
                        o.mm(ps[pa_][:, :], W1[i2][:, kk, hc], h2b[:, kk, hs], kk == 0, kk == 7, [wk + "a", "c_h2b"], [P[pa_]])
                    for kk in range(8):
                        o.mm(ps[pb_][:, :], W3[i2][:, kk, hc], h2b[:, kk, hs], kk == 0, kk == 7, [wk + "b", "c_h2b"], [P[pb_]])
                    s2 = ht % 2
                    o.act(sil[s2][:], ps[pa_][:, :], AF.Silu, [P[pa_]], [f"c_sil{s2}"])
                    o.tt(t3[s2][:], ps[pb_][:, :], gbc[i2][:, hs], ALU.mult, [P[pb_], f"c_gbc{i2}"], [f"c_t3{s2}"])
                    o.tt(hid[j2][:, ht, :], sil[s2][:], t3[s2][:], ALU.mult, [f"c_sil{s2}", f"c_t3{s2}"], [(f"c_hid{j2}", ht)],
                         eng=("pool" if ht % 2 == 0 else "dve"))

            def stage2(iu):
                e, hf = units[iu]
                i2 = e % 2
                wk = f"c_w{i2}"
                j2 = iu % 2
                hs = slice(hf * 512, (hf + 1) * 512)
                for f in range(8):
                    po = 4 + f % 4
                    fc = slice(f * 128, (f + 1) * 128)
                    for ht in range(4):
                        o.mm(ps[po][:, :], W2[i2][:, ht, fc], hid[j2][:, ht, :], ht == 0, ht == 3, [wk + "c", (f"c_hid{j2}", ht)], [P[po]])
                    if e == 0:
                        o.copy(acc[:, f, hs], ps[po][:, :], [P[po]], [("c_acc", f)], eng="act")
                    else:
                        o.tt(acc[:, f, hs], ps[po][:, :], acc[:, f, hs], ALU.add, [P[po], ("c_acc", f)], [("c_acc", f)])

            stage1(0)
            for iu in range(len(units)):
                if iu + 1 < len(units):
                    stage1(iu + 1)
                stage2(iu)
            for f in range(8):
                i2 = f % 2
                o.dma("sp", xr[i2][:], x1v[:, f, t0:t0 + TB], f"c_xr{i2}", r=["x1T"], w=[f"c_xr{i2}"])
                o.stt(xr[i2][:], acc[:, f, :], k.mods[:, l, 40 + f:41 + f], xr[i2][:], ALU.mult, ALU.add, [("c_acc", f), "mods", f"c_xr{i2}"], [f"c_xr{i2}"])
                o.dma("pool", xov[:, f, t0:t0 + TB], xr[i2][:], f"c_xo{i2}", r=[f"c_xr{i2}"], w=["xout%d" % l])
                if stop == "C1":
                    o.dma("sp", dr["dbg"][f * 128:(f + 1) * 128, t0:t0 + TB], xr[i2][:], "dbg", r=[f"c_xr{i2}"], w=["dbg"])
        S_.barrier()
        S_.flush(k.st)


def make_shapes(sh, cst, pc):
    shapes = {}
    for d_ in (sh, cst, pc):
        for kname, v in d_.items():
            shapes[kname] = (v.shape, I32 if v.dtype == np.int32 else F32)
    return shapes


def run(inputs, S, L, stop=None, dbg=None):
    sh, cst, per_core = prep_inputs(inputs, S)
    sh = {kk: (v[:L] if v.shape[0] == inputs["ada_w"].shape[0] and kk not in () else v) for kk, v in sh.items()}
    shapes = make_shapes(sh, cst, per_core[0])
    nc = build(S, L, shapes, stop=stop, dbg=dbg)
    in_maps = []
    for b in range(NCORES):
        m = dict(sh)
        m.update(cst)
        m.update(per_core[b])
        in_maps.append(m)
    res = run_bass_kernel_spmd(nc, in_maps, core_ids=list(range(NCORES)))
    return res


def kernel(**inputs):
    S = inputs["x"].shape[1]
    L = inputs["ada_w"].shape[0]
    res = run(inputs, S, L)
    out = np.stack([np.ascontiguousarray(res.results[b]["outT"].T) for b in range(NCORES)], axis=0)
    return out.astype(np.float32)
```

```python
import math
import numpy as np
from contextlib import ExitStack
import concourse.bass as bass
import concourse.mybir as mybir
from concourse.bass_utils import run_bass_kernel_spmd

F32 = mybir.dt.float32
BF16 = mybir.dt.bfloat16
I32 = mybir.dt.int32
AF = mybir.ActivationFunctionType
ALU = mybir.AluOpType
AX = mybir.AxisListType

D = 1024
DIN = 2080
NCORES = 8
ENGS = ["pe", "act", "dve", "pool", "sp"]


class Sched:
    def __init__(self, nc):
        self.nc = nc
        self.ops = {e: [] for e in ENGS}
        self.count = {e: 0 for e in ENGS}
        self.seen = {e: {} for e in ENGS}
        self.last_write = {}
        self.readers = {}
        self.dma_count = {}
        self.sem_names = list(ENGS)
        self.sems = {}
        self.rr = 0

    def _deps(self, eng, reads, writes):
        deps = {}

        def add(tok):
            if tok is not None and deps.get(tok[0], 0) < tok[1]:
                deps[tok[0]] = tok[1]

        for r in reads:
            add(self.last_write.get(r))
        for w in writes:
            add(self.last_write.get(w))
            for t in self.readers.get(w, ()):
                add(t)
        out = []
        for k, v in deps.items():
            if eng == "pe" and k == "pe":
                continue
            if self.seen[eng].get(k, 0) < v:
                self.seen[eng][k] = v
                out.append((k, v))
        return out

    def _commit(self, tok, reads, writes):
        for r in reads:
            lst = self.readers.setdefault(r, [])
            lst[:] = [t for t in lst if t[0] != tok[0]]
            lst.append(tok)
        for w in writes:
            self.last_write[w] = tok
            self.readers[w] = []

    def op(self, eng, fn, reads=(), writes=()):
        waits = self._deps(eng, reads, writes)
        self.count[eng] += 1
        tok = (eng, self.count[eng])
        self.ops[eng].append((waits, fn, (eng, 1)))
        self._commit(tok, reads, writes)
        return tok

    def dma(self, eng, fn, key, reads=(), writes=()):
        waits = self._deps(eng, reads, writes)
        k = ("dma", key)
        if k not in self.dma_count:
            self.dma_count[k] = 0
            self.sem_names.append(k)
        self.dma_count[k] += 16
        tok = (k, self.dma_count[k])
        self.ops[eng].append((waits, fn, (k, 16)))
        self._commit(tok, reads, writes)
        return tok

    def barrier(self):
        toks = [(e, self.count[e]) for e in ENGS if self.count[e] > 0]
        toks += [(k, v) for k, v in self.dma_count.items()]
        for e in ENGS:
            waits = []
            for k, v in toks:
                if k != e and self.seen[e].get(k, 0) < v:
                    self.seen[e][k] = v
                    waits.append((k, v))
            if waits:
                self.ops[e].append((waits, None, None))

    def flush(self, stack):
        nc = self.nc
        for k in self.sem_names:
            if k not in self.sems:
                nm = "s_" + "_".join(str(x) for x in (k if isinstance(k, tuple) else (k,)))
                self.sems[k] = stack.enter_context(nc.semaphore(nm))
        sems = self.sems
        ops = self.ops
        self.ops = {e: [] for e in ENGS}
        with nc.Block() as block:
            def replay(engname):
                def body(e):
                    for waits, fn, inc in ops[engname]:
                        for k, v in waits:
                            e.wait_ge(sems[k], v)
                        if fn is not None:
                            fn(e).then_inc(sems[inc[0]], inc[1])
                return body

            block.tensor(replay("pe"))
            block.scalar(replay("act"))
            block.vector(replay("dve"))
            block.gpsimd(replay("pool"))
            block.sync(replay("sp"))


def _pk(v, p=128):
    v = np.asarray(v)
    n = v.shape[-1] // p
    return np.ascontiguousarray(np.swapaxes(v.reshape(v.shape[:-1] + (n, p)), -1, -2))


def prep_inputs(inp, S):
    L = inp["ada_w"].shape[0]
    f32 = np.float32
    sh = {}
    sh["ada_w"] = np.ascontiguousarray(inp["ada_w"], f32)
    sh["ada_b"] = _pk(inp["ada_b"])
    sh["n1g"] = _pk(inp["norm1_g"])
    sh["n2g"] = _pk(inp["norm2_g"])
    sh["w_in"] = np.ascontiguousarray(inp["w_in"], f32)
    sh["w_out"] = np.ascontiguousarray(inp["w_out"], f32)
    sh["mu"] = _pk(inp["rwkv_mu"])
    rv = np.stack([inp["rwkv_w0"], inp["rwkv_a0"], inp["rwkv_k_k"], inp["rwkv_k_a"],
                   inp["rwkv_r_k"].reshape(L, 256), inp["rwkv_ln_w"], inp["rwkv_ln_b"]], axis=1)
    sh["rvec"] = np.ascontiguousarray(np.transpose(_pk(rv), (0, 2, 1, 3)))
    sh["lora"] = np.ascontiguousarray(np.concatenate([inp["rwkv_w2"], inp["rwkv_a2"], inp["rwkv_g2"]], axis=1))

    def s5vec(a):
        return np.ascontiguousarray(a.reshape(L, 8, 2, 64).transpose(0, 2, 3, 1).reshape(L, 128, 8))
    ldt = np.broadcast_to(inp["s5_log_dt"][:, :, None], (L, 16, 64))
    sh["s5v"] = np.ascontiguousarray(np.stack([s5vec(inp["s5_lambda_re"]), s5vec(inp["s5_lambda_im"]), s5vec(ldt)], axis=2))
    bpad = np.zeros((L, 2, 8, 128, 128), f32)
    cpad = np.zeros((L, 2, 8, 128, 128), f32)
    for ri, (bb, cc) in enumerate([(inp["s5_b_re"], inp["s5_c_re"]), (inp["s5_b_im"], inp["s5_c_im"])]):
        for g in range(16):
            jt, q = g // 2, g % 2
            r0 = (g % 8) * 16
            bpad[:, ri, jt, r0:r0 + 16, q * 64:(q + 1) * 64] = np.transpose(bb[:, g], (0, 2, 1))
            cpad[:, ri, jt, q * 64:(q + 1) * 64, r0:r0 + 16] = np.transpose(cc[:, g], (0, 2, 1))
    sh["s5b"] = bpad
    sh["s5c"] = cpad
    sv = np.stack([inp["s5_d"], inp["s5_glu_b"], inp["branch_norm_g"][:, 0]], axis=1)
    sh["s5vec2"] = np.ascontiguousarray(np.transpose(_pk(sv), (0, 2, 1, 3)))
    sh["glu_w"] = np.ascontiguousarray(inp["s5_glu_w"], f32)
    sh["qng"] = _pk(inp["mla_q_norm_g"])
    sh["w_uq"] = np.ascontiguousarray(inp["mla_w_uq"], f32)
    sh["kvng"] = _pk(inp["mla_kv_norm_g"])
    wkv = inp["mla_w_ukv"].reshape(L, 128, 4, 128)
    kn = np.zeros((L, 128, 4, 96), f32)
    kn[..., :64] = wkv[..., :64]
    sh["w_ukn"] = kn
    sh["w_ukv"] = np.ascontiguousarray(wkv[..., 64:].reshape(L, 128, 256))
    sh["qkhg"] = np.ascontiguousarray(np.stack([inp["mla_q_head_g"], inp["mla_k_head_g"]], axis=2))
    cw = inp["lru_conv_w"]
    lv = np.concatenate([cw, inp["lru_conv_b"][:, None], inp["lru_b_a"][:, None], inp["lru_b_x"][:, None],
                         inp["lru_lambda"][:, None], inp["branch_norm_g"][:, 2][:, None]], axis=1)
    sh["lruv"] = np.ascontiguousarray(np.transpose(_pk(lv), (0, 2, 1, 3)))
    bd = np.zeros((L, 2, 2, 128, 128), f32)
    for wi, wmat in enumerate([inp["lru_w_a"], inp["lru_w_x"]]):
        for n in range(4):
            t, q = n // 2, n % 2
            bd[:, wi, t, q * 64:(q + 1) * 64, q * 64:(q + 1) * 64] = wmat[:, n]
    sh["lrubd"] = bd
    sh["bng_c"] = np.ascontiguousarray(inp["branch_norm_g"][:, 1].reshape(L, 4, 64).transpose(0, 2, 1))
    sh["wr"] = np.ascontiguousarray(np.concatenate([inp["moe_w_group"], inp["moe_w_expert"]], axis=2))
    br = np.concatenate([inp["moe_b_group"], inp["moe_b_expert"]], axis=1)
    sh["br"] = np.ascontiguousarray(np.broadcast_to(br[:, None, :], (L, 128, 36)))
    sh["w1"] = np.ascontiguousarray(inp["moe_w1"], f32)
    sh["w3"] = np.ascontiguousarray(inp["moe_w3"], f32)
    sh["w2"] = np.ascontiguousarray(inp["moe_w2"], f32)
    cst = {}
    cst["ident"] = np.eye(128, dtype=f32)
    bo = np.zeros((128, 128), f32)
    bo[:64, :64] = 1.0
    bo[64:, 64:] = 1.0
    cst["blk"] = bo
    half = 16
    invf = np.power(np.float32(10000.0), -np.arange(half, dtype=f32) * np.float32(2.0) / np.float32(32)).astype(f32)
    iv = np.zeros((96, 1), f32)
    iv[64:80, 0] = invf
    iv[80:96, 0] = invf
    cst["invf"] = iv
    PT = np.zeros((128, 128), f32)
    for i in range(16):
        PT[80 + i, 64 + i] = -1.0
        PT[64 + i, 80 + i] = 1.0
    cst["rotT"] = PT
    E = np.zeros((32, 96), f32)
    for i in range(32):
        E[i, 64 + i] = 1.0
    cst["epe"] = E
    jj = np.arange(64)[:, None]
    ii = np.arange(64)[None, :]
    mk = np.concatenate([(jj < ii), (jj <= ii)], axis=1).astype(f32)
    cst["mk"] = np.ascontiguousarray(np.broadcast_to(mk[:, None, :], (64, 4, 128)))
    cst["mkl"] = np.ascontiguousarray(np.broadcast_to((jj > ii).astype(f32)[:, None, :], (64, 4, 64)))
    cst["id4"] = np.ascontiguousarray(np.broadcast_to(np.eye(64, dtype=f32)[:, None, :], (64, 4, 64)))
    sel = np.zeros((128, 64), f32)
    sel[64, :] = 1.0
    cst["sel65"] = sel
    s32 = np.zeros((128, 32, 128), f32)
    for e in range(32):
        s32[e, e, :] = 1.0
    cst["sel32"] = s32
    cst["tvals"] = np.ascontiguousarray(np.broadcast_to(np.arange(NB, dtype=f32)[None, :], (128, NB)))
    per_core = []
    for b in range(NCORES):
        d = {}
        d["xT"] = np.ascontiguousarray(inp["x"][b, :S].T, f32)
        d["c"] = _pk(inp["c"][b])
        d["pos"] = np.ascontiguousarray(np.broadcast_to(inp["pos_offset"][b].astype(np.int32).reshape(1, 1), (96, 1)))
        per_core.append(d)
    return sh, cst, per_core


class K:
    pass


def build(S, L, shapes, stop=None, dbg=None):
    nc = bass.Bass("TRN2", target_bir_lowering=False)
    k = K()
    k.nc = nc
    k.S = S
    k.L = L
    dr = {}
    for name, (shp, dt) in shapes.items():
        dr[name] = nc.dram_tensor(name, list(shp), dt, kind="ExternalInput").ap()
    dr["out"] = nc.dram_tensor("outT", [D, S], F32, kind="ExternalOutput").ap()
    k.mixT = nc.dram_tensor("mixT", [D, S], BF16, kind="Internal").ap()
    k.x1T = nc.dram_tensor("x1T", [D, S], F32, kind="Internal").ap()
    k.x2T = nc.dram_tensor("x2T", [D, S], F32, kind="Internal").ap()
    k.h2T = nc.dram_tensor("h2T", [D, S], BF16, kind="Internal").ap()
    k.gT = nc.dram_tensor("gT", [32, S], F32, kind="Internal").ap()
    NE = shapes["w1"][0][1]
    k.w1b = nc.dram_tensor("w1b", [NE, 128, 4096], BF16, kind="Internal").ap()
    k.w3b = nc.dram_tensor("w3b", [NE, 128, 4096], BF16, kind="Internal").ap()
    k.w2b = nc.dram_tensor("w2b", [NE, 128, 4096], BF16, kind="Internal").ap()
    k.qs = nc.dram_tensor("qs", [96, 4, S], BF16, kind="Internal").ap()
    k.ks = nc.dram_tensor("ks", [96, 4, S], BF16, kind="Internal").ap()
    k.vs = nc.dram_tensor("vs", [S // 128, 128, 260], BF16, kind="Internal").ap()
    if dbg is not None:
        dr["dbg"] = nc.dram_tensor("dbg", list(dbg), F32, kind="ExternalOutput").ap()
    k.dr = dr
    with ExitStack() as st:
        k.st = st
        S_ = Sched(nc)
        k.S_ = S_
        emit_all(k, stop)
        S_.barrier()
        S_.flush(st)
    return nc


def sb(k, st, name, shape, dt=F32):
    k.uid = getattr(k, "uid", 0) + 1
    return st.enter_context(k.nc.sbuf_tensor("sb%d_%s" % (k.uid, name), list(shape), dt))


class Ops:
    def __init__(self, S_):
        self.S = S_

    def dma(self, eng, out, in_, key, r=(), w=()):
        return self.S.dma(eng, lambda e: e.dma_start(out=out, in_=in_), key, r, w)

    def act(self, out, in_, func, r, w, bias=None, scale=None, eng="act"):
        kw = {}
        if bias is not None:
            kw["bias"] = bias
        if scale is not None:
            kw["scale"] = scale
        return self.S.op(eng, lambda e: e.activation(out=out, in_=in_, func=func, **kw), r, w)

    def tt(self, out, in0, in1, op, r, w, eng="dve"):
        return self.S.op(eng, lambda e: e.tensor_tensor(out=out, in0=in0, in1=in1, op=op), r, w)

    def ts(self, out, in0, s1, s2, op0, op1, r, w, eng="dve"):
        if s2 is None:
            return self.S.op(eng, lambda e: e.tensor_scalar(out=out, in0=in0, scalar1=s1, scalar2=None, op0=op0), r, w)
        return self.S.op(eng, lambda e: e.tensor_scalar(out=out, in0=in0, scalar1=s1, scalar2=s2, op0=op0, op1=op1), r, w)

    def stt(self, out, in0, scalar, in1, op0, op1, r, w, eng="dve"):
        return self.S.op(eng, lambda e: e.scalar_tensor_tensor(out=out, in0=in0, scalar=scalar, in1=in1, op0=op0, op1=op1), r, w)

    def copy(self, out, in_, r, w, eng="dve"):
        if eng == "act":
            return self.S.op(eng, lambda e: e.activation(out=out, in_=in_, func=AF.Copy), r, w)
        return self.S.op(eng, lambda e: e.tensor_copy(out=out, in_=in_), r, w)

    def memset(self, out, val, w, eng="pool"):
        return self.S.op(eng, lambda e: e.memset(out, val), (), w)

    def scan(self, out, d0, d1, init, r, w, eng="dve"):
        return self.S.op(eng, lambda e: e.tensor_tensor_scan(out=out, data0=d0, data1=d1, initial=init, op0=ALU.mult, op1=ALU.add), r, w)

    def mm(self, out, lhsT, rhs, start, stop, r, w):
        return self.S.op("pe", lambda e: e.matmul(out, lhsT, rhs, start=start, stop=stop), r, w)

    def tr(self, out, in_, ident, r, w):
        return self.S.op("pe", lambda e: e.transpose(out, in_, ident), r, w)


NB = 256


def emit_all(k, stop):
    nc, S_, dr, S, L = k.nc, k.S_, k.dr, k.S, k.L
    st = k.st
    o = Ops(S_)
    nblk = S // NB
    ps = [st.enter_context(nc.psum_tensor(f"ps{i}", [128, 512], F32)) for i in range(8)]
    P = [f"ps{i}" for i in range(8)]
    ident = sb(k, st, "ident", [128, 128])
    blk = sb(k, st, "blk", [128, 128])
    blkb = sb(k, st, "blkb", [128, 128], BF16)
    onesb = sb(k, st, "onesb", [128, 128], BF16)
    mods = sb(k, st, "mods", [128, L, 48])
    g1s = sb(k, st, "g1s", [128, L, 8])
    g2s = sb(k, st, "g2s", [128, L, 8])
    k.ident, k.blk, k.blkb, k.onesb, k.mods, k.g1s, k.g2s, k.ps, k.P, k.o = ident, blk, blkb, onesb, mods, g1s, g2s, ps, P, o
    o.dma("sp", ident[:], dr["ident"], "ident", w=["ident"])
    o.dma("sp", blk[:], dr["blk"], "blk", w=["blk"])
    o.copy(blkb[:], blk[:], ["blk"], ["blkb"])
    o.memset(onesb[:], 1.0, ["onesb"])
    ones32 = sb(k, st, "ones32", [128, 128])
    k.ones32 = ones32
    o.memset(ones32[:], 1.0, ["ones32"])
    cc = sb(k, st, "cc", [128, 8])
    k.cc = cc
    for val, c in CC.items():
        o.memset(cc[:, c:c + 1], float(val), ["cc"])

    with ExitStack() as p0:
        stg = [sb(k, p0, f"adst{i}", [128, 8, 768]) for i in range(2)]
        cond = sb(k, p0, "cond", [128, 8])
        adab = sb(k, p0, "adab", [128, 48])
        ng = sb(k, p0, "ng", [128, 8])
        tmp8 = sb(k, p0, "tmp8", [128, 8])
        o.dma("sp", cond[:], dr["c"], "cond", w=["cond"])
        o.act(cond[:], cond[:], AF.Silu, ["cond"], ["cond"])
        for l in range(L):
            o.dma("sp", adab[:], dr["ada_b"][l], "adab", w=["adab"])
            wv = dr["ada_w"][l].rearrange("(kk p) n -> p kk n", p=128)
            for c in range(8):
                bk = f"adst{c % 2}"
                o.dma("sp" if c % 2 == 0 else "pool", stg[c % 2][:], wv[:, :, c * 768:(c + 1) * 768], bk, w=[bk])
                for j in range(6):
                    col = c * 6 + j
                    for kk in range(8):
                        o.mm(ps[0][:, col:col + 1], stg[c % 2][:, kk, j * 128:(j + 1) * 128], cond[:, kk:kk + 1],
                             kk == 0, kk == 7, [bk, "cond"], [P[0]])
            o.tt(mods[:, l, :], ps[0][:, 0:48], adab[:], ALU.add, [P[0], "adab"], ["mods"])
            for (gs, nm, c0) in ((g1s, "n1g", 8), (g2s, "n2g", 32)):
                o.dma("sp", ng[:], dr[nm][l], "ng", w=["ng"])
                o.ts(tmp8[:], mods[:, l, c0:c0 + 8], 1.0, None, ALU.add, None, ["mods"], ["tmp8"])
                o.tt(gs[:, l, :], tmp8[:], ng[:], ALU.mult, ["tmp8", "ng"], ["g1s" if c0 == 8 else "g2s"])
        S_.barrier()
        S_.flush(st)
    if stop == "p0":
        o.dma("sp", dr["dbg"][:, 0:L * 48], mods[:].rearrange("p l c -> p (l c)"), "dbg", r=["mods"], w=["dbg"])
        return

    xin = dr["xT"]
    for l in range(L):
        phaseA(k, l, xin, stop)
        if stop is not None and stop.startswith("A"):
            return
        phaseB(k, l, xin, k.x1T, stop)
        if stop is not None and stop.startswith("B"):
            return
        phaseW(k, l)
        xo = dr["out"] if l == L - 1 else k.x2T
        phaseC(k, l, k.x1T, xo, stop)
        if stop is not None and stop.startswith("C"):
            return
        xin = xo


CC = {1e-6: 0, 64e-5: 1, 1.0: 2, -math.pi: 3, 0.0: 4, 1e-24: 5}


def rsqrt(k, out, in_, scale, eps, rkeys, wkey, rows=128):
    c = CC[eps]
    k.o.act(out, in_, AF.Sqrt, list(rkeys) + ["cc"], [wkey], bias=k.cc[0:rows, c:c + 1], scale=scale)
    k.o.S.op("dve", lambda e: e.reciprocal(out=out, in_=out), [wkey], [wkey])


def rms_stats(k, srcs, n_feat, eps, ps_ap, rstd_ap, sq_bufs, rkeys, pkey, wkey, lhsT=None, rows=128):
    o = k.o
    n = len(srcs)
    lhs = k.onesb[:rows, :rows] if lhsT is None else lhsT
    for i, (ap, key) in enumerate(srcs):
        sq, sqk = sq_bufs[i]
        o.act(sq, ap, AF.Square, [key], [sqk])
        o.mm(ps_ap, lhs, sq, i == 0, i == n - 1, [sqk, "onesb"], [pkey])
    rsqrt(k, rstd_ap, ps_ap, 1.0 / n_feat, eps, [pkey], wkey, rows)


def phaseA(k, l, xin, stop):
    nc, S_, dr, S, L, o = k.nc, k.S_, k.dr, k.S, k.L, k.o
    ps, P = k.ps, k.P
    nblk = S // NB
    with ExitStack() as pa:
        w_in = sb(k, pa, "w_in", [128, 8, DIN], BF16)
        xblk = sb(k, pa, "xblk", [128, 8, NB])
        wv = dr["w_in"][l].rearrange("(kk p) n -> p kk n", p=128)
        xflat = xblk[:].rearrange("p a b -> p (a b)")
        for c in range(20):
            stv = xflat[:, (c % 2) * 832:(c % 2 + 1) * 832].rearrange("p (kk n) -> p kk n", n=104)
            o.dma("sp", stv, wv[:, :, c * 104:(c + 1) * 104], f"xblk{c % 2}", w=["xblk"])
            o.copy(w_in[:, :, c * 104:(c + 1) * 104], stv, ["xblk"], ["w_in"], eng=("dve" if c % 2 == 0 else "pool"))
        rstd = sb(k, pa, "rstd", [128, NB])
        xt = sb(k, pa, "xt", [128, NB])
        hT = sb(k, pa, "hT", [128, 8, NB], BF16)
        sqb = hT
        zr = sb(k, pa, "zr", [128, 7, NB + 1])
        s5u = sb(k, pa, "s5u", [128, 2, NB])
        qab = sb(k, pa, "qab", [128, 2, NB], BF16)
        kvab = sb(k, pa, "kvab", [128, NB], BF16)
        kpeb = sb(k, pa, "kpeb", [32, NB], BF16)
        lrx = sb(k, pa, "lrx", [128, 2, NB + 3])
        lrg = sb(k, pa, "lrg", [128, 2, NB])
        R = rwkv_setup(k, pa, l)
        Q = s5_setup(k, pa, l, xblk) if stop not in ("A1",) and not (stop or "").startswith("A2") else None
        U = lru_setup(k, pa, l, xblk) if stop not in ("A1",) and not (stop or "").startswith("A2") else None
        mixT = k.mixT
        A = mla_setup(k, pa, l, xblk, Q, U) if Q is not None else None
        o.memset(zr[:, :, 0:1], 0.0, ["zr"])
        o.memset(lrx[:, :, 0:3], 0.0, ["lrx"])
        xv = xin.rearrange("(kk p) s -> p kk s", p=128)
        tiles = [(t * 128, 128) for t in range(12)] + [(1536, 32)] + [(1568 + t * 128, 128) for t in range(4)]
        for i in range(nblk):
            t0 = i * NB
            o.dma("sp", xblk[:], xv[:, :, t0:t0 + NB], "xblk", w=["xblk"])
            rms_stats(k, [(xblk[:, kk, :], "xblk") for kk in range(8)], 1024.0, 1e-6, ps[0][:, 0:NB], rstd[:],
                      [(sqb[:, kk, :], "hT") for kk in range(8)], None, P[0], "rstd")
            for kk in range(8):
                o.tt(xt[:], xblk[:, kk, :], rstd[:], ALU.mult, ["xblk", "rstd"], ["xt"])
                o.act(hT[:, kk, :], xt[:], AF.Identity, ["xt", "g1s", "mods"], ["hT"],
                      bias=k.mods[:, l, kk:kk + 1], scale=k.g1s[:, l, kk:kk + 1])
            for ti, (c0, m) in enumerate(tiles):
                pb = 1 + (ti % 3)
                pt = ps[pb][0:m, 0:NB]
                for kk in range(8):
                    o.mm(pt, w_in[:, kk, c0:c0 + m], hT[:, kk, :], kk == 0, kk == 7, ["w_in", "hT"], [P[pb]])
                if ti < 7:
                    o.copy(zr[:, ti, 1:NB + 1], pt, [P[pb]], ["zr"], eng=("act" if ti % 2 == 0 else "dve"))
                elif ti < 9:
                    o.copy(s5u[:, ti - 7, :], pt, [P[pb]], ["s5u"], eng="dve")
                elif ti < 11:
                    o.copy(qab[:, ti - 9, :], pt, [P[pb]], ["qab"], eng="act")
                elif ti == 11:
                    o.copy(kvab[:], pt, [P[pb]], ["kvab"], eng="act")
                elif ti == 12:
                    o.copy(kpeb[:], pt, [P[pb]], ["kpeb"], eng="act")
                elif ti < 15:
                    o.copy(lrx[:, ti - 13, 3:NB + 3], pt, [P[pb]], ["lrx"], eng="dve")
                else:
                    o.copy(lrg[:, ti - 15, :], pt, [P[pb]], ["lrg"], eng="act")
            gens = []
            if stop != "A1":
                R["mix_dst"] = (mixT, t0) if (stop is None or not stop.startswith("A2")) else None
                gens.append(rwkv_block(k, l, i, R, zr, stop))
            if stop is None or not stop.startswith("A2"):
                gens.append(seq_gen(par_gen([s5_block(k, l, i, Q, s5u, mixT, t0), lru_block(k, l, i, U, lrx, lrg, mixT, t0)]),
                                    mla_block(k, l, i, A, qab, kvab, kpeb, t0)))
            interleave(gens)
            if stop == "A4":
                dbg = dr["dbg"]
                o.copy(A["t1"][:], A["qrb"][:, 1, :], ["a_out"], ["l_aa"], eng="dve")
                o.dma("sp", dbg[0:96, t0:t0 + NB], A["t1"][:], "dbg", r=["l_aa"], w=["dbg"])
                o.copy(A["t2"][:], A["krb"][:, 2, :], ["a_out"], ["l_t1"], eng="dve")
                o.dma("sp", dbg[96:192, t0:t0 + NB], A["t2"][:], "dbg", r=["l_t1"], w=["dbg"])
                for tt in range(NB // 128):
                    o.copy(A["qf"][0:64, 0:128], A["Vt"][0:64, tt, 3, 0:128].rearrange("p c -> p c") if False else A["Vt"][0:64, tt, :, :].rearrange("p h c -> p (h c)")[:, 0:128], ["a_Vt"], ["l_xc"], eng="dve")
                    o.dma("sp", dbg[192:256, t0 + tt * 128:t0 + (tt + 1) * 128], A["qf"][0:64, 0:128], "dbg", r=["l_xc"], w=["dbg"])
                continue
            if stop == "A3":
                dbg = dr["dbg"]
                o.dma("sp", dbg[256:512, t0:t0 + NB].rearrange("(t p) s -> p t s", p=128), Q["yo"][:], "dbg", r=["s_yo"], w=["dbg"])
                o.dma("sp", dbg[768:1024, t0:t0 + NB].rearrange("(t p) s -> p t s", p=128), U["yo"][:], "dbg", r=["l_yo"], w=["dbg"])
                o.dma("sp", dbg[0:256, t0:t0 + NB].rearrange("(t p) s -> p t s", p=128), R["yout"][:], "dbg", r=["yout"], w=["dbg"])
                continue
            if stop is not None and stop.startswith("A2"):
                dbg = dr["dbg"]
                o.dma("sp", dbg[0:256, t0:t0 + NB].rearrange("(t p) s -> p t s", p=128), R["yout"][:], "dbg", r=["yout"], w=["dbg"])
                continue
            if stop == "A1":
                dbg = dr["dbg"]
                o.dma("sp", dbg[0:896, t0:t0 + NB].rearrange("(t p) s -> p t s", p=128), zr[:, :, 1:NB + 1], "dbg", r=["zr"], w=["dbg"])
                o.dma("sp", dbg[896:1152, t0:t0 + NB].rearrange("(t p) s -> p t s", p=128), s5u[:], "dbg", r=["s5u"], w=["dbg"])
                o.dma("sp", dbg[1568:1824, t0:t0 + NB].rearrange("(t p) s -> p t s", p=128), lrx[:, :, 3:NB + 3], "dbg", r=["lrx"], w=["dbg"])
                o.dma("sp", dbg[1824:2080, t0:t0 + NB].rearrange("(t p) s -> p t s", p=128), lrg[:], "dbg", r=["lrg"], w=["dbg"])
                continue
        S_.barrier()
        S_.flush(k.st)


def par_gen(gens):
    gens = list(gens)
    while gens:
        for g in list(gens):
            try:
                next(g)
            except StopIteration:
                gens.remove(g)
        yield


def seq_gen(*gens):
    for g in gens:
        yield from g


def interleave(gens):
    gens = [g for g in gens if g is not None]
    first = gens[0] if gens else None
    while gens:
        for g in list(gens):
            for _ in range(2 if g is first else 1):
                try:
                    next(g)
                except StopIteration:
                    gens.remove(g)
                    break


def rwkv_setup(k, pa, l):
    o, dr = k.o, k.dr
    R = {}
    nch = NB // 64
    f2 = [128, 2, NB]
    for nm in ("gg", "bonus", "epos", "bt", "kt", "ya", "yout"):
        R[nm] = sb(k, pa, "r_" + nm, f2)
    for nm in ("sg", "aa", "kk", "tA", "tB", "kmod", "cls", "eneg", "eex"):
        R[nm] = sb(k, pa, "r_" + nm, [128, 1, NB])
    R["zs"] = sb(k, pa, "r_zs", [128, 7, NB])
    R["youtb"] = sb(k, pa, "r_youtb", [128, 2, NB], BF16)
    R["lob"] = sb(k, pa, "r_lob", [128, NB], BF16)
    R["sqb"] = sb(k, pa, "r_sqb", [128, NB], BF16)
    R["ar"] = sb(k, pa, "r_ar", [128, 2, nch, 2, 64])
    R["tok"] = sb(k, pa, "r_tok", [128, 3, 2, 128])
    R["NL"] = [sb(k, pa, f"r_NL{i}", [64, 2, 4, 64]) for i in range(2)]
    R["Pm"] = [sb(k, pa, f"r_Pm{i}", [64, 4, 64]) for i in range(2)]
    R["LakT"] = sb(k, pa, "r_LakT", [64, 4, 64])
    R["QrbT"] = sb(k, pa, "r_QrbT", [128, 4, 64])
    R["QrkT"] = sb(k, pa, "r_QrkT", [128, 4, 64])
    R["Xs"] = sb(k, pa, "r_Xs", [64, 256])
    R["Mtmp"] = sb(k, pa, "r_Mtmp", [128, 128])
    R["Us"] = sb(k, pa, "r_Us", [128, 256])
    R["M"] = [sb(k, pa, f"r_M{i}", [128, 2, 128]) for i in range(2)]
    R["mu"] = sb(k, pa, "r_mu", [128, 7])
    R["omu"] = sb(k, pa, "r_omu", [128, 7])
    R["rvec"] = sb(k, pa, "r_rvec", [128, 7, 2])
    R["lst"] = sb(k, pa, "r_lst", [128, 256])
    R["lora"] = sb(k, pa, "r_lora", [128, 256], BF16)
    R["mk"] = sb(k, pa, "r_mk", [64, 4, 128])
    R["mkl"] = sb(k, pa, "r_mkl", [64, 4, 64])
    R["id4"] = sb(k, pa, "r_id4", [64, 4, 64])
    R["cmask"] = sb(k, pa, "r_cmask", [128, NB])
    o.dma("sp", R["mu"][:], dr["mu"][l], "r_mu", w=["r_mu"])
    o.ts(R["omu"][:], R["mu"][:], -1.0, 1.0, ALU.mult, ALU.add, ["r_mu"], ["r_omu"])
    o.dma("sp", R["rvec"][:], dr["rvec"][l], "r_rvec", w=["r_rvec"])
    o.dma("sp", R["lst"][:], dr["lora"][l], "r_lst", w=["r_lst"])
    o.copy(R["lora"][:], R["lst"][:], ["r_lst"], ["r_lora"])
    o.dma("sp", R["mk"][:], dr["mk"], "r_mk", w=["r_mk"])
    o.dma("sp", R["mkl"][:], dr["mkl"], "r_mkl", w=["r_mkl"])
    o.dma("sp", R["id4"][:], dr["id4"], "r_id4", w=["r_id4"])
    o.memset(R["cmask"][:], 1.0, ["r_cmask"])
    o.memset(R["cmask"][:].rearrange("p (c j) -> p c j", j=64)[:, :, 0:1], 0.0, ["r_cmask"])
    o.memset(R["M"][0][:], 0.0, ["r_M0"])
    o.memset(R["tok"][:], 0.0, ["r_tok"])
    o.memset(R["Us"][:], 0.0, ["r_Us"])
    o.memset(R["QrbT"][:], 0.0, ["r_QrbT"])
    o.memset(R["QrkT"][:], 0.0, ["r_QrkT"])
    return R


C0 = -math.exp(-0.5)


def rwkv_block(k, l, i, R, zr, stop):
    o, ps, P = k.o, k.ps, k.P
    nch = NB // 64
    zs, rv = R["zs"], R["rvec"]
    W0, A0, KK, KA, RK, LNW, LNB = range(7)
    for t in range(7):
        eng = "dve" if t % 2 == 0 else "pool"
        o.ts(zs[:, t, :], zr[:, t, 0:NB], R["mu"][:, t:t + 1], None, ALU.mult, None, ["zr", "r_mu"], [("zs", t)], eng=eng)
        o.stt(zs[:, t, :], zr[:, t, 1:NB + 1], R["omu"][:, t:t + 1], zs[:, t, :], ALU.mult, ALU.add, ["zr", "r_omu", ("zs", t)], [("zs", t)])
    o.copy(zr[:, :, 0:1], zr[:, :, NB:NB + 1], ["zr"], ["zr"], eng="dve")
    lob = R["lob"]
    o.act(lob[0:32, :], zs[0:32, 6, :], AF.Tanh, [("zs", 6)], ["r_lob"])
    o.act(lob[64:128, :], zs[64:128, 6, :], AF.Sigmoid, [("zs", 6)], ["r_lob"])
    o.copy(lob[32:64, :], zs[32:64, 6, :], [("zs", 6)], ["r_lob"], eng="dve")
    lora = R["lora"]
    for p in range(2):
        cs = slice(p * 128, (p + 1) * 128)
        rT, kT, vT = zs[:, p, :], zs[:, 2 + p, :], zs[:, 4 + p, :]
        rk_, kk_, vk_ = ("zs", p), ("zs", 2 + p), ("zs", 4 + p)
        o.mm(ps[1][:, 0:NB], lora[0:32, cs], lob[0:32, :], True, True, ["r_lora", "r_lob"], [P[1]])
        o.mm(ps[2][:, 0:NB], lora[32:64, cs], lob[32:64, :], True, True, ["r_lora", "r_lob"], [P[2]])
        o.mm(ps[3][:, 0:NB], lora[64:128, cs], lob[64:128, :], True, True, ["r_lora", "r_lob"], [P[3]])
        o.act(R["sg"][:, 0, :], ps[1][:, 0:NB], AF.Sigmoid, [P[1], "r_rvec"], ["r_sg"], bias=rv[:, W0, p:p + 1])
        o.act(R["aa"][:, 0, :], ps[2][:, 0:NB], AF.Sigmoid, [P[2], "r_rvec"], ["r_aa"], bias=rv[:, A0, p:p + 1])
        o.copy(R["gg"][:, p, :], ps[3][:, 0:NB], [P[3]], ["r_gg"], eng="dve")
        kk = R["kk"][:, 0, :]
        o.ts(kk, kT, rv[:, KK, p:p + 1], None, ALU.mult, None, [kk_, "r_rvec"], ["r_kk"])
        o.act(R["sqb"][:], kk, AF.Square, ["r_kk"], ["r_sqb"])
        o.mm(ps[1][:, 0:NB], k.blkb[:], R["sqb"][:], True, True, ["blkb", "r_sqb"], [P[1]])
        rsqrt(k, R["tA"][:, 0, :], ps[1][:, 0:NB], 1.0, 1e-24, [P[1]], "r_tA")
        o.tt(kk, kk, R["tA"][:, 0, :], ALU.mult, ["r_kk", "r_tA"], ["r_kk"])
        o.ts(R["tA"][:, 0, :], R["aa"][:, 0, :], -1.0, rv[:, KA, p:p + 1], ALU.add, ALU.mult, ["r_aa", "r_rvec"], ["r_tA"])
        o.stt(R["kmod"][:, 0, :], R["tA"][:, 0, :], 1.0, kT, ALU.add, ALU.mult, ["r_tA", kk_], ["r_kmod"])
        o.tt(R["tA"][:, 0, :], rT, R["kmod"][:, 0, :], ALU.mult, [rk_, "r_kmod"], ["r_tA"])
        o.ts(R["sqb"][:], R["tA"][:, 0, :], rv[:, RK, p:p + 1], None, ALU.mult, None, ["r_tA", "r_rvec"], ["r_sqb"])
        o.mm(ps[2][:, 0:NB], k.blkb[:], R["sqb"][:], True, True, ["blkb", "r_sqb"], [P[2]])
        o.tt(R["bonus"][:, p, :], ps[2][:, 0:NB], vT, ALU.mult, [P[2], vk_], ["r_bonus"])
        o.scan(R["cls"][:, 0, :], R["cmask"][:], R["sg"][:, 0, :], 0.0, ["r_cmask", "r_sg"], ["r_cls"])
        o.act(R["epos"][:, p, :], R["cls"][:, 0, :], AF.Exp, ["r_cls"], ["r_epos"], scale=C0)
        o.act(R["eneg"][:, 0, :], R["cls"][:, 0, :], AF.Exp, ["r_cls"], ["r_eneg"], scale=-C0)
        o.tt(R["tB"][:, 0, :], R["cls"][:, 0, :], R["sg"][:, 0, :], ALU.subtract, ["r_cls", "r_sg"], ["r_tB"])
        o.act(R["eex"][:, 0, :], R["tB"][:, 0, :], AF.Exp, ["r_tB"], ["r_eex"], scale=C0)
        arv = R["ar"][:, p, :, :, :]
        o.tt(arv[:, :, 1, :], rT.rearrange("p (c j) -> p c j", j=64), R["epos"][:, p, :].rearrange("p (c j) -> p c j", j=64),
             ALU.mult, [rk_, "r_epos"], ["r_ar"])
        o.stt(arv[:, :, 0, :], kk.rearrange("p (c j) -> p c j", j=64), -1.0, R["eex"][:, 0, :].rearrange("p (c j) -> p c j", j=64),
              ALU.mult, ALU.mult, ["r_kk", "r_eex"], ["r_ar"])
        o.tt(R["kt"][:, p, :], R["kmod"][:, 0, :], R["eneg"][:, 0, :], ALU.mult, ["r_kmod", "r_eneg"], ["r_kt"])
        o.tt(R["tA"][:, 0, :], kk, R["aa"][:, 0, :], ALU.mult, ["r_kk", "r_aa"], ["r_tA"])
        o.tt(R["bt"][:, p, :], R["tA"][:, 0, :], R["eneg"][:, 0, :], ALU.mult, ["r_tA", "r_eneg"], ["r_bt"])
        yield
    ar, bt, kt, tok = R["ar"], R["bt"], R["kt"], R["tok"]
    NL, Pm = R["NL"], R["Pm"]
    if stop == "A2a":
        return
    for c in range(nch):
        gc = i * nch + c
        cs = slice(c * 64, (c + 1) * 64)
        for p in range(2):
            o.tr(ps[4][0:64, p * 128:(p + 1) * 128], bt[:, p, cs], k.ident[:], ["r_bt", "ident"], [P[4]])
            o.tr(ps[4][0:64, 256 + p * 128:256 + (p + 1) * 128], kt[:, p, cs], k.ident[:], ["r_kt", "ident"], [P[4]])
            o.tr(ps[5][0:64, p * 128:(p + 1) * 128], zs[:, 4 + p, cs], k.ident[:], [("zs", 4 + p), "ident"], [P[5]])
        o.copy(tok[0:64, 0:2, :, :].rearrange("j a p c -> j (a p c)"), ps[4][0:64, :], [P[4]], ["r_tok"], eng="act")
        o.copy(tok[0:64, 2, :, :].rearrange("j p c -> j (p c)"), ps[5][0:64, 0:256], [P[5]], ["r_tok"], eng="dve")
        yield
        if stop == "A2b":
            continue
        for h in range(4):
            p, q = h // 2, h % 2
            rs = slice(q * 64, (q + 1) * 64)
            arh = ar[rs, p, c, :, :].rearrange("k a j -> k (a j)")
            o.mm(ps[6][0:64, h * 128:(h + 1) * 128], bt[rs, p, cs], arh, True, True, ["r_bt", "r_ar"], [P[6]])
            o.mm(ps[7][0:64, h * 128:(h + 1) * 128], kt[rs, p, cs], arh, True, True, ["r_kt", "r_ar"], [P[7]])
            o.mm(ps[5][0:64, 256 + h * 64:256 + (h + 1) * 64], ar[rs, p, c, 0, :], bt[rs, p, cs], True, True, ["r_bt", "r_ar"], [P[5]])
        v6 = ps[6][0:64, :].rearrange("j (h x) -> j h x", x=128)
        v7 = ps[7][0:64, :].rearrange("j (h x) -> j h x", x=128)
        mk = R["mk"]
        o.tt(NL[0][:, 0, :, :], v6[:, :, 0:64], mk[:, :, 0:64], ALU.mult, [P[6], "r_mk"], ["r_NL0"])
        o.tt(R["QrbT"][0:64], v6[:, :, 64:128], mk[:, :, 64:128], ALU.mult, [P[6], "r_mk"], ["r_QrbT"])
        o.tt(R["LakT"][:], v7[:, :, 0:64], mk[:, :, 0:64], ALU.mult, [P[7], "r_mk"], ["r_LakT"])
        o.tt(R["QrkT"][0:64], v7[:, :, 64:128], mk[:, :, 64:128], ALU.mult, [P[7], "r_mk"], ["r_QrkT"])
        o.tt(NL[0][:, 1, :, :], ps[5][0:64, 256:512].rearrange("j (h x) -> j h x", x=64), R["mkl"][:], ALU.mult, [P[5], "r_mkl"], ["r_NL0"])
        o.tt(Pm[1][:], NL[0][:, 0, :, :], R["id4"][:], ALU.add, ["r_NL0", "r_id4"], ["r_Pm1"], eng="pool")
        yield
        if stop == "A2c":
            continue
        for m in range(1, 7):
            src, dst = NL[(m - 1) % 2], NL[m % 2]
            sk, dk = f"r_NL{(m - 1) % 2}", f"r_NL{m % 2}"
            pin, pout = Pm[(m - 1) % 2], Pm[m % 2]
            pik, pok = f"r_Pm{(m - 1) % 2}", f"r_Pm{m % 2}"
            for h in range(4):
                if m <= 5:
                    o.mm(ps[6][0:64, h * 64:(h + 1) * 64], src[:, 1, h, :], src[:, 0, h, :], True, True, [sk], [P[6]])
                    o.mm(ps[6][0:64, 256 + h * 64:256 + (h + 1) * 64], src[:, 0, h, :], src[:, 1, h, :], True, True, [sk], [P[6]])
                if m >= 2:
                    o.mm(ps[7][0:64, h * 64:(h + 1) * 64], src[:, 1, h, :], pin[:, h, :], True, True, [sk, pik], [P[7]])
            if m <= 5:
                o.copy(dst[:].rearrange("j a h x -> j (a h x)"), ps[6][0:64, :], [P[6]], [dk], eng="act")
            if m >= 2:
                o.tt(pout[:].rearrange("j h x -> j (h x)"), ps[7][0:64, 0:256], pin[:].rearrange("j h x -> j (h x)"), ALU.add, [P[7], pik], [pok])
                yield
            else:
                pass
        TT_ = Pm[0]
        if stop == "A2d":
            continue
        Mc, Mn = R["M"][gc % 2], R["M"][(gc + 1) % 2]
        mck, mnk = f"r_M{gc % 2}", f"r_M{(gc + 1) % 2}"
        for p in range(2):
            o.mm(ps[4][0:64, p * 128:(p + 1) * 128], ar[:, p, c, 0, :], Mc[:, p, :], True, False, ["r_ar", mck], [P[4]])
            for q in range(2):
                h = 2 * p + q
                o.mm(ps[4][0:64, p * 128 + q * 64:p * 128 + (q + 1) * 64], R["LakT"][:, h, :], tok[0:64, 2, p, q * 64:(q + 1) * 64],
                     False, q == 1, ["r_LakT", "r_tok"], [P[4]])
        o.copy(R["Xs"][:], ps[4][0:64, 0:256], [P[4]], ["r_Xs"], eng="dve")
        yield
        if stop == "A2e":
            continue
        for h in range(4):
            o.mm(ps[4][0:64, 256 + h * 64:256 + (h + 1) * 64], TT_[:, h, :], R["Xs"][:, h * 64:(h + 1) * 64], True, True, ["r_Pm0", "r_Xs"], [P[4]])
        o.copy(R["Us"][0:64, :], ps[4][0:64, 256:512], [P[4]], ["r_Us"], eng="dve")
        yield
        if stop == "A2f":
            continue
        for p in range(2):
            pc = slice(p * 128, (p + 1) * 128)
            o.mm(ps[5][:, pc], k.ident[:], Mc[:, p, :], True, False, ["ident", mck], [P[5]])
            o.mm(ps[5][:, pc], tok[:, 0, p, :], R["Us"][:, pc], False, False, ["r_tok", "r_Us"], [P[5]])
            o.mm(ps[5][:, pc], tok[:, 1, p, :], tok[:, 2, p, :], False, True, ["r_tok"], [P[5]])
            for q in range(2):
                if stop == "A2g":
                    continue
                h = 2 * p + q
                yc = slice(256 + h * 64, 256 + (h + 1) * 64)
                o.mm(ps[5][:, yc], Mc[:, p, :], ar[:, p, c, 1, :], True, False, [mck, "r_ar"], [P[5]])
                o.mm(ps[5][:, yc], R["Us"][:, pc], R["QrbT"][:, h, :], False, False, ["r_Us", "r_QrbT"], [P[5]])
                o.mm(ps[5][:, yc], tok[:, 2, p, :], R["QrkT"][:, h, :], False, True, ["r_tok", "r_QrkT"], [P[5]])
        for p in range(2):
            pc = slice(p * 128, (p + 1) * 128)
            o.act(R["Mtmp"][:], ps[5][:, pc], AF.Identity, [P[5], "r_epos"], ["r_Mtmp"], scale=R["epos"][:, p, c * 64 + 63:c * 64 + 64])
            o.tt(Mn[:, p, :], R["Mtmp"][:], k.blk[:], ALU.mult, ["r_Mtmp", "blk"], [mnk])
            for q in range(2):
                if stop == "A2g":
                    continue
                h = 2 * p + q
                rs = slice(q * 64, (q + 1) * 64)
                o.copy(R["ya"][rs, p, cs], ps[5][rs, 256 + h * 64:256 + (h + 1) * 64], [P[5]], ["r_ya"], eng="act")
    for p in range(2):
        ya = R["ya"][:, p, :]
        o.mm(ps[1][:, 0:NB], k.blk[:], ya, True, True, ["blk", "r_ya"], [P[1]])
        o.stt(R["tA"][:, 0, :], ps[1][:, 0:NB], -1.0 / 64, ya, ALU.mult, ALU.add, [P[1], "r_ya"], ["r_tA"])
        o.act(R["tB"][:, 0, :], R["tA"][:, 0, :], AF.Square, ["r_tA"], ["r_tB"])
        o.mm(ps[2][:, 0:NB], k.blk[:], R["tB"][:, 0, :], True, True, ["blk", "r_tB"], [P[2]])
        rsqrt(k, R["tB"][:, 0, :], ps[2][:, 0:NB], 1.0 / 64, 64e-5, [P[2]], "r_tB")
        o.tt(R["tA"][:, 0, :], R["tA"][:, 0, :], R["tB"][:, 0, :], ALU.mult, ["r_tA", "r_tB"], ["r_tA"])
        o.ts(R["tA"][:, 0, :], R["tA"][:, 0, :], rv[:, LNW, p:p + 1], rv[:, LNB, p:p + 1], ALU.mult, ALU.add, ["r_tA", "r_rvec"], ["r_tA"])
        o.tt(R["tA"][:, 0, :], R["tA"][:, 0, :], R["bonus"][:, p, :], ALU.add, ["r_tA", "r_bonus"], ["r_tA"])
        o.tt(R["yout"][:, p, :], R["tA"][:, 0, :], R["gg"][:, p, :], ALU.mult, ["r_tA", "r_gg"], ["yout"])
        yield
    o.copy(R["youtb"][:], R["yout"][:], ["yout"], ["youtb"], eng="pool")
    if R.get("mix_dst") is not None:
        mixT, t0 = R["mix_dst"]
        o.dma("pool", mixT[0:256, t0:t0 + NB].rearrange("(t p) s -> p t s", p=128), R["youtb"][:], "mix_a", r=["youtb"], w=["mixT_a"])
    yield


TWO_PI = 2.0 * math.pi
CW1 = 6.28125
CW2 = TWO_PI - CW1


def sin_of(k, out, ang, shift, tmp, tmpi, shape_rows, r, w, tk):
    o = k.o
    o.ts(tmp, ang, 1.0 / TWO_PI, 0.5 + shift / TWO_PI, ALU.mult, ALU.add, r, [tk])
    o.copy(tmpi, tmp, [tk], [tk + "i"])
    o.copy(tmp, tmpi, [tk + "i"], [tk])
    o.stt(out, tmp, -CW1, ang, ALU.mult, ALU.add, [tk] + list(r), w)
    o.stt(out, tmp, -CW2, out, ALU.mult, ALU.add, [tk] + list(w), w)
    if shift != 0.0:
        o.ts(out, out, float(shift), None, ALU.add, None, w, w)
    o.ts(tmp, out, -math.pi, TWO_PI, ALU.is_lt, ALU.mult, w, [tk])
    o.tt(out, out, tmp, ALU.add, list(w) + [tk], w)
    o.ts(tmp, out, math.pi, -TWO_PI, ALU.is_gt, ALU.mult, w, [tk])
    o.tt(out, out, tmp, ALU.add, list(w) + [tk], w)
    o.ts(out, out, math.pi, -math.pi, ALU.min, ALU.max, w, w)
    o.act(out, out, AF.Sin, w, w)


def gelu_tanh(k, out, x, t1, r, w, tk):
    o = k.o
    o.tt(t1, x, x, ALU.mult, r, [tk])
    o.ts(t1, t1, 0.044715, 1.0, ALU.mult, ALU.add, [tk], [tk])
    o.tt(t1, t1, x, ALU.mult, [tk] + list(r), [tk])
    o.act(t1, t1, AF.Sigmoid, [tk], [tk], scale=1.5957691216057308)
    o.tt(out, x, t1, ALU.mult, [tk] + list(r), w)


def s5_setup(k, pa, l, xblk):
    o, dr, ps, P = k.o, k.dr, k.ps, k.P
    Q = {}
    Q["v"] = sb(k, pa, "s_v", [128, 3, 8])
    for nm in ("dt", "mag", "th", "c8", "s8", "qre", "qim", "den", "t8a", "t8b", "Ere", "Eim", "cre", "cim", "glr", "gli"):
        Q[nm] = sb(k, pa, "s_" + nm, [128, 8])
    Q["t8i"] = sb(k, pa, "s_t8i", [128, 8], I32)
    for nm in ("cosT", "sinT"):
        Q[nm] = sb(k, pa, "s_" + nm, [128, 8, NB])
    Q["hre"] = sb(k, pa, "s_hre", [128, 8, NB], BF16)
    Q["him"] = sb(k, pa, "s_him", [128, 8, NB], BF16)
    Q["tv"] = sb(k, pa, "s_tv", [128, NB])
    Q["B"] = sb(k, pa, "s_B", [128, 2, 8, 128], BF16)
    Q["C"] = sb(k, pa, "s_C", [128, 2, 8, 128], BF16)
    Q["vec2"] = sb(k, pa, "s_vec2", [128, 3, 2])
    Q["glu"] = sb(k, pa, "s_glu", [128, 2, 256], BF16)
    Q["ub"] = sb(k, pa, "s_ub", [128, 2, NB], BF16)
    for nm in ("w1", "w2", "w3", "w4", "w5", "w6", "w7"):
        Q[nm] = sb(k, pa, "s_" + nm, [128, NB])
    Q["wi"] = sb(k, pa, "s_wi", [128, NB], I32)
    Q["yv"] = sb(k, pa, "s_yv", [128, 2, NB])
    Q["ge"] = sb(k, pa, "s_ge", [128, 2, NB])
    Q["geb"] = sb(k, pa, "s_geb", [128, 2, NB], BF16)
    Q["sqb"] = sb(k, pa, "s_sqb", [128, 2, NB], BF16)
    Q["yo"] = sb(k, pa, "s_yo", [128, 2, NB])
    Q["yob"] = sb(k, pa, "s_yob", [128, 2, NB], BF16)
    Q["dq"] = sb(k, pa, "s_dq", [128, 2, 128])
    v = Q["v"]
    wst = xblk[:].rearrange("p a b -> p (a b)").rearrange("p (a j c) -> p a j c", a=2, j=8)
    o.dma("sp", v[:], dr["s5v"][l], "s_v", w=["s_v"])
    o.dma("sp", Q["tv"][:], dr["tvals"], "s_tv", w=["s_tv"])
    o.dma("sp", Q["vec2"][:], dr["s5vec2"][l], "s_vec2", w=["s_vec2"])
    S = ["s_small"]
    o.act(Q["dt"][:], v[:, 2, :], AF.Exp, ["s_v"], S)
    o.tt(Q["t8a"][:], v[:, 0, :], Q["dt"][:], ALU.mult, ["s_v"] + S, S)
    o.act(Q["mag"][:], Q["t8a"][:], AF.Exp, S, S)
    o.tt(Q["th"][:], v[:, 1, :], Q["dt"][:], ALU.mult, ["s_v"] + S, S)
    sin_of(k, Q["s8"][:], Q["th"][:], 0.0, Q["t8b"][:], Q["t8i"][:], 128, S, S, "s_t8")
    sin_of(k, Q["c8"][:], Q["th"][:], math.pi / 2, Q["t8b"][:], Q["t8i"][:], 128, S, S, "s_t8")
    o.tt(Q["t8a"][:], Q["mag"][:], Q["c8"][:], ALU.mult, S, S)
    o.ts(Q["t8a"][:], Q["t8a"][:], -1.0, None, ALU.add, None, S, S)
    o.tt(Q["t8b"][:], Q["mag"][:], Q["s8"][:], ALU.mult, S + ["s_t8"], ["s_t8"])
    o.tt(Q["den"][:], v[:, 0, :], v[:, 0, :], ALU.mult, ["s_v"], S)
    o.tt(Q["qre"][:], v[:, 1, :], v[:, 1, :], ALU.mult, ["s_v"], S)
    o.tt(Q["den"][:], Q["den"][:], Q["qre"][:], ALU.add, S, S)
    o.S.op("dve", lambda e: e.reciprocal(out=Q["den"][:], in_=Q["den"][:]), S, S)
    o.tt(Q["qre"][:], Q["t8a"][:], v[:, 0, :], ALU.mult, S + ["s_v"], S)
    o.tt(Q["qim"][:], Q["t8b"][:], v[:, 1, :], ALU.mult, S + ["s_v", "s_t8"], S)
    o.tt(Q["qre"][:], Q["qre"][:], Q["qim"][:], ALU.add, S, S)
    o.tt(Q["qre"][:], Q["qre"][:], Q["den"][:], ALU.mult, S, S)
    o.tt(Q["qim"][:], Q["t8b"][:], v[:, 0, :], ALU.mult, S + ["s_v", "s_t8"], S)
    o.tt(Q["cre"][:], Q["t8a"][:], v[:, 1, :], ALU.mult, S + ["s_v"], S)
    o.tt(Q["qim"][:], Q["qim"][:], Q["cre"][:], ALU.subtract, S, S)
    o.tt(Q["qim"][:], Q["qim"][:], Q["den"][:], ALU.mult, S, S)
    o.ts(Q["t8a"][:], Q["th"][:], float(NB), None, ALU.mult, None, S, S)
    sin_of(k, Q["Eim"][:], Q["t8a"][:], 0.0, Q["t8b"][:], Q["t8i"][:], 128, S, S, "s_t8")
    sin_of(k, Q["Ere"][:], Q["t8a"][:], math.pi / 2, Q["t8b"][:], Q["t8i"][:], 128, S, S, "s_t8")
    o.dma("sp", wst, dr["s5b"][l].rearrange("a j p c -> p a j c"), "xblk0", w=["xblk"])
    for jt in range(8):
        o.ts(Q["dq"][:, 0, :], k.ident[:], Q["qre"][:, jt:jt + 1], None, ALU.mult, None, ["ident"] + S, ["s_dq"])
        o.ts(Q["dq"][:, 1, :], k.ident[:], Q["qim"][:, jt:jt + 1], None, ALU.mult, None, ["ident"] + S, ["s_dq"])
        o.mm(ps[1][:, 0:128], k.ones32[:], Q["dq"][:, 0, :], True, True, ["ones32", "s_dq"], [P[1]])
        o.mm(ps[1][:, 128:256], k.ones32[:], Q["dq"][:, 1, :], True, True, ["ones32", "s_dq"], [P[1]])
        qrb, qib = ps[1][:, 0:128], ps[1][:, 128:256]
        bre, bim = wst[:, 0, jt, :], wst[:, 1, jt, :]
        w1, w2 = Q["w1"][:, 0:128], Q["w2"][:, 0:128]
        o.tt(w1, qrb, bre, ALU.mult, [P[1], "xblk"], ["s_w1"])
        o.tt(w2, qib, bim, ALU.mult, [P[1], "xblk"], ["s_w2"])
        o.tt(Q["B"][:, 0, jt, :], w1, w2, ALU.subtract, ["s_w1", "s_w2"], ["s_B"])
        o.tt(w1, qrb, bim, ALU.mult, [P[1], "xblk"], ["s_w1"])
        o.tt(w2, qib, bre, ALU.mult, [P[1], "xblk"], ["s_w2"])
        o.tt(Q["B"][:, 1, jt, :], w1, w2, ALU.add, ["s_w1", "s_w2"], ["s_B"])
    o.dma("sp", wst, dr["s5c"][l].rearrange("a j p c -> p a j c"), "xblk0", r=["s_B"], w=["xblk"])
    o.copy(Q["C"][:, 0], wst[:, 0], ["xblk"], ["s_C"])
    o.ts(Q["C"][:, 1], wst[:, 1], -1.0, None, ALU.mult, None, ["xblk"], ["s_C"])
    gst = xblk[:, 0:2, :].rearrange("p a b -> p (a b)")[:, 0:512].rearrange("p (kt n) -> p kt n", n=256)
    o.dma("sp", gst, dr["glu_w"][l].rearrange("(kt p) n -> p kt n", p=128), "xblk0", r=["s_C"], w=["xblk"])
    o.copy(Q["glu"][:], gst, ["xblk"], ["s_glu"])
    T = ["s_tab"]
    for jt in range(8):
        o.ts(Q["w5"][:], Q["tv"][:], Q["th"][:, jt:jt + 1], None, ALU.mult, None, ["s_tv"] + S, ["s_w5"])
        sin_of(k, Q["sinT"][:, jt, :], Q["w5"][:], 0.0, Q["w6"][:], Q["wi"][:], 128, ["s_w5"], T, "s_w6")
        sin_of(k, Q["cosT"][:, jt, :], Q["w5"][:], math.pi / 2, Q["w6"][:], Q["wi"][:], 128, ["s_w5"], T, "s_w6")
    o.memset(Q["cre"][:], 0.0, ["s_carry"], eng="dve")
    o.memset(Q["cim"][:], 0.0, ["s_carry"], eng="dve")
    return Q


def s5_block(k, l, i, Q, s5u, mixT, t0):
    o, ps, P = k.o, k.ps, k.P
    o.copy(Q["ub"][:], s5u[:], ["s5u"], ["s_ub"], eng="pool")
    T = ["s_tab"]
    w1, w2, w3, w4, w5, w6 = (Q[n][:] for n in ("w1", "w2", "w3", "w4", "w5", "w6"))
    for jt in range(8):
        ct = jt // 4
        pb = 1 + (jt % 2)
        o.mm(ps[pb][:, 0:NB], Q["B"][:, 0, jt, :], Q["ub"][:, ct, :], True, True, ["s_B", "s_ub"], [P[pb]])
        o.mm(ps[pb][:, NB:2 * NB], Q["B"][:, 1, jt, :], Q["ub"][:, ct, :], True, True, ["s_B", "s_ub"], [P[pb]])
        bre, bim = ps[pb][:, 0:NB], ps[pb][:, NB:2 * NB]
        cs_, sn_ = Q["cosT"][:, jt, :], Q["sinT"][:, jt, :]
        o.tt(w1, bre, cs_, ALU.mult, [P[pb]] + T, ["s_w1"])
        o.tt(w2, bim, sn_, ALU.mult, [P[pb]] + T, ["s_w2"])
        o.tt(w3, bim, cs_, ALU.mult, [P[pb]] + T, ["s_w3"])
        o.tt(w4, bre, sn_, ALU.mult, [P[pb]] + T, ["s_w4"])
        o.tt(w1, w1, w2, ALU.add, ["s_w1", "s_w2"], ["s_w1"], eng="pool")
        o.tt(w3, w3, w4, ALU.subtract, ["s_w3", "s_w4"], ["s_w3"], eng="pool")
        magb = Q["w7"][:]
        o.ts(magb, Q["tv"][:], 0.0, Q["mag"][:, jt:jt + 1], ALU.mult, ALU.add, ["s_tv", "s_small"], ["s_w7"], eng="pool")
        o.scan(w5, magb, w1, Q["cre"][:, jt:jt + 1], ["s_w7", "s_w1", "s_carry"], ["s_w5"])
        o.scan(w6, magb, w3, Q["cim"][:, jt:jt + 1], ["s_w7", "s_w3", "s_carry"], ["s_w6"])
        o.copy(Q["glr"][:, jt:jt + 1], w5[:, NB - 1:NB], ["s_w5"], ["s_gl"], eng="dve")
        o.copy(Q["gli"][:, jt:jt + 1], w6[:, NB - 1:NB], ["s_w6"], ["s_gl"], eng="dve")
        o.tt(w2, w5, cs_, ALU.mult, ["s_w5"] + T, ["s_w2"], eng="pool")
        o.tt(w4, w6, sn_, ALU.mult, ["s_w6"] + T, ["s_w4"], eng="pool")
        o.tt(Q["hre"][:, jt, :], w2, w4, ALU.subtract, ["s_w2", "s_w4"], [("s_h", jt)], eng="pool")
        o.tt(w2, w5, sn_, ALU.mult, ["s_w5"] + T, ["s_w2"], eng="pool")
        o.tt(w4, w6, cs_, ALU.mult, ["s_w6"] + T, ["s_w4"], eng="pool")
        o.tt(Q["him"][:, jt, :], w2, w4, ALU.add, ["s_w2", "s_w4"], [("s_h", jt)], eng="pool")
        yield
    o.tt(Q["t8a"][:], Q["glr"][:], Q["Ere"][:], ALU.mult, ["s_gl", "s_small"], ["s_c1"])
    o.tt(Q["t8b"][:], Q["gli"][:], Q["Eim"][:], ALU.mult, ["s_gl", "s_small"], ["s_c2"])
    o.tt(Q["cre"][:], Q["t8a"][:], Q["t8b"][:], ALU.subtract, ["s_c1", "s_c2"], ["s_carry"])
    o.tt(Q["t8a"][:], Q["glr"][:], Q["Eim"][:], ALU.mult, ["s_gl", "s_small"], ["s_c1"])
    o.tt(Q["t8b"][:], Q["gli"][:], Q["Ere"][:], ALU.mult, ["s_gl", "s_small"], ["s_c2"])
    o.tt(Q["cim"][:], Q["t8a"][:], Q["t8b"][:], ALU.add, ["s_c1", "s_c2"], ["s_carry"])
    vec2 = Q["vec2"]
    for ct in range(2):
        pb = 3
        for j in range(4):
            jt = ct * 4 + j
            o.mm(ps[pb][:, 0:NB], Q["C"][:, 0, jt, :], Q["hre"][:, jt, :], j == 0, False, ["s_C", ("s_h", jt)], [P[pb]])
            o.mm(ps[pb][:, 0:NB], Q["C"][:, 1, jt, :], Q["him"][:, jt, :], False, j == 3, ["s_C", ("s_h", jt)], [P[pb]])
        o.copy(Q["yv"][:, ct, :], ps[pb][:, 0:NB], [P[pb]], ["s_yv"], eng="act")
        o.stt(Q["yv"][:, ct, :], s5u[:, ct, :], vec2[:, 0, ct:ct + 1], Q["yv"][:, ct, :], ALU.mult, ALU.add, ["s5u", "s_vec2", "s_yv"], ["s_yv"])
        gelu_tanh(k, Q["ge"][:, ct, :], Q["yv"][:, ct, :], Q["w1"][:], ["s_yv"], ["s_ge"], "s_w1")
        o.copy(Q["geb"][:, ct, :], Q["ge"][:, ct, :], ["s_ge"], ["s_geb"], eng="pool")
        yield
    for ct in range(2):
        pb = 3
        for kt in range(2):
            o.mm(ps[pb][:, 0:NB], Q["glu"][:, kt, ct * 128:(ct + 1) * 128], Q["geb"][:, kt, :], kt == 0, kt == 1, ["s_glu", "s_geb"], [P[pb]])
        o.act(Q["w1"][:], ps[pb][:, 0:NB], AF.Sigmoid, [P[pb], "s_vec2"], ["s_w1"], bias=vec2[:, 1, ct:ct + 1])
        o.tt(Q["yv"][:, ct, :], Q["ge"][:, ct, :], Q["w1"][:], ALU.mult, ["s_ge", "s_w1"], ["s_yv"])
        yield
    branch_norm(k, Q["yv"], "s_yv", Q["sqb"], "s_sqb", Q["w2"], "s_w2", vec2[:, 2, :], "s_vec2", Q["yo"], "s_yo", Q["yob"], "s_yob", 2)
    o.dma("pool", mixT[256:512, t0:t0 + NB].rearrange("(t p) s -> p t s", p=128), Q["yob"][:], "mix_b", r=["s_yob"], w=["mixT_b"])


def branch_norm(k, y, yk, sqb, sqk, rstd, rk, g, gk, yo, yok, yob, yobk, nt, rows=128):
    o, ps, P = k.o, k.ps, k.P
    pb = 2
    for t in range(nt):
        o.act(sqb[0:rows, t, :], y[0:rows, t, :], AF.Square, [yk], [sqk])
    for t in range(nt):
        o.mm(ps[pb][0:rows, 0:y.shape[2]], k.onesb[0:rows, 0:rows], sqb[0:rows, t, :], t == 0, t == nt - 1, [sqk, "onesb"], [P[pb]])
    rsqrt(k, rstd[0:rows, :], ps[pb][0:rows, 0:y.shape[2]], 1.0 / (nt * rows), 1e-6, [P[pb]], rk, rows)
    for t in range(nt):
        o.stt(yo[0:rows, t, :], y[0:rows, t, :], g[0:rows, t:t + 1], rstd[0:rows, :], ALU.mult, ALU.mult, [yk, gk, rk], [yok])
    o.copy(yob[0:rows], yo[0:rows], [yok], [yobk], eng="pool")


def lru_setup(k, pa, l, xblk):
    o, dr = k.o, k.dr
    U = {}
    U["v"] = sb(k, pa, "l_v", [128, 9, 2])
    U["c1"] = sb(k, pa, "l_c1", [128, 2])
    U["bd"] = sb(k, pa, "l_bd", [128, 2, 2, 128], BF16)
    for nm in ("xc", "rr", "ii", "aa", "t1", "t2", "hh", "gg"):
        U[nm] = sb(k, pa, "l_" + nm, [128, NB])
    U["xcb"] = sb(k, pa, "l_xcb", [128, NB], BF16)
    U["hc"] = sb(k, pa, "l_hc", [128, 2])
    U["yd"] = sb(k, pa, "l_yd", [128, 2, NB])
    U["sqb"] = sb(k, pa, "l_sqb", [128, 2, NB], BF16)
    U["yo"] = sb(k, pa, "l_yo", [128, 2, NB])
    U["yob"] = sb(k, pa, "l_yob", [128, 2, NB], BF16)
    o.dma("sp", U["v"][:], dr["lruv"][l], "l_v", w=["l_v"])
    bst = xblk[:, 0:2, :].rearrange("p a b -> p (a b)")[:, 0:512].rearrange("p (a t c) -> p a t c", a=2, t=2)
    o.dma("sp", bst, dr["lrubd"][l].rearrange("a t p c -> p a t c"), "xblk0", w=["xblk"])
    o.copy(U["bd"][:], bst, ["xblk"], ["l_bd"])
    o.act(U["c1"][:], U["v"][:, 7, :], AF.Exp, ["l_v"], ["l_c1"], scale=-1.0)
    o.act(U["c1"][:], U["c1"][:], AF.Ln, ["l_c1", "cc"], ["l_c1"], bias=k.cc[:, CC[1.0]:CC[1.0] + 1])
    o.ts(U["c1"][:], U["c1"][:], -8.0, None, ALU.mult, None, ["l_c1"], ["l_c1"])
    o.memset(U["hc"][:], 0.0, ["l_hc"], eng="dve")
    return U


def lru_block(k, l, i, U, lrx, lrg, mixT, t0):
    o, ps, P = k.o, k.ps, k.P
    v = U["v"]
    for t in range(2):
        xc = U["xc"][:]
        o.ts(xc, lrx[:, t, 3:NB + 3], v[:, 3, t:t + 1], v[:, 4, t:t + 1], ALU.mult, ALU.add, ["lrx", "l_v"], ["l_xc"])
        for j in range(3):
            o.stt(xc, lrx[:, t, j:NB + j], v[:, j, t:t + 1], xc, ALU.mult, ALU.add, ["lrx", "l_v", "l_xc"], ["l_xc"])
        o.copy(U["xcb"][:], xc, ["l_xc"], ["l_xcb"], eng="pool")
        yield
        o.mm(ps[1][:, 0:NB], U["bd"][:, 0, t, :], U["xcb"][:], True, True, ["l_bd", "l_xcb"], [P[1]])
        o.mm(ps[1][:, NB:2 * NB], U["bd"][:, 1, t, :], U["xcb"][:], True, True, ["l_bd", "l_xcb"], [P[1]])
        o.act(U["rr"][:], ps[1][:, 0:NB], AF.Sigmoid, [P[1], "l_v"], ["l_rr"], bias=v[:, 5, t:t + 1])
        o.act(U["ii"][:], ps[1][:, NB:2 * NB], AF.Sigmoid, [P[1], "l_v"], ["l_ii"], bias=v[:, 6, t:t + 1])
        o.act(U["aa"][:], U["rr"][:], AF.Exp, ["l_rr", "l_c1"], ["l_aa"], scale=U["c1"][:, t:t + 1])
        yield
        o.tt(U["t1"][:], U["aa"][:], U["aa"][:], ALU.mult, ["l_aa"], ["l_t1"])
        o.ts(U["t1"][:], U["t1"][:], -1.0, 1.0, ALU.mult, ALU.add, ["l_t1"], ["l_t1"])
        o.ts(U["t1"][:], U["t1"][:], 0.0, None, ALU.max, None, ["l_t1"], ["l_t1"])
        o.act(U["t1"][:], U["t1"][:], AF.Sqrt, ["l_t1"], ["l_t1"])
        o.tt(U["t2"][:], U["ii"][:], xc, ALU.mult, ["l_ii", "l_xc"], ["l_t2"])
        o.tt(U["t2"][:], U["t2"][:], U["t1"][:], ALU.mult, ["l_t2", "l_t1"], ["l_t2"])
        o.scan(U["hh"][:], U["aa"][:], U["t2"][:], U["hc"][:, t:t + 1], ["l_aa", "l_t2", "l_hc"], ["l_hh"])
        o.copy(U["hc"][:, t:t + 1], U["hh"][:, NB - 1:NB], ["l_hh"], ["l_hc"], eng="dve")
        yield
        gelu_tanh(k, U["gg"][:], lrg[:, t, :], U["t1"][:], ["lrg"], ["l_gg"], "l_t1")
        o.tt(U["yd"][:, t, :], U["hh"][:], U["gg"][:], ALU.mult, ["l_hh", "l_gg"], ["l_yd"])
        yield
    o.copy(lrx[:, :, 0:3], lrx[:, :, NB:NB + 3], ["lrx"], ["lrx"], eng="dve")
    branch_norm(k, U["yd"], "l_yd", U["sqb"], "l_sqb", U["t2"], "l_t2", v[:, 8, :], "l_v", U["yo"], "l_yo", U["yob"], "l_yob", 2)
    o.dma("pool", mixT[768:1024, t0:t0 + NB].rearrange("(t p) s -> p t s", p=128), U["yob"][:], "mix_d", r=["l_yob"], w=["mixT_d"])


QSCALE = 96 ** -0.5


def mla_setup(k, pa, l, xblk, Q, U):
    o, dr = k.o, k.dr
    A = {}
    xflat = xblk[:].rearrange("p a b -> p (a b)")
    A["wuq"] = sb(k, pa, "a_wuq", [128, 2, 384], BF16)
    A["wukn"] = sb(k, pa, "a_wukn", [128, 4, 96], BF16)
    A["wukv"] = sb(k, pa, "a_wukv", [128, 256], BF16)
    A["qng"] = sb(k, pa, "a_qng", [128, 2])
    A["kvng"] = sb(k, pa, "a_kvng", [128, 1])
    A["qkhg"] = sb(k, pa, "a_qkhg", [96, 2])
    A["invf"] = sb(k, pa, "a_invf", [96, 1])
    A["rotT"] = sb(k, pa, "a_rotT", [128, 128])
    A["epe"] = sb(k, pa, "a_epe", [32, 96], BF16)
    A["posi"] = sb(k, pa, "a_posi", [96, 1], I32)
    A["posf"] = sb(k, pa, "a_posf", [96, 1])
    A["tv"] = Q["tv"]
    for nm in ("rk96", "COS", "SIN"):
        A[nm] = sb(k, pa, "a_" + nm, [96, NB])
    A["rq"] = U["t2"]
    A["ang"], A["tmpS"], A["angi"] = Q["w5"][0:96, :], Q["w6"][0:96, :], Q["wi"][0:96, :]
    A["qf"], A["rsh"], A["qn"], A["t1"], A["t2"] = (U[n][0:96, :] for n in ("xc", "rr", "ii", "aa", "t1"))
    A["qn128"] = U["ii"]
    A["sq"] = sb(k, pa, "a_sq", [128, 2, NB], BF16)
    A["sqk"] = sb(k, pa, "a_sqk", [128, NB], BF16)
    A["sqh"] = sb(k, pa, "a_sqh", [96, NB], BF16)
    A["rkt"] = sb(k, pa, "a_rkt", [128, NB // 128])
    A["qrb"] = sb(k, pa, "a_qrb", [96, 4, NB], BF16)
    A["krb"] = sb(k, pa, "a_krb", [96, 4, NB], BF16)
    A["Vt"] = sb(k, pa, "a_Vt", [128, NB // 128, 4, 65], BF16)
    for nm, src in (("qng", "qng"), ("kvng", "kvng"), ("qkhg", "qkhg")):
        o.dma("sp", A[nm][:], dr[src][l], "a_" + nm, w=["a_small"])
    o.dma("sp", A["invf"][:], dr["invf"], "a_invf", w=["a_small"])
    o.dma("sp", A["rotT"][:], dr["rotT"], "a_rotT", w=["a_small"])
    o.dma("sp", A["posi"][:], dr["pos"], "a_posi", w=["a_posi"])
    o.copy(A["posf"][:], A["posi"][:], ["a_posi"], ["a_small"])
    ste = xflat[0:32, 0:96]
    o.dma("sp", ste, dr["epe"], "xblk0", w=["xblk"])
    o.copy(A["epe"][:], ste, ["xblk"], ["a_w"])
    stq = xflat[:, 0:768].rearrange("p (kt n) -> p kt n", n=384)
    o.dma("sp", stq, dr["w_uq"][l].rearrange("(kt p) n -> p kt n", p=128), "xblk0", r=["a_w"], w=["xblk"])
    for kt in range(2):
        o.ts(A["wuq"][:, kt, :], stq[:, kt, :], A["qng"][:, kt:kt + 1], None, ALU.mult, None, ["xblk", "a_small"], ["a_w"])
    stk = xflat[:, 0:384]
    o.dma("sp", stk, dr["w_ukn"][l].rearrange("p h c -> p (h c)"), "xblk0", r=["a_w"], w=["xblk"])
    o.ts(A["wukn"][:].rearrange("p h c -> p (h c)"), stk, A["kvng"][:, 0:1], None, ALU.mult, None, ["xblk", "a_small"], ["a_w"])
    stv = xflat[:, 0:256]
    o.dma("sp", stv, dr["w_ukv"][l], "xblk0", r=["a_w"], w=["xblk"])
    o.ts(A["wukv"][:], stv, A["kvng"][:, 0:1], None, ALU.mult, None, ["xblk", "a_small"], ["a_w"])
    o.memset(A["rk96"][:], 1.0, ["a_rk96"], eng="dve")
    o.memset(A["Vt"][:], 1.0, ["a_Vt"], eng="dve")
    return A


def head_norm_rope(k, A, src, gcol, out):
    o, ps, P = k.o, k.ps, k.P
    o.act(A["sqh"][:], src, AF.Square, ["l_xc"], ["a_sqh"])
    o.mm(ps[4][0:96, 0:NB], k.onesb[0:96, 0:96], A["sqh"][:], True, True, ["onesb", "a_sqh"], [P[4]])
    rsqrt(k, A["rsh"][:], ps[4][0:96, 0:NB], 1.0 / 96, 1e-6, [P[4]], "l_rr", 96)
    o.stt(A["qn"][:], src, A["qkhg"][:, gcol:gcol + 1], A["rsh"][:], ALU.mult, ALU.mult, ["l_xc", "a_small", "l_rr"], ["l_ii"])
    o.mm(ps[4][:, NB:2 * NB], A["rotT"][:], A["qn128"][:], True, True, ["a_small", "l_ii"], [P[4]])
    o.tt(A["t1"][:], A["qn"][:], A["COS"][:], ALU.mult, ["l_ii", "a_cs"], ["l_aa"])
    o.tt(A["t2"][:], ps[4][0:96, NB:2 * NB], A["SIN"][:], ALU.mult, [P[4], "a_cs"], ["l_t1"])
    o.tt(out, A["t1"][:], A["t2"][:], ALU.add, ["l_aa", "l_t1"], ["a_out"])


def mla_block(k, l, i, A, qab, kvab, kpeb, t0):
    o, ps, P = k.o, k.ps, k.P
    ntt = NB // 128
    rms_stats(k, [(qab[:, kt, :], "qab") for kt in range(2)], 256.0, 1e-6, ps[1][:, 0:NB], A["rq"][:],
              [(A["sq"][:, kt, :], "a_sq") for kt in range(2)], None, P[1], "l_t2")
    o.act(A["sqk"][:], kvab[:], AF.Square, ["kvab"], ["a_sqk"])
    o.mm(ps[2][:, 0:NB], k.onesb[:], A["sqk"][:], True, True, ["onesb", "a_sqk"], [P[2]])
    for tt in range(ntt):
        o.mm(ps[2][:, NB + tt:NB + tt + 1], A["sqk"][:, tt * 128:(tt + 1) * 128], k.onesb[:, 0:1], True, True, ["onesb", "a_sqk"], [P[2]])
    rsqrt(k, A["rk96"][0:64, :], ps[2][0:64, 0:NB], 1.0 / 128, 1e-6, [P[2]], "a_rk96", 64)
    rsqrt(k, A["rkt"][:], ps[2][:, NB:NB + ntt], 1.0 / 128, 1e-6, [P[2]], "a_rkt", 128)
    yield
    o.ts(A["ang"][:], A["tv"][0:96, :], A["posf"][:, 0:1], None, ALU.add, None, ["s_tv", "a_small"], ["s_w5"])
    o.ts(A["ang"][:], A["ang"][:], float(t0), A["invf"][:, 0:1], ALU.add, ALU.mult, ["s_w5", "a_small"], ["s_w5"])
    sin_of(k, A["SIN"][:], A["ang"][:], 0.0, A["tmpS"][:], A["angi"][:], 96, ["s_w5"], ["a_cs"], "s_w6")
    sin_of(k, A["COS"][:], A["ang"][:], math.pi / 2, A["tmpS"][:], A["angi"][:], 96, ["s_w5"], ["a_cs"], "s_w6")
    yield
    for h in range(4):
        for kt in range(2):
            o.mm(ps[3][0:96, 0:NB], A["wuq"][:, kt, h * 96:(h + 1) * 96], qab[:, kt, :], kt == 0, kt == 1, ["a_w", "qab"], [P[3]])
        o.tt(A["qf"][:], ps[3][0:96, 0:NB], A["rq"][0:96, :], ALU.mult, [P[3], "l_t2"], ["l_xc"])
        head_norm_rope(k, A, A["qf"][:], 0, A["qrb"][:, h, :])
        yield
        o.mm(ps[3][0:96, NB:2 * NB], A["wukn"][:, h, :], kvab[:], True, False, ["a_w", "kvab"], [P[3]])
        o.mm(ps[3][0:96, NB:2 * NB], A["epe"][:], kpeb[:], False, True, ["a_w", "kpeb"], [P[3]])
        o.tt(A["qf"][:], ps[3][0:96, NB:2 * NB], A["rk96"][:], ALU.mult, [P[3], "a_rk96"], ["l_xc"])
        head_norm_rope(k, A, A["qf"][:], 1, A["krb"][:, h, :])
        yield
    for tt in range(ntt):
        o.mm(ps[1][:, 0:256], kvab[:, tt * 128:(tt + 1) * 128], A["wukv"][:], True, True, ["kvab", "a_w"], [P[1]])
        o.act(A["Vt"][:, tt, :, 0:64], ps[1][:, 0:256].rearrange("p (h c) -> p h c", c=64), AF.Identity, [P[1], "a_rkt"], ["a_Vt"],
              scale=A["rkt"][:, tt:tt + 1])
    o.dma("pool", k.qs[:, :, t0:t0 + NB], A["qrb"][:], "st_q", r=["a_out"], w=["qs"])
    o.dma("pool", k.ks[:, :, t0:t0 + NB], A["krb"][:], "st_k", r=["a_out"], w=["ks"])
    o.dma("pool", k.vs[t0 // 128:t0 // 128 + ntt].rearrange("t p c -> p t c"), A["Vt"][:].rearrange("p t h c -> p t (h c)"), "st_v", r=["a_Vt"], w=["vs"])


QB = 256


def phaseB(k, l, xin, x1T, stop):
    nc, S_, dr, S, L, o = k.nc, k.S_, k.dr, k.S, k.L, k.o
    ps, P = k.ps, k.P
    nqb = S // QB
    with ExitStack() as pb_:
        Kall = sb(k, pb_, "Kall", [96, 4, S], BF16)
        Vall = sb(k, pb_, "Vall", [128, S // 128, 260], BF16)
        qblk = sb(k, pb_, "qblk", [96, 4, QB], BF16)
        PT = [sb(k, pb_, f"PT{i}", [128, QB], BF16) for i in range(2)]
        Oext = sb(k, pb_, "Oext", [128, QB])
        rden = sb(k, pb_, "rden", [64, QB])
        yc = sb(k, pb_, "yc", [64, 4, QB])
        ycsq = sb(k, pb_, "ycsq", [64, 4, QB], BF16)
        ycn = sb(k, pb_, "ycn", [64, 4, QB])
        ycnb = sb(k, pb_, "ycnb", [64, 4, QB], BF16)
        rstc = sb(k, pb_, "rstc", [64, QB])
        mixb = sb(k, pb_, "mixb", [128, 6, QB], BF16)
        wo = sb(k, pb_, "wo", [128, 6, D], BF16)
        woc = sb(k, pb_, "woc", [64, 4, D], BF16)
        xblk = sb(k, pb_, "xblkB", [128, 8, QB])
        sq2 = sb(k, pb_, "sq2", [128, 8, QB], BF16)
        rst2 = sb(k, pb_, "rst2", [128, QB])
        xt2 = sb(k, pb_, "xt2", [128, QB])
        h2 = sb(k, pb_, "h2", [128, 8, QB])
        h2b = sb(k, pb_, "h2b", [128, 8, QB], BF16)
        wr = sb(k, pb_, "wr", [128, 8, 36])
        brt = sb(k, pb_, "brt", [128, 36])
        sel65 = sb(k, pb_, "sel65", [128, 64])
        bngc = sb(k, pb_, "bngc", [64, 4])
        lg = sb(k, pb_, "lg", [128, 36])
        rt = {nm: sb(k, pb_, "rt_" + nm, [128, w_]) for nm, w_ in (("m4", 1), ("nm4", 1), ("e4", 4), ("s4", 1), ("gp", 1), ("ohg", 4), ("sel", 8),
                                                                  ("l1", 1), ("oh1", 8), ("sel2", 8), ("l2", 1), ("oh2", 8), ("nl1", 1), ("d", 1),
                                                                  ("t", 1), ("w1", 1), ("w2", 1), ("ge", 8))}
        gates = sb(k, pb_, "gates", [128, 32])
        gT = sb(k, pb_, "gTb", [32, QB])
        wst = h2[:].rearrange("p a b -> p (a b)")
        wov = dr["w_out"][l]
        rows = [0, 128, 256, 384, 768, 896]
        for t, r0 in enumerate(rows):
            for hf in range(1):
                o.dma("sp", wst[:, 0:1024], wov[r0:r0 + 128, :], "h2st", w=["h2"])
                o.copy(wo[:, t, :], wst[:, 0:1024], ["h2"], ["wo"], eng=("dve" if t % 2 == 0 else "pool"))
        for h in range(4):
            o.dma("sp", wst[0:64, 0:1024], wov[512 + h * 64:512 + (h + 1) * 64, :], "h2st", w=["h2"])
            o.copy(woc[:, h, :], wst[0:64, 0:1024], ["h2"], ["wo"], eng="dve")
        o.dma("sp", wr[:], dr["wr"][l].rearrange("(kk p) n -> p kk n", p=128), "wr", w=["wr"])
        o.dma("sp", brt[:], dr["br"][l], "brt", w=["wr"])
        o.dma("sp", sel65[:], dr["sel65"], "sel65", w=["sel65"])
        o.memset(Oext[:], 0.0, ["Oext"], eng="dve")
        o.dma("sp", bngc[:], dr["bng_c"][l], "bngc", w=["bngc"])
        xv = xin.rearrange("(kk p) s -> p kk s", p=128)
        x1v = x1T.rearrange("(kk p) s -> p kk s", p=128)
        h2v = k.h2T.rearrange("(kk p) s -> p kk s", p=128)
        for qb in range(nqb):
            t0 = qb * QB
            nkt = QB // 128
            o.dma("sp", Kall[:, :, t0:t0 + QB], k.ks[:, :, t0:t0 + QB], "ldK", r=["ks"], w=["Kall"])
            o.dma("sp", Vall[:, t0 // 128:t0 // 128 + nkt, :], k.vs[t0 // 128:t0 // 128 + nkt].rearrange("t p c -> p t c"), "ldV", r=["vs"], w=["Vall"])
            o.dma("sp", qblk[:], k.qs[:, :, t0:t0 + QB], "ldQ", r=["qs"], w=["qblk"])
            o.dma("pool", mixb[:, 0:2, :], k.mixT[0:256, t0:t0 + QB].rearrange("(t p) s -> p t s", p=128), "ldm", r=["mixT_a"], w=["mixb"])
            o.dma("pool", mixb[:, 2:4, :], k.mixT[256:512, t0:t0 + QB].rearrange("(t p) s -> p t s", p=128), "ldm", r=["mixT_b"], w=["mixb"])
            o.dma("pool", mixb[:, 4:6, :], k.mixT[768:1024, t0:t0 + QB].rearrange("(t p) s -> p t s", p=128), "ldm", r=["mixT_d"], w=["mixb"])
            o.dma("sp", xblk[:], xv[:, :, t0:t0 + QB], "xblkB", w=["xblkB"])
            nk_tot = (t0 + QB) // 128
            units = [(h, kt) for h in range(4) for kt in range(nk_tot)]

            def s_stage(iu):
                h, kt = units[iu]
                a = kt - (t0 // 128)
                c0 = max(a, 0) * 128
                sb_ = 1 + (iu % 2)
                o.mm(ps[sb_][:, c0:QB], Kall[:, h, kt * 128:(kt + 1) * 128], qblk[:, h, c0:QB], True, True, ["Kall", "qblk"], [P[sb_]])

            s_stage(0)
            for iu, (h, kt) in enumerate(units):
                if iu + 1 < len(units):
                    s_stage(iu + 1)
                a = kt - (t0 // 128)
                c0 = max(a, 0) * 128
                sb_ = 1 + (iu % 2)
                pt = PT[iu % 2]
                ptk = f"PT{iu % 2}"
                o.act(pt[:, c0:QB], ps[sb_][:, c0:QB], AF.Exp, [P[sb_]], [ptk], scale=QSCALE)
                if a >= 0:
                    o.memset(pt[64:128, c0:c0 + 64], 0.0, [ptk], eng="pool")
                o.mm(ps[3][0:65, c0:QB], Vall[:, kt, h * 65:(h + 1) * 65], pt[:, c0:QB], kt == 0, kt == nk_tot - 1, ["Vall", ptk], [P[3]])
                if kt == nk_tot - 1:
                    o.copy(Oext[0:65, :], ps[3][0:65, 0:QB], [P[3]], ["Oext"], eng="dve")
                    o.mm(ps[4][0:64, 0:QB], sel65[:], Oext[:], True, True, ["sel65", "Oext"], [P[4]])
                    o.S.op("dve", lambda e: e.reciprocal(out=rden[:], in_=ps[4][0:64, 0:QB]), [P[4]], ["rden"])
                    o.tt(yc[:, h, :], Oext[0:64, :], rden[:], ALU.mult, ["Oext", "rden"], ["yc"])
            branch_norm(k, yc, "yc", ycsq, "ycsq", rstc, "rstc", bngc, "bngc", ycn, "ycn", ycnb, "ycnb", 4, rows=64)
            if stop == "B1":
                dbg = dr["dbg"]
                o.dma("sp", dbg[512:768, t0:t0 + QB].rearrange("(h p) s -> p h s", p=64), ycn[:], "dbg", r=["ycn"], w=["dbg"])
            for f in range(8):
                pb = 5 + (f % 2)
                fc = slice(f * 128, (f + 1) * 128)
                for t in range(6):
                    o.mm(ps[pb][:, 0:QB], wo[:, t, fc], mixb[:, t, :], t == 0, False, ["wo", "mixb"], [P[pb]])
                for h in range(4):
                    o.mm(ps[pb][:, 0:QB], woc[:, h, fc], ycnb[:, h, :], False, h == 3, ["wo", "ycnb"], [P[pb]])
                o.act(xt2[:], ps[pb][:, 0:QB], AF.Identity, [P[pb], "mods"], ["xt2"], scale=k.mods[:, l, 16 + f:17 + f])
                o.tt(xblk[:, f, :], xt2[:], xblk[:, f, :], ALU.add, ["xt2", "xblkB"], ["xblkB"])
            o.dma("pool", x1v[:, :, t0:t0 + QB], xblk[:], "stx1", r=["xblkB"], w=["x1T"])
            if stop == "B1":
                dbg = dr["dbg"]
                o.dma("sp", dbg[1024:2048, t0:t0 + QB].rearrange("(t p) s -> p t s", p=128), xblk[:], "dbg", r=["xblkB"], w=["dbg"])
            rms_stats(k, [(xblk[:, kk, :], "xblkB") for kk in range(8)], 1024.0, 1e-6, ps[7][:, 0:QB], rst2[:],
                      [(sq2[:, kk, :], "sq2") for kk in range(8)], None, P[7], "rst2")
            for kk in range(8):
                o.tt(xt2[:], xblk[:, kk, :], rst2[:], ALU.mult, ["xblkB", "rst2"], ["xt2"])
                o.act(h2[:, kk, :], xt2[:], AF.Identity, ["xt2", "g2s", "mods"], ["h2"], bias=k.mods[:, l, 24 + kk:25 + kk], scale=k.g2s[:, l, kk:kk + 1])
            o.copy(h2b[:], h2[:], ["h2"], ["h2b"], eng="pool")
            o.dma("pool", h2v[:, :, t0:t0 + QB], h2b[:], "sth2", r=["h2b"], w=["h2T"])
            for tq in range(QB // 128):
                tsl = slice(tq * 128, (tq + 1) * 128)
                for kk in range(8):
                    o.mm(ps[7][:, 256:292], h2[:, kk, tsl], wr[:, kk, :], kk == 0, kk == 7, ["h2", "wr"], [P[7]])
                o.tt(lg[:], ps[7][:, 256:292], brt[:], ALU.add, [P[7], "wr"], ["lg"])
                route(k, lg, rt, gates)
                o.tr(ps[7][0:32, 384:512], gates[:], k.ident[:], ["gates", "ident"], [P[7]])
                o.copy(gT[:, tsl], ps[7][0:32, 384:512], [P[7]], ["gTb"], eng="dve")
            o.dma("pool", k.gT[:, t0:t0 + QB], gT[:], "stg", r=["gTb"], w=["gT"])
            if stop == "B1":
                o.dma("sp", dr["dbg"][0:32, t0:t0 + QB], gT[:], "dbg", r=["gTb"], w=["dbg"])
        S_.barrier()
        S_.flush(k.st)


def route(k, lg, rt, gates):
    o = k.o
    R_ = ["rt"]
    red = lambda out, in_, op: o.S.op("dve", lambda e: e.tensor_reduce(out=out, in_=in_, axis=AX.X, op=op), ["lg"] + R_, R_)
    red(rt["m4"][:], lg[:, 0:4], ALU.max)
    o.ts(rt["nm4"][:], rt["m4"][:], -1.0, None, ALU.mult, None, R_, R_)
    o.act(rt["e4"][:], lg[:, 0:4], AF.Exp, ["lg"] + R_, R_, bias=rt["nm4"][:, 0:1])
    red(rt["s4"][:], rt["e4"][:], ALU.add)
    o.S.op("dve", lambda e: e.reciprocal(out=rt["gp"][:], in_=rt["s4"][:]), R_, R_)
    o.ts(rt["ohg"][:], lg[:, 0:4], rt["m4"][:, 0:1], None, ALU.is_equal, None, ["lg"] + R_, R_)
    for g in range(4):
        le = lg[:, 4 + 8 * g:12 + 8 * g]
        if g == 0:
            o.ts(rt["sel"][:], le, rt["ohg"][:, 0:1], None, ALU.mult, None, ["lg"] + R_, R_)
        else:
            o.stt(rt["sel"][:], le, rt["ohg"][:, g:g + 1], rt["sel"][:], ALU.mult, ALU.add, ["lg"] + R_, R_)
    red(rt["l1"][:], rt["sel"][:], ALU.max)
    o.ts(rt["oh1"][:], rt["sel"][:], rt["l1"][:, 0:1], None, ALU.is_equal, None, R_, R_)
    o.stt(rt["sel2"][:], rt["oh1"][:], -1e30, rt["sel"][:], ALU.mult, ALU.add, R_, R_)
    red(rt["l2"][:], rt["sel2"][:], ALU.max)
    o.ts(rt["oh2"][:], rt["sel2"][:], rt["l2"][:, 0:1], None, ALU.is_equal, None, R_, R_)
    o.ts(rt["nl1"][:], rt["l1"][:], -1.0, None, ALU.mult, None, R_, R_)
    o.act(rt["d"][:], rt["l2"][:], AF.Exp, R_, R_, bias=rt["nl1"][:, 0:1])
    o.ts(rt["t"][:], rt["d"][:], 1.0, None, ALU.add, None, R_, R_)
    o.S.op("dve", lambda e: e.reciprocal(out=rt["t"][:], in_=rt["t"][:]), R_, R_)
    o.tt(rt["w1"][:], rt["gp"][:], rt["t"][:], ALU.mult, R_, R_)
    o.tt(rt["w2"][:], rt["w1"][:], rt["d"][:], ALU.mult, R_, R_)
    o.ts(rt["ge"][:], rt["oh1"][:], rt["w1"][:, 0:1], None, ALU.mult, None, R_, R_)
    o.stt(rt["ge"][:], rt["oh2"][:], rt["w2"][:, 0:1], rt["ge"][:], ALU.mult, ALU.add, R_, R_)
    for g in range(4):
        o.ts(gates[:, 8 * g:8 * g + 8], rt["ge"][:], rt["ohg"][:, g:g + 1], None, ALU.mult, None, R_, ["gates"])


def phaseW(k, l):
    nc, S_, dr, o = k.nc, k.S_, k.dr, k.o
    NE = dr["w1"].shape[1]
    with ExitStack() as pw:
        stg = [sb(k, pw, f"wstg{i}", [128, 4096]) for i in range(2)]
        wb = [sb(k, pw, f"wbf{i}", [128, 4096], BF16) for i in range(2)]
        it = 0
        for e in range(NE):
            for nm, dst, kk in (("w1", k.w1b, 8), ("w3", k.w3b, 8), ("w2", k.w2b, 4)):
                i2 = it % 2
                n = 4096 // kk
                src = dr[nm][l, e].rearrange("(kk p) n -> p kk n", p=128)
                o.dma("sp", stg[i2][:].rearrange("p (kk n) -> p kk n", kk=kk), src, f"wstg{i2}", w=[f"wstg{i2}"])
                o.copy(wb[i2][:], stg[i2][:], [f"wstg{i2}"], [f"wbf{i2}"], eng=("dve", "pool", "act")[it % 3])
                o.dma("pool", dst[e], wb[i2][:], f"wbst{i2}", r=[f"wbf{i2}"], w=["wscr"])
                it += 1
        S_.barrier()
        S_.flush(k.st)


def phaseC(k, l, x1T, xoutT, stop):
    nc, S_, dr, S, o = k.nc, k.S_, k.dr, k.S, k.o
    ps, P = k.ps, k.P
    NE = dr["w1"].shape[1]
    TB = min(1024, S)
    nh = TB // 512
    with ExitStack() as pc:
        h2b = sb(k, pc, "c_h2b", [128, 8, TB], BF16)
        acc = sb(k, pc, "c_acc", [128, 8, TB])
        gbc = [sb(k, pc, f"c_gbc{i}", [128, TB]) for i in range(2)]
        W1 = [sb(k, pc, f"c_w1_{i}", [128, 8, 512], BF16) for i in range(2)]
        W3 = [sb(k, pc, f"c_w3_{i}", [128, 8, 512], BF16) for i in range(2)]
        W2 = [sb(k, pc, f"c_w2_{i}", [128, 4, 1024], BF16) for i in range(2)]
        hid = [sb(k, pc, f"c_hid{i}", [128, 4, 512], BF16) for i in range(2)]
        sil = [sb(k, pc, f"c_sil{i}", [128, 512]) for i in range(2)]
        t3 = [sb(k, pc, f"c_t3{i}", [128, 512]) for i in range(2)]
        xr = [sb(k, pc, f"c_xr{i}", [128, TB]) for i in range(2)]
        sel32 = sb(k, pc, "c_sel32", [128, 32, 128])
        gt128 = sb(k, pc, "c_gt128", [128, TB])
        o.dma("sp", sel32[:], dr["sel32"], "c_sel32", w=["sel32"])
        o.memset(gt128[:], 0.0, ["gt128"], eng="dve")
        h2v = k.h2T.rearrange("(kk p) s -> p kk s", p=128)
        x1v = x1T.rearrange("(kk p) s -> p kk s", p=128)
        xov = xoutT.rearrange("(kk p) s -> p kk s", p=128)
        for tb in range(S // TB):
            t0 = tb * TB
            o.dma("sp", h2b[:], h2v[:, :, t0:t0 + TB], "c_h2b", r=["h2T"], w=["c_h2b"])
            o.dma("sp", gt128[0:32, :], k.gT[:, t0:t0 + TB], "c_gt128", r=["gT"], w=["gt128"])
            units = [(e, hf) for e in range(NE) for hf in range(nh)]

            def stage1(iu):
                e, hf = units[iu]
                i2 = e % 2
                wk = f"c_w{i2}"
                j2 = iu % 2
                if hf == 0:
                    o.dma("sp", W1[i2][:].rearrange("p a b -> p (a b)"), k.w1b[e], f"c_w1_{i2}", r=["wscr"], w=[wk + "a"])
                    o.dma("sp", W3[i2][:].rearrange("p a b -> p (a b)"), k.w3b[e], f"c_w3_{i2}", r=["wscr"], w=[wk + "b"])
                    o.dma("sp", W2[i2][:].rearrange("p a b -> p (a b)"), k.w2b[e], f"c_w2_{i2}", r=["wscr"], w=[wk + "c"])
                    for h_ in range(nh):
                        o.mm(ps[h_][:, :], sel32[:, e, :], gt128[:, h_ * 512:(h_ + 1) * 512], True, True, ["sel32", "gt128"], [P[h_]])
                        o.copy(gbc[i2][:, h_ * 512:(h_ + 1) * 512], ps[h_][:, :], [P[h_]], [f"c_gbc{i2}"], eng="act")
                hs = slice(hf * 512, (hf + 1) * 512)
                for ht in range(4):
                    pa_, pb_ = ht % 2, 2 + ht % 2
                    hc = slice(ht * 128, (ht + 1) * 128)
                    for kk in range(8):
                        o.mm(ps[pa_][:, :], W1[i2][:, kk, hc], h2b[:, kk, hs], kk == 0, kk == 7, [wk + "a", "c_h2b"], [P[pa_]])
                    for kk in range(8):
                        o.mm(ps[pb_][:, :], W3[i2][:, kk, hc], h2b[:, kk, hs], kk == 0, kk == 7, [wk + "b", "c_h2b"], [P[pb_]])
                    s2 = ht % 2
                    o.act(sil[s2][:], ps[pa_][:, :], AF.Silu, [P[pa_]], [f"c_sil{s2}"])
                    o.tt(t3[s2][:], ps[pb_][:, :], gbc[i2][:, hs], ALU.mult, [P[pb_], f"c_gbc{i2}"], [f"c_t3{s2}"])
                    o.tt(hid[j2][:, ht, :], sil[s2][:], t3[s2][:], ALU.mult, [f"c_sil{s2}", f"c_t3{s2}"], [(f"c_hid{j2}", ht)],
                         eng=("pool" if ht % 2 == 0 else "dve"))

            def stage2(iu):
                e, hf = units[iu]
                i2 = e % 2
                wk = f"c_w{i2}"
                j2 = iu % 2
                hs = slice(hf * 512, (hf + 1) * 512)
                for f in range(8):
                    po = 4 + f % 4
                    fc = slice(f * 128, (f + 1) * 128)
                    for ht in range(4):
                        o.mm(ps[po][:, :], W2[i2][:, ht, fc], hid[j2][:, ht, :], ht == 0, ht == 3, [wk + "c", (f"c_hid{j2}", ht)], [P[po]])
                    if e == 0:
                        o.copy(acc[:, f, hs], ps[po][:, :], [P[po]], [("c_acc", f)], eng="act")
                    else:
                        o.tt(acc[:, f, hs], ps[po][:, :], acc[:, f, hs], ALU.add, [P[po], ("c_acc", f)], [("c_acc", f)])

            stage1(0)
            for iu in range(len(units)):
                if iu + 1 < len(units):
                    stage1(iu + 1)
                stage2(iu)
            for f in range(8):
                i2 = f % 2
                o.dma("sp", xr[i2][:], x1v[:, f, t0:t0 + TB], f"c_xr{i2}", r=["x1T"], w=[f"c_xr{i2}"])
                o.stt(xr[i2][:], acc[:, f, :], k.mods[:, l, 40 + f:41 + f], xr[i2][:], ALU.mult, ALU.add, [("c_acc", f), "mods", f"c_xr{i2}"], [f"c_xr{i2}"])
                o.dma("pool", xov[:, f, t0:t0 + TB], xr[i2][:], f"c_xo{i2}", r=[f"c_xr{i2}"], w=["xout%d" % l])
                if stop == "C1":
                    o.dma("sp", dr["dbg"][f * 128:(f + 1) * 128, t0:t0 + TB], xr[i2][:], "dbg", r=[f"c_xr{i2}"], w=["dbg"])
        S_.barrier()
        S_.flush(k.st)


def make_shapes(sh, cst, pc):
    shapes = {}
    for d_ in (sh, cst, pc):
        for kname, v in d_.items():
            shapes[kname] = (v.shape, I32 if v.dtype == np.int32 else F32)
    return shapes


def run(inputs, S, L, stop=None, dbg=None):
    sh, cst, per_core = prep_inputs(inputs, S)
    sh = {kk: (v[:L] if v.shape[0] == inputs["ada_w"].shape[0] and kk not in () else v) for kk, v in sh.items()}
    shapes = make_shapes(sh, cst, per_core[0])
    nc = build(S, L, shapes, stop=stop, dbg=dbg)
    in_maps = []
    for b in range(NCORES):
        m = dict(sh)
        m.update(cst)
        m.update(per_core[b])
        in_maps.append(m)
    res = run_bass_kernel_spmd(nc, in_maps, core_ids=list(range(NCORES)))
    return res


def kernel(**inputs):
    S = inputs["x"].shape[1]
    L = inputs["ada_w"].shape[0]
    res = run(inputs, S, L)
    out = np.stack([np.ascontiguousarray(res.results[b]["outT"].T) for b in range(NCORES)], axis=0)
    return out.astype(np.float32)
```
